# Optimizing a Trainium2 kernel written in Bass

```python
import jax, jax.numpy as jnp
from jax import lax
import numpy as np

D_MODEL = 1024
BATCH = 2
SEQ = 8192
DEPTH = 2

GLA_HEADS = 4
GLA_DK = 64
GLA_DV = 128
GLA_GATE_RANK = 16
GLA_TAU = 16.0
GLA_CHUNK = 64
NSA_HEADS = 8
NSA_KV_GROUPS = 2
NSA_HPG = NSA_HEADS // NSA_KV_GROUPS
NSA_DH = 64
CMP_LEN = 32
CMP_STRIDE = 16
CMP_HIDDEN = 256
SEL_BLOCK = 64
SEL_TOPK = 16
WINDOW = 512
Q_BLOCK = 128
ROPE_THETA = 500000.0
ROT_DIM = NSA_DH // 4
SGU_CHUNK = 128
SGU_GROUPS = 8
SGU_WIDTH = 2048
SGU_GROUP_DIM = SGU_WIDTH // SGU_GROUPS
N_EXPERTS = 16
N_EXPERT_GROUPS = 4
EXPERTS_PER_GROUP = N_EXPERTS // N_EXPERT_GROUPS
MOE_TOPK = 2
EXPERT_HIDDEN = 512

GLA_QK_W = GLA_HEADS * GLA_DK
GLA_V_W = GLA_HEADS * GLA_DV
NSA_Q_W = NSA_HEADS * NSA_DH
NSA_KV_W = NSA_KV_GROUPS * NSA_DH
AB_SPLITS = (GLA_QK_W, GLA_QK_W, GLA_V_W, GLA_GATE_RANK, GLA_V_W,
             NSA_Q_W, NSA_KV_W, NSA_KV_W, NSA_KV_W, NSA_KV_W, NSA_KV_W, NSA_KV_W,
             NSA_HEADS * 3)
AB_IN_WIDTH = sum(AB_SPLITS)
AB_MIX_WIDTH = GLA_V_W + NSA_Q_W

NORM_EPS = 1e-6
NEG_INF = -1e30
FORCE_BONUS = 1e4

kernel_name = "hybrid_gla_nsa_gmlp_moe_trunk"


def rms_norm(x, g):
    xf = x.astype(jnp.float32)
    y = xf * lax.rsqrt(jnp.mean(xf * xf, axis=-1, keepdims=True) + NORM_EPS)
    return (y * g.astype(jnp.float32)).astype(x.dtype)


def modulate(h, shift, scale):
    return h * (1 + scale[:, None, :]) + shift[:, None, :]


def rope(x, pos):
    half = ROT_DIM // 2
    inv_freq = jnp.float32(ROPE_THETA) ** (-jnp.arange(half, dtype=jnp.float32) / half)
    ang = pos.astype(jnp.float32)[..., None] * inv_freq
    cos = jnp.cos(ang)[..., None, :]
    sin = jnp.sin(ang)[..., None, :]
    xr = x[..., :ROT_DIM].astype(jnp.float32)
    x1, x2 = xr[..., :half], xr[..., half:]
    rot = jnp.concatenate([x1 * cos - x2 * sin, x2 * cos + x1 * sin], axis=-1).astype(x.dtype)
    return jnp.concatenate([rot, x[..., ROT_DIM:]], axis=-1)


def masked_softmax(s, mask):
    s = jnp.where(mask, s.astype(jnp.float32), NEG_INF)
    return jax.nn.softmax(s, axis=-1) * mask


def gla_mixer(q, k, v, g_lr, r, w_gate2, b_gate, out_g):
    B, S, _ = q.shape
    n = S // GLA_CHUNK
    f32 = jnp.float32
    shp = (B, n, GLA_CHUNK, GLA_HEADS, -1)
    qf = q.astype(f32).reshape(shp) * (GLA_DK ** -0.5)
    kf = k.astype(f32).reshape(shp)
    vf = v.astype(f32).reshape(shp)
    log_a = jax.nn.log_sigmoid((g_lr @ w_gate2 + b_gate).astype(f32)) / GLA_TAU
    b = jnp.cumsum(log_a.reshape(shp), axis=2)
    b_last = b[:, :, -1:]
    b_mid = b[:, :, GLA_CHUNK // 2:GLA_CHUNK // 2 + 1]
    scores = jnp.einsum("bnihd,bnjhd->bnhij", qf * jnp.exp(b - b_mid), kf * jnp.exp(b_mid - b))
    causal = jnp.tril(jnp.ones((GLA_CHUNK, GLA_CHUNK), dtype=bool))
    scores = jnp.where(causal, scores, 0.0)
    o_intra = jnp.einsum("bnhij,bnjhe->bnihe", scores, vf)
    kv = jnp.einsum("bnchd,bnche->bnhde", kf * jnp.exp(b_last - b), vf)
    decay = jnp.exp(b_last[:, :, 0])

    def step(state, inp):
        dec, kv_n = inp
        return dec[..., None] * state + kv_n, state

    init = jnp.zeros((B, GLA_HEADS, GLA_DK, GLA_DV), f32)
    _, states = lax.scan(step, init, (jnp.moveaxis(decay, 1, 0), jnp.moveaxis(kv, 1, 0)))
    states = jnp.moveaxis(states, 0, 1)
    o_inter = jnp.einsum("bnchd,bnhde->bnche", qf * jnp.exp(b), states)
    o = (o_intra + o_inter).reshape(B, S, GLA_HEADS, GLA_DV)
    o = rms_norm(o, out_g) * jax.nn.silu(r.astype(f32)).reshape(B, S, GLA_HEADS, GLA_DV)
    return o.reshape(B, S, GLA_V_W)


def nsa_mixer(q, kc, vc, ks, vs, kw, vw, g_logits, positions,
              q_gain, k_gain, cmp_pe, cmp_w1, cmp_w2):
    B, S, _ = q.shape
    G, HPG, DH = NSA_KV_GROUPS, NSA_HPG, NSA_DH
    scale = DH ** -0.5
    heads_kv = lambda t: t.reshape(B, S, G, DH)
    qh = rope(rms_norm(q.reshape(B, S, NSA_HEADS, DH), q_gain), positions)
    k_win = rope(rms_norm(heads_kv(kw), k_gain[2]), positions)
    v_win = heads_kv(vw)
    k_sel = rope(rms_norm(heads_kv(ks), k_gain[1]), positions)
    v_sel = heads_kv(vs)
    n_cmp = (S - CMP_LEN) // CMP_STRIDE + 1
    starts = jnp.arange(n_cmp) * CMP_STRIDE
    tok = starts[:, None] + jnp.arange(CMP_LEN)[None, :]

    def compress(t, pe, w1, w2):
        blk = heads_kv(t)[:, tok] + pe[None, None, :, None, :]
        blk = jnp.moveaxis(blk, 3, 2).reshape(B, n_cmp, G, CMP_LEN * DH)
        return jax.nn.gelu(blk @ w1) @ w2

    k_cmp = compress(kc, cmp_pe[0], cmp_w1[0], cmp_w2[0])
    k_cmp = rope(rms_norm(k_cmp, k_gain[0]), positions[:, tok[:, -1]])
    v_cmp = compress(vc, cmp_pe[1], cmp_w1[1], cmp_w2[1])

    q_g = qh.reshape(B, S, G, HPG, DH).transpose(0, 2, 3, 1, 4)
    gm = lambda t: t.transpose(0, 2, 1, 3)
    kcg, vcg = gm(k_cmp), gm(v_cmp)
    n_sel = S // SEL_BLOCK
    topk = min(SEL_TOPK, n_sel)
    ksb = gm(k_sel).reshape(B, G, n_sel, SEL_BLOCK, DH)
    vsb = gm(v_sel).reshape(B, G, n_sel, SEL_BLOCK, DH)
    pad = ((0, 0), (0, 0), (WINDOW, 0), (0, 0))
    kwp = jnp.pad(gm(k_win), pad)
    vwp = jnp.pad(gm(v_win), pad)
    gates = jax.nn.sigmoid(g_logits.astype(jnp.float32)).reshape(B, S, G, HPG, 3).transpose(0, 2, 3, 1, 4)
    ci = starts[:, None]
    sj = jnp.arange(n_sel)[None, :] * SEL_BLOCK
    cover = ((ci < sj + SEL_BLOCK) & (ci + CMP_LEN > sj)).astype(jnp.float32)
    cmp_end = starts + CMP_LEN - 1
    jj = jnp.arange(n_sel)
    bi = jnp.arange(B)[:, None, None, None]
    gi = jnp.arange(G)[None, :, None, None]

    def block(i):
        qs = i * Q_BLOCK
        qb = lax.dynamic_slice_in_dim(q_g, qs, Q_BLOCK, axis=3)
        t = qs + jnp.arange(Q_BLOCK)
        s_c = jnp.einsum("bghqd,bgkd->bghqk", qb, kcg).astype(jnp.float32) * scale
        p_c = masked_softmax(s_c, cmp_end[None, :] <= t[:, None])
        o_c = jnp.einsum("bghqk,bgkd->bghqd", p_c, vcg)
        imp = jnp.einsum("bghqk,kj->bgqj", p_c, cover)
        cur = t // SEL_BLOCK
        forced = (jj[None] == 0) | (jj[None] == cur[:, None]) | (jj[None] == cur[:, None] - 1)
        valid = jj[None] * SEL_BLOCK <= t[:, None]
        sel_score = jnp.where(valid, imp + jnp.where(forced, FORCE_BONUS, 0.0), NEG_INF)
        _, idx = lax.top_k(sel_score, topk)
        kb = ksb[bi, gi, idx]
        vb = vsb[bi, gi, idx]
        kpos = idx[..., None] * SEL_BLOCK + jnp.arange(SEL_BLOCK)
        smask = (kpos <= t[:, None, None]).reshape(B, G, 1, Q_BLOCK, topk * SEL_BLOCK)
        s_s = jnp.einsum("bghqd,bgqnkd->bghqnk", qb, kb).astype(jnp.float32) * scale
        p_s = masked_softmax(s_s.reshape(B, G, HPG, Q_BLOCK, topk * SEL_BLOCK), smask)
        o_s = jnp.einsum("bghqm,bgqmd->bghqd", p_s,
                         vb.reshape(B, G, Q_BLOCK, topk * SEL_BLOCK, DH))
        kwb = lax.dynamic_slice_in_dim(kwp, qs, WINDOW + Q_BLOCK, axis=2)
        vwb = lax.dynamic_slice_in_dim(vwp, qs, WINDOW + Q_BLOCK, axis=2)
        kpw = qs - WINDOW + jnp.arange(WINDOW + Q_BLOCK)
        diff = t[:, None] - kpw[None, :]
        wmask = (diff >= 0) & (diff < WINDOW) & (kpw[None, :] >= 0)
        s_w = jnp.einsum("bghqd,bgkd->bghqk", qb, kwb).astype(jnp.float32) * scale
        p_w = masked_softmax(s_w, wmask)
        o_w = jnp.einsum("bghqk,bgkd->bghqd", p_w, vwb)
        gb = lax.dynamic_slice_in_dim(gates, qs, Q_BLOCK, axis=3)
        return gb[..., 0:1] * o_c + gb[..., 1:2] * o_s + gb[..., 2:3] * o_w

    outs = lax.map(block, jnp.arange(S // Q_BLOCK))
    return outs.transpose(1, 0, 4, 2, 3, 5).reshape(B, S, NSA_Q_W)


def mixer_ab(h, positions, w_in, w_out, gla_w_gate2, gla_b_gate, gla_norm_g,
             q_gain, k_gain, cmp_pe, cmp_w1, cmp_w2):
    offs = np.cumsum(AB_SPLITS)[:-1].tolist()
    (gq, gk, gv, g_lr, g_r, nq, kc, vc, ks, vs, kw, vw, ng) = jnp.split(h @ w_in, offs, axis=-1)
    o_a = gla_mixer(gq, gk, gv, g_lr, g_r, gla_w_gate2, gla_b_gate, gla_norm_g)
    o_b = nsa_mixer(nq, kc, vc, ks, vs, kw, vw, ng, positions, q_gain, k_gain, cmp_pe, cmp_w1, cmp_w2)
    return jnp.concatenate([o_a.astype(h.dtype), o_b.astype(h.dtype)], axis=-1) @ w_out


def mixer_c(h, w_in, norm_g, w_s, b_s, w_out):
    B, S, _ = h.shape
    z = jax.nn.gelu(h @ w_in)
    u, v = jnp.split(z, 2, axis=-1)
    v = rms_norm(v, norm_g)
    n = S // SGU_CHUNK
    v = v.reshape(B, n, SGU_CHUNK, SGU_GROUPS, SGU_GROUP_DIM)
    w = w_s * jnp.tril(jnp.ones((SGU_CHUNK, SGU_CHUNK), dtype=w_s.dtype))
    mix = jnp.einsum("gts,bnsgc->bntgc", w, v) + b_s.T[None, None, :, :, None]
    return (u * mix.reshape(B, S, SGU_WIDTH)) @ w_out


def moe(h, w_router, router_bias, w_gate, w_up, w_down):
    B, S, D = h.shape
    ht = h.reshape(B * S, D)
    scores = jax.nn.sigmoid((ht @ w_router).astype(jnp.float32))
    sel = scores + router_bias.astype(jnp.float32)
    grp = sel.reshape(-1, N_EXPERT_GROUPS, EXPERTS_PER_GROUP)
    group_score = lax.top_k(grp, MOE_TOPK)[0].sum(-1)
    g_idx = jnp.argmax(group_score, axis=-1)
    in_grp = (jnp.arange(N_EXPERTS) // EXPERTS_PER_GROUP)[None, :] == g_idx[:, None]
    _, e_idx = lax.top_k(jnp.where(in_grp, sel, NEG_INF), MOE_TOPK)
    wts = jnp.take_along_axis(scores, e_idx, axis=-1)
    wts = wts / jnp.sum(wts, axis=-1, keepdims=True)
    combine = jnp.sum(jax.nn.one_hot(e_idx, N_EXPERTS, dtype=jnp.float32) * wts[..., None], axis=1)
    out = jnp.zeros((B * S, D), jnp.float32)
    for e in range(N_EXPERTS):
        hid = jax.nn.silu(ht @ w_gate[e]) * (ht @ w_up[e])
        out = out + combine[:, e:e + 1] * (hid @ w_down[e])
    return out.reshape(B, S, D).astype(h.dtype)


def setup_inputs(seed: int = 0) -> dict:
    key = jax.random.key(seed)
    ks = jax.random.split(key, 32)
    f32 = jnp.float32
    n_even = (DEPTH + 1) // 2
    n_odd = DEPTH // 2
    nrm = lambda k, shape, s: jax.random.normal(k, shape, f32) * s
    x = nrm(ks[0], (BATCH, SEQ, D_MODEL), 1.0)
    c = nrm(ks[1], (BATCH, D_MODEL), 1.0)
    positions = (jax.random.randint(ks[2], (BATCH, 1), 0, 4096)
                 + jnp.arange(SEQ, dtype=jnp.int32)[None, :]).astype(jnp.int32)
    return {
        "x": x,
        "c": c,
        "positions": positions,
        "w_ada": nrm(ks[3], (DEPTH, D_MODEL, 6 * D_MODEL), 0.5 * D_MODEL ** -0.5),
        "b_ada": nrm(ks[4], (DEPTH, 6 * D_MODEL), 0.02),
        "norm_g": 1.0 + nrm(ks[5], (DEPTH, 2, D_MODEL), 0.01),
        "w_in_ab": nrm(ks[6], (n_even, D_MODEL, AB_IN_WIDTH), D_MODEL ** -0.5),
        "w_out_ab": nrm(ks[7], (n_even, AB_MIX_WIDTH, D_MODEL), AB_MIX_WIDTH ** -0.5),
        "gla_w_gate2": nrm(ks[8], (n_even, GLA_GATE_RANK, GLA_QK_W), GLA_GATE_RANK ** -0.5),
        "gla_b_gate": nrm(ks[9], (n_even, GLA_QK_W), 0.1),
        "gla_norm_g": 1.0 + nrm(ks[10], (n_even, GLA_DV), 0.01),
        "nsa_q_gain": 1.0 + nrm(ks[11], (n_even, NSA_DH), 0.01),
        "nsa_k_gain": 1.0 + nrm(ks[12], (n_even, 3, NSA_DH), 0.01),
        "nsa_cmp_pe": nrm(ks[13], (n_even, 2, CMP_LEN, NSA_DH), 0.1),
        "nsa_cmp_w1": nrm(ks[14], (n_even, 2, CMP_LEN * NSA_DH, CMP_HIDDEN), (CMP_LEN * NSA_DH) ** -0.5),
        "nsa_cmp_w2": nrm(ks[15], (n_even, 2, CMP_HIDDEN, NSA_DH), CMP_HIDDEN ** -0.5),
        "w_in_c": nrm(ks[16], (n_odd, D_MODEL, 2 * SGU_WIDTH), D_MODEL ** -0.5),
        "sgu_norm_g": 1.0 + nrm(ks[17], (n_odd, SGU_WIDTH), 0.01),
        "sgu_w_s": nrm(ks[18], (n_odd, SGU_GROUPS, SGU_CHUNK, SGU_CHUNK), SGU_CHUNK ** -0.5),
        "sgu_b_s": 1.0 + nrm(ks[19], (n_odd, SGU_GROUPS, SGU_CHUNK), 0.02),
        "w_out_c": nrm(ks[20], (n_odd, SGU_WIDTH, D_MODEL), SGU_WIDTH ** -0.5),
        "w_router": nrm(ks[21], (D_MODEL, N_EXPERTS), D_MODEL ** -0.5),
        "router_bias": nrm(ks[22], (N_EXPERTS,), 0.01),
        "w_gate": nrm(ks[23], (DEPTH, N_EXPERTS, D_MODEL, EXPERT_HIDDEN), D_MODEL ** -0.5),
        "w_up": nrm(ks[24], (DEPTH, N_EXPERTS, D_MODEL, EXPERT_HIDDEN), D_MODEL ** -0.5),
        "w_down": nrm(ks[25], (DEPTH, N_EXPERTS, EXPERT_HIDDEN, D_MODEL), EXPERT_HIDDEN ** -0.5),
    }


def reference(x, c, positions, w_ada, b_ada, norm_g, w_in_ab, w_out_ab, gla_w_gate2,
              gla_b_gate, gla_norm_g, nsa_q_gain, nsa_k_gain, nsa_cmp_pe, nsa_cmp_w1,
              nsa_cmp_w2, w_in_c, sgu_norm_g, sgu_w_s, sgu_b_s, w_out_c, w_router,
              router_bias, w_gate, w_up, w_down):
    cond = jax.nn.silu(c)
    for layer in range(DEPTH):
        mod = cond @ w_ada[layer] + b_ada[layer]
        shift_t, scale_t, gate_t, shift_c, scale_c, gate_c = jnp.split(mod, 6, axis=-1)
        h = modulate(rms_norm(x, norm_g[layer, 0]), shift_t, scale_t)
        i = layer // 2
        if layer % 2 == 0:
            y = mixer_ab(h, positions, w_in_ab[i], w_out_ab[i], gla_w_gate2[i], gla_b_gate[i],
                         gla_norm_g[i], nsa_q_gain[i], nsa_k_gain[i], nsa_cmp_pe[i],
                         nsa_cmp_w1[i], nsa_cmp_w2[i])
        else:
            y = mixer_c(h, w_in_c[i], sgu_norm_g[i], sgu_w_s[i], sgu_b_s[i], w_out_c[i])
        x = x + gate_t[:, None, :] * y.astype(x.dtype)
        h = modulate(rms_norm(x, norm_g[layer, 1]), shift_c, scale_c)
        x = x + gate_c[:, None, :] * moe(h, w_router, router_bias, w_gate[layer], w_up[layer], w_down[layer])
    return x
```

```python
import contextlib
import types
import numpy as np
import ml_dtypes
import concourse.bass as bass
import concourse.mybir as mybir
from concourse.bass_utils import run_bass_kernel_spmd

F32 = mybir.dt.float32
BF16 = mybir.dt.bfloat16
I32 = mybir.dt.int32
AF = mybir.ActivationFunctionType
ALU = mybir.AluOpType
AX = mybir.AxisListType

ENGS = ("pe", "dve", "act", "pool", "sp")
NCORES = 8
D = 1024
NT = 16
TOK = 2048
EPS = 1e-6


PSUM_KEYS = set(["b%d" % i for i in range(8)] + ["nb%d" % i for i in range(8)] +
                ["cph0", "cph1", "cpb", "cpo0", "cpo1", "cpt", "pT0", "pT1", "pg0", "pg1", "pu0", "pu1", "py0", "py1",
                 "pv0", "pv1", "pm0", "pm1", "pcol", "prow0", "prow1", "opy0", "opy1"] + ["gb%d" % i for i in range(8)])


class Sched:
    def __init__(self, nc, stack):
        self.nc = nc
        self.stack = stack
        self.q = {e: [] for e in ENGS}
        self.last_w = {}
        self.readers = {}
        self.dma_sems = {}
        self.esem = {e: stack.enter_context(nc.semaphore("s_" + e)) for e in ENGS}
        self.ecount = {e: 0 for e in ENGS}
        self.seen = {e: {} for e in ENGS}
        self.phase_end = {}
        self.phase = 0
        self.alias = {}
        self.slot_map = {}
        self.sem_pool = []

    def _deps(self, eng, r, w):
        deps = []
        r = [self.alias.get(k, k) for k in r]
        w = [self.alias.get(k, k) for k in w]
        for k in r:
            t = self.last_w.get(k)
            if t is not None:
                deps.append(t)
            if k in PSUM_KEYS:
                deps.extend(x for x in self.readers.get(k, ()) if not (x[0] == "eng" and x[1] == eng))
        for k in w:
            t = self.last_w.get(k)
            if t is not None:
                deps.append(t)
            deps.extend(self.readers.get(k, ()))
        best = {}
        for t in deps:
            key = (t[0], t[1], t[3] if t[0] == "eng" else 0)
            if key not in best or best[key] < t[2]:
                best[key] = t[2]
        out = []
        for (kind, name, ph), v in best.items():
            if kind == "eng" and name == eng and (eng == "pe" or not Sched.same_engine_sync):
                continue
            out.append((kind, name, v, ph))
        return out

    def _commit(self, tok, r, w):
        r = [self.alias.get(k, k) for k in r]
        w = [self.alias.get(k, k) for k in w]
        for k in w:
            self.last_w[k] = tok
            self.readers[k] = []
        for k in r:
            self.readers.setdefault(k, []).append(tok)

    limit = None
    count = 0
    recycle = True
    sim_mode = False
    uniq = 0
    same_engine_sync = True

    @staticmethod
    def _freeze(fn):
        if fn.__closure__ is None:
            return fn
        cells = tuple(types.CellType(c.cell_contents) for c in fn.__closure__)
        g = types.FunctionType(fn.__code__, fn.__globals__, fn.__name__, fn.__defaults__, cells)
        g.__kwdefaults__ = fn.__kwdefaults__
        return g

    def op(self, eng, fn, r=(), w=()):
        fn = self._freeze(fn)
        Sched.count += 1
        if Sched.limit is not None and Sched.count > Sched.limit:
            return
        deps = self._deps(eng, r, w)
        idx = len(self.q[eng])
        self.q[eng].append(dict(kind="op", fn=fn, deps=deps))
        self._commit(("eng", eng, idx, self.phase), r, w)

    def dma(self, eng, out, in_, r=(), w=(), slot=None, **kw):
        Sched.count += 1
        if Sched.limit is not None and Sched.count > Sched.limit:
            return
        deps = self._deps(eng, r, w)
        if slot is None:
            slot = w[0]
        if eng == "pool":
            Sched.uniq += 1
            sid = "sw%d" % Sched.uniq
            self.dma_sems[sid] = [self.stack.enter_context(self.nc.semaphore(sid)), 0]
            slot = sid
        else:
            if slot not in self.slot_map:
                n = len(self.slot_map)
                if n >= len(self.sem_pool):
                    self.sem_pool.append(n)
                    self.dma_sems[n] = [self.stack.enter_context(self.nc.semaphore("d%d" % n)), 0]
                self.slot_map[slot] = n
            slot = self.slot_map[slot]
        self.dma_sems[slot][1] += 16
        val = self.dma_sems[slot][1]
        self.q[eng].append(dict(kind="dma", out=out, in_=in_, deps=deps, slot=slot, kw=kw))
        self._commit(("dma", slot, val, 0), r, w)

    def wait_all(self, eng, keys):
        deps = self._deps(eng, keys, ())
        self.q[eng].append(dict(kind="wait", deps=deps))

    def flush(self, barrier=True):
        nc = self.nc
        ph = self.phase
        miles = {e: set() for e in ENGS}
        for e in ENGS:
            for o in self.q[e]:
                for (kind, name, v, p) in o["deps"]:
                    if kind == "eng" and p == ph:
                        miles[name].add(v)
        for e in ENGS:
            n = len(self.q[e])
            if n:
                last = max(i for i, o in enumerate(self.q[e]) if o["kind"] != "wait") if any(
                    o["kind"] != "wait" for o in self.q[e]) else None
                if last is not None and self.q[e][last]["kind"] == "op":
                    miles[e].add(last)
        rank = {}
        for e in ENGS:
            for i, v in enumerate(sorted(miles[e])):
                rank[(e, v)] = self.ecount[e] + i + 1
        prev_end = dict(self.phase_end)
        prev_dma = dict(getattr(self, 'dma_totals', {}))
        with nc.Block() as block:
            engobj = {"pe": block.tensor, "dve": block.vector, "act": block.scalar,
                      "pool": block.gpsimd, "sp": block.sync}

            def make(ename):
                def body(eng):
                    seen = self.seen[ename]

                    def wait(sem, key, v):
                        if seen.get(key, 0) >= v:
                            return
                        eng.wait_ge(sem, v)
                        seen[key] = v
                    if barrier:
                        for oe, v in prev_end.items():
                            if oe != ename and v > 0:
                                wait(self.esem[oe], oe, v)
                        for slot, tot in prev_dma.items():
                            wait(self.dma_sems[slot][0], "d:%s" % (slot,), tot)
                    for i, o in enumerate(self.q[ename]):
                        for (kind, name, v, p) in o["deps"]:
                            if kind == "eng":
                                if p == ph:
                                    if name == ename:
                                        wait(self.esem[name], name, rank[(name, v)])
                                    else:
                                        wait(self.esem[name], name, rank[(name, v)])
                                else:
                                    if name != ename and prev_end.get(name, 0) > 0:
                                        wait(self.esem[name], name, prev_end[name])
                            else:
                                wait(self.dma_sems[name][0], "d:%s" % (name,), v)
                        if o["kind"] == "op":
                            ins = o["fn"](eng)
                            if i in miles[ename]:
                                ins.then_inc(self.esem[ename], 1)
                        elif o["kind"] == "dma":
                            ins = eng.dma_start(out=o["out"], in_=o["in_"], **o["kw"])
                            ins.then_inc(self.dma_sems[o["slot"]][0], 16)
                return body
            for e in ENGS:
                if self.q[e] or barrier:
                    engobj[e](make(e))
        for e in ENGS:
            self.ecount[e] += len(miles[e])
            self.phase_end[e] = self.ecount[e]
            self.q[e] = []
        self.phase += 1
        self.dma_totals = {slot: v[1] for slot, v in self.dma_sems.items()}
        if Sched.recycle:
            self.slot_map = {}


class Rec:
    def __init__(self):
        self.items = []

    def op(self, eng, fn, r=(), w=()):
        self.items.append(("op", eng, Sched._freeze(fn), tuple(r), tuple(w), None))

    def dma(self, eng, out, in_, r=(), w=(), slot=None, **kw):
        self.items.append(("dma", eng, (out, in_, slot, kw), tuple(r), tuple(w), None))


def interleave(S, a, b):
    ia = ib = 0
    na, nb = len(a.items), len(b.items)
    while ia < na or ib < nb:
        take_a = ib >= nb or (ia < na and ia * nb <= ib * na)
        it = a.items[ia] if take_a else b.items[ib]
        if take_a:
            ia += 1
        else:
            ib += 1
        if it[0] == "op":
            S.op(it[1], it[2], r=it[3], w=it[4])
        else:
            out, in_, slot, kw = it[2]
            S.dma(it[1], out, in_, r=it[3], w=it[4], slot=slot, **kw)


class Ctx:
    def __init__(self):
        self.nc = bass.Bass("TRN2", target_bir_lowering=False)
        self.outer = contextlib.ExitStack()
        self.S = Sched(self.nc, self.outer)
        self.uid = 0

    def dram_in(self, name, shape, dt=F32):
        return self.nc.dram_tensor(name, list(shape), dt, kind="ExternalInput").ap()

    def dram_out(self, name, shape, dt=F32):
        return self.nc.dram_tensor(name, list(shape), dt, kind="ExternalOutput").ap()

    def dram_tmp(self, name, shape, dt=F32):
        return self.nc.dram_tensor(name, list(shape), dt, kind="Internal").ap()

    def sb(self, stack, name, shape, dt):
        self.uid += 1
        return stack.enter_context(self.nc.sbuf_tensor("%s_%d" % (name, self.uid), list(shape), dt))

    def ps(self, stack, name, shape, dt=F32):
        self.uid += 1
        return stack.enter_context(self.nc.psum_tensor("%s_%d" % (name, self.uid), list(shape), dt))


def norm_transpose(C, S, xt, xkey, tmp, ident, pTs, tag):
    junk, ss, rs, xn = tmp["junk"], tmp["ss"], tmp["rs"], tmp["xn"]
    kj, kss, krs, kxn = [tag + s for s in ("junk", "ss", "rs", "xn")]
    S.op("act", lambda e: e.activation(out=junk[:], in_=xt, func=AF.Square, accum_out=ss[:]),
         r=[xkey], w=[kj, kss])
    if tmp.get("lnexp"):
        S.op("act", lambda e: e.activation(out=rs[:], in_=ss[:], func=AF.Ln, scale=1.0 / D, bias=tmp["eps"][:]),
             r=[kss], w=[krs])
        S.op("act", lambda e: e.activation(out=rs[:], in_=rs[:], func=AF.Exp, scale=-0.5), r=[krs], w=[krs])
    else:
        S.op("act", lambda e: e.activation(out=rs[:], in_=ss[:], func=AF.Sqrt, scale=1.0 / D, bias=tmp["eps"][:]),
             r=[kss], w=[krs])
        S.op("dve", lambda e: e.reciprocal(out=rs[:], in_=rs[:]), r=[krs], w=[krs])
    S.op("dve", lambda e: e.tensor_scalar(out=xn[:], in0=xt, scalar1=rs[:, 0:1], scalar2=None, op0=ALU.mult),
         r=[xkey, krs], w=[kxn])
    for k in range(8):
        p, pk = pTs[k // 4]
        S.op("pe", lambda e, k=k, p=p: e.transpose(out=p[:, k % 4, :], in_=xn[:, k * 128:(k + 1) * 128],
                                                   identity=ident[:]),
             r=[kxn, "ident"], w=[pk])


def phase_ada(C, P, dr):
    nc, S = C.nc, C.S
    with contextlib.ExitStack() as st:
        ccol = C.sb(st, "ccol", [128, 8], F32)
        cond = C.sb(st, "cond", [128, 8], F32)
        condbc = C.sb(st, "condbc", [128, 8, 128], F32)
        ones = C.sb(st, "ones", [128, 128], F32)
        bcol = C.sb(st, "bcol", [128, 2, 48], F32)
        gcol = C.sb(st, "gcol", [128, 2, 2, 8], F32)
        mcol = C.sb(st, "mcol", [128, 2, 48], F32)
        wblk = [C.sb(st, "wblk%d" % i, [128, 8, 512], F32) for i in range(2)]
        brow = [C.sb(st, "brow%d" % i, [128, 512], F32) for i in range(2)]
        pcol = C.ps(st, "pcol", [128, 512], F32)
        prow = [C.ps(st, "prow%d" % i, [128, 512], F32) for i in range(2)]
        S.dma("sp", ccol[:], dr["c_col"], w=["ccol"])
        S.dma("sp", bcol[:], dr["b_ada_col"], w=["bcol"])
        S.dma("sp", gcol[:], dr["norm_g_col"], w=["gcol"])
        S.op("act", lambda e: e.activation(out=cond[:], in_=ccol[:], func=AF.Silu), r=["ccol"], w=["cond"])
        S.op("dve", lambda e: e.memset(ones[:], 1.0), w=["ones"])
        for k in range(8):
            S.op("dve", lambda e, k=k: e.tensor_scalar(out=condbc[:, k, :], in0=ones[:], scalar1=cond[:, k:k + 1],
                                                       scalar2=None, op0=ALU.mult),
                 r=["ones", "cond"], w=["condbc"])
        nrow = 0
        modrow = [C.sb(st, "modrow%d" % i, [128, 512], F32) for i in range(2)]
        pcT = C.ps(st, "pcT", [128, 4, 128], F32)
        for l in range(2):
            for nb in range(12):
                i = (l * 12 + nb) % 2
                for hk_ in range(2):
                    S.dma("sp" if hk_ == 0 else "act", wblk[i][:, hk_ * 4:(hk_ + 1) * 4, :],
                          dr["w_ada"][l, hk_ * 512:(hk_ + 1) * 512, nb * 512:(nb + 1) * 512].rearrange("(k p) n -> p k n", p=128),
                          w=["wblk%d_%d" % (i, hk_)])
                sub6 = nb // 2
                j = nrow % 2
                nrow += 1
                S.dma("sp", brow[j][:], dr["b_ada"][l, nb * 512:(nb + 1) * 512].partition_broadcast(128), w=["brow%d" % j])
                for k in range(8):
                    S.op("pe", lambda e, k=k, i=i, j=j: e.matmul(prow[j][:], lhsT=condbc[:, k, :], rhs=wblk[i][:, k, :],
                                                                 start=(k == 0), stop=(k == 7)),
                         r=["condbc", "wblk%d_0" % i, "wblk%d_1" % i], w=["prow%d" % j])
                if sub6 in (2, 5):
                    gs = 0 if sub6 == 2 else 1
                    half = nb % 2
                    S.op("dve", lambda e, j=j, l=l, gs=gs, half=half: e.tensor_tensor(
                        out=P["gates"][:, l, gs, half * 512:(half + 1) * 512], in0=prow[j][:], in1=brow[j][:], op=ALU.add),
                        r=["prow%d" % j, "brow%d" % j], w=["gates"])
                else:
                    S.op("dve", lambda e, j=j: e.tensor_tensor(out=modrow[j][:], in0=prow[j][:], in1=brow[j][:], op=ALU.add),
                         r=["prow%d" % j, "brow%d" % j], w=["modrow%d" % j])
                    for m in range(4):
                        S.op("pe", lambda e, j=j, m=m: e.transpose(out=pcT[:, m, :], in_=modrow[j][:, m * 128:(m + 1) * 128], identity=P["ident"][:]),
                             r=["modrow%d" % j, "ident"], w=["pcol"])
                    S.op("dve", lambda e, l=l, nb=nb: e.tensor_copy(out=mcol[:, l, nb * 4:nb * 4 + 4], in_=pcT[:, :, 0]),
                         r=["pcol"], w=["mcol"])
            for s in range(2):
                S.op("dve", lambda e, l=l, s=s: e.scalar_tensor_tensor(
                    out=P["modA"][:, l, s, :], in0=mcol[:, l, s * 24 + 8:s * 24 + 16], scalar=1.0, in1=gcol[:, l, s, :],
                    op0=ALU.add, op1=ALU.mult), r=["mcol", "gcol"], w=["modA"])
                S.op("dve", lambda e, l=l, s=s: e.tensor_copy(out=P["modB"][:, l, s, :], in_=mcol[:, l, s * 24:s * 24 + 8]),
                     r=["mcol"], w=["modB"])
        S.flush()


MOE_INTERLEAVE = True
MOE_LNEXP = False


def phase_moe(C, P, dr, layer, x_in, x_out):
    nc, S = C.nc, C.S
    L = layer
    with contextlib.ExitStack() as st:
        X = C.sb(st, "X", [128, NT, D], F32)
        hT = C.sb(st, "hT", [128, 8, TOK], BF16)
        comb = C.sb(st, "comb", [128, NT, 16], F32)
        tmpn2 = [dict(junk=C.sb(st, "junk%d" % i, [128, D], BF16), ss=C.sb(st, "ss%d" % i, [128, 1], F32),
                      rs=C.sb(st, "rs%d" % i, [128, 1], F32), xn=C.sb(st, "xn%d" % i, [128, D], F32), eps=P["eps"], lnexp=MOE_LNEXP) for i in range(2)]
        hTf = [C.sb(st, "hTf%d" % i, [128, 8, 128], F32) for i in range(2)]
        wr = C.sb(st, "wr", [128, 8, 16], F32)
        rb = C.sb(st, "rb", [128, 16], F32)
        rt2 = [{n: C.sb(st, "rt%d_" % i + n, [128, 16], F32) for n in
                ("sc", "sel", "eq1", "sel2", "eq2", "selm", "wts")} for i in range(2)]
        r42 = [{n: C.sb(st, "r4%d_" % i + n, [128, 4], F32) for n in ("m1", "m2", "gs", "ing")} for i in range(2)]
        r12 = [{n: C.sb(st, "r1%d_" % i + n, [128, 1], F32) for n in ("gmax", "den")} for i in range(2)]
        wpk = [C.sb(st, "wpk%d" % i, [128, 12288], BF16) for i in range(2)]
        wg = [wpk[i][:, 0:4096].rearrange("p (k n) -> p k n", k=8) for i in range(2)]
        wu = [wpk[i][:, 4096:8192].rearrange("p (k n) -> p k n", k=8) for i in range(2)]
        wd = [wpk[i][:, 8192:12288].rearrange("p (k n) -> p k n", k=4) for i in range(2)]
        sg = [C.sb(st, "sg%d" % i, [128, 512], F32) for i in range(2)]
        hid = [C.sb(st, "hid%d" % i, [128, 4, 512], BF16) for i in range(2)]
        ev = [C.sb(st, "ev%d" % i, [128, 512], F32) for i in range(2)]
        pT = [C.ps(st, "pT%d" % i, [128, 4, 128], F32) for i in range(2)]
        pg = [C.ps(st, "pg%d" % i, [128, 512], F32) for i in range(2)]
        pu = [C.ps(st, "pu%d" % i, [128, 512], F32) for i in range(2)]
        py = [C.ps(st, "py%d" % i, [128, 512], F32) for i in range(2)]
        plog2 = [pg[0], pg[1]]

        S.dma("sp", wr[:], dr["w_router"].rearrange("(k p) n -> p k n", p=128), w=["wr"])
        S.dma("sp", rb[:], dr["router_bias"].partition_broadcast(128), w=["rb"])

        def load_w(e):
            i = e % 2
            S.dma("pool", wpk[i][:], dr["w_exp"][L, e], w=["wg%d" % i, "wu%d" % i, "wd%d" % i], slot="wpk%d" % i, max_dma_last_dim=8192)

        for t in range(NT):
            S.dma("sp", X[:, t, :], x_in[t * 128:(t + 1) * 128, :], w=["X%d" % t])
        load_w(0)
        load_w(1)
        A = P["modA"]
        B = P["modB"]
        def moe_pro(t, S):
            q_ = t % 2
            tmpn = tmpn2[q_]
            rt, r4, r1 = rt2[q_], r42[q_], r12[q_]
            plog = plog2[q_]
            T_ = "m%d" % q_
            pTq = [(pT[0], "pT0"), (pT[1], "pT1")] if q_ == 0 else [(pu[0][:].rearrange("p (a b) -> p a b", a=4), "pu0"),
                                                                      (pu[1][:].rearrange("p (a b) -> p a b", a=4), "pu1")]
            norm_transpose(C, S, X[:, t, :], "X%d" % t, tmpn, P["ident"], pTq, T_)
            hf = hTf[t % 2]
            hk = "hTf%d" % (t % 2)
            for k in range(8):
                p, pk = pTq[k // 4]
                if k % 2 == 0:
                    S.op("dve", lambda e, k=k, p=p, hf=hf: e.tensor_scalar(
                        out=hf[:, k, :], in0=p[:, k % 4, :], scalar1=A[:, L, 1, k:k + 1], scalar2=B[:, L, 1, k:k + 1],
                        op0=ALU.mult, op1=ALU.add), r=[pk, "modA", "modB"], w=[hk])
                else:
                    S.op("act", lambda e, k=k, p=p, hf=hf: e.activation(
                        out=hf[:, k, :], in_=p[:, k % 4, :], func=AF.Identity, scale=A[:, L, 1, k:k + 1],
                        bias=B[:, L, 1, k:k + 1]), r=[pk, "modA", "modB"], w=[hk])
            S.op("pool", lambda e, t=t, hf=hf: e.tensor_copy(out=hT[:, :, t * 128:(t + 1) * 128], in_=hf[:]),
                 r=[hk], w=["hT%d" % t])
            for k in range(8):
                S.op("pe", lambda e, k=k, hf=hf: e.matmul(plog[:, 0:16], lhsT=hf[:, k, :], rhs=wr[:, k, :],
                                                          start=(k == 0), stop=(k == 7)),
                     r=[hk, "wr"], w=["pg%d" % q_])
            sc, sel, eq1, sel2, eq2, selm, wts = [rt[n] for n in ("sc", "sel", "eq1", "sel2", "eq2", "selm", "wts")]
            m1, m2, gs, ing = [r4[n] for n in ("m1", "m2", "gs", "ing")]
            gmax, den = r1["gmax"], r1["den"]
            v4 = lambda a: a[:].rearrange("p (g e) -> p g e", e=4)
            b4 = lambda a: a[:].unsqueeze(2).to_broadcast([128, 4, 4])
            S.op("act", lambda e: e.activation(out=sc[:], in_=plog[:, 0:16], func=AF.Sigmoid), r=["pg%d" % q_], w=[T_ + "r_sc"])
            S.op("dve", lambda e: e.tensor_tensor(out=sel[:], in0=sc[:], in1=rb[:], op=ALU.add), r=[T_ + "r_sc", "rb"], w=[T_ + "r_sel"])
            S.op("dve", lambda e: e.tensor_reduce(out=m1[:], in_=v4(sel), axis=AX.X, op=ALU.max), r=[T_ + "r_sel"], w=[T_ + "r_m1"])
            S.op("dve", lambda e: e.tensor_tensor(out=v4(eq1), in0=v4(sel), in1=b4(m1), op=ALU.is_equal),
                 r=[T_ + "r_sel", T_ + "r_m1"], w=[T_ + "r_eq1"])
            S.op("dve", lambda e: e.scalar_tensor_tensor(out=sel2[:], in0=eq1[:], scalar=-1e9, in1=sel[:],
                                                         op0=ALU.mult, op1=ALU.add), r=[T_ + "r_eq1", T_ + "r_sel"], w=[T_ + "r_sel2"])
            S.op("dve", lambda e: e.tensor_reduce(out=m2[:], in_=v4(sel2), axis=AX.X, op=ALU.max), r=[T_ + "r_sel2"], w=[T_ + "r_m2"])
            S.op("dve", lambda e: e.tensor_tensor(out=gs[:], in0=m1[:], in1=m2[:], op=ALU.add), r=[T_ + "r_m1", T_ + "r_m2"], w=[T_ + "r_gs"])
            S.op("dve", lambda e: e.tensor_reduce(out=gmax[:], in_=gs[:], axis=AX.X, op=ALU.max), r=[T_ + "r_gs"], w=[T_ + "r_gmax"])
            S.op("dve", lambda e: e.tensor_scalar(out=ing[:], in0=gs[:], scalar1=gmax[:, 0:1], scalar2=None, op0=ALU.is_equal),
                 r=[T_ + "r_gs", T_ + "r_gmax"], w=[T_ + "r_ing"])
            S.op("dve", lambda e: e.tensor_tensor(out=v4(eq2), in0=v4(sel2), in1=b4(m2), op=ALU.is_equal),
                 r=[T_ + "r_sel2", T_ + "r_m2"], w=[T_ + "r_eq2"])
            S.op("dve", lambda e: e.tensor_tensor(out=selm[:], in0=eq1[:], in1=eq2[:], op=ALU.add), r=[T_ + "r_eq1", T_ + "r_eq2"], w=[T_ + "r_selm"])
            S.op("dve", lambda e: e.tensor_tensor(out=v4(selm), in0=v4(selm), in1=b4(ing), op=ALU.mult),
                 r=[T_ + "r_selm", T_ + "r_ing"], w=[T_ + "r_selm"])
            S.op("dve", lambda e: e.tensor_tensor(out=wts[:], in0=sc[:], in1=selm[:], op=ALU.mult), r=[T_ + "r_sc", T_ + "r_selm"], w=[T_ + "r_wts"])
            S.op("dve", lambda e: e.tensor_reduce(out=den[:], in_=wts[:], axis=AX.X, op=ALU.add), r=[T_ + "r_wts"], w=[T_ + "r_den"])
            S.op("dve", lambda e: e.reciprocal(out=den[:], in_=den[:]), r=[T_ + "r_den"], w=[T_ + "r_den"])
            S.op("dve", lambda e, t=t: e.tensor_scalar(out=comb[:, t, :], in0=wts[:], scalar1=den[:, 0:1], scalar2=None, op0=ALU.mult),
                 r=[T_ + "r_wts", T_ + "r_den"], w=["comb%d" % t])

        for t in range(0, NT, 2):
            if MOE_INTERLEAVE:
                ra, rb_ = Rec(), Rec()
                moe_pro(t, ra)
                moe_pro(t + 1, rb_)
                interleave(S, ra, rb_)
            else:
                moe_pro(t, S)
                moe_pro(t + 1, S)
        gate = P["gates"]
        n_g = 0
        n_y = 0
        for ex in range(16):
            i = ex % 2
            for tg in range(4):
                hb = hid[(ex * 4 + tg) % 2]
                hbk = "hid%d" % ((ex * 4 + tg) % 2)
                for hc in range(4):
                    j = n_g % 2
                    n_g += 1
                    hkeys = ["hT%d" % t for t in range(tg * 4, tg * 4 + 4)]
                    for k in range(8):
                        S.op("pe", lambda e, k=k, i=i, j=j, hc=hc, tg=tg: e.matmul(
                            pg[j][:], lhsT=wg[i][:, k, hc * 128:(hc + 1) * 128], rhs=hT[:, k, tg * 512:(tg + 1) * 512],
                            start=(k == 0), stop=(k == 7)), r=hkeys + ["wg%d" % i], w=["pg%d" % j])
                    for k in range(8):
                        S.op("pe", lambda e, k=k, i=i, j=j, hc=hc, tg=tg: e.matmul(
                            pu[j][:], lhsT=wu[i][:, k, hc * 128:(hc + 1) * 128], rhs=hT[:, k, tg * 512:(tg + 1) * 512],
                            start=(k == 0), stop=(k == 7)), r=hkeys + ["wu%d" % i], w=["pu%d" % j])
                    S.op("act", lambda e, j=j: e.activation(out=sg[j][:], in_=pg[j][:], func=AF.Silu),
                         r=["pg%d" % j], w=["sg%d" % j])
                    S.op("dve", lambda e, j=j, hb=hb, hc=hc: e.tensor_tensor(out=hb[:, hc, :], in0=sg[j][:], in1=pu[j][:], op=ALU.mult),
                         r=["sg%d" % j, "pu%d" % j], w=[hbk + "_%d" % hc])
                for tt in range(4):
                    t = tg * 4 + tt
                    for half in range(2):
                        j = n_y % 2
                        n_y += 1
                        for hc in range(4):
                            S.op("pe", lambda e, j=j, hb=hb, hc=hc, tt=tt, half=half, i=i: e.matmul(
                                py[j][:], lhsT=hb[:, hc, tt * 128:(tt + 1) * 128], rhs=wd[i][:, hc, half * 512:(half + 1) * 512],
                                start=(hc == 0), stop=(hc == 3)), r=[hbk + "_%d" % hc, "wd%d" % i], w=["py%d" % j])
                        S.op("dve", lambda e, j=j, t=t, ex=ex, half=half: e.scalar_tensor_tensor(
                            out=ev[j][:], in0=py[j][:], scalar=comb[:, t, ex:ex + 1], in1=gate[:, L, 1, half * 512:(half + 1) * 512],
                            op0=ALU.mult, op1=ALU.mult), r=["py%d" % j, "comb%d" % t, "gates"], w=["ev%d" % j])
                        S.op("pool", lambda e, j=j, t=t, half=half: e.tensor_tensor(
                            out=X[:, t, half * 512:(half + 1) * 512], in0=X[:, t, half * 512:(half + 1) * 512], in1=ev[j][:], op=ALU.add),
                            r=["ev%d" % j, "X%d" % t], w=["X%d" % t])
            if ex + 2 < 16:
                load_w(ex + 2)
        for t in range(NT):
            S.dma("sp", x_out[t * 128:(t + 1) * 128, :], X[:, t, :], r=["X%d" % t], w=["xo_%s_%d" % (x_out.name, t % 4)])
        S.wait_all("sp", ["xo_%s_%d" % (x_out.name, j) for j in range(4)])
        S.flush()


def alloc_persistent(C):
    st = C.outer
    P = {}
    P["ident"] = C.sb(st, "ident", [128, 128], F32)
    P["eps"] = C.sb(st, "eps", [128, 1], F32)
    P["modA"] = C.sb(st, "modA", [128, 2, 2, 8], F32)
    P["modB"] = C.sb(st, "modB", [128, 2, 2, 8], F32)
    P["gates"] = C.sb(st, "gates", [128, 2, 2, D], F32)
    return P


def phase_init(C, P, dr):
    S = C.S
    S.dma("sp", P["ident"][:], dr["ident"], w=["ident"])
    S.op("dve", lambda e: e.memset(P["eps"][:], EPS), w=["eps"])


GELU = AF.Gelu_apprx_tanh


def phase_gmlp(C, P, dr, x_in, x_out):
    nc, S0 = C.nc, C.S
    L = 1
    with contextlib.ExitStack() as st:
        win = C.sb(st, "win", [128, 8, 4096], BF16)
        wout = C.sb(st, "wout", [128, 16, 1024], BF16)
        WT = C.sb(st, "WT", [128, 8, 128], BF16)
        wsf = C.sb(st, "wsf", [128, 8, 128], F32)
        tril = C.sb(st, "tril", [128, 128], F32)
        grow = C.sb(st, "grow", [128, 2048], F32)
        bsr = C.sb(st, "bsr", [1, 8, 128], BF16)
        bsf = C.sb(st, "bsf", [1, 8, 128], F32)
        ones1 = C.sb(st, "ones1", [1, 128], BF16)
        xt = [C.sb(st, "xt%d" % i, [128, D], F32) for i in range(2)]
        xo = [C.sb(st, "xo%d" % i, [128, D], F32) for i in range(2)]
        junk_sh = C.sb(st, "junk_sh", [128, D], BF16)
        tmpn2 = [dict(junk=junk_sh, ss=C.sb(st, "ss%d" % i, [128, 1], F32),
                      rs=C.sb(st, "rs%d" % i, [128, 1], F32), xn=C.sb(st, "xn%d" % i, [128, D], F32), eps=P["eps"]) for i in range(2)]
        hT = [C.sb(st, "hT%d" % i, [128, 8, 128], BF16) for i in range(2)]
        vz2 = [C.sb(st, "vz%d" % i, [128, 2048], F32) for i in range(2)]
        vss2 = [C.sb(st, "vss%d" % i, [128, 4], F32) for i in range(2)]
        vs12 = [C.sb(st, "vs1%d" % i, [128, 1], F32) for i in range(2)]
        vn2 = [C.sb(st, "vn%d" % i, [128, 2048], BF16) for i in range(2)]
        uT2 = [C.sb(st, "uT%d" % i, [128, 16, 128], BF16) for i in range(2)]
        pTt2 = [C.sb(st, "pTt%d" % i, [128, 16, 128], BF16) for i in range(2)]
        ev2 = [C.sb(st, "ev%d" % i, [128, 512], F32) for i in range(2)]
        bks = [bank(C, st, "gb%d" % i) for i in range(8)]
        v3 = lambda a_, n: a_.rearrange("p (a b) -> p a b", a=n)
        S = S0
        wsrc = dr["w_in_c"].rearrange("(k p) n -> p k n", p=128)
        S.dma("pool", win[:, :, 2048:4096], wsrc[:, :, 2048:4096], w=["win%d" % k for k in range(4)], slot="winv", max_dma_last_dim=8192)
        S.dma("pool", win[:, :, 0:2048], wsrc[:, :, 0:2048], w=["win%d" % k for k in range(4, 8)], slot="winu", max_dma_last_dim=8192)
        S.dma("pool", wout[:], dr["w_out_c"].rearrange("(k p) n -> p k n", p=128), w=["wout"])
        S.dma("sp", wsf[:], dr["sgu_w_s"].rearrange("g t s -> t g s"), w=["wsf"])
        S.dma("sp", tril[:], dr["tril"], w=["tril"])
        S.dma("sp", grow[:], dr["sgu_norm_g"].partition_broadcast(128), w=["grow"])
        S.dma("sp", bsf[:], dr["sgu_b_s"].rearrange("(o g) t -> o g t", o=1), w=["bsf"])
        S.op("dve", lambda e: e.tensor_copy(out=bsr[:], in_=bsf[:]), r=["bsf"], w=["bsr"])
        S.op("dve", lambda e: e.memset(ones1[:], 1.0), w=["ones1"])
        for g in range(8):
            S.op("dve", lambda e, g=g: e.tensor_tensor(out=wsf[:, g, :], in0=wsf[:, g, :], in1=tril[:], op=ALU.mult),
                 r=["wsf", "tril"], w=["wsf"])
        for g in range(8):
            p = v3(bks[g // 4][:], 4)
            S.op("pe", lambda e, g=g, p=p: e.transpose(out=p[:, g % 4, :], in_=wsf[:, g, :], identity=P["ident"][:]),
                 r=["wsf", "ident"], w=["gb%d" % (g // 4)])
        for i in range(2):
            S.op("dve", lambda e, i=i: e.tensor_copy(out=WT[:, i * 4:(i + 1) * 4, :], in_=v3(bks[i][:], 4)), r=["gb%d" % i], w=["WT"])

        A = P["modA"]
        B = P["modB"]
        gate = P["gates"]
        winkeys_v = ["win%d" % k for k in range(4)]
        winkeys_u = ["win%d" % k for k in range(4, 8)]

        def tile(t, S):
            i = t % 2
            q0, q1, q2, q3 = [bks[i * 4 + n] for n in range(4)]
            k0, k1, k2, k3 = ["gb%d" % (i * 4 + n) for n in range(4)]
            pT = [(v3(q0[:], 4), k0), (v3(q1[:], 4), k1)]
            pvb = [(q0, k0), (q1, k1)]
            pu, puk = v3(q2[:], 4), k2
            pm, pmk = v3(q3[:], 4), k3
            tmpn, vz, vss, vs1, vn, uT, pTt, ev = tmpn2[i], vz2[i], vss2[i], vs12[i], vn2[i], uT2[i], pTt2[i], ev2[i]
            T_ = "g%d" % i
            S.dma("sp", xt[i][:], x_in[t * 128:(t + 1) * 128, :], w=["xt%d" % i])
            norm_transpose(C, S, xt[i][:], "xt%d" % i, tmpn, P["ident"], pT, T_)
            h = hT[i]
            hk = "hT%d" % i
            for k in range(8):
                p, pk = pT[k // 4]
                if k % 2 == 0:
                    S.op("dve", lambda e, k=k, p=p, h=h: e.tensor_scalar(
                        out=h[:, k, :], in0=p[:, k % 4, :], scalar1=A[:, L, 0, k:k + 1], scalar2=B[:, L, 0, k:k + 1],
                        op0=ALU.mult, op1=ALU.add), r=[pk, "modA", "modB"], w=[hk])
                else:
                    S.op("act", lambda e, k=k, p=p, h=h: e.activation(
                        out=h[:, k, :], in_=p[:, k % 4, :], func=AF.Identity, scale=A[:, L, 0, k:k + 1],
                        bias=B[:, L, 0, k:k + 1]), r=[pk, "modA", "modB"], w=[hk])
            for n in range(4):
                pv_, pvk = pvb[n % 2]
                for k in range(8):
                    S.op("pe", lambda e, k=k, n=n, pv_=pv_, h=h: e.matmul(
                        pv_[:], lhsT=h[:, k, :], rhs=win[:, k, 2048 + n * 512: 2048 + (n + 1) * 512],
                        start=(k == 0), stop=(k == 7)), r=[hk] + winkeys_v, w=[pvk])
                S.op("act", lambda e, n=n, pv_=pv_: e.activation(out=vz[:, n * 512:(n + 1) * 512], in_=pv_[:], func=GELU),
                     r=[pvk], w=[T_ + "vz%d" % n])
                S.op("act", lambda e, n=n: e.activation(out=junk_sh[:, 0:512], in_=vz[:, n * 512:(n + 1) * 512], func=AF.Square,
                                                        accum_out=vss[:, n:n + 1]), r=[T_ + "vz%d" % n], w=[T_ + "vss%d" % n])
            S.op("dve", lambda e: e.tensor_reduce(out=vs1[:], in_=vss[:], axis=AX.X, op=ALU.add),
                 r=[T_ + "vss%d" % n for n in range(4)], w=[T_ + "vs1"])
            S.op("act", lambda e: e.activation(out=vs1[:], in_=vs1[:], func=AF.Sqrt, scale=1.0 / 2048, bias=P["eps"][:]),
                 r=[T_ + "vs1"], w=[T_ + "vs1"])
            S.op("dve", lambda e: e.reciprocal(out=vs1[:], in_=vs1[:]), r=[T_ + "vs1"], w=[T_ + "vs1"])
            for n in range(4):
                S.op("dve", lambda e, n=n: e.scalar_tensor_tensor(
                    out=vn[:, n * 512:(n + 1) * 512], in0=vz[:, n * 512:(n + 1) * 512], scalar=vs1[:, 0:1],
                    in1=grow[:, n * 512:(n + 1) * 512], op0=ALU.mult, op1=ALU.mult),
                    r=[T_ + "vz%d" % n, T_ + "vs1", "grow"], w=[T_ + "vn%d" % n])
            for q in range(4):
                for m in range(4):
                    c = q * 4 + m
                    for k in range(8):
                        S.op("pe", lambda e, k=k, c=c, m=m, h=h: e.matmul(
                            pu[:, m, :], lhsT=win[:, k, c * 128:(c + 1) * 128], rhs=h[:, k, :],
                            start=(k == 0), stop=(k == 7)), r=[hk] + winkeys_u, w=[puk])
                S.op("act", lambda e, q=q: e.activation(out=uT[:, q * 4:(q + 1) * 4, :], in_=pu, func=GELU),
                     r=[puk], w=[T_ + "uT%d" % q])
                for m in range(4):
                    c = q * 4 + m
                    g = c // 2
                    S.op("pe", lambda e, c=c, m=m, g=g: e.matmul(
                        pm[:, m, :], lhsT=vn[:, c * 128:(c + 1) * 128], rhs=WT[:, g, :], start=True, stop=False),
                        r=[T_ + "vn%d" % (c // 4), "WT"], w=[pmk])
                    S.op("pe", lambda e, c=c, m=m, g=g: e.matmul(
                        pm[:, m, :], lhsT=ones1[0:1, :], rhs=bsr[0:1, g, :], start=False, stop=True),
                        r=["ones1", "bsr"], w=[pmk])
                S.op("dve", lambda e, q=q: e.tensor_tensor(out=pTt[:, q * 4:(q + 1) * 4, :], in0=pm, in1=uT[:, q * 4:(q + 1) * 4, :],
                                                           op=ALU.mult), r=[pmk, T_ + "uT%d" % q], w=[T_ + "pTt%d" % q])
            o = xo[i]
            for half in range(2):
                pv_, pvk = pvb[half]
                for c in range(16):
                    S.op("pe", lambda e, c=c, half=half, pv_=pv_: e.matmul(
                        pv_[:], lhsT=pTt[:, c, :], rhs=wout[:, c, half * 512:(half + 1) * 512],
                        start=(c == 0), stop=(c == 15)), r=[T_ + "pTt%d" % (c // 4), "wout"], w=[pvk])
                S.op("dve", lambda e, half=half, pv_=pv_: e.tensor_tensor(
                    out=ev[:], in0=pv_[:], in1=gate[:, L, 0, half * 512:(half + 1) * 512], op=ALU.mult),
                    r=[pvk, "gates"], w=[T_ + "ev"])
                S.op("dve", lambda e, half=half, o=o: e.tensor_tensor(
                    out=o[:, half * 512:(half + 1) * 512], in0=ev[:], in1=xt[i][:, half * 512:(half + 1) * 512], op=ALU.add),
                    r=[T_ + "ev", "xt%d" % i], w=["xo%d_%d" % (i, half)])
            S.dma("sp", x_out[t * 128:(t + 1) * 128, :], o[:], r=["xo%d_0" % i, "xo%d_1" % i], w=["xo_%s_%d" % (x_out.name, i)])

        for t in range(0, NT, 2):
            ra, rb_ = Rec(), Rec()
            tile(t, ra)
            tile(t + 1, rb_)
            interleave(S0, ra, rb_)
        S0.wait_all("sp", ["xo_%s_%d" % (x_out.name, j) for j in range(2)])
        S0.flush()


NWT = 64
OWN0 = 48
DBG_SKIP = set()
TWO_PI = 6.283185307179586
CW1 = 6.28125
CW2 = TWO_PI - CW1
CA_Q, CA_K, CA_V, CA_GLR, CA_R = 0, 256, 512, 1024, 1040
CA_KC, CA_VC, CA_KV4 = 1552, 1680, 1808
NA = 2320


def bank(C, st, name):
    return C.ps(st, name, [128, 512], F32)


def rope_tables(C, S, st, pos_i, ncol, invf, out_cs, tag):
    pf = C.sb(st, tag + "pf", [128, ncol], F32)
    ang = C.sb(st, tag + "ang", [128, ncol, 8], F32)
    ki = C.sb(st, tag + "ki", [128, ncol, 8], I32)
    kf = C.sb(st, tag + "kf", [128, ncol, 8], F32)
    r = C.sb(st, tag + "r", [128, ncol, 8], F32)
    y = C.sb(st, tag + "y", [128, ncol, 8], F32)
    m = C.sb(st, tag + "m", [128, ncol, 8], F32)
    k = lambda s: tag + s
    S.op("dve", lambda e: e.tensor_copy(out=pf[:], in_=pos_i[:]), r=[k("pos")], w=[k("pf")])
    S.op("dve", lambda e: e.tensor_tensor(out=ang[:], in0=pf[:].unsqueeze(2).to_broadcast([128, ncol, 8]),
                                          in1=invf[:].unsqueeze(1).to_broadcast([128, ncol, 8]), op=ALU.mult),
         r=[k("pf"), "invf"], w=[k("ang")])
    S.op("dve", lambda e: e.tensor_scalar(out=ki[:], in0=ang[:], scalar1=1.0 / TWO_PI, scalar2=None, op0=ALU.mult),
         r=[k("ang")], w=[k("ki")])
    S.op("dve", lambda e: e.tensor_copy(out=kf[:], in_=ki[:]), r=[k("ki")], w=[k("kf")])
    S.op("dve", lambda e: e.scalar_tensor_tensor(out=r[:], in0=kf[:], scalar=-CW1, in1=ang[:], op0=ALU.mult, op1=ALU.add),
         r=[k("kf"), k("ang")], w=[k("r")])
    S.op("dve", lambda e: e.scalar_tensor_tensor(out=r[:], in0=kf[:], scalar=-CW2, in1=r[:], op0=ALU.mult, op1=ALU.add),
         r=[k("kf"), k("r")], w=[k("r")])
    for which, shift in ((1, 0.0), (0, np.pi / 2)):
        S.op("dve", lambda e, shift=shift: e.tensor_scalar(out=y[:], in0=r[:], scalar1=float(shift), scalar2=None, op0=ALU.add),
             r=[k("r")], w=[k("y")])
        S.op("dve", lambda e: e.tensor_scalar(out=m[:], in0=y[:], scalar1=float(np.pi), scalar2=None, op0=ALU.is_gt),
             r=[k("y")], w=[k("m")])
        S.op("dve", lambda e: e.scalar_tensor_tensor(out=y[:], in0=m[:], scalar=-TWO_PI, in1=y[:], op0=ALU.mult, op1=ALU.add),
             r=[k("m"), k("y")], w=[k("y")])
        S.op("dve", lambda e: e.tensor_scalar(out=y[:], in0=y[:], scalar1=float(np.pi), scalar2=-float(np.pi), op0=ALU.min, op1=ALU.max),
             r=[k("y")], w=[k("y")])
        S.op("act", lambda e, which=which: e.activation(out=out_cs[:, :, which * 8:(which + 1) * 8], in_=y[:], func=AF.Sin),
             r=[k("y")], w=[k("cs")])


def rms_gain_rope(C, S, src, srckey, dst, dstkey, ngrp, gain_row, gainkey, cs, cskey, T, tag):
    sq, ss, t1, t2 = T["sq"], T["ss"], T["t1"], T["t2"]
    k = lambda s: tag + s
    S.op("act", lambda e: e.activation(out=sq[:, 0:ngrp, :], in_=src, func=AF.Square), r=[srckey], w=[k("sq")])
    S.op("dve", lambda e: e.tensor_reduce(out=ss[:, 0:ngrp], in_=sq[:, 0:ngrp, :], axis=AX.X, op=ALU.add), r=[k("sq")], w=[k("ss")])
    S.op("act", lambda e: e.activation(out=ss[:, 0:ngrp], in_=ss[:, 0:ngrp], func=AF.Ln, scale=1.0 / 64, bias=T["eps"][:]),
         r=[k("ss")], w=[k("ss")])
    S.op("act", lambda e: e.activation(out=ss[:, 0:ngrp], in_=ss[:, 0:ngrp], func=AF.Exp, scale=-0.5), r=[k("ss")], w=[k("ss")])
    S.op("dve", lambda e: e.tensor_tensor(out=dst, in0=src, in1=ss[:, 0:ngrp].unsqueeze(2).to_broadcast([128, ngrp, 64]), op=ALU.mult),
         r=[srckey, k("ss")], w=[dstkey])
    S.op("dve", lambda e: e.tensor_tensor(out=dst, in0=dst, in1=gain_row.unsqueeze(1).to_broadcast([128, ngrp, 64]), op=ALU.mult),
         r=[dstkey, gainkey], w=[dstkey])
    cosb = cs[:, 0:8].unsqueeze(1).to_broadcast([128, ngrp, 8])
    sinb = cs[:, 8:16].unsqueeze(1).to_broadcast([128, ngrp, 8])
    x1 = dst[:, :, 0:8]
    x2 = dst[:, :, 8:16]
    S.op("dve", lambda e: e.tensor_tensor(out=t1[:, 0:ngrp, 0:8], in0=x1, in1=cosb, op=ALU.mult), r=[dstkey, cskey], w=[k("t1a")])
    S.op("dve", lambda e: e.tensor_tensor(out=t1[:, 0:ngrp, 8:16], in0=x2, in1=cosb, op=ALU.mult), r=[dstkey, cskey], w=[k("t1b")])
    S.op("dve", lambda e: e.tensor_tensor(out=t2[:, 0:ngrp, 0:8], in0=x2, in1=sinb, op=ALU.mult), r=[dstkey, cskey], w=[k("t2a")])
    S.op("dve", lambda e: e.tensor_tensor(out=t2[:, 0:ngrp, 8:16], in0=x1, in1=sinb, op=ALU.mult), r=[dstkey, cskey], w=[k("t2b")])
    S.op("dve", lambda e: e.tensor_tensor(out=x1, in0=t1[:, 0:ngrp, 0:8], in1=t2[:, 0:ngrp, 0:8], op=ALU.subtract),
         r=[k("t1a"), k("t2a"), k("t1b"), k("t2b")], w=[dstkey])
    S.op("dve", lambda e: e.tensor_tensor(out=x2, in0=t1[:, 0:ngrp, 8:16], in1=t2[:, 0:ngrp, 8:16], op=ALU.add),
         r=[k("t1b"), k("t2b")], w=[dstkey])


def alloc_mixer(C, st):
    M = {}
    M["KselT"] = C.sb(st, "KselT", [128, NWT * 128], BF16)
    M["Vsel"] = C.sb(st, "Vsel", [128, NWT, 2, 65], BF16)
    M["KwinT"] = C.sb(st, "KwinT", [128, 20 * 128], BF16)
    M["Vwin"] = C.sb(st, "Vwin", [128, 20, 2, 65], BF16)
    M["mixT"] = C.sb(st, "mixT", [128, 8, TOK], BF16)
    M["csT"] = C.sb(st, "csT", [128, NWT, 16], F32)
    M["kbias"] = C.sb(st, "kbias", [128, NWT], F32)
    M["gain"] = C.sb(st, "gain", [128, 4, 64], F32)
    M["invf"] = C.sb(st, "invf", [128, 8], F32)
    return M


def phase_mix_a(C, P, M, dr, dbg=None, tiles=None):
    nc, S = C.nc, C.S
    L = 0
    with contextlib.ExitStack() as st:
        wA = C.sb(st, "wA", [128, 8, NA], BF16)
        w2g = C.sb(st, "w2g", [16, 256], F32)
        negb = C.sb(st, "negb", [128, 2], F32)
        gng = C.sb(st, "gng", [128, 128], F32)
        maskT = C.sb(st, "maskT", [128, 4, 128], F32)
        posi = C.sb(st, "posi", [128, NWT], I32)
        valid = C.sb(st, "valid", [128, NWT], F32)
        onesc = C.sb(st, "onesc", [128, 64], F32)
        state = C.sb(st, "state", [128, 4, 128], F32)
        sbf = [C.sb(st, "sbf%d" % i, [128, 4, 128], BF16) for i in range(2)]
        xt = [C.sb(st, "xt%d" % i, [128, D], F32) for i in range(2)]
        tmpn = dict(junk=C.sb(st, "junk", [128, D], BF16), ss=C.sb(st, "ss", [128, 1], F32),
                    rs=C.sb(st, "rs", [128, 1], F32), xn=C.sb(st, "xn", [128, D], F32), eps=P["eps"], lnexp=True)
        hT = C.sb(st, "hT", [128, 8, 128], BF16)
        glrT = C.sb(st, "glrT", [16, 128], F32)
        vb2 = [C.sb(st, "vb%d" % i, [128, 512], BF16) for i in range(2)]
        qk2 = [C.sb(st, "qk%d" % i, [128, 4, 128], F32) for i in range(2)]
        sp2 = [C.sb(st, "sp%d" % i, [128, 2, 128], F32) for i in range(2)]
        kv42 = [C.sb(st, "kv4s%d" % i, [128, 512], F32) for i in range(2)]
        rs2 = [C.sb(st, "rsl%d" % i, [128, 512], F32) for i in range(2)]
        cmpT = [C.sb(st, "cmpT%d" % i, [128, 2, 128], BF16) for i in range(2)]
        cs_ = C.sb(st, "cs_", [128, 2, 128], F32)
        m16 = C.sb(st, "m16", [128, 2, 2, 2], F32)
        nm16 = C.sb(st, "nm16", [128, 2, 2, 2], F32)
        oaf = C.sb(st, "oaf", [128, 512], F32)
        E1 = C.sb(st, "E1", [128, 2, 128], F32)
        E2 = C.sb(st, "E2", [128, 2, 128], F32)
        E3 = C.sb(st, "E3", [128, 2, 128], F32)
        E4 = C.sb(st, "E4", [128, 2, 128], F32)
        qtl = C.sb(st, "qtl", [128, 2, 2, 128], BF16)
        ktl = C.sb(st, "ktl", [128, 2, 128], BF16)
        khT = C.sb(st, "khT", [128, 2, 128], F32)
        kh = C.sb(st, "kh", [128, 2, 128], BF16)
        qhz = C.sb(st, "qhz", [128, 2, 2, 128], BF16)
        sT = C.sb(st, "sT", [128, 4, 128], BF16)
        ssq = C.sb(st, "ssq", [128, 4], F32)
        sqj = C.sb(st, "sqj", [128, 128], F32)
        on = C.sb(st, "on", [128, 512], F32)
        oa = C.sb(st, "oa", [128, 512], BF16)
        identb = C.sb(st, "identb", [128, 128], BF16)
        kn = C.sb(st, "kn", [128, 2, 2, 64], F32)
        RT = dict(sq=C.sb(st, "r_sq", [128, 2, 64], F32), ss=C.sb(st, "r_ss", [128, 2], F32),
                  t1=C.sb(st, "r_t1", [128, 2, 16], F32), t2=C.sb(st, "r_t2", [128, 2, 16], F32), eps=P["eps"])
        b = [bank(C, st, "bk%d" % i) for i in range(8)]
        v3 = lambda a, n: a.rearrange("p (a b) -> p a b", a=n)
        pT0, pT1 = v3(b[0][:], 4), v3(b[1][:], 4)
        pv, pr = b[0], b[0]
        pkv4 = b[1]
        pqk = v3(b[2][:], 4)
        pcmp = v3(b[3][:, 0:256], 2)
        pz = v3(b[3][:, 256:512], 2)
        pglr = b[3][0:16, 256:384]
        pkvs = v3(b[4][:], 2)
        pkh = v3(b[5][:, 0:256], 2)
        pkT = v3(b[5][:, 256:512], 2)
        psc = v3(b[6][:], 4)
        pmx = b[6][:, 0:256].bitcast(BF16).rearrange("p (a b) -> p a b", a=4)
        po = v3(b[7][:], 4)

        S.dma("pool", wA[:, :, 0:1552], dr["w_in_ab"][:, 0:1552].rearrange("(k p) n -> p k n", p=128), w=["wA0"])
        S.dma("pool", wA[:, :, 1552:NA], dr["w_in_ab"][:, 2064:2832].rearrange("(k p) n -> p k n", p=128), w=["wA1"])
        S.dma("sp", w2g[:], dr["gla_w_gate2"], w=["w2g"])
        S.dma("sp", negb[:], dr["gla_b_gate_col"], w=["negb"])
        S.dma("sp", gng[:], dr["gla_norm_g"].partition_broadcast(128), w=["gng"])
        S.dma("sp", maskT[:], dr["gla_maskT"], w=["maskT"])
        S.dma("sp", posi[:], dr["pos_col"], w=["Tpos"])
        S.dma("sp", valid[:], dr["valid_col"], w=["valid"])
        S.dma("sp", M["invf"][:], dr["invf"].partition_broadcast(128), w=["invf"])
        S.dma("sp", M["gain"][:, 0:3, :], dr["nsa_k_gain"].partition_broadcast(128), w=["gain"])
        S.dma("sp", M["gain"][:, 3, :], dr["nsa_q_gain"].partition_broadcast(128), w=["gainq"])
        S.op("dve", lambda e: e.tensor_scalar(out=negb[:], in0=negb[:], scalar1=-1.0, scalar2=None, op0=ALU.mult), r=["negb"], w=["negb"])
        S.op("dve", lambda e: e.memset(onesc[:], 1.0), w=["onesc"])
        S.op("dve", lambda e: e.memset(state[:], 0.0), w=["state"])
        S.op("pool", lambda e: e.memset(qhz[:], 0.0), w=["qhz"])
        S.op("pool", lambda e: e.memset(qtl[:], 0.0), w=["qtl"])
        S.op("pool", lambda e: e.memset(M["Vsel"][:, :, :, 64:65], 1.0), w=["Vsel1"])
        S.op("pool", lambda e: e.memset(M["Vwin"][:, :, :, 64:65], 1.0), w=["Vwin1"])
        S.op("dve", lambda e: e.tensor_copy(out=identb[:], in_=P["ident"][:]), r=["ident"], w=["identb"])
        S.op("dve", lambda e: e.tensor_scalar(out=M["kbias"][:], in0=valid[:], scalar1=-1.0, scalar2=1e4, op0=ALU.add, op1=ALU.mult),
             r=["valid"], w=["kbias"])
        rope_tables(C, S, st, posi, NWT, M["invf"], M["csT"], "T")

        A, B = P["modA"], P["modB"]
        wk = ["wA0", "wA1"]
        tl = list(tiles if tiles is not None else range(NWT))

        def stage_a(t, S):
            own = t >= OWN0
            i = t % 2
            S.dma("sp", xt[i][:], dr["xw"][t * 128:(t + 1) * 128, :], w=["xt%d" % i])
            norm_transpose(C, S, xt[i][:], "xt%d" % i, tmpn, P["ident"], [(pT0, "b0"), (pT1, "b1")], "a")
            for k in range(8):
                p, pk = ((pT0, "b0"), (pT1, "b1"))[k // 4]
                if k % 2 == 0:
                    S.op("dve", lambda e, k=k, p=p: e.tensor_scalar(
                        out=hT[:, k, :], in0=p[:, k % 4, :], scalar1=A[:, L, 0, k:k + 1], scalar2=B[:, L, 0, k:k + 1],
                        op0=ALU.mult, op1=ALU.add), r=[pk, "modA", "modB"], w=["hT"])
                else:
                    S.op("act", lambda e, k=k, p=p: e.activation(
                        out=hT[:, k, :], in_=p[:, k % 4, :], func=AF.Identity, scale=A[:, L, 0, k:k + 1],
                        bias=B[:, L, 0, k:k + 1]), r=[pk, "modA", "modB"], w=["hT"])
            for m in range(4):
                if m < 2 and not own:
                    continue
                for k in range(8):
                    S.op("pe", lambda e, k=k, m=m: e.matmul(pqk[:, m, :], lhsT=wA[:, k, m * 128:(m + 1) * 128], rhs=hT[:, k, :],
                                                            start=(k == 0), stop=(k == 7)), r=["hT"] + wk, w=["b2"])
            for k in range(8):
                S.op("pe", lambda e, k=k: e.matmul(pglr, lhsT=wA[:, k, CA_GLR:CA_GLR + 16], rhs=hT[:, k, :],
                                                   start=(k == 0), stop=(k == 7)), r=["hT"] + wk, w=["b3"])
            for m in range(2):
                for k in range(8):
                    S.op("pe", lambda e, k=k, m=m: e.matmul(pcmp[:, m, :], lhsT=wA[:, k, CA_KC + m * 128:CA_KC + (m + 1) * 128],
                                                            rhs=hT[:, k, :], start=(k == 0), stop=(k == 7)), r=["hT"] + wk, w=["b3"])
            for k in range(8):
                S.op("pe", lambda e, k=k: e.matmul(pv[:], lhsT=hT[:, k, :], rhs=wA[:, k, CA_V:CA_V + 512],
                                                   start=(k == 0), stop=(k == 7)), r=["hT"] + wk, w=["b0"])
            for k in range(8):
                S.op("pe", lambda e, k=k: e.matmul(pkv4[:], lhsT=hT[:, k, :], rhs=wA[:, k, CA_KV4:CA_KV4 + 512],
                                                   start=(k == 0), stop=(k == 7)), r=["hT"] + wk, w=["b1"])
            S.op("dve", lambda e: e.tensor_copy(out=glrT[:], in_=pglr), r=["b3"], w=["glrT"])
            ct = cmpT[i]
            S.op("act", lambda e: e.activation(out=ct[:], in_=pcmp, func=AF.Copy), r=["b3"], w=["cmpT%d" % i])
            for m in range(2):
                S.dma("act", dr["cmp_scr"][m, :, t * 128:(t + 1) * 128], ct[:, m, :], r=["cmpT%d" % i], w=["cmp_scr%d" % i])
            m0 = 0 if own else 2
            S.op("dve", lambda e: e.tensor_copy(out=qk2[i][:, m0:4, :], in_=pqk[:, m0:4, :]), r=["b2"], w=["qk%d" % i])
            S.op("act", lambda e: e.activation(out=vb2[i][:], in_=pv[:], func=AF.Copy), r=["b0"], w=["vb%d" % i])
            S.op("dve", lambda e: e.tensor_copy(out=kv42[i][:], in_=pkv4[:]), r=["b1"], w=["kv4s%d" % i])
            for p_ in range(2):
                S.op("pe", lambda e, p_=p_: e.matmul(pz[:, p_, :], lhsT=w2g[:, p_ * 128:(p_ + 1) * 128], rhs=glrT[:],
                                                     start=True, stop=True), r=["w2g", "glrT"], w=["b3"])
            if own:
                for k in range(8):
                    S.op("pe", lambda e, k=k: e.matmul(pr[:], lhsT=hT[:, k, :], rhs=wA[:, k, CA_R:CA_R + 512],
                                                       start=(k == 0), stop=(k == 7)), r=["hT"] + wk, w=["b0"])
            for p_ in range(2):
                S.op("act", lambda e, p_=p_: e.activation(out=sp2[i][:, p_, :], in_=pz[:, p_, :], func=AF.Exp, scale=-1.0,
                                                          bias=negb[:, p_:p_ + 1]), r=["b3", "negb"], w=["sp%d" % i])
            if own:
                S.op("act", lambda e: e.activation(out=rs2[i][:], in_=pr[:], func=AF.Exp, scale=-1.0), r=["b0"], w=["rsl%d" % i])
                S.op("dve", lambda e: e.tensor_scalar(out=rs2[i][:], in0=rs2[i][:], scalar1=1.0, scalar2=None, op0=ALU.add), r=["rsl%d" % i], w=["rsl%d" % i])
                S.op("dve", lambda e: e.reciprocal(out=rs2[i][:], in_=rs2[i][:]), r=["rsl%d" % i], w=["rsl%d" % i])
                S.op("dve", lambda e: e.tensor_tensor(out=rs2[i][:], in0=rs2[i][:], in1=pr[:], op=ALU.mult), r=["rsl%d" % i, "b0"], w=["rsl%d" % i])

        def stage_b(t, S):
            own = t >= OWN0
            i = t % 2
            sp, qk, vb, kv4s, rs_ = sp2[i], qk2[i], vb2[i], kv42[i], rs2[i]
            spk, qkk, vbk, kvk, rsk = "sp%d" % i, "qk%d" % i, "vb%d" % i, "kv4s%d" % i, "rsl%d" % i
            S.op("act", lambda e: e.activation(out=sp[:], in_=sp[:], func=AF.Ln, bias=1.0, scale=1.0), r=[spk], w=[spk])
            for p_ in range(2):
                for c in range(2):
                    S.op("dve", lambda e, p_=p_, c=c: e.tensor_tensor_scan(
                        out=cs_[:, p_, c * 64:(c + 1) * 64], data0=onesc[:], data1=sp[:, p_, c * 64:(c + 1) * 64], initial=0.0,
                        op0=ALU.mult, op1=ALU.add), r=[spk, "onesc"], w=["cs_"])
            csv = cs_[:].rearrange("p a (c t) -> p a c t", c=2)
            S.op("dve", lambda e: e.tensor_scalar(out=m16[:, :, :, 0:1], in0=csv[:, :, :, 32:33], scalar1=1.0 / 16, scalar2=None, op0=ALU.mult),
                 r=["cs_"], w=["m16"])
            S.op("dve", lambda e: e.tensor_scalar(out=m16[:, :, :, 1:2], in0=csv[:, :, :, 63:64], scalar1=1.0 / 16, scalar2=None, op0=ALU.mult),
                 r=["cs_"], w=["m16"])
            S.op("dve", lambda e: e.tensor_scalar(out=nm16[:], in0=m16[:], scalar1=-1.0, scalar2=None, op0=ALU.mult), r=["m16"], w=["nm16"])
            for p_ in range(2):
                for c in range(2):
                    sl = slice(c * 64, (c + 1) * 64)
                    S.op("act", lambda e, p_=p_, c=c, sl=sl: e.activation(out=E3[:, p_, sl], in_=cs_[:, p_, sl], func=AF.Exp,
                                                                          scale=1.0 / 16, bias=nm16[:, p_, c, 1:2]), r=["cs_", "nm16"], w=["E3"])
            S.op("act", lambda e: e.activation(out=E4[:], in_=cs_[:], func=AF.Exp, scale=-1.0 / 16), r=["cs_"], w=["E4"])
            if own:
                for p_ in range(2):
                    for c in range(2):
                        sl = slice(c * 64, (c + 1) * 64)
                        S.op("act", lambda e, p_=p_, c=c, sl=sl: e.activation(out=E1[:, p_, sl], in_=cs_[:, p_, sl], func=AF.Exp,
                                                                              scale=-1.0 / 16, bias=m16[:, p_, c, 0:1]), r=["cs_", "m16"], w=["E1"])
                        S.op("act", lambda e, p_=p_, c=c, sl=sl: e.activation(out=E2[:, p_, sl], in_=cs_[:, p_, sl], func=AF.Exp,
                                                                              scale=1.0 / 16, bias=nm16[:, p_, c, 0:1]), r=["cs_", "nm16"], w=["E2"])
            S.op("dve", lambda e: e.tensor_tensor(out=khT[:], in0=qk[:, 2:4, :], in1=E3[:], op=ALU.mult), r=[qkk, "E3"], w=["khT"])
            for p_ in range(2):
                S.op("pe", lambda e, p_=p_: e.transpose(out=pkh[:, p_, :], in_=khT[:, p_, :], identity=P["ident"][:]),
                     r=["khT", "ident"], w=["b5"])
            S.op("dve", lambda e: e.tensor_scalar(out=kh[:], in0=pkh, scalar1=valid[:, t:t + 1], scalar2=None, op0=ALU.mult),
                 r=["b5", "valid"], w=["kh"])
            if own:
                S.op("dve", lambda e: e.tensor_tensor(out=ktl[:], in0=qk[:, 2:4, :], in1=E2[:], op=ALU.mult), r=[qkk, "E2"], w=["ktl"])
                for p_ in range(2):
                    for hh in range(2):
                        rs64 = slice(hh * 64, (hh + 1) * 64)
                        S.op("dve", lambda e, p_=p_, hh=hh, rs64=rs64: e.scalar_tensor_tensor(
                            out=qtl[rs64, p_, hh, :], in0=qk[rs64, p_, :], scalar=0.125, in1=E1[rs64, p_, :],
                            op0=ALU.mult, op1=ALU.mult), r=[qkk, "E1"], w=["qtl"])
                    for c in range(2):
                        sl = slice(c * 64, (c + 1) * 64)
                        S.op("dve", lambda e, p_=p_, c=c, sl=sl: e.scalar_tensor_tensor(
                            out=qhz[:, p_, c, sl], in0=qk[:, p_, sl], scalar=0.125, in1=E4[:, p_, sl], op0=ALU.mult, op1=ALU.mult),
                            r=[qkk, "E4"], w=["qhz"])
                for hd in range(4):
                    p_ = hd // 2
                    S.op("pe", lambda e, hd=hd, p_=p_: e.matmul(psc[:, hd, :], lhsT=ktl[:, p_, :], rhs=qtl[:, p_, hd % 2, :],
                                                                start=True, stop=True), r=["ktl", "qtl"], w=["b6"])
                S.op("dve", lambda e: e.tensor_tensor(out=sT[:], in0=psc, in1=maskT[:], op=ALU.mult), r=["b6", "maskT"], w=["sT"])
            for c in range(2):
                if own:
                    S.op("act", lambda e, c=c: e.activation(out=sbf[c][:], in_=state[:], func=AF.Copy), r=["state"], w=["sbf%d" % c])
                for p_ in range(2):
                    cs64 = slice(c * 64, (c + 1) * 64)
                    S.op("pe", lambda e, p_=p_, cs64=cs64: e.matmul(pkvs[:, p_, :], lhsT=kh[cs64, p_, :], rhs=vb[cs64, p_ * 256:(p_ + 1) * 256],
                                                                    start=True, stop=True), r=["kh", vbk], w=["b4"])
                for hd in range(4):
                    p_, o_ = hd // 2, (hd % 2) * 64
                    dcol = E4[o_:o_ + 64, p_, c * 64 + 63:c * 64 + 64]
                    S.op("dve", lambda e, hd=hd, p_=p_, o_=o_, dcol=dcol: e.scalar_tensor_tensor(
                        out=state[o_:o_ + 64, hd, :], in0=state[o_:o_ + 64, hd, :], scalar=dcol,
                        in1=pkvs[o_:o_ + 64, p_, (hd % 2) * 128:(hd % 2) * 128 + 128], op0=ALU.mult, op1=ALU.add),
                        r=["state", "E4", "b4"], w=["state"])
            if own:
                for hd in range(4):
                    p_, o_ = hd // 2, (hd % 2) * 64
                    S.op("pe", lambda e, hd=hd: e.matmul(po[:, hd, :], lhsT=sT[:, hd, :], rhs=vb[:, hd * 128:(hd + 1) * 128],
                                                         start=True, stop=False), r=["sT", vbk], w=["b7"])
                    for c in range(2):
                        S.op("pe", lambda e, hd=hd, p_=p_, o_=o_, c=c: e.matmul(
                            po[:, hd, :], lhsT=qhz[o_:o_ + 64, p_, c, :], rhs=sbf[c][o_:o_ + 64, hd, :], start=False, stop=(c == 1)),
                            r=["qhz", "sbf%d" % c], w=["b7"])
                for hd in range(4):
                    S.op("act", lambda e, hd=hd: e.activation(out=sqj[:], in_=po[:, hd, :], func=AF.Square, accum_out=ssq[:, hd:hd + 1]),
                         r=["b7"], w=["sqj", "ssq"])
                S.op("act", lambda e: e.activation(out=ssq[:], in_=ssq[:], func=AF.Ln, scale=1.0 / 128, bias=P["eps"][:]), r=["ssq"], w=["ssq"])
                S.op("act", lambda e: e.activation(out=ssq[:], in_=ssq[:], func=AF.Exp, scale=-0.5), r=["ssq"], w=["ssq"])
                for hd in range(4):
                    S.op("dve", lambda e, hd=hd: e.scalar_tensor_tensor(out=on[:, hd * 128:(hd + 1) * 128], in0=po[:, hd, :], scalar=ssq[:, hd:hd + 1],
                                                                        in1=gng[:], op0=ALU.mult, op1=ALU.mult), r=["b7", "ssq", "gng"], w=["on"])
                S.op("dve", lambda e: e.tensor_tensor(out=oa[:], in0=on[:], in1=rs_[:], op=ALU.mult), r=["on", rsk], w=["oa"])
                if dbg is not None:
                    S.op("dve", lambda e: e.tensor_tensor(out=oaf[:], in0=on[:], in1=rs_[:], op=ALU.mult), r=["on", rsk], w=["oaf"])
                    S.dma("sp", dbg["o_a"][(t - OWN0) * 128:(t - OWN0 + 1) * 128, :], oaf[:], r=["oaf"], w=["dbg_oa"])
                for hd in range(4):
                    S.op("pe", lambda e, hd=hd: e.transpose(out=pmx[:, hd, :], in_=oa[:, hd * 128:(hd + 1) * 128], identity=identb[:]),
                         r=["oa", "identb"], w=["b6"])
                S.op("act", lambda e: e.activation(out=M["mixT"][:, 0:4, (t - OWN0) * 128:(t - OWN0 + 1) * 128], in_=pmx, func=AF.Copy),
                     r=["b6"], w=["mixTa%d" % (t - OWN0)])
            kv4 = kv4s[:].rearrange("p (s g d) -> p s g d", s=4, g=2)
            branches = [(0, 1)] + ([(1, 2)] if t >= 44 else [])
            for br, gi in branches:
                rms_gain_rope(C, S, kv4[:, 2 * br, :, :], kvk, kn[:, br, :, :], "kn%d" % br, 2, M["gain"][:, gi, :], "gain",
                              M["csT"][:, t, :], "Tcs", RT, "rk")
                S.op("pe", lambda e, br=br: e.transpose(out=pkT[:, br, :], in_=kn[:, br, :, :].rearrange("p g d -> p (g d)"), identity=P["ident"][:]),
                     r=["kn%d" % br, "ident"], w=["b5"])
            S.op("act", lambda e: e.activation(out=M["KselT"][:, t * 128:(t + 1) * 128], in_=pkT[:, 0, :], func=AF.Copy),
                 r=["b5"], w=["KselT%d" % t])
            S.op("pool", lambda e: e.tensor_copy(out=M["Vsel"][:, t, :, 0:64], in_=kv4[:, 1, :, :]), r=[kvk], w=["Vsel%d" % t])
            if t >= 44:
                S.op("act", lambda e: e.activation(out=M["KwinT"][:, (t - 44) * 128:(t - 43) * 128], in_=pkT[:, 1, :], func=AF.Copy),
                     r=["b5"], w=["KwinT%d" % (t - 44)])
                S.op("pool", lambda e: e.tensor_copy(out=M["Vwin"][:, t - 44, :, 0:64], in_=kv4[:, 3, :, :]), r=[kvk], w=["Vwin%d" % (t - 44)])

        if tl:
            stage_a(tl[0], S)
        for n_, t in enumerate(tl):
            ra, rb = Rec(), Rec()
            if n_ + 1 < len(tl):
                stage_a(tl[n_ + 1], ra)
            stage_b(t, rb)
            interleave(S, ra, rb)
        if dbg is not None:
            S.wait_all("sp", ["dbg_oa"])
        S.wait_all("act", ["cmp_scr0", "cmp_scr1"])
        S.flush()


def alloc_cmp(C, st):
    M2 = {}
    M2["KcT"] = C.sb(st, "KcT", [128, 512], F32)
    M2["Vc"] = C.sb(st, "Vc", [128, 4, 2, 65], F32)
    M2["cbias"] = C.sb(st, "cbias", [128, 4], F32)
    return M2


def phase_cmp(C, P, M, M2, dr):
    nc, S = C.nc, C.S
    with contextlib.ExitStack() as st:
        KC2 = C.sb(st, "KC2", [128, 2, 2, 8192], BF16)
        w1 = C.sb(st, "w1", [128, 2, 16, 256], BF16)
        w2 = C.sb(st, "w2", [128, 2, 2, 64], BF16)
        pecol = C.sb(st, "pecol", [128, 2, 16], F32)
        pecb = C.sb(st, "pecb", [128, 2, 16], BF16)
        pbias = C.sb(st, "pbias", [128, 2, 2], F32)
        hT = [C.sb(st, "chT%d" % i, [128, 2, 512], BF16) for i in range(2)]
        posb = C.sb(st, "posb", [128, 4], I32)
        csB = C.sb(st, "csB", [128, 4, 16], F32)
        cval = C.sb(st, "cval", [128, 4], F32)
        kcn = C.sb(st, "kcn", [128, 2, 64], F32)
        RT = dict(sq=C.sb(st, "c_sq", [128, 2, 64], F32), ss=C.sb(st, "c_ss", [128, 2], F32),
                  t1=C.sb(st, "c_t1", [128, 2, 16], F32), t2=C.sb(st, "c_t2", [128, 2, 16], F32), eps=P["eps"])
        ph = [bank(C, st, "cph%d" % i) for i in range(2)]
        pb = bank(C, st, "cpb")
        po = [bank(C, st, "cpo%d" % i) for i in range(2)]
        pt = bank(C, st, "cpt")

        for kv in range(2):
            for g in range(2):
                S.dma("sp", KC2[0:64, kv, g, :], dr["cmp_scr"][kv, g * 64:(g + 1) * 64, :], r=["cmp_scr0", "cmp_scr1"], w=["KC2a%d%d" % (kv, g)])
                S.dma("sp", KC2[64:128, kv, g, 0:8191], dr["cmp_scr"][kv, g * 64:(g + 1) * 64, 1:8192], r=["cmp_scr0", "cmp_scr1"],
                      w=["KC2b%d%d" % (kv, g)])
            S.dma("pool", w1[:, kv, :, :], dr["nsa_cmp_w1"][kv].rearrange("(lp p) n -> p lp n", p=128), w=["w1_%d" % kv])
            S.dma("pool", w2[:, kv, :, :], dr["nsa_cmp_w2"][kv].rearrange("(hc p) n -> p hc n", p=128), w=["w2_%d" % kv])
        S.dma("sp", pecol[:], dr["cmp_pe_col"], w=["pecol"])
        S.dma("sp", posb[:], dr["posb_col"], w=["Bpos"])
        S.dma("sp", cval[:], dr["cvalid_col"], w=["cval"])
        S.op("dve", lambda e: e.tensor_copy(out=pecb[:], in_=pecol[:]), r=["pecol"], w=["pecb"])
        S.op("dve", lambda e: e.tensor_scalar(out=M2["cbias"][:], in0=cval[:], scalar1=-1.0, scalar2=1e4, op0=ALU.add, op1=ALU.mult),
             r=["cval"], w=["cbias"])
        S.op("pool", lambda e: e.memset(M2["Vc"][:, :, :, 64:65], 1.0), w=["Vc1"])
        for i in range(2):
            S.op("pool", lambda e, i=i: e.memset(hT[i][:, :, 511:512], 0.0), w=["chT%d" % i])
        rope_tables(C, S, st, posb, 4, M["invf"], csB, "B")
        for kv in range(2):
            for hc in range(2):
                for lp in range(16):
                    S.op("pe", lambda e, kv=kv, hc=hc, lp=lp: e.matmul(
                        pb[:, kv * 2 + hc:kv * 2 + hc + 1], lhsT=w1[:, kv, lp, hc * 128:(hc + 1) * 128], rhs=pecb[:, kv, lp:lp + 1],
                        start=(lp == 0), stop=(lp == 15)), r=["w1_%d" % kv, "pecb"], w=["cpb"])
        S.op("dve", lambda e: e.tensor_copy(out=pbias[:].rearrange("p a b -> p (a b)"), in_=pb[:, 0:4]), r=["cpb"], w=["pbias"])
        n = 0
        for kv in range(2):
            for g in range(2):
                h = hT[n % 2]
                hk = "chT%d" % (n % 2)
                n += 1
                for hc in range(2):
                    p = ph[hc]
                    for lp in range(16):
                        S.op("pe", lambda e, kv=kv, g=g, hc=hc, lp=lp, p=p: e.matmul(
                            p[:, 0:511], lhsT=w1[:, kv, lp, hc * 128:(hc + 1) * 128],
                            rhs=KC2[:, kv, g, 2 * lp:2 * lp + 16 * 510 + 1:16], start=(lp == 0), stop=(lp == 15)),
                            r=["w1_%d" % kv, "KC2a%d%d" % (kv, g), "KC2b%d%d" % (kv, g)], w=["cph%d" % hc])
                    S.op("act", lambda e, hc=hc, h=h, p=p, kv=kv: e.activation(out=h[:, hc, 0:511], in_=p[:, 0:511], func=GELU,
                                                                               bias=pbias[:, kv, hc:hc + 1]), r=["cph%d" % hc, "pbias"], w=[hk])
                for bc in range(4):
                    o = po[kv]
                    ok = "cpo%d" % kv
                    for hc in range(2):
                        S.op("pe", lambda e, kv=kv, g=g, hc=hc, bc=bc, h=h, o=o: e.matmul(
                            o[:, (bc * 2 + g) * 64:(bc * 2 + g + 1) * 64], lhsT=h[:, hc, bc * 128:(bc + 1) * 128], rhs=w2[:, kv, hc, :],
                            start=(hc == 0), stop=(hc == 1)), r=[hk, "w2_%d" % kv], w=[ok])
        pk4 = po[0][:].rearrange("p (b g d) -> p b g d", b=4, g=2)
        pv4 = po[1][:].rearrange("p (b g d) -> p b g d", b=4, g=2)
        ptv = pt[:].rearrange("p (a b) -> p a b", a=4)
        for bc in range(4):
            rms_gain_rope(C, S, pk4[:, bc, :, :], "cpo0", kcn[:], "kcn", 2, M["gain"][:, 0, :], "gain", csB[:, bc, :], "Bcs", RT, "ck")
            S.op("pe", lambda e, bc=bc: e.transpose(out=ptv[:, bc, :], in_=kcn[:].rearrange("p g d -> p (g d)"), identity=P["ident"][:]),
                 r=["kcn", "ident"], w=["cpt"])
        S.op("act", lambda e: e.activation(out=M2["KcT"][:], in_=pt[:], func=AF.Copy), r=["cpt"], w=["KcT"])
        S.op("dve", lambda e: e.tensor_copy(out=M2["Vc"][:, :, :, 0:64], in_=pv4), r=["cpo1"], w=["Vc"])
        S.flush()


NEGB = -30000.0


def phase_nsa(C, P, M, M2, dr, dbg=None, qtiles=None):
    nc, S = C.nc, C.S
    L = 0
    with contextlib.ExitStack() as st:
        wB = C.sb(st, "wB", [128, 8, 536], BF16)
        Esel = C.sb(st, "Esel", [128, 64, 128], BF16)
        cover = C.sb(st, "cover", [128, 4, 129], F32)
        corebias = C.sb(st, "corebias", [128, 128], F32)
        causb = C.sb(st, "causb", [128, 4, 128], BF16)
        winb = C.sb(st, "winb", [128, 4, 128], BF16)
        identb = C.sb(st, "identb", [128, 128], BF16)
        kbC = C.sb(st, "kbC", [128, NWT], F32)
        cbC = C.sb(st, "cbC", [128, 4], F32)
        gm = C.sb(st, "gm", [128, 4], F32)
        Cc = C.sb(st, "Cc", [128, 1], F32)
        xt = [C.sb(st, "xt%d" % i, [128, D], F32) for i in range(2)]
        tmpn = dict(junk=C.sb(st, "junk", [128, D], BF16), ss=C.sb(st, "ss", [128, 1], F32),
                    rs=C.sb(st, "rs", [128, 1], F32), xn=C.sb(st, "xn", [128, D], F32), eps=P["eps"], lnexp=True)
        hT = C.sb(st, "hT", [128, 8, 128], BF16)
        qn = C.sb(st, "qn", [128, 8, 64], F32)
        RT = dict(sq=C.sb(st, "q_sq", [128, 8, 64], F32), ss=C.sb(st, "q_ss", [128, 8], F32),
                  t1=C.sb(st, "q_t1", [128, 8, 16], F32), t2=C.sb(st, "q_t2", [128, 8, 16], F32), eps=P["eps"])
        QT32 = C.sb(st, "QT32", [128, 2, 4, 128], F32)
        QTb = C.sb(st, "QTb", [128, 2, 4, 128], BF16)
        gts2 = [C.sb(st, "gts%d" % i, [128, 24], F32) for i in range(2)]
        cm = [C.sb(st, "cm%d" % i, [128, 4, 128], F32) for i in range(2)]
        sbias = [C.sb(st, "sbias%d" % i, [128, 128], F32) for i in range(2)]
        PcT = [C.sb(st, "PcT%d" % i, [128, 4, 128], F32) for i in range(4)]
        PTs = [[C.sb(st, "PT%d_%d" % (g_, i), [128, 512], BF16) for i in range(3)] for g_ in range(2)]
        oTs = [[C.sb(st, "oTs%d%d" % (g_, i), [65, 512], F32) for i in range(2)] for g_ in range(2)]
        rden = C.sb(st, "rden", [128, 4], F32)
        acc = C.sb(st, "acc", [128, 128], F32)
        m8a = C.sb(st, "m8a", [128, 8], F32)
        m8b = C.sb(st, "m8b", [128, 8], F32)
        sc2 = C.sb(st, "sc2", [128, 128], F32)
        selm = C.sb(st, "selm", [128, 128], F32)
        selv = C.sb(st, "selv", [128, 128], F32)
        NBt2 = [C.sb(st, "NBt%d" % i, [128, 4, 128], BF16) for i in range(2)]
        oT = C.sb(st, "oT", [65, 512], F32)
        ob2 = [C.sb(st, "ob%d" % i, [128, 512], F32) for i in range(2)]
        obb = C.sb(st, "obb", [128, 512], BF16)
        coef = C.sb(st, "coef", [128, 4], F32)
        bks = [bank(C, st, "nb%d" % i) for i in range(8)]
        v3 = lambda a, n: a.rearrange("p (a b) -> p a b", a=n)
        pT0, pT1 = v3(bks[0][:], 4), v3(bks[1][:], 4)
        pq = bks[2]
        pqT = v3(bks[3][:], 4)
        pS = [bks[4], bks[5]]
        pO = bks[6]
        pX = bks[7]
        pW = bks[1]
        pS3 = [bks[4], bks[5], bks[3]]
        pS3k = ["nb4", "nb5", "nb3"]

        S.dma("pool", wB[:, :, 0:512], dr["w_nq_perm"].rearrange("(k p) n -> p k n", p=128), w=["wB0"])
        S.dma("pool", wB[:, :, 512:536], dr["w_in_ab"][:, 2832:2856].rearrange("(k p) n -> p k n", p=128), w=["wB1"])
        S.dma("sp", Esel[:], dr["Esel"], w=["Esel"])
        S.dma("sp", cover[:], dr["cover"], w=["cover"])
        S.dma("sp", corebias[:], dr["corebias"], w=["corebias"])
        S.dma("sp", causb[:], dr["causb"], w=["causb"])
        S.dma("sp", winb[:], dr["winb"], w=["winb"])
        S.op("dve", lambda e: e.tensor_copy(out=identb[:], in_=P["ident"][:]), r=["ident"], w=["identb"])
        S.op("pool", lambda e: e.memset(QT32[:], 0.0), w=["QT32"])
        S.op("pool", lambda e: e.memset(QTb[:], 0.0), w=["QTb"])
        S.op("dve", lambda e: e.tensor_reduce(out=gm[:], in_=M["gain"][:], axis=AX.X, op=ALU.max, apply_absolute_value=True),
             r=["gain", "gainq"], w=["gm"])
        S.op("dve", lambda e: e.tensor_reduce(out=Cc[:], in_=gm[:, 0:3], axis=AX.X, op=ALU.max), r=["gm"], w=["Cc"])
        S.op("dve", lambda e: e.tensor_scalar(out=Cc[:], in0=Cc[:], scalar1=gm[:, 3:4], scalar2=-8.0, op0=ALU.mult, op1=ALU.mult),
             r=["Cc", "gm"], w=["Cc"])
        S.op("dve", lambda e: e.tensor_scalar(out=kbC[:], in0=M["kbias"][:], scalar1=Cc[:, 0:1], scalar2=None, op0=ALU.add),
             r=["kbias", "Cc"], w=["kbC"])
        S.op("dve", lambda e: e.tensor_scalar(out=cbC[:], in0=M2["cbias"][:], scalar1=Cc[:, 0:1], scalar2=None, op0=ALU.add),
             r=["cbias", "Cc"], w=["cbC"])

        A, B = P["modA"], P["modB"]
        nS = 0
        nP = 0
        def pro(qt, S):
            T = OWN0 + qt
            i = qt % 2
            gts = gts2[i]
            ob = ob2[i]
            gk = "gts%d" % i
            obk = "ob%d" % i
            S.dma("sp", xt[i][:], dr["xw"][T * 128:(T + 1) * 128, :], w=["xt%d" % i])
            S.dma("sp", cm[i][:], dr["cmaskT"][qt], w=["cm%d" % i])
            S.dma("sp", sbias[i][:], dr["selbias"][qt], w=["sbias%d" % i])
            norm_transpose(C, S, xt[i][:], "xt%d" % i, tmpn, P["ident"], [(pT0, "nb0"), (pT1, "nb1")], "n")
            for k in range(8):
                p, pk = ((pT0, "nb0"), (pT1, "nb1"))[k // 4]
                if k % 2 == 0:
                    S.op("dve", lambda e, k=k, p=p: e.tensor_scalar(
                        out=hT[:, k, :], in0=p[:, k % 4, :], scalar1=A[:, L, 0, k:k + 1], scalar2=B[:, L, 0, k:k + 1],
                        op0=ALU.mult, op1=ALU.add), r=[pk, "modA", "modB"], w=["hT"])
                else:
                    S.op("act", lambda e, k=k, p=p: e.activation(
                        out=hT[:, k, :], in_=p[:, k % 4, :], func=AF.Identity, scale=A[:, L, 0, k:k + 1],
                        bias=B[:, L, 0, k:k + 1]), r=[pk, "modA", "modB"], w=["hT"])
            for k in range(8):
                S.op("pe", lambda e, k=k: e.matmul(pq[:], lhsT=hT[:, k, :], rhs=wB[:, k, 0:512], start=(k == 0), stop=(k == 7)),
                     r=["hT", "wB0"], w=["nb2"])
            for k in range(8):
                S.op("pe", lambda e, k=k: e.matmul(bks[0][:, 0:24], lhsT=hT[:, k, :], rhs=wB[:, k, 512:536], start=(k == 0), stop=(k == 7)),
                     r=["hT", "wB1"], w=["nb0"])
            S.op("act", lambda e: e.activation(out=gts[:], in_=bks[0][:, 0:24], func=AF.Exp, scale=-1.0), r=["nb0"], w=[gk])
            S.op("dve", lambda e: e.tensor_scalar(out=gts[:], in0=gts[:], scalar1=1.0, scalar2=None, op0=ALU.add), r=[gk], w=[gk])
            S.op("dve", lambda e: e.reciprocal(out=gts[:], in_=gts[:]), r=[gk], w=[gk])
            rms_gain_rope(C, S, pq[:].rearrange("p (h d) -> p h d", h=8), "nb2", qn[:], "qn", 8, M["gain"][:, 3, :], "gainq",
                          M["csT"][:, T, :], "Tcs", RT, "rq")
            for hh in range(4):
                S.op("pe", lambda e, hh=hh: e.transpose(out=pqT[:, hh, :], in_=qn[:, 2 * hh:2 * hh + 2, :].rearrange("p g d -> p (g d)"),
                                                        identity=P["ident"][:]), r=["qn", "ident"], w=["nb3"])
            for g in range(2):
                rg = slice(g * 64, (g + 1) * 64)
                S.op("dve", lambda e, g=g, rg=rg: e.tensor_copy(out=QT32[rg, g, :, :], in_=pqT[rg, :, :]), r=["nb3"], w=["QT32"])
                S.op("dve", lambda e, g=g, rg=rg: e.tensor_copy(out=QTb[rg, g, :, :], in_=pqT[rg, :, :]), r=["nb3"], w=["QTb"])
            S.op("dve", lambda e: e.memset(ob[:], 0.0), w=[obk])


        qlist = list(qtiles if qtiles is not None else range(NT))
        pro(qlist[0], S)
        for qi, qt in enumerate(qlist):
            T = OWN0 + qt
            i = qt % 2
            gts = gts2[i]
            ob = ob2[i]
            gk = "gts%d" % i
            obk = "ob%d" % i
            def finish_branch(g, br):
                S.op("act", lambda e: e.activation(out=oT[:], in_=pO[0:65, :], func=AF.Copy), r=["nb6"], w=["oT"])
                pXv = pX[:, 0:260].rearrange("p (h d) -> p h d", h=4)
                for hh in range(4):
                    S.op("pe", lambda e, hh=hh: e.transpose(out=pXv[:, hh, :], in_=oT[:, hh * 128:(hh + 1) * 128], identity=P["ident"][0:65, 0:65]),
                         r=["oT", "ident"], w=["nb7"])
                S.op("dve", lambda e: e.tensor_scalar(out=coef[:], in0=pXv[:, :, 64], scalar1=1e-30, scalar2=None, op0=ALU.max), r=["nb7"], w=["coef"])
                S.op("dve", lambda e: e.reciprocal(out=coef[:], in_=coef[:]), r=["coef"], w=["coef"])
                gv = gts[:].rearrange("p (g h b) -> p g h b", g=2, h=4)
                S.op("dve", lambda e: e.tensor_tensor(out=coef[:], in0=coef[:], in1=gv[:, g, :, br], op=ALU.mult), r=["coef", gk], w=["coef"])
                for hh in range(4):
                    c0 = (g * 4 + hh) * 64
                    S.op("dve", lambda e, hh=hh, c0=c0: e.scalar_tensor_tensor(
                        out=ob[:, c0:c0 + 64], in0=pXv[:, hh, 0:64], scalar=coef[:, hh:hh + 1], in1=ob[:, c0:c0 + 64],
                        op0=ALU.mult, op1=ALU.add), r=["nb7", "coef", obk], w=[obk])

            def finish_branch2(g, br, pacc, pacck):
                S.op("act", lambda e: e.activation(out=oT[:], in_=pacc[0:65, :], func=AF.Copy), r=[pacck], w=["oT"])
                pXv = pX[:, 0:260].rearrange("p (h d) -> p h d", h=4)
                for hh in range(4):
                    S.op("pe", lambda e, hh=hh: e.transpose(out=pXv[:, hh, :], in_=oT[:, hh * 128:(hh + 1) * 128], identity=P["ident"][0:65, 0:65]),
                         r=["oT", "ident"], w=["nb7"])
                S.op("dve", lambda e: e.tensor_scalar(out=coef[:], in0=pXv[:, :, 64], scalar1=1e-30, scalar2=None, op0=ALU.max), r=["nb7"], w=["coef"])
                S.op("dve", lambda e: e.reciprocal(out=coef[:], in_=coef[:]), r=["coef"], w=["coef"])
                gv = gts[:].rearrange("p (g h b) -> p g h b", g=2, h=4)
                S.op("dve", lambda e: e.tensor_tensor(out=coef[:], in0=coef[:], in1=gv[:, g, :, br], op=ALU.mult), r=["coef", gk], w=["coef"])
                for hh in range(4):
                    c0 = (g * 4 + hh) * 64
                    S.op("dve", lambda e, hh=hh, c0=c0: e.scalar_tensor_tensor(
                        out=ob[:, c0:c0 + 64], in0=pXv[:, hh, 0:64], scalar=coef[:, hh:hh + 1], in1=ob[:, c0:c0 + 64],
                        op0=ALU.mult, op1=ALU.add), r=["nb7", "coef", obk], w=[obk])

            pI2 = [[(bks[0], "nb0"), (bks[1], "nb1")], [(bks[2], "nb2"), (bks[3], "nb3")]]
            pI = [bks[0], bks[1]]
            for g in range(2):
                for bc in range(4):
                    j = nS % 2
                    nS += 1
                    pc = PcT[bc]
                    pck = "PcT%d" % bc
                    S.op("pe", lambda e, g=g, bc=bc, j=j: e.matmul(pS[j][:], lhsT=M2["KcT"][:, bc * 128:(bc + 1) * 128],
                                                                   rhs=QT32[:, g, :, :], start=True, stop=True),
                         r=["KcT", "QT32"], w=["nb%d" % (4 + j)])
                    S.op("act", lambda e, bc=bc, j=j, pc=pc: e.activation(out=pc[:].rearrange("p a b -> p (a b)"), in_=pS[j][:], func=AF.Exp,
                                                                          scale=0.125, bias=cbC[:, bc:bc + 1]),
                         r=["nb%d" % (4 + j), "cbC"], w=[pck])
                    S.op("dve", lambda e, bc=bc, pc=pc, i=i: e.tensor_tensor(out=pc[:], in0=pc[:],
                                                                             in1=cm[i][:, bc, :].unsqueeze(1).to_broadcast([128, 4, 128]), op=ALU.mult),
                         r=[pck, "cm%d" % i], w=[pck])
                for bc in range(4):
                    pc = PcT[bc]
                    pck = "PcT%d" % bc
                    S.op("pe", lambda e, g=g, bc=bc, pc=pc: e.matmul(pO[0:65, :], lhsT=M2["Vc"][:, bc, g, :], rhs=pc[:].rearrange("p a b -> p (a b)"),
                                                                     start=(bc == 0), stop=(bc == 3)), r=[pck, "Vc", "Vc1"], w=["nb6"])
                for hh in range(4):
                    pi, pik = pI2[g][hh // 2]
                    for bc in range(4):
                        pc = PcT[bc]
                        pck = "PcT%d" % bc
                        S.op("pe", lambda e, bc=bc, pc=pc, hh=hh, pi=pi: e.matmul(
                            pi[:, (hh % 2) * 129:(hh % 2) * 129 + 129], lhsT=pc[:, hh, :], rhs=cover[:, bc, :],
                            start=(bc == 0), stop=(bc == 3)), r=[pck, "cover"], w=[pik])
                finish_branch2(g, 0, pO, "nb6")
            for g in range(2):
                for hh in range(4):
                    pi, pik = pI2[g][hh // 2]
                    S.op("dve", lambda e, hh=hh, pi=pi: e.tensor_scalar(out=rden[:, hh:hh + 1], in0=pi[:, (hh % 2) * 129 + 128:(hh % 2) * 129 + 129],
                                                                        scalar1=1e-30, scalar2=None, op0=ALU.max), r=[pik], w=["rden"])
                S.op("dve", lambda e: e.reciprocal(out=rden[:], in_=rden[:]), r=["rden"], w=["rden"])
                for hh in range(4):
                    pi, pik = pI2[g][hh // 2]
                    src = pi[:, (hh % 2) * 129:(hh % 2) * 129 + 128]
                    if hh == 0:
                        S.op("dve", lambda e, src=src: e.scalar_tensor_tensor(out=acc[:], in0=src, scalar=rden[:, 0:1], in1=sbias[i][:],
                                                                              op0=ALU.mult, op1=ALU.add), r=[pik, "rden", "sbias%d" % i], w=["acc"])
                    else:
                        S.op("dve", lambda e, src=src, hh=hh: e.scalar_tensor_tensor(out=acc[:], in0=src, scalar=rden[:, hh:hh + 1], in1=acc[:],
                                                                                     op0=ALU.mult, op1=ALU.add), r=[pik, "rden", "acc"], w=["acc"])
                S.op("dve", lambda e: e.tensor_tensor(out=acc[:], in0=acc[:], in1=corebias[:], op=ALU.add), r=["acc", "corebias"], w=["acc"])
                S.op("dve", lambda e: e.max(out=m8a[:], in_=acc[:]), r=["acc"], w=["m8a"])
                S.op("dve", lambda e: e.match_replace(out=sc2[:], in_to_replace=m8a[:], in_values=acc[:], imm_value=-3e38),
                     r=["acc", "m8a"], w=["sc2"])
                S.op("dve", lambda e: e.max(out=m8b[:], in_=sc2[:]), r=["sc2"], w=["m8b"])
                S.op("dve", lambda e: e.tensor_scalar(out=selm[:], in0=acc[:], scalar1=m8b[:, 7:8], scalar2=None, op0=ALU.is_ge),
                     r=["acc", "m8b"], w=["selm"])
                S.op("dve", lambda e: e.tensor_scalar(out=selv[:], in0=acc[:], scalar1=-1e29, scalar2=None, op0=ALU.is_gt), r=["acc"], w=["selv"])
                S.op("dve", lambda e: e.tensor_tensor(out=selm[:], in0=selm[:], in1=selv[:], op=ALU.mult), r=["selm", "selv"], w=["selm"])
                if dbg is not None and "selm" in dbg:
                    S.dma("sp", dbg["selm"][qt, g], selm[:], r=["selm"], w=["dbg_selm"])
                S.op("pe", lambda e: e.transpose(out=pX[:, 384:512], in_=selm[:], identity=P["ident"][:]), r=["selm", "ident"], w=["nb7"])
                S.op("dve", lambda e, g=g: e.tensor_scalar(out=NBt2[g][:], in0=pX[:, 384:512].unsqueeze(1).to_broadcast([128, 4, 128]), scalar1=-1.0,
                                                           scalar2=-NEGB, op0=ALU.add, op1=ALU.mult), r=["nb7"], w=["NBt%d" % g])
            def finish_tail(g, br, src):
                pXv = pX[:, 0:260].rearrange("p (h d) -> p h d", h=4)
                srck = "oTs%d%d" % (g, br)
                for hh in range(4):
                    S.op("pe", lambda e, hh=hh: e.transpose(out=pXv[:, hh, :], in_=src[:, hh * 128:(hh + 1) * 128], identity=P["ident"][0:65, 0:65]),
                         r=[srck, "ident"], w=["nb7"])
                S.op("dve", lambda e: e.tensor_scalar(out=coef[:], in0=pXv[:, :, 64], scalar1=1e-30, scalar2=None, op0=ALU.max), r=["nb7"], w=["coef"])
                S.op("dve", lambda e: e.reciprocal(out=coef[:], in_=coef[:]), r=["coef"], w=["coef"])
                gv = gts[:].rearrange("p (g h b) -> p g h b", g=2, h=4)
                S.op("dve", lambda e: e.tensor_tensor(out=coef[:], in0=coef[:], in1=gv[:, g, :, br], op=ALU.mult), r=["coef", gk], w=["coef"])
                for hh in range(4):
                    c0 = (g * 4 + hh) * 64
                    S.op("dve", lambda e, hh=hh, c0=c0: e.scalar_tensor_tensor(
                        out=ob[:, c0:c0 + 64], in0=pXv[:, hh, 0:64], scalar=coef[:, hh:hh + 1], in1=ob[:, c0:c0 + 64],
                        op0=ALU.mult, op1=ALU.add), r=["nb7", "coef", obk], w=[obk])

            def stream(g, R):
                psb = [(bks[4], "nb4"), (bks[5], "nb5")] if g == 0 else [(bks[2], "nb2"), (bks[3], "nb3")]
                pOW, pOWk = (bks[6], "nb6") if g == 0 else (bks[1], "nb1")
                chunks = [("sel", kc) for kc in range(T + 1)] + [("win", wi) for wi in range(5)]
                nck = len(chunks)

                def emit_S(ci):
                    kind, a_ = chunks[ci]
                    ps, psk = psb[ci % 2]
                    if kind == "sel":
                        kc = a_
                        last = (kc == T)
                        R.op("pe", lambda e: e.matmul(ps[:], lhsT=M["KselT"][:, kc * 128:(kc + 1) * 128], rhs=QTb[:, g, :, :], start=True, stop=False),
                             r=["KselT%d" % kc, "QTb"], w=[psk])
                        R.op("pe", lambda e: e.matmul(ps[:], lhsT=Esel[:, kc, :], rhs=NBt2[g][:], start=False, stop=(not last)),
                             r=["Esel", "NBt%d" % g], w=[psk])
                        if last:
                            R.op("pe", lambda e: e.matmul(ps[:], lhsT=identb[:], rhs=causb[:], start=False, stop=True),
                                 r=["identb", "causb"], w=[psk])
                    else:
                        wi = a_
                        kc = T - 4 + wi
                        edge = wi in (0, 4)
                        R.op("pe", lambda e: e.matmul(ps[:], lhsT=M["KwinT"][:, (kc - 44) * 128:(kc - 43) * 128], rhs=QTb[:, g, :, :],
                                                      start=True, stop=(not edge)), r=["KwinT%d" % (kc - 44), "QTb"], w=[psk])
                        if edge:
                            mb = winb if wi == 0 else causb
                            R.op("pe", lambda e: e.matmul(ps[:], lhsT=identb[:], rhs=mb[:], start=False, stop=True),
                                 r=["identb", "causb", "winb"], w=[psk])

                def emit_PV(ci):
                    kind, a_ = chunks[ci]
                    ps, psk = psb[ci % 2]
                    pt_ = PTs[g][ci % 3]
                    ptk = "PT%d_%d" % (g, ci % 3)
                    kc = a_ if kind == "sel" else T - 4 + a_
                    R.op("act", lambda e: e.activation(out=pt_[:], in_=ps[:], func=AF.Exp, scale=0.125, bias=kbC[:, kc:kc + 1]),
                         r=[psk, "kbC"], w=[ptk])
                    if kind == "sel":
                        R.op("pe", lambda e: e.matmul(pOW[0:65, :], lhsT=M["Vsel"][:, kc, g, :], rhs=pt_[:], start=(kc == 0), stop=(kc == T)),
                             r=[ptk, "Vsel%d" % kc, "Vsel1"], w=[pOWk])
                        if kc == T:
                            R.op("act", lambda e: e.activation(out=oTs[g][0][:], in_=pOW[0:65, :], func=AF.Copy), r=[pOWk], w=["oTs%d0" % g])
                    else:
                        R.op("pe", lambda e: e.matmul(pOW[0:65, :], lhsT=M["Vwin"][:, kc - 44, g, :], rhs=pt_[:], start=(a_ == 0), stop=(a_ == 4)),
                             r=[ptk, "Vwin%d" % (kc - 44), "Vwin1"], w=[pOWk])
                        if a_ == 4:
                            R.op("act", lambda e: e.activation(out=oTs[g][1][:], in_=pOW[0:65, :], func=AF.Copy), r=[pOWk], w=["oTs%d1" % g])

                emit_S(0)
                for ci in range(nck):
                    if ci + 1 < nck:
                        emit_S(ci + 1)
                    emit_PV(ci)

            r0, r1 = Rec(), Rec()
            stream(0, r0)
            stream(1, r1)
            interleave(S, r0, r1)
            S_main = S
            S = Rec()
            for g in range(2):
                finish_tail(g, 1, oTs[g][0])
                finish_tail(g, 2, oTs[g][1])
            if dbg is not None and "o_b" in dbg:
                S.dma("sp", dbg["o_b"][qt * 128:(qt + 1) * 128, :], ob[:], r=[obk], w=["dbg_ob"])
            S.op("act", lambda e: e.activation(out=obb[:], in_=ob[:], func=AF.Copy), r=[obk], w=["obb"])
            pmx = bks[7][:, 0:256].bitcast(BF16).rearrange("p (a b) -> p a b", a=4)
            for hh in range(4):
                S.op("pe", lambda e, hh=hh: e.transpose(out=pmx[:, hh, :], in_=obb[:, hh * 128:(hh + 1) * 128], identity=identb[:]),
                     r=["obb", "identb"], w=["nb7"])
            S.op("act", lambda e, qt=qt: e.activation(out=M["mixT"][:, 4:8, qt * 128:(qt + 1) * 128], in_=pmx, func=AF.Copy),
                 r=["nb7"], w=["mixTb%d" % qt])
            tail = S
            S = S_main
            nxt = Rec()
            if qi + 1 < len(qlist):
                pro(qlist[qi + 1], nxt)
            interleave(S, tail, nxt)
        if dbg is not None:
            S.wait_all("sp", ["dbg_ob", "dbg_selm"])
        S.flush()


def phase_outproj(C, P, M, dr, x_out):
    nc, S = C.nc, C.S
    L = 0
    with contextlib.ExitStack() as st:
        wo = C.sb(st, "wo", [128, 8, D], BF16)
        xt = [C.sb(st, "xt%d" % i, [128, D], F32) for i in range(2)]
        xo = [C.sb(st, "xo%d" % i, [128, D], F32) for i in range(2)]
        ev = C.sb(st, "ev", [128, 512], F32)
        py = [bank(C, st, "opy%d" % i) for i in range(2)]
        S.dma("pool", wo[:], dr["w_out_ab"].rearrange("(k p) n -> p k n", p=128), w=["wo"])
        gate = P["gates"]
        for t in range(NT):
            i = t % 2
            S.dma("sp", xt[i][:], dr["xw"][(OWN0 + t) * 128:(OWN0 + t + 1) * 128, :], w=["xt%d" % i])
            for half in range(2):
                for k in range(8):
                    S.op("pe", lambda e, k=k, half=half, t=t: e.matmul(py[half][:], lhsT=M["mixT"][:, k, t * 128:(t + 1) * 128],
                                                                       rhs=wo[:, k, half * 512:(half + 1) * 512], start=(k == 0), stop=(k == 7)),
                         r=["wo", "mixTa%d" % t, "mixTb%d" % t], w=["opy%d" % half])
                S.op("dve", lambda e, half=half: e.tensor_tensor(out=ev[:], in0=py[half][:], in1=gate[:, L, 0, half * 512:(half + 1) * 512], op=ALU.mult),
                     r=["opy%d" % half, "gates"], w=["ev"])
                S.op("dve", lambda e, half=half, i=i: e.tensor_tensor(out=xo[i][:, half * 512:(half + 1) * 512], in0=ev[:],
                                                                      in1=xt[i][:, half * 512:(half + 1) * 512], op=ALU.add),
                     r=["ev", "xt%d" % i], w=["xo%d_%d" % (i, half)])
            S.dma("sp", x_out[t * 128:(t + 1) * 128, :], xo[i][:], r=["xo%d_0" % i, "xo%d_1" % i], w=["xo_%s_%d" % (x_out.name, i)])
        S.wait_all("sp", ["xo_%s_%d" % (x_out.name, j) for j in range(2)])
        S.flush()


IN_SPECS = [("ident", [128, 128], F32), ("c_col", [128, 8], F32), ("b_ada_col", [128, 2, 48], F32), ("norm_g_col", [128, 2, 2, 8], F32),
            ("w_ada", [2, 1024, 6144], F32), ("b_ada", [2, 6144], F32),
            ("xw", [8192, 1024], F32), ("w_in_ab", [1024, 2856], F32), ("gla_w_gate2", [16, 256], F32), ("gla_b_gate_col", [128, 2], F32),
            ("gla_norm_g", [128], F32), ("gla_maskT", [128, 4, 128], F32), ("valid_col", [128, 64], F32), ("invf", [8], F32),
            ("nsa_k_gain", [3, 64], F32), ("nsa_q_gain", [64], F32), ("pos_col", [128, 64], I32),
            ("nsa_cmp_w1", [2, 2048, 256], F32), ("nsa_cmp_w2", [2, 256, 64], F32), ("cmp_pe_col", [128, 2, 16], F32),
            ("posb_col", [128, 4], I32), ("cvalid_col", [128, 4], F32), ("w_nq_perm", [1024, 512], F32), ("Esel", [128, 64, 128], BF16),
            ("cover", [128, 4, 129], F32), ("corebias", [128, 128], F32), ("causb", [128, 4, 128], BF16), ("winb", [128, 4, 128], BF16),
            ("cmaskT", [16, 128, 4, 128], F32), ("selbias", [16, 128, 128], F32), ("w_out_ab", [1024, 1024], F32),
            ("w_in_c", [1024, 4096], F32), ("w_out_c", [2048, 1024], F32), ("sgu_w_s", [8, 128, 128], F32), ("sgu_b_s", [8, 128], F32),
            ("sgu_norm_g", [2048], F32), ("tril", [128, 128], F32),
            ("w_router", [1024, 16], F32), ("router_bias", [16], F32),
            ("w_exp", [2, 16, 128, 12288], F32)]


def build_full(upto=4):
    C = Ctx()
    dr = {n: C.dram_in(n, s, dt) for n, s, dt in IN_SPECS}
    dr["cmp_scr"] = C.dram_tmp("cmp_scr", [2, 128, 8192], BF16)
    xs = [C.dram_tmp("xs%d" % i, [TOK, D], F32) for i in range(3)]
    y = C.dram_out("y", [TOK, D])
    dst = lambda i: y if upto == i + 1 else xs[i]
    P = alloc_persistent(C)
    phase_init(C, P, dr)
    phase_ada(C, P, dr)
    mst = contextlib.ExitStack()
    M = alloc_mixer(C, mst)
    phase_mix_a(C, P, M, dr)
    M2 = alloc_cmp(C, mst)
    phase_cmp(C, P, M, M2, dr)
    phase_nsa(C, P, M, M2, dr)
    phase_outproj(C, P, M, dr, dst(0))
    mst.close()
    if upto >= 2:
        phase_moe(C, P, dr, 0, xs[0], dst(1))
    if upto >= 3:
        phase_gmlp(C, P, dr, xs[1], dst(2))
    if upto >= 4:
        phase_moe(C, P, dr, 1, xs[2], y)
    C.outer.close()
    return C.nc


def _col(v):
    return np.ascontiguousarray(np.asarray(v).reshape(-1, 128).T)


def _shared_inputs(d):
    f32 = np.float32
    sh = {}
    sh["ident"] = np.eye(128, dtype=f32)
    sh["b_ada_col"] = np.ascontiguousarray(d["b_ada"].reshape(2, 48, 128).transpose(2, 0, 1))
    sh["norm_g_col"] = np.ascontiguousarray(d["norm_g"].reshape(2, 2, 8, 128).transpose(3, 0, 1, 2))
    sh["w_ada"] = d["w_ada"]
    sh["b_ada"] = d["b_ada"]
    w_in = d["w_in_ab"][0]
    sh["w_in_ab"] = w_in
    sh["gla_w_gate2"] = d["gla_w_gate2"][0]
    sh["gla_b_gate_col"] = _col(d["gla_b_gate"][0])
    sh["gla_norm_g"] = d["gla_norm_g"][0]
    j = np.arange(128)[:, None]
    i = np.arange(128)[None, :]
    m = ((j // 64 == i // 64) & (j <= i)).astype(f32)
    sh["gla_maskT"] = np.ascontiguousarray(np.repeat(m[:, None, :], 4, axis=1))
    sh["invf"] = (f32(500000.0) ** (-np.arange(8, dtype=f32) / f32(8))).astype(f32)
    sh["nsa_k_gain"] = d["nsa_k_gain"][0]
    sh["nsa_q_gain"] = d["nsa_q_gain"][0]
    sh["nsa_cmp_w1"] = d["nsa_cmp_w1"][0]
    sh["nsa_cmp_w2"] = d["nsa_cmp_w2"][0]
    sh["cmp_pe_col"] = np.ascontiguousarray(d["nsa_cmp_pe"][0].reshape(2, 16, 128).transpose(2, 0, 1))
    nq = w_in[:, 1552:2064].reshape(1024, 2, 4, 64)
    sh["w_nq_perm"] = np.ascontiguousarray(nq.transpose(0, 2, 1, 3).reshape(1024, 512))
    E = np.zeros((128, 64, 128), f32)
    for c in range(64):
        E[2 * c, c, 0:64] = 1.0
        E[2 * c + 1, c, 64:128] = 1.0
    sh["Esel"] = E.astype(ml_dtypes.bfloat16)
    blk = np.arange(512)
    jj = np.arange(128)
    cov = np.zeros((512, 129), f32)
    cov[:, :128] = ((16 * blk[:, None] < 64 * jj[None, :] + 64) & (16 * blk[:, None] + 32 > 64 * jj[None, :])).astype(f32)
    cov[:, 128] = 1.0
    cov[511] = 0.0
    sh["cover"] = np.ascontiguousarray(cov.reshape(4, 128, 129).transpose(1, 0, 2))
    cm = np.zeros((16, 128, 4, 128), f32)
    sb = np.zeros((16, 128, 128), f32)
    ii = np.arange(128)
    for qt in range(16):
        wq = (OWN0 + qt) * 128 + ii
        for bc in range(4):
            b_ = bc * 128 + np.arange(128)
            cm[qt, :, bc, :] = ((16 * b_ + 31)[:, None] <= wq[None, :]).astype(f32)
        cur = wq // 64
        forced = (jj[None, :] == cur[:, None]) | (jj[None, :] == cur[:, None] - 1)
        inval = jj[None, :] > cur[:, None]
        sb[qt] = np.where(inval, -1e30, np.where(forced, 1e4, 0.0))
    sh["cmaskT"] = cm
    sh["selbias"] = sb
    caus = np.where(j <= i, 0.0, NEGB).astype(f32)
    win = np.where(j > i, 0.0, NEGB).astype(f32)
    rep4 = lambda a: np.ascontiguousarray(np.repeat(a[:, None, :], 4, axis=1))
    sh["causb"] = rep4(caus).astype(ml_dtypes.bfloat16)
    sh["winb"] = rep4(win).astype(ml_dtypes.bfloat16)
    sh["w_out_ab"] = d["w_out_ab"][0]
    sh["w_in_c"] = d["w_in_c"][0]
    sh["w_out_c"] = d["w_out_c"][0]
    sh["sgu_w_s"] = d["sgu_w_s"][0]
    sh["sgu_b_s"] = d["sgu_b_s"][0]
    sh["sgu_norm_g"] = d["sgu_norm_g"][0]
    sh["tril"] = np.tril(np.ones((128, 128), f32))
    for k in ("w_router", "router_bias"):
        sh[k] = d[k]
    wg_ = d["w_gate"].reshape(2, 16, 8, 128, 512).transpose(0, 1, 3, 2, 4).reshape(2, 16, 128, 4096)
    wu_ = d["w_up"].reshape(2, 16, 8, 128, 512).transpose(0, 1, 3, 2, 4).reshape(2, 16, 128, 4096)
    wd_ = d["w_down"].reshape(2, 16, 4, 128, 1024).transpose(0, 1, 3, 2, 4).reshape(2, 16, 128, 4096)
    sh["w_exp"] = np.ascontiguousarray(np.concatenate([wg_, wu_, wd_], axis=-1))
    return sh


def _core_inputs(d, core):
    f32 = np.float32
    b, qtr = core // 4, core % 4
    own = qtr * TOK
    a = np.arange(8192) - 6144 + own
    ok = a >= 0
    xw = np.zeros((8192, D), f32)
    xw[ok] = d["x"][b, a[ok]]
    pos = np.zeros(8192, np.int32)
    pos[ok] = d["positions"][b, a[ok]]
    co = {"xw": xw, "pos_col": _col(pos), "valid_col": _col(ok.astype(f32)), "c_col": _col(d["c"][b])}
    first_blk = 96 - 32 * qtr
    jj = np.arange(128)
    row = np.where(jj < first_blk, -1e30, np.where(jj == first_blk, 1e4, 0.0)).astype(f32)
    co["corebias"] = np.ascontiguousarray(np.broadcast_to(row, (128, 128)))
    blk = np.arange(512)
    a_start = 16 * blk - 6144 + own
    cvalid = ((a_start >= 0) & (blk <= 510))
    a_end = np.clip(16 * blk + 31 - 6144 + own, 0, 8191)
    posb = np.where(cvalid, d["positions"][b, a_end], 0).astype(np.int32)
    co["cvalid_col"] = _col(cvalid.astype(f32))
    co["posb_col"] = _col(posb)
    return co


_NC_CACHE = {}


def kernel(**inputs):
    d = {k: np.asarray(v) for k, v in inputs.items()}
    if "nc" not in _NC_CACHE:
        _NC_CACHE["nc"] = build_full()
    nc = _NC_CACHE["nc"]
    sh = _shared_inputs(d)
    in_maps = []
    for core in range(NCORES):
        m = dict(sh)
        m.update(_core_inputs(d, core))
        in_maps.append(m)
    res = run_bass_kernel_spmd(nc, in_maps, core_ids=list(range(NCORES)))
    out = np.zeros((2, 8192, D), np.float32)
    for core in range(NCORES):
        b, qtr = core // 4, core % 4
        out[b, qtr * TOK:(qtr + 1) * TOK] = res.results[core]["y"]
    return out
```

```python
import contextlib
import types
import numpy as np
import ml_dtypes
import concourse.bass as bass
import concourse.mybir as mybir
from concourse.bass_utils import run_bass_kernel_spmd

F32 = mybir.dt.float32
BF16 = mybir.dt.bfloat16
I32 = mybir.dt.int32
AF = mybir.ActivationFunctionType
ALU = mybir.AluOpType
AX = mybir.AxisListType

ENGS = ("pe", "dve", "act", "pool", "sp")
NCORES = 8
D = 1024
NT = 16
TOK = 2048
EPS = 1e-6


PSUM_KEYS = set(["b%d" % i for i in range(8)] + ["nb%d" % i for i in range(8)] +
                ["cph0", "cph1", "cpb", "cpo0", "cpo1", "cpt", "pT0", "pT1", "pg0", "pg1", "pu0", "pu1", "py0", "py1",
                 "pv0", "pv1", "pm0", "pm1", "pcol", "prow0", "prow1", "opy0", "opy1"] + ["gb%d" % i for i in range(8)])


class Sched:
    def __init__(self, nc, stack):
        self.nc = nc
        self.stack = stack
        self.q = {e: [] for e in ENGS}
        self.last_w = {}
        self.readers = {}
        self.dma_sems = {}
        self.esem = {e: stack.enter_context(nc.semaphore("s_" + e)) for e in ENGS}
        self.ecount = {e: 0 for e in ENGS}
        self.seen = {e: {} for e in ENGS}
        self.phase_end = {}
        self.phase = 0
        self.alias = {}
        self.slot_map = {}
        self.sem_pool = []

    def _deps(self, eng, r, w):
        deps = []
        r = [self.alias.get(k, k) for k in r]
        w = [self.alias.get(k, k) for k in w]
        for k in r:
            t = self.last_w.get(k)
            if t is not None:
                deps.append(t)
            if k in PSUM_KEYS:
                deps.extend(x for x in self.readers.get(k, ()) if not (x[0] == "eng" and x[1] == eng))
        for k in w:
            t = self.last_w.get(k)
            if t is not None:
                deps.append(t)
            deps.extend(self.readers.get(k, ()))
        best = {}
        for t in deps:
            key = (t[0], t[1], t[3] if t[0] == "eng" else 0)
            if key not in best or best[key] < t[2]:
                best[key] = t[2]
        out = []
        for (kind, name, ph), v in best.items():
            if kind == "eng" and name == eng and (eng == "pe" or not Sched.same_engine_sync):
                continue
            out.append((kind, name, v, ph))
        return out

    def _commit(self, tok, r, w):
        r = [self.alias.get(k, k) for k in r]
        w = [self.alias.get(k, k) for k in w]
        for k in w:
            self.last_w[k] = tok
            self.readers[k] = []
        for k in r:
            self.readers.setdefault(k, []).append(tok)

    limit = None
    count = 0
    recycle = True
    sim_mode = False
    uniq = 0
    same_engine_sync = True

    @staticmethod
    def _freeze(fn):
        if fn.__closure__ is None:
            return fn
        cells = tuple(types.CellType(c.cell_contents) for c in fn.__closure__)
        g = types.FunctionType(fn.__code__, fn.__globals__, fn.__name__, fn.__defaults__, cells)
        g.__kwdefaults__ = fn.__kwdefaults__
        return g

    def op(self, eng, fn, r=(), w=()):
        fn = self._freeze(fn)
        Sched.count += 1
        if Sched.limit is not None and Sched.count > Sched.limit:
            return
        deps = self._deps(eng, r, w)
        idx = len(self.q[eng])
        self.q[eng].append(dict(kind="op", fn=fn, deps=deps))
        self._commit(("eng", eng, idx, self.phase), r, w)

    def dma(self, eng, out, in_, r=(), w=(), slot=None, **kw):
        Sched.count += 1
        if Sched.limit is not None and Sched.count > Sched.limit:
            return
        deps = self._deps(eng, r, w)
        if slot is None:
            slot = w[0]
        if eng == "pool":
            Sched.uniq += 1
            sid = "sw%d" % Sched.uniq
            self.dma_sems[sid] = [self.stack.enter_context(self.nc.semaphore(sid)), 0]
            slot = sid
        else:
            if slot not in self.slot_map:
                n = len(self.slot_map)
                if n >= len(self.sem_pool):
                    self.sem_pool.append(n)
                    self.dma_sems[n] = [self.stack.enter_context(self.nc.semaphore("d%d" % n)), 0]
                self.slot_map[slot] = n
            slot = self.slot_map[slot]
        self.dma_sems[slot][1] += 16
        val = self.dma_sems[slot][1]
        self.q[eng].append(dict(kind="dma", out=out, in_=in_, deps=deps, slot=slot, kw=kw))
        self._commit(("dma", slot, val, 0), r, w)

    def wait_all(self, eng, keys):
        deps = self._deps(eng, keys, ())
        self.q[eng].append(dict(kind="wait", deps=deps))

    def flush(self, barrier=True):
        nc = self.nc
        ph = self.phase
        miles = {e: set() for e in ENGS}
        for e in ENGS:
            for o in self.q[e]:
                for (kind, name, v, p) in o["deps"]:
                    if kind == "eng" and p == ph:
                        miles[name].add(v)
        for e in ENGS:
            n = len(self.q[e])
            if n:
                last = max(i for i, o in enumerate(self.q[e]) if o["kind"] != "wait") if any(
                    o["kind"] != "wait" for o in self.q[e]) else None
                if last is not None and self.q[e][last]["kind"] == "op":
                    miles[e].add(last)
        rank = {}
        for e in ENGS:
            for i, v in enumerate(sorted(miles[e])):
                rank[(e, v)] = self.ecount[e] + i + 1
        prev_end = dict(self.phase_end)
        prev_dma = dict(getattr(self, 'dma_totals', {}))
        with nc.Block() as block:
            engobj = {"pe": block.tensor, "dve": block.vector, "act": block.scalar,
                      "pool": block.gpsimd, "sp": block.sync}

            def make(ename):
                def body(eng):
                    seen = self.seen[ename]

                    def wait(sem, key, v):
                        if seen.get(key, 0) >= v:
                            return
                        eng.wait_ge(sem, v)
                        seen[key] = v
                    if barrier:
                        for oe, v in prev_end.items():
                            if oe != ename and v > 0:
                                wait(self.esem[oe], oe, v)
                        for slot, tot in prev_dma.items():
                            wait(self.dma_sems[slot][0], "d:%s" % (slot,), tot)
                    for i, o in enumerate(self.q[ename]):
                        for (kind, name, v, p) in o["deps"]:
                            if kind == "eng":
                                if p == ph:
                                    if name == ename:
                                        wait(self.esem[name], name, rank[(name, v)])
                                    else:
                                        wait(self.esem[name], name, rank[(name, v)])
                                else:
                                    if name != ename and prev_end.get(name, 0) > 0:
                                        wait(self.esem[name], name, prev_end[name])
                            else:
                                wait(self.dma_sems[name][0], "d:%s" % (name,), v)
                        if o["kind"] == "op":
                            ins = o["fn"](eng)
                            if i in miles[ename]:
                                ins.then_inc(self.esem[ename], 1)
                        elif o["kind"] == "dma":
                            ins = eng.dma_start(out=o["out"], in_=o["in_"], **o["kw"])
                            ins.then_inc(self.dma_sems[o["slot"]][0], 16)
                return body
            for e in ENGS:
                if self.q[e] or barrier:
                    engobj[e](make(e))
        for e in ENGS:
            self.ecount[e] += len(miles[e])
            self.phase_end[e] = self.ecount[e]
            self.q[e] = []
        self.phase += 1
        self.dma_totals = {slot: v[1] for slot, v in self.dma_sems.items()}
        if Sched.recycle:
            self.slot_map = {}


class Rec:
    def __init__(self):
        self.items = []

    def op(self, eng, fn, r=(), w=()):
        self.items.append(("op", eng, Sched._freeze(fn), tuple(r), tuple(w), None))

    def dma(self, eng, out, in_, r=(), w=(), slot=None, **kw):
        self.items.append(("dma", eng, (out, in_, slot, kw), tuple(r), tuple(w), None))


def interleave(S, a, b):
    ia = ib = 0
    na, nb = len(a.items), len(b.items)
    while ia < na or ib < nb:
        take_a = ib >= nb or (ia < na and ia * nb <= ib * na)
        it = a.items[ia] if take_a else b.items[ib]
        if take_a:
            ia += 1
        else:
            ib += 1
        if it[0] == "op":
            S.op(it[1], it[2], r=it[3], w=it[4])
        else:
            out, in_, slot, kw = it[2]
            S.dma(it[1], out, in_, r=it[3], w=it[4], slot=slot, **kw)


class Ctx:
    def __init__(self):
        self.nc = bass.Bass("TRN2", target_bir_lowering=False)
        self.outer = contextlib.ExitStack()
        self.S = Sched(self.nc, self.outer)
        self.uid = 0

    def dram_in(self, name, shape, dt=F32):
        return self.nc.dram_tensor(name, list(shape), dt, kind="ExternalInput").ap()

    def dram_out(self, name, shape, dt=F32):
        return self.nc.dram_tensor(name, list(shape), dt, kind="ExternalOutput").ap()

    def dram_tmp(self, name, shape, dt=F32):
        return self.nc.dram_tensor(name, list(shape), dt, kind="Internal").ap()

    def sb(self, stack, name, shape, dt):
        self.uid += 1
        return stack.enter_context(self.nc.sbuf_tensor("%s_%d" % (name, self.uid), list(shape), dt))

    def ps(self, stack, name, shape, dt=F32):
        self.uid += 1
        return stack.enter_context(self.nc.psum_tensor("%s_%d" % (name, self.uid), list(shape), dt))


def norm_transpose(C, S, xt, xkey, tmp, ident, pTs, tag):
    junk, ss, rs, xn = tmp["junk"], tmp["ss"], tmp["rs"], tmp["xn"]
    kj, kss, krs, kxn = [tag + s for s in ("junk", "ss", "rs", "xn")]
    S.op("act", lambda e: e.activation(out=junk[:], in_=xt, func=AF.Square, accum_out=ss[:]),
         r=[xkey], w=[kj, kss])
    if tmp.get("lnexp"):
        S.op("act", lambda e: e.activation(out=rs[:], in_=ss[:], func=AF.Ln, scale=1.0 / D, bias=tmp["eps"][:]),
             r=[kss], w=[krs])
        S.op("act", lambda e: e.activation(out=rs[:], in_=rs[:], func=AF.Exp, scale=-0.5), r=[krs], w=[krs])
    else:
        S.op("act", lambda e: e.activation(out=rs[:], in_=ss[:], func=AF.Sqrt, scale=1.0 / D, bias=tmp["eps"][:]),
             r=[kss], w=[krs])
        S.op("dve", lambda e: e.reciprocal(out=rs[:], in_=rs[:]), r=[krs], w=[krs])
    S.op("dve", lambda e: e.tensor_scalar(out=xn[:], in0=xt, scalar1=rs[:, 0:1], scalar2=None, op0=ALU.mult),
         r=[xkey, krs], w=[kxn])
    for k in range(8):
        p, pk = pTs[k // 4]
        S.op("pe", lambda e, k=k, p=p: e.transpose(out=p[:, k % 4, :], in_=xn[:, k * 128:(k + 1) * 128],
                                                   identity=ident[:]),
             r=[kxn, "ident"], w=[pk])


def phase_ada(C, P, dr):
    nc, S = C.nc, C.S
    with contextlib.ExitStack() as st:
        ccol = C.sb(st, "ccol", [128, 8], F32)
        cond = C.sb(st, "cond", [128, 8], F32)
        condbc = C.sb(st, "condbc", [128, 8, 128], F32)
        ones = C.sb(st, "ones", [128, 128], F32)
        bcol = C.sb(st, "bcol", [128, 2, 48], F32)
        gcol = C.sb(st, "gcol", [128, 2, 2, 8], F32)
        mcol = C.sb(st, "mcol", [128, 2, 48], F32)
        wblk = [C.sb(st, "wblk%d" % i, [128, 8, 512], F32) for i in range(2)]
        brow = [C.sb(st, "brow%d" % i, [128, 512], F32) for i in range(2)]
        pcol = C.ps(st, "pcol", [128, 512], F32)
        prow = [C.ps(st, "prow%d" % i, [128, 512], F32) for i in range(2)]
        S.dma("sp", ccol[:], dr["c_col"], w=["ccol"])
        S.dma("sp", bcol[:], dr["b_ada_col"], w=["bcol"])
        S.dma("sp", gcol[:], dr["norm_g_col"], w=["gcol"])
        S.op("act", lambda e: e.activation(out=cond[:], in_=ccol[:], func=AF.Silu), r=["ccol"], w=["cond"])
        S.op("dve", lambda e: e.memset(ones[:], 1.0), w=["ones"])
        for k in range(8):
            S.op("dve", lambda e, k=k: e.tensor_scalar(out=condbc[:, k, :], in0=ones[:], scalar1=cond[:, k:k + 1],
                                                       scalar2=None, op0=ALU.mult),
                 r=["ones", "cond"], w=["condbc"])
        nrow = 0
        modrow = [C.sb(st, "modrow%d" % i, [128, 512], F32) for i in range(2)]
        pcT = C.ps(st, "pcT", [128, 4, 128], F32)
        for l in range(2):
            for nb in range(12):
                i = (l * 12 + nb) % 2
                for hk_ in range(2):
                    S.dma("sp" if hk_ == 0 else "act", wblk[i][:, hk_ * 4:(hk_ + 1) * 4, :],
                          dr["w_ada"][l, hk_ * 512:(hk_ + 1) * 512, nb * 512:(nb + 1) * 512].rearrange("(k p) n -> p k n", p=128),
                          w=["wblk%d_%d" % (i, hk_)])
                sub6 = nb // 2
                j = nrow % 2
                nrow += 1
                S.dma("sp", brow[j][:], dr["b_ada"][l, nb * 512:(nb + 1) * 512].partition_broadcast(128), w=["brow%d" % j])
                for k in range(8):
                    S.op("pe", lambda e, k=k, i=i, j=j: e.matmul(prow[j][:], lhsT=condbc[:, k, :], rhs=wblk[i][:, k, :],
                                                                 start=(k == 0), stop=(k == 7)),
                         r=["condbc", "wblk%d_0" % i, "wblk%d_1" % i], w=["prow%d" % j])
                if sub6 in (2, 5):
                    gs = 0 if sub6 == 2 else 1
                    half = nb % 2
                    S.op("dve", lambda e, j=j, l=l, gs=gs, half=half: e.tensor_tensor(
                        out=P["gates"][:, l, gs, half * 512:(half + 1) * 512], in0=prow[j][:], in1=brow[j][:], op=ALU.add),
                        r=["prow%d" % j, "brow%d" % j], w=["gates"])
                else:
                    S.op("dve", lambda e, j=j: e.tensor_tensor(out=modrow[j][:], in0=prow[j][:], in1=brow[j][:], op=ALU.add),
                         r=["prow%d" % j, "brow%d" % j], w=["modrow%d" % j])
                    for m in range(4):
                        S.op("pe", lambda e, j=j, m=m: e.transpose(out=pcT[:, m, :], in_=modrow[j][:, m * 128:(m + 1) * 128], identity=P["ident"][:]),
                             r=["modrow%d" % j, "ident"], w=["pcol"])
                    S.op("dve", lambda e, l=l, nb=nb: e.tensor_copy(out=mcol[:, l, nb * 4:nb * 4 + 4], in_=pcT[:, :, 0]),
                         r=["pcol"], w=["mcol"])
            for s in range(2):
                S.op("dve", lambda e, l=l, s=s: e.scalar_tensor_tensor(
                    out=P["modA"][:, l, s, :], in0=mcol[:, l, s * 24 + 8:s * 24 + 16], scalar=1.0, in1=gcol[:, l, s, :],
                    op0=ALU.add, op1=ALU.mult), r=["mcol", "gcol"], w=["modA"])
                S.op("dve", lambda e, l=l, s=s: e.tensor_copy(out=P["modB"][:, l, s, :], in_=mcol[:, l, s * 24:s * 24 + 8]),
                     r=["mcol"], w=["modB"])
        S.flush()


MOE_INTERLEAVE = True
MOE_LNEXP = False


def phase_moe(C, P, dr, layer, x_in, x_out):
    nc, S = C.nc, C.S
    L = layer
    with contextlib.ExitStack() as st:
        X = C.sb(st, "X", [128, NT, D], F32)
        hT = C.sb(st, "hT", [128, 8, TOK], BF16)
        comb = C.sb(st, "comb", [128, NT, 16], F32)
        tmpn2 = [dict(junk=C.sb(st, "junk%d" % i, [128, D], BF16), ss=C.sb(st, "ss%d" % i, [128, 1], F32),
                      rs=C.sb(st, "rs%d" % i, [128, 1], F32), xn=C.sb(st, "xn%d" % i, [128, D], F32), eps=P["eps"], lnexp=MOE_LNEXP) for i in range(2)]
        hTf = [C.sb(st, "hTf%d" % i, [128, 8, 128], F32) for i in range(2)]
        wr = C.sb(st, "wr", [128, 8, 16], F32)
        rb = C.sb(st, "rb", [128, 16], F32)
        rt2 = [{n: C.sb(st, "rt%d_" % i + n, [128, 16], F32) for n in
                ("sc", "sel", "eq1", "sel2", "eq2", "selm", "wts")} for i in range(2)]
        r42 = [{n: C.sb(st, "r4%d_" % i + n, [128, 4], F32) for n in ("m1", "m2", "gs", "ing")} for i in range(2)]
        r12 = [{n: C.sb(st, "r1%d_" % i + n, [128, 1], F32) for n in ("gmax", "den")} for i in range(2)]
        wpk = [C.sb(st, "wpk%d" % i, [128, 12288], BF16) for i in range(2)]
        wg = [wpk[i][:, 0:4096].rearrange("p (k n) -> p k n", k=8) for i in range(2)]
        wu = [wpk[i][:, 4096:8192].rearrange("p (k n) -> p k n", k=8) for i in range(2)]
        wd = [wpk[i][:, 8192:12288].rearrange("p (k n) -> p k n", k=4) for i in range(2)]
        sg = [C.sb(st, "sg%d" % i, [128, 512], F32) for i in range(2)]
        hid = [C.sb(st, "hid%d" % i, [128, 4, 512], BF16) for i in range(2)]
        ev = [C.sb(st, "ev%d" % i, [128, 512], F32) for i in range(2)]
        pT = [C.ps(st, "pT%d" % i, [128, 4, 128], F32) for i in range(2)]
        pg = [C.ps(st, "pg%d" % i, [128, 512], F32) for i in range(2)]
        pu = [C.ps(st, "pu%d" % i, [128, 512], F32) for i in range(2)]
        py = [C.ps(st, "py%d" % i, [128, 512], F32) for i in range(2)]
        plog2 = [pg[0], pg[1]]

        S.dma("sp", wr[:], dr["w_router"].rearrange("(k p) n -> p k n", p=128), w=["wr"])
        S.dma("sp", rb[:], dr["router_bias"].partition_broadcast(128), w=["rb"])

        def load_w(e):
            i = e % 2
            S.dma("pool", wpk[i][:], dr["w_exp"][L, e], w=["wg%d" % i, "wu%d" % i, "wd%d" % i], slot="wpk%d" % i, max_dma_last_dim=8192)

        for t in range(NT):
            S.dma("sp", X[:, t, :], x_in[t * 128:(t + 1) * 128, :], w=["X%d" % t])
        load_w(0)
        load_w(1)
        A = P["modA"]
        B = P["modB"]
        def moe_pro(t, S):
            q_ = t % 2
            tmpn = tmpn2[q_]
            rt, r4, r1 = rt2[q_], r42[q_], r12[q_]
            plog = plog2[q_]
            T_ = "m%d" % q_
            pTq = [(pT[0], "pT0"), (pT[1], "pT1")] if q_ == 0 else [(pu[0][:].rearrange("p (a b) -> p a b", a=4), "pu0"),
                                                                      (pu[1][:].rearrange("p (a b) -> p a b", a=4), "pu1")]
            norm_transpose(C, S, X[:, t, :], "X%d" % t, tmpn, P["ident"], pTq, T_)
            hf = hTf[t % 2]
            hk = "hTf%d" % (t % 2)
            for k in range(8):
                p, pk = pTq[k // 4]
                if k % 2 == 0:
                    S.op("dve", lambda e, k=k, p=p, hf=hf: e.tensor_scalar(
                        out=hf[:, k, :], in0=p[:, k % 4, :], scalar1=A[:, L, 1, k:k + 1], scalar2=B[:, L, 1, k:k + 1],
                        op0=ALU.mult, op1=ALU.add), r=[pk, "modA", "modB"], w=[hk])
                else:
                    S.op("act", lambda e, k=k, p=p, hf=hf: e.activation(
                        out=hf[:, k, :], in_=p[:, k % 4, :], func=AF.Identity, scale=A[:, L, 1, k:k + 1],
                        bias=B[:, L, 1, k:k + 1]), r=[pk, "modA", "modB"], w=[hk])
            S.op("pool", lambda e, t=t, hf=hf: e.tensor_copy(out=hT[:, :, t * 128:(t + 1) * 128], in_=hf[:]),
                 r=[hk], w=["hT%d" % t])
            for k in range(8):
                S.op("pe", lambda e, k=k, hf=hf: e.matmul(plog[:, 0:16], lhsT=hf[:, k, :], rhs=wr[:, k, :],
                                                          start=(k == 0), stop=(k == 7)),
                     r=[hk, "wr"], w=["pg%d" % q_])
            sc, sel, eq1, sel2, eq2, selm, wts = [rt[n] for n in ("sc", "sel", "eq1", "sel2", "eq2", "selm", "wts")]
            m1, m2, gs, ing = [r4[n] for n in ("m1", "m2", "gs", "ing")]
            gmax, den = r1["gmax"], r1["den"]
            v4 = lambda a: a[:].rearrange("p (g e) -> p g e", e=4)
            b4 = lambda a: a[:].unsqueeze(2).to_broadcast([128, 4, 4])
            S.op("act", lambda e: e.activation(out=sc[:], in_=plog[:, 0:16], func=AF.Sigmoid), r=["pg%d" % q_], w=[T_ + "r_sc"])
            S.op("dve", lambda e: e.tensor_tensor(out=sel[:], in0=sc[:], in1=rb[:], op=ALU.add), r=[T_ + "r_sc", "rb"], w=[T_ + "r_sel"])
            S.op("dve", lambda e: e.tensor_reduce(out=m1[:], in_=v4(sel), axis=AX.X, op=ALU.max), r=[T_ + "r_sel"], w=[T_ + "r_m1"])
            S.op("dve", lambda e: e.tensor_tensor(out=v4(eq1), in0=v4(sel), in1=b4(m1), op=ALU.is_equal),
                 r=[T_ + "r_sel", T_ + "r_m1"], w=[T_ + "r_eq1"])
            S.op("dve", lambda e: e.scalar_tensor_tensor(out=sel2[:], in0=eq1[:], scalar=-1e9, in1=sel[:],
                                                         op0=ALU.mult, op1=ALU.add), r=[T_ + "r_eq1", T_ + "r_sel"], w=[T_ + "r_sel2"])
            S.op("dve", lambda e: e.tensor_reduce(out=m2[:], in_=v4(sel2), axis=AX.X, op=ALU.max), r=[T_ + "r_sel2"], w=[T_ + "r_m2"])
            S.op("dve", lambda e: e.tensor_tensor(out=gs[:], in0=m1[:], in1=m2[:], op=ALU.add), r=[T_ + "r_m1", T_ + "r_m2"], w=[T_ + "r_gs"])
            S.op("dve", lambda e: e.tensor_reduce(out=gmax[:], in_=gs[:], axis=AX.X, op=ALU.max), r=[T_ + "r_gs"], w=[T_ + "r_gmax"])
            S.op("dve", lambda e: e.tensor_scalar(out=ing[:], in0=gs[:], scalar1=gmax[:, 0:1], scalar2=None, op0=ALU.is_equal),
                 r=[T_ + "r_gs", T_ + "r_gmax"], w=[T_ + "r_ing"])
            S.op("dve", lambda e: e.tensor_tensor(out=v4(eq2), in0=v4(sel2), in1=b4(m2), op=ALU.is_equal),
                 r=[T_ + "r_sel2", T_ + "r_m2"], w=[T_ + "r_eq2"])
            S.op("dve", lambda e: e.tensor_tensor(out=selm[:], in0=eq1[:], in1=eq2[:], op=ALU.add), r=[T_ + "r_eq1", T_ + "r_eq2"], w=[T_ + "r_selm"])
            S.op("dve", lambda e: e.tensor_tensor(out=v4(selm), in0=v4(selm), in1=b4(ing), op=ALU.mult),
                 r=[T_ + "r_selm", T_ + "r_ing"], w=[T_ + "r_selm"])
            S.op("dve", lambda e: e.tensor_tensor(out=wts[:], in0=sc[:], in1=selm[:], op=ALU.mult), r=[T_ + "r_sc", T_ + "r_selm"], w=[T_ + "r_wts"])
            S.op("dve", lambda e: e.tensor_reduce(out=den[:], in_=wts[:], axis=AX.X, op=ALU.add), r=[T_ + "r_wts"], w=[T_ + "r_den"])
            S.op("dve", lambda e: e.reciprocal(out=den[:], in_=den[:]), r=[T_ + "r_den"], w=[T_ + "r_den"])
            S.op("dve", lambda e, t=t: e.tensor_scalar(out=comb[:, t, :], in0=wts[:], scalar1=den[:, 0:1], scalar2=None, op0=ALU.mult),
                 r=[T_ + "r_wts", T_ + "r_den"], w=["comb%d" % t])

        for t in range(0, NT, 2):
            if MOE_INTERLEAVE:
                ra, rb_ = Rec(), Rec()
                moe_pro(t, ra)
                moe_pro(t + 1, rb_)
                interleave(S, ra, rb_)
            else:
                moe_pro(t, S)
                moe_pro(t + 1, S)
        gate = P["gates"]
        cnt = {"g": 0, "y": 0}

        def emit_gu(ex, tg):
            i = ex % 2
            hb = hid[(ex * 4 + tg) % 2]
            hbk = "hid%d" % ((ex * 4 + tg) % 2)
            for hc in range(4):
                j = cnt["g"] % 2
                cnt["g"] += 1
                hkeys = ["hT%d" % t for t in range(tg * 4, tg * 4 + 4)]
                for k in range(8):
                    S.op("pe", lambda e, k=k, i=i, j=j, hc=hc, tg=tg: e.matmul(
                        pg[j][:], lhsT=wg[i][:, k, hc * 128:(hc + 1) * 128], rhs=hT[:, k, tg * 512:(tg + 1) * 512],
                        start=(k == 0), stop=(k == 7)), r=hkeys + ["wg%d" % i], w=["pg%d" % j])
                for k in range(8):
                    S.op("pe", lambda e, k=k, i=i, j=j, hc=hc, tg=tg: e.matmul(
                        pu[j][:], lhsT=wu[i][:, k, hc * 128:(hc + 1) * 128], rhs=hT[:, k, tg * 512:(tg + 1) * 512],
                        start=(k == 0), stop=(k == 7)), r=hkeys + ["wu%d" % i], w=["pu%d" % j])
                S.op("act", lambda e, j=j: e.activation(out=sg[j][:], in_=pg[j][:], func=AF.Silu),
                     r=["pg%d" % j], w=["sg%d" % j])
                S.op("dve", lambda e, j=j, hb=hb, hc=hc: e.tensor_tensor(out=hb[:, hc, :], in0=sg[j][:], in1=pu[j][:], op=ALU.mult),
                     r=["sg%d" % j, "pu%d" % j], w=[hbk + "_%d" % hc])

        def emit_down(ex, tg):
            i = ex % 2
            hb = hid[(ex * 4 + tg) % 2]
            hbk = "hid%d" % ((ex * 4 + tg) % 2)
            for tt in range(4):
                t = tg * 4 + tt
                for half in range(2):
                    j = cnt["y"] % 2
                    cnt["y"] += 1
                    for hc in range(4):
                        S.op("pe", lambda e, j=j, hb=hb, hc=hc, tt=tt, half=half, i=i: e.matmul(
                            py[j][:], lhsT=hb[:, hc, tt * 128:(tt + 1) * 128], rhs=wd[i][:, hc, half * 512:(half + 1) * 512],
                            start=(hc == 0), stop=(hc == 3)), r=[hbk + "_%d" % hc, "wd%d" % i], w=["py%d" % j])
                    S.op("dve", lambda e, j=j, t=t, ex=ex, half=half: e.scalar_tensor_tensor(
                        out=ev[j][:], in0=py[j][:], scalar=comb[:, t, ex:ex + 1], in1=gate[:, L, 1, half * 512:(half + 1) * 512],
                        op0=ALU.mult, op1=ALU.mult), r=["py%d" % j, "comb%d" % t, "gates"], w=["ev%d" % j])
                    S.op("pool", lambda e, j=j, t=t, half=half: e.tensor_tensor(
                        out=X[:, t, half * 512:(half + 1) * 512], in0=X[:, t, half * 512:(half + 1) * 512], in1=ev[j][:], op=ALU.add),
                        r=["ev%d" % j, "X%d" % t], w=["X%d" % t])

        steps = [(ex, tg) for ex in range(16) for tg in range(4)]
        for si, (ex, tg) in enumerate(steps):
            emit_gu(ex, tg)
            if si > 0:
                pex, ptg = steps[si - 1]
                emit_down(pex, ptg)
                if ptg == 3 and pex + 2 < 16:
                    load_w(pex + 2)
        emit_down(*steps[-1])
        for t in range(NT):
            S.dma("sp", x_out[t * 128:(t + 1) * 128, :], X[:, t, :], r=["X%d" % t], w=["xo_%s_%d" % (x_out.name, t % 4)])
        S.wait_all("sp", ["xo_%s_%d" % (x_out.name, j) for j in range(4)])
        S.flush()


def alloc_persistent(C):
    st = C.outer
    P = {}
    P["ident"] = C.sb(st, "ident", [128, 128], F32)
    P["eps"] = C.sb(st, "eps", [128, 1], F32)
    P["modA"] = C.sb(st, "modA", [128, 2, 2, 8], F32)
    P["modB"] = C.sb(st, "modB", [128, 2, 2, 8], F32)
    P["gates"] = C.sb(st, "gates", [128, 2, 2, D], F32)
    return P


def phase_init(C, P, dr):
    S = C.S
    S.dma("sp", P["ident"][:], dr["ident"], w=["ident"])
    S.op("dve", lambda e: e.memset(P["eps"][:], EPS), w=["eps"])


GELU = AF.Gelu_apprx_tanh


def phase_gmlp(C, P, dr, x_in, x_out):
    nc, S0 = C.nc, C.S
    L = 1
    with contextlib.ExitStack() as st:
        win = C.sb(st, "win", [128, 8, 4096], BF16)
        wout = C.sb(st, "wout", [128, 16, 1024], BF16)
        WT = C.sb(st, "WT", [128, 8, 128], BF16)
        wsf = C.sb(st, "wsf", [128, 8, 128], F32)
        tril = C.sb(st, "tril", [128, 128], F32)
        grow = C.sb(st, "grow", [128, 2048], F32)
        bsr = C.sb(st, "bsr", [1, 8, 128], BF16)
        bsf = C.sb(st, "bsf", [1, 8, 128], F32)
        ones1 = C.sb(st, "ones1", [1, 128], BF16)
        xt = [C.sb(st, "xt%d" % i, [128, D], F32) for i in range(2)]
        xo = [C.sb(st, "xo%d" % i, [128, D], F32) for i in range(2)]
        junk_sh = C.sb(st, "junk_sh", [128, D], BF16)
        tmpn2 = [dict(junk=junk_sh, ss=C.sb(st, "ss%d" % i, [128, 1], F32),
                      rs=C.sb(st, "rs%d" % i, [128, 1], F32), xn=C.sb(st, "xn%d" % i, [128, D], F32), eps=P["eps"]) for i in range(2)]
        hT = [C.sb(st, "hT%d" % i, [128, 8, 128], BF16) for i in range(2)]
        vz2 = [C.sb(st, "vz%d" % i, [128, 2048], F32) for i in range(2)]
        vss2 = [C.sb(st, "vss%d" % i, [128, 4], F32) for i in range(2)]
        vs12 = [C.sb(st, "vs1%d" % i, [128, 1], F32) for i in range(2)]
        vn2 = [C.sb(st, "vn%d" % i, [128, 2048], BF16) for i in range(2)]
        uT2 = [C.sb(st, "uT%d" % i, [128, 16, 128], BF16) for i in range(2)]
        pTt2 = [C.sb(st, "pTt%d" % i, [128, 16, 128], BF16) for i in range(2)]
        ev2 = [C.sb(st, "ev%d" % i, [128, 512], F32) for i in range(2)]
        bks = [bank(C, st, "gb%d" % i) for i in range(8)]
        v3 = lambda a_, n: a_.rearrange("p (a b) -> p a b", a=n)
        S = S0
        wsrc = dr["w_in_c"].rearrange("(k p) n -> p k n", p=128)
        S.dma("pool", win[:, :, 2048:4096], wsrc[:, :, 2048:4096], w=["win%d" % k for k in range(4)], slot="winv", max_dma_last_dim=8192)
        S.dma("pool", win[:, :, 0:2048], wsrc[:, :, 0:2048], w=["win%d" % k for k in range(4, 8)], slot="winu", max_dma_last_dim=8192)
        S.dma("pool", wout[:], dr["w_out_c"].rearrange("(k p) n -> p k n", p=128), w=["wout"])
        S.dma("sp", wsf[:], dr["sgu_w_s"].rearrange("g t s -> t g s"), w=["wsf"])
        S.dma("sp", tril[:], dr["tril"], w=["tril"])
        S.dma("sp", grow[:], dr["sgu_norm_g"].partition_broadcast(128), w=["grow"])
        S.dma("sp", bsf[:], dr["sgu_b_s"].rearrange("(o g) t -> o g t", o=1), w=["bsf"])
        S.op("dve", lambda e: e.tensor_copy(out=bsr[:], in_=bsf[:]), r=["bsf"], w=["bsr"])
        S.op("dve", lambda e: e.memset(ones1[:], 1.0), w=["ones1"])
        for g in range(8):
            S.op("dve", lambda e, g=g: e.tensor_tensor(out=wsf[:, g, :], in0=wsf[:, g, :], in1=tril[:], op=ALU.mult),
                 r=["wsf", "tril"], w=["wsf"])
        for g in range(8):
            p = v3(bks[g // 4][:], 4)
            S.op("pe", lambda e, g=g, p=p: e.transpose(out=p[:, g % 4, :], in_=wsf[:, g, :], identity=P["ident"][:]),
                 r=["wsf", "ident"], w=["gb%d" % (g // 4)])
        for i in range(2):
            S.op("dve", lambda e, i=i: e.tensor_copy(out=WT[:, i * 4:(i + 1) * 4, :], in_=v3(bks[i][:], 4)), r=["gb%d" % i], w=["WT"])

        A = P["modA"]
        B = P["modB"]
        gate = P["gates"]
        winkeys = ["win%d" % k for k in range(8)]

        def tile(t, S):
            i = t % 2
            q0, q1, q2, q3 = [bks[i * 4 + n] for n in range(4)]
            k0, k1, k2, k3 = ["gb%d" % (i * 4 + n) for n in range(4)]
            pT = [(v3(q0[:], 4), k0), (v3(q1[:], 4), k1)]
            pvb = [(q0, k0), (q1, k1)]
            pu, puk = v3(q2[:], 4), k2
            pm, pmk = v3(q3[:], 4), k3
            tmpn, vz, vss, vs1, vn, uT, pTt, ev = tmpn2[i], vz2[i], vss2[i], vs12[i], vn2[i], uT2[i], pTt2[i], ev2[i]
            T_ = "g%d" % i
            S.dma("sp", xt[i][:], x_in[t * 128:(t + 1) * 128, :], w=["xt%d" % i])
            norm_transpose(C, S, xt[i][:], "xt%d" % i, tmpn, P["ident"], pT, T_)
            h = hT[i]
            hk = "hT%d" % i
            for k in range(8):
                p, pk = pT[k // 4]
                if k % 2 == 0:
                    S.op("dve", lambda e, k=k, p=p, h=h: e.tensor_scalar(
                        out=h[:, k, :], in0=p[:, k % 4, :], scalar1=A[:, L, 0, k:k + 1], scalar2=B[:, L, 0, k:k + 1],
                        op0=ALU.mult, op1=ALU.add), r=[pk, "modA", "modB"], w=[hk])
                else:
                    S.op("act", lambda e, k=k, p=p, h=h: e.activation(
                        out=h[:, k, :], in_=p[:, k % 4, :], func=AF.Identity, scale=A[:, L, 0, k:k + 1],
                        bias=B[:, L, 0, k:k + 1]), r=[pk, "modA", "modB"], w=[hk])
            for n in range(4):
                pv_, pvk = pvb[n % 2]
                for k in range(8):
                    S.op("pe", lambda e, k=k, n=n, pv_=pv_, h=h: e.matmul(
                        pv_[:], lhsT=h[:, k, :], rhs=win[:, k, 2048 + n * 512: 2048 + (n + 1) * 512],
                        start=(k == 0), stop=(k == 7)), r=[hk] + winkeys, w=[pvk])
                S.op("act", lambda e, n=n, pv_=pv_: e.activation(out=vz[:, n * 512:(n + 1) * 512], in_=pv_[:], func=GELU),
                     r=[pvk], w=[T_ + "vz%d" % n])
                S.op("act", lambda e, n=n: e.activation(out=junk_sh[:, 0:512], in_=vz[:, n * 512:(n + 1) * 512], func=AF.Square,
                                                        accum_out=vss[:, n:n + 1]), r=[T_ + "vz%d" % n], w=[T_ + "vss%d" % n])
            S.op("dve", lambda e: e.tensor_reduce(out=vs1[:], in_=vss[:], axis=AX.X, op=ALU.add),
                 r=[T_ + "vss%d" % n for n in range(4)], w=[T_ + "vs1"])
            S.op("act", lambda e: e.activation(out=vs1[:], in_=vs1[:], func=AF.Sqrt, scale=1.0 / 2048, bias=P["eps"][:]),
                 r=[T_ + "vs1"], w=[T_ + "vs1"])
            S.op("dve", lambda e: e.reciprocal(out=vs1[:], in_=vs1[:]), r=[T_ + "vs1"], w=[T_ + "vs1"])
            for n in range(4):
                S.op("dve", lambda e, n=n: e.scalar_tensor_tensor(
                    out=vn[:, n * 512:(n + 1) * 512], in0=vz[:, n * 512:(n + 1) * 512], scalar=vs1[:, 0:1],
                    in1=grow[:, n * 512:(n + 1) * 512], op0=ALU.mult, op1=ALU.mult),
                    r=[T_ + "vz%d" % n, T_ + "vs1", "grow"], w=[T_ + "vn%d" % n])
            for q in range(4):
                for m in range(4):
                    c = q * 4 + m
                    for k in range(8):
                        S.op("pe", lambda e, k=k, c=c, m=m, h=h: e.matmul(
                            pu[:, m, :], lhsT=win[:, k, c * 128:(c + 1) * 128], rhs=h[:, k, :],
                            start=(k == 0), stop=(k == 7)), r=[hk] + winkeys, w=[puk])
                S.op("act", lambda e, q=q: e.activation(out=uT[:, q * 4:(q + 1) * 4, :], in_=pu, func=GELU),
                     r=[puk], w=[T_ + "uT%d" % q])
                for m in range(4):
                    c = q * 4 + m
                    g = c // 2
                    S.op("pe", lambda e, c=c, m=m, g=g: e.matmul(
                        pm[:, m, :], lhsT=vn[:, c * 128:(c + 1) * 128], rhs=WT[:, g, :], start=True, stop=False),
                        r=[T_ + "vn%d" % (c // 4), "WT"], w=[pmk])
                    S.op("pe", lambda e, c=c, m=m, g=g: e.matmul(
                        pm[:, m, :], lhsT=ones1[0:1, :], rhs=bsr[0:1, g, :], start=False, stop=True),
                        r=["ones1", "bsr"], w=[pmk])
                S.op("dve", lambda e, q=q: e.tensor_tensor(out=pTt[:, q * 4:(q + 1) * 4, :], in0=pm, in1=uT[:, q * 4:(q + 1) * 4, :],
                                                           op=ALU.mult), r=[pmk, T_ + "uT%d" % q], w=[T_ + "pTt%d" % q])
            o = xo[i]
            for half in range(2):
                pv_, pvk = pvb[half]
                for c in range(16):
                    S.op("pe", lambda e, c=c, half=half, pv_=pv_: e.matmul(
                        pv_[:], lhsT=pTt[:, c, :], rhs=wout[:, c, half * 512:(half + 1) * 512],
                        start=(c == 0), stop=(c == 15)), r=[T_ + "pTt%d" % (c // 4), "wout"], w=[pvk])
                S.op("dve", lambda e, half=half, pv_=pv_: e.tensor_tensor(
                    out=ev[:], in0=pv_[:], in1=gate[:, L, 0, half * 512:(half + 1) * 512], op=ALU.mult),
                    r=[pvk, "gates"], w=[T_ + "ev"])
                S.op("dve", lambda e, half=half, o=o: e.tensor_tensor(
                    out=o[:, half * 512:(half + 1) * 512], in0=ev[:], in1=xt[i][:, half * 512:(half + 1) * 512], op=ALU.add),
                    r=[T_ + "ev", "xt%d" % i], w=["xo%d_%d" % (i, half)])
            S.dma("sp", x_out[t * 128:(t + 1) * 128, :], o[:], r=["xo%d_0" % i, "xo%d_1" % i], w=["xo_%s_%d" % (x_out.name, i)])

        for t in range(0, NT, 2):
            ra, rb_ = Rec(), Rec()
            tile(t, ra)
            tile(t + 1, rb_)
            interleave(S0, ra, rb_)
        S0.wait_all("sp", ["xo_%s_%d" % (x_out.name, j) for j in range(2)])
        S0.flush()


NWT = 64
OWN0 = 48
DBG_SKIP = set()
TWO_PI = 6.283185307179586
CW1 = 6.28125
CW2 = TWO_PI - CW1
CA_Q, CA_K, CA_V, CA_GLR, CA_R = 0, 256, 512, 1024, 1040
CA_KC, CA_VC, CA_KV4 = 1552, 1680, 1808
NA = 2320


def bank(C, st, name):
    return C.ps(st, name, [128, 512], F32)


def rope_tables(C, S, st, pos_i, ncol, invf, out_cs, tag):
    pf = C.sb(st, tag + "pf", [128, ncol], F32)
    ang = C.sb(st, tag + "ang", [128, ncol, 8], F32)
    ki = C.sb(st, tag + "ki", [128, ncol, 8], I32)
    kf = C.sb(st, tag + "kf", [128, ncol, 8], F32)
    r = C.sb(st, tag + "r", [128, ncol, 8], F32)
    y = C.sb(st, tag + "y", [128, ncol, 8], F32)
    m = C.sb(st, tag + "m", [128, ncol, 8], F32)
    k = lambda s: tag + s
    S.op("dve", lambda e: e.tensor_copy(out=pf[:], in_=pos_i[:]), r=[k("pos")], w=[k("pf")])
    S.op("dve", lambda e: e.tensor_tensor(out=ang[:], in0=pf[:].unsqueeze(2).to_broadcast([128, ncol, 8]),
                                          in1=invf[:].unsqueeze(1).to_broadcast([128, ncol, 8]), op=ALU.mult),
         r=[k("pf"), "invf"], w=[k("ang")])
    S.op("dve", lambda e: e.tensor_scalar(out=ki[:], in0=ang[:], scalar1=1.0 / TWO_PI, scalar2=None, op0=ALU.mult),
         r=[k("ang")], w=[k("ki")])
    S.op("dve", lambda e: e.tensor_copy(out=kf[:], in_=ki[:]), r=[k("ki")], w=[k("kf")])
    S.op("dve", lambda e: e.scalar_tensor_tensor(out=r[:], in0=kf[:], scalar=-CW1, in1=ang[:], op0=ALU.mult, op1=ALU.add),
         r=[k("kf"), k("ang")], w=[k("r")])
    S.op("dve", lambda e: e.scalar_tensor_tensor(out=r[:], in0=kf[:], scalar=-CW2, in1=r[:], op0=ALU.mult, op1=ALU.add),
         r=[k("kf"), k("r")], w=[k("r")])
    for which, shift in ((1, 0.0), (0, np.pi / 2)):
        S.op("dve", lambda e, shift=shift: e.tensor_scalar(out=y[:], in0=r[:], scalar1=float(shift), scalar2=None, op0=ALU.add),
             r=[k("r")], w=[k("y")])
        S.op("dve", lambda e: e.tensor_scalar(out=m[:], in0=y[:], scalar1=float(np.pi), scalar2=None, op0=ALU.is_gt),
             r=[k("y")], w=[k("m")])
        S.op("dve", lambda e: e.scalar_tensor_tensor(out=y[:], in0=m[:], scalar=-TWO_PI, in1=y[:], op0=ALU.mult, op1=ALU.add),
             r=[k("m"), k("y")], w=[k("y")])
        S.op("dve", lambda e: e.tensor_scalar(out=y[:], in0=y[:], scalar1=float(np.pi), scalar2=-float(np.pi), op0=ALU.min, op1=ALU.max),
             r=[k("y")], w=[k("y")])
        S.op("act", lambda e, which=which: e.activation(out=out_cs[:, :, which * 8:(which + 1) * 8], in_=y[:], func=AF.Sin),
             r=[k("y")], w=[k("cs")])


def rms_gain_rope(C, S, src, srckey, dst, dstkey, ngrp, gain_row, gainkey, cs, cskey, T, tag):
    sq, ss, t1, t2 = T["sq"], T["ss"], T["t1"], T["t2"]
    k = lambda s: tag + s
    S.op("act", lambda e: e.activation(out=sq[:, 0:ngrp, :], in_=src, func=AF.Square), r=[srckey], w=[k("sq")])
    S.op("dve", lambda e: e.tensor_reduce(out=ss[:, 0:ngrp], in_=sq[:, 0:ngrp, :], axis=AX.X, op=ALU.add), r=[k("sq")], w=[k("ss")])
    S.op("act", lambda e: e.activation(out=ss[:, 0:ngrp], in_=ss[:, 0:ngrp], func=AF.Ln, scale=1.0 / 64, bias=T["eps"][:]),
         r=[k("ss")], w=[k("ss")])
    S.op("act", lambda e: e.activation(out=ss[:, 0:ngrp], in_=ss[:, 0:ngrp], func=AF.Exp, scale=-0.5), r=[k("ss")], w=[k("ss")])
    S.op("dve", lambda e: e.tensor_tensor(out=dst, in0=src, in1=ss[:, 0:ngrp].unsqueeze(2).to_broadcast([128, ngrp, 64]), op=ALU.mult),
         r=[srckey, k("ss")], w=[dstkey])
    S.op("dve", lambda e: e.tensor_tensor(out=dst, in0=dst, in1=gain_row.unsqueeze(1).to_broadcast([128, ngrp, 64]), op=ALU.mult),
         r=[dstkey, gainkey], w=[dstkey])
    cosb = cs[:, 0:8].unsqueeze(1).to_broadcast([128, ngrp, 8])
    sinb = cs[:, 8:16].unsqueeze(1).to_broadcast([128, ngrp, 8])
    x1 = dst[:, :, 0:8]
    x2 = dst[:, :, 8:16]
    S.op("dve", lambda e: e.tensor_tensor(out=t1[:, 0:ngrp, 0:8], in0=x1, in1=cosb, op=ALU.mult), r=[dstkey, cskey], w=[k("t1a")])
    S.op("dve", lambda e: e.tensor_tensor(out=t1[:, 0:ngrp, 8:16], in0=x2, in1=cosb, op=ALU.mult), r=[dstkey, cskey], w=[k("t1b")])
    S.op("dve", lambda e: e.tensor_tensor(out=t2[:, 0:ngrp, 0:8], in0=x2, in1=sinb, op=ALU.mult), r=[dstkey, cskey], w=[k("t2a")])
    S.op("dve", lambda e: e.tensor_tensor(out=t2[:, 0:ngrp, 8:16], in0=x1, in1=sinb, op=ALU.mult), r=[dstkey, cskey], w=[k("t2b")])
    S.op("dve", lambda e: e.tensor_tensor(out=x1, in0=t1[:, 0:ngrp, 0:8], in1=t2[:, 0:ngrp, 0:8], op=ALU.subtract),
         r=[k("t1a"), k("t2a"), k("t1b"), k("t2b")], w=[dstkey])
    S.op("dve", lambda e: e.tensor_tensor(out=x2, in0=t1[:, 0:ngrp, 8:16], in1=t2[:, 0:ngrp, 8:16], op=ALU.add),
         r=[k("t1b"), k("t2b")], w=[dstkey])


def alloc_mixer(C, st):
    M = {}
    M["KselT"] = C.sb(st, "KselT", [128, NWT * 128], BF16)
    M["Vsel"] = C.sb(st, "Vsel", [128, NWT, 2, 65], BF16)
    M["KwinT"] = C.sb(st, "KwinT", [128, 20 * 128], BF16)
    M["Vwin"] = C.sb(st, "Vwin", [128, 20, 2, 65], BF16)
    M["mixT"] = C.sb(st, "mixT", [128, 8, TOK], BF16)
    M["csT"] = C.sb(st, "csT", [128, NWT, 16], F32)
    M["kbias"] = C.sb(st, "kbias", [128, NWT], F32)
    M["gain"] = C.sb(st, "gain", [128, 4, 64], F32)
    M["invf"] = C.sb(st, "invf", [128, 8], F32)
    return M


def phase_mix_a(C, P, M, dr, dbg=None, tiles=None):
    nc, S = C.nc, C.S
    L = 0
    with contextlib.ExitStack() as st:
        wA = C.sb(st, "wA", [128, 8, NA], BF16)
        w2g = C.sb(st, "w2g", [16, 256], F32)
        negb = C.sb(st, "negb", [128, 2], F32)
        gng = C.sb(st, "gng", [128, 128], F32)
        maskT = C.sb(st, "maskT", [128, 4, 128], F32)
        posi = C.sb(st, "posi", [128, NWT], I32)
        valid = C.sb(st, "valid", [128, NWT], F32)
        onesc = C.sb(st, "onesc", [128, 64], F32)
        state = C.sb(st, "state", [128, 4, 128], F32)
        sbf = [C.sb(st, "sbf%d" % i, [128, 4, 128], BF16) for i in range(2)]
        xt = [C.sb(st, "xt%d" % i, [128, D], F32) for i in range(2)]
        tmpn = dict(junk=C.sb(st, "junk", [128, D], BF16), ss=C.sb(st, "ss", [128, 1], F32),
                    rs=C.sb(st, "rs", [128, 1], F32), xn=C.sb(st, "xn", [128, D], F32), eps=P["eps"], lnexp=True)
        hT = C.sb(st, "hT", [128, 8, 128], BF16)
        glrT = C.sb(st, "glrT", [16, 128], F32)
        vb2 = [C.sb(st, "vb%d" % i, [128, 512], BF16) for i in range(2)]
        qk2 = [C.sb(st, "qk%d" % i, [128, 4, 128], F32) for i in range(2)]
        sp2 = [C.sb(st, "sp%d" % i, [128, 2, 128], F32) for i in range(2)]
        kv42 = [C.sb(st, "kv4s%d" % i, [128, 512], F32) for i in range(2)]
        rs2 = [C.sb(st, "rsl%d" % i, [128, 512], F32) for i in range(2)]
        cmpT = [C.sb(st, "cmpT%d" % i, [128, 2, 128], BF16) for i in range(2)]
        cs_ = C.sb(st, "cs_", [128, 2, 128], F32)
        m16 = C.sb(st, "m16", [128, 2, 2, 2], F32)
        nm16 = C.sb(st, "nm16", [128, 2, 2, 2], F32)
        oaf = C.sb(st, "oaf", [128, 512], F32)
        E1 = C.sb(st, "E1", [128, 2, 128], F32)
        E2 = C.sb(st, "E2", [128, 2, 128], F32)
        E3 = C.sb(st, "E3", [128, 2, 128], F32)
        E4 = C.sb(st, "E4", [128, 2, 128], F32)
        qtl = C.sb(st, "qtl", [128, 2, 2, 128], BF16)
        ktl = C.sb(st, "ktl", [128, 2, 128], BF16)
        khT = C.sb(st, "khT", [128, 2, 128], F32)
        kh = C.sb(st, "kh", [128, 2, 128], BF16)
        qhz = C.sb(st, "qhz", [128, 2, 2, 128], BF16)
        sT = C.sb(st, "sT", [128, 4, 128], BF16)
        ssq = C.sb(st, "ssq", [128, 4], F32)
        sqj = C.sb(st, "sqj", [128, 128], F32)
        on = C.sb(st, "on", [128, 512], F32)
        oa = C.sb(st, "oa", [128, 512], BF16)
        identb = C.sb(st, "identb", [128, 128], BF16)
        kn = C.sb(st, "kn", [128, 2, 2, 64], F32)
        RT = dict(sq=C.sb(st, "r_sq", [128, 2, 64], F32), ss=C.sb(st, "r_ss", [128, 2], F32),
                  t1=C.sb(st, "r_t1", [128, 2, 16], F32), t2=C.sb(st, "r_t2", [128, 2, 16], F32), eps=P["eps"])
        b = [bank(C, st, "bk%d" % i) for i in range(8)]
        v3 = lambda a, n: a.rearrange("p (a b) -> p a b", a=n)
        pT0, pT1 = v3(b[0][:], 4), v3(b[1][:], 4)
        pv, pr = b[0], b[0]
        pkv4 = b[1]
        pqk = v3(b[2][:], 4)
        pcmp = v3(b[3][:, 0:256], 2)
        pz = v3(b[3][:, 256:512], 2)
        pglr = b[3][0:16, 256:384]
        pkvs = v3(b[4][:], 2)
        pkh = v3(b[5][:, 0:256], 2)
        pkT = v3(b[5][:, 256:512], 2)
        psc = v3(b[6][:], 4)
        pmx = b[6][:, 0:256].bitcast(BF16).rearrange("p (a b) -> p a b", a=4)
        po = v3(b[7][:], 4)

        S.dma("pool", wA[:, :, 0:1552], dr["w_in_ab"][:, 0:1552].rearrange("(k p) n -> p k n", p=128), w=["wA0"])
        S.dma("pool", wA[:, :, 1552:NA], dr["w_in_ab"][:, 2064:2832].rearrange("(k p) n -> p k n", p=128), w=["wA1"])
        S.dma("sp", w2g[:], dr["gla_w_gate2"], w=["w2g"])
        S.dma("sp", negb[:], dr["gla_b_gate_col"], w=["negb"])
        S.dma("sp", gng[:], dr["gla_norm_g"].partition_broadcast(128), w=["gng"])
        S.dma("sp", maskT[:], dr["gla_maskT"], w=["maskT"])
        S.dma("sp", posi[:], dr["pos_col"], w=["Tpos"])
        S.dma("sp", valid[:], dr["valid_col"], w=["valid"])
        S.dma("sp", M["invf"][:], dr["invf"].partition_broadcast(128), w=["invf"])
        S.dma("sp", M["gain"][:, 0:3, :], dr["nsa_k_gain"].partition_broadcast(128), w=["gain"])
        S.dma("sp", M["gain"][:, 3, :], dr["nsa_q_gain"].partition_broadcast(128), w=["gainq"])
        S.op("dve", lambda e: e.tensor_scalar(out=negb[:], in0=negb[:], scalar1=-1.0, scalar2=None, op0=ALU.mult), r=["negb"], w=["negb"])
        S.op("dve", lambda e: e.memset(onesc[:], 1.0), w=["onesc"])
        S.op("dve", lambda e: e.memset(state[:], 0.0), w=["state"])
        S.op("pool", lambda e: e.memset(qhz[:], 0.0), w=["qhz"])
        S.op("pool", lambda e: e.memset(qtl[:], 0.0), w=["qtl"])
        S.op("pool", lambda e: e.memset(M["Vsel"][:, :, :, 64:65], 1.0), w=["Vsel1"])
        S.op("pool", lambda e: e.memset(M["Vwin"][:, :, :, 64:65], 1.0), w=["Vwin1"])
        S.op("dve", lambda e: e.tensor_copy(out=identb[:], in_=P["ident"][:]), r=["ident"], w=["identb"])
        S.op("dve", lambda e: e.tensor_scalar(out=M["kbias"][:], in0=valid[:], scalar1=-1.0, scalar2=1e4, op0=ALU.add, op1=ALU.mult),
             r=["valid"], w=["kbias"])
        rope_tables(C, S, st, posi, NWT, M["invf"], M["csT"], "T")

        A, B = P["modA"], P["modB"]
        wk = ["wA0", "wA1"]
        tl = list(tiles if tiles is not None else range(NWT))

        def stage_a(t, S):
            own = t >= OWN0
            i = t % 2
            S.dma("sp", xt[i][:], dr["xw"][t * 128:(t + 1) * 128, :], w=["xt%d" % i])
            norm_transpose(C, S, xt[i][:], "xt%d" % i, tmpn, P["ident"], [(pT0, "b0"), (pT1, "b1")], "a")
            for k in range(8):
                p, pk = ((pT0, "b0"), (pT1, "b1"))[k // 4]
                if k % 2 == 0:
                    S.op("dve", lambda e, k=k, p=p: e.tensor_scalar(
                        out=hT[:, k, :], in0=p[:, k % 4, :], scalar1=A[:, L, 0, k:k + 1], scalar2=B[:, L, 0, k:k + 1],
                        op0=ALU.mult, op1=ALU.add), r=[pk, "modA", "modB"], w=["hT"])
                else:
                    S.op("act", lambda e, k=k, p=p: e.activation(
                        out=hT[:, k, :], in_=p[:, k % 4, :], func=AF.Identity, scale=A[:, L, 0, k:k + 1],
                        bias=B[:, L, 0, k:k + 1]), r=[pk, "modA", "modB"], w=["hT"])
            for m in range(4):
                if m < 2 and not own:
                    continue
                for k in range(8):
                    S.op("pe", lambda e, k=k, m=m: e.matmul(pqk[:, m, :], lhsT=wA[:, k, m * 128:(m + 1) * 128], rhs=hT[:, k, :],
                                                            start=(k == 0), stop=(k == 7)), r=["hT"] + wk, w=["b2"])
            for k in range(8):
                S.op("pe", lambda e, k=k: e.matmul(pglr, lhsT=wA[:, k, CA_GLR:CA_GLR + 16], rhs=hT[:, k, :],
                                                   start=(k == 0), stop=(k == 7)), r=["hT"] + wk, w=["b3"])
            for m in range(2):
                for k in range(8):
                    S.op("pe", lambda e, k=k, m=m: e.matmul(pcmp[:, m, :], lhsT=wA[:, k, CA_KC + m * 128:CA_KC + (m + 1) * 128],
                                                            rhs=hT[:, k, :], start=(k == 0), stop=(k == 7)), r=["hT"] + wk, w=["b3"])
            for k in range(8):
                S.op("pe", lambda e, k=k: e.matmul(pv[:], lhsT=hT[:, k, :], rhs=wA[:, k, CA_V:CA_V + 512],
                                                   start=(k == 0), stop=(k == 7)), r=["hT"] + wk, w=["b0"])
            for k in range(8):
                S.op("pe", lambda e, k=k: e.matmul(pkv4[:], lhsT=hT[:, k, :], rhs=wA[:, k, CA_KV4:CA_KV4 + 512],
                                                   start=(k == 0), stop=(k == 7)), r=["hT"] + wk, w=["b1"])
            S.op("dve", lambda e: e.tensor_copy(out=glrT[:], in_=pglr), r=["b3"], w=["glrT"])
            ct = cmpT[i]
            S.op("act", lambda e: e.activation(out=ct[:], in_=pcmp, func=AF.Copy), r=["b3"], w=["cmpT%d" % i])
            for m in range(2):
                S.dma("act", dr["cmp_scr"][m, :, t * 128:(t + 1) * 128], ct[:, m, :], r=["cmpT%d" % i], w=["cmp_scr%d" % i])
            m0 = 0 if own else 2
            S.op("dve", lambda e: e.tensor_copy(out=qk2[i][:, m0:4, :], in_=pqk[:, m0:4, :]), r=["b2"], w=["qk%d" % i])
            S.op("act", lambda e: e.activation(out=vb2[i][:], in_=pv[:], func=AF.Copy), r=["b0"], w=["vb%d" % i])
            S.op("dve", lambda e: e.tensor_copy(out=kv42[i][:], in_=pkv4[:]), r=["b1"], w=["kv4s%d" % i])
            for p_ in range(2):
                S.op("pe", lambda e, p_=p_: e.matmul(pz[:, p_, :], lhsT=w2g[:, p_ * 128:(p_ + 1) * 128], rhs=glrT[:],
                                                     start=True, stop=True), r=["w2g", "glrT"], w=["b3"])
            if own:
                for k in range(8):
                    S.op("pe", lambda e, k=k: e.matmul(pr[:], lhsT=hT[:, k, :], rhs=wA[:, k, CA_R:CA_R + 512],
                                                       start=(k == 0), stop=(k == 7)), r=["hT"] + wk, w=["b0"])
            for p_ in range(2):
                S.op("act", lambda e, p_=p_: e.activation(out=sp2[i][:, p_, :], in_=pz[:, p_, :], func=AF.Exp, scale=-1.0,
                                                          bias=negb[:, p_:p_ + 1]), r=["b3", "negb"], w=["sp%d" % i])
            if own:
                S.op("act", lambda e: e.activation(out=rs2[i][:], in_=pr[:], func=AF.Exp, scale=-1.0), r=["b0"], w=["rsl%d" % i])
                S.op("dve", lambda e: e.tensor_scalar(out=rs2[i][:], in0=rs2[i][:], scalar1=1.0, scalar2=None, op0=ALU.add), r=["rsl%d" % i], w=["rsl%d" % i])
                S.op("dve", lambda e: e.reciprocal(out=rs2[i][:], in_=rs2[i][:]), r=["rsl%d" % i], w=["rsl%d" % i])
                S.op("dve", lambda e: e.tensor_tensor(out=rs2[i][:], in0=rs2[i][:], in1=pr[:], op=ALU.mult), r=["rsl%d" % i, "b0"], w=["rsl%d" % i])

        def stage_b(t, S):
            own = t >= OWN0
            i = t % 2
            sp, qk, vb, kv4s, rs_ = sp2[i], qk2[i], vb2[i], kv42[i], rs2[i]
            spk, qkk, vbk, kvk, rsk = "sp%d" % i, "qk%d" % i, "vb%d" % i, "kv4s%d" % i, "rsl%d" % i
            S.op("act", lambda e: e.activation(out=sp[:], in_=sp[:], func=AF.Ln, bias=1.0, scale=1.0), r=[spk], w=[spk])
            for p_ in range(2):
                for c in range(2):
                    S.op("dve", lambda e, p_=p_, c=c: e.tensor_tensor_scan(
                        out=cs_[:, p_, c * 64:(c + 1) * 64], data0=onesc[:], data1=sp[:, p_, c * 64:(c + 1) * 64], initial=0.0,
                        op0=ALU.mult, op1=ALU.add), r=[spk, "onesc"], w=["cs_"])
            csv = cs_[:].rearrange("p a (c t) -> p a c t", c=2)
            S.op("dve", lambda e: e.tensor_scalar(out=m16[:, :, :, 0:1], in0=csv[:, :, :, 32:33], scalar1=1.0 / 16, scalar2=None, op0=ALU.mult),
                 r=["cs_"], w=["m16"])
            S.op("dve", lambda e: e.tensor_scalar(out=m16[:, :, :, 1:2], in0=csv[:, :, :, 63:64], scalar1=1.0 / 16, scalar2=None, op0=ALU.mult),
                 r=["cs_"], w=["m16"])
            S.op("dve", lambda e: e.tensor_scalar(out=nm16[:], in0=m16[:], scalar1=-1.0, scalar2=None, op0=ALU.mult), r=["m16"], w=["nm16"])
            for p_ in range(2):
                for c in range(2):
                    sl = slice(c * 64, (c + 1) * 64)
                    S.op("act", lambda e, p_=p_, c=c, sl=sl: e.activation(out=E3[:, p_, sl], in_=cs_[:, p_, sl], func=AF.Exp,
                                                                          scale=1.0 / 16, bias=nm16[:, p_, c, 1:2]), r=["cs_", "nm16"], w=["E3"])
            S.op("act", lambda e: e.activation(out=E4[:], in_=cs_[:], func=AF.Exp, scale=-1.0 / 16), r=["cs_"], w=["E4"])
            if own:
                for p_ in range(2):
                    for c in range(2):
                        sl = slice(c * 64, (c + 1) * 64)
                        S.op("act", lambda e, p_=p_, c=c, sl=sl: e.activation(out=E1[:, p_, sl], in_=cs_[:, p_, sl], func=AF.Exp,
                                                                              scale=-1.0 / 16, bias=m16[:, p_, c, 0:1]), r=["cs_", "m16"], w=["E1"])
                        S.op("act", lambda e, p_=p_, c=c, sl=sl: e.activation(out=E2[:, p_, sl], in_=cs_[:, p_, sl], func=AF.Exp,
                                                                              scale=1.0 / 16, bias=nm16[:, p_, c, 0:1]), r=["cs_", "nm16"], w=["E2"])
            S.op("dve", lambda e: e.tensor_tensor(out=khT[:], in0=qk[:, 2:4, :], in1=E3[:], op=ALU.mult), r=[qkk, "E3"], w=["khT"])
            for p_ in range(2):
                S.op("pe", lambda e, p_=p_: e.transpose(out=pkh[:, p_, :], in_=khT[:, p_, :], identity=P["ident"][:]),
                     r=["khT", "ident"], w=["b5"])
            S.op("dve", lambda e: e.tensor_scalar(out=kh[:], in0=pkh, scalar1=valid[:, t:t + 1], scalar2=None, op0=ALU.mult),
                 r=["b5", "valid"], w=["kh"])
            if own:
                S.op("dve", lambda e: e.tensor_tensor(out=ktl[:], in0=qk[:, 2:4, :], in1=E2[:], op=ALU.mult), r=[qkk, "E2"], w=["ktl"])
                for p_ in range(2):
                    for hh in range(2):
                        rs64 = slice(hh * 64, (hh + 1) * 64)
                        S.op("dve", lambda e, p_=p_, hh=hh, rs64=rs64: e.scalar_tensor_tensor(
                            out=qtl[rs64, p_, hh, :], in0=qk[rs64, p_, :], scalar=0.125, in1=E1[rs64, p_, :],
                            op0=ALU.mult, op1=ALU.mult), r=[qkk, "E1"], w=["qtl"])
                    for c in range(2):
                        sl = slice(c * 64, (c + 1) * 64)
                        S.op("dve", lambda e, p_=p_, c=c, sl=sl: e.scalar_tensor_tensor(
                            out=qhz[:, p_, c, sl], in0=qk[:, p_, sl], scalar=0.125, in1=E4[:, p_, sl], op0=ALU.mult, op1=ALU.mult),
                            r=[qkk, "E4"], w=["qhz"])
                for hd in range(4):
                    p_ = hd // 2
                    S.op("pe", lambda e, hd=hd, p_=p_: e.matmul(psc[:, hd, :], lhsT=ktl[:, p_, :], rhs=qtl[:, p_, hd % 2, :],
                                                                start=True, stop=True), r=["ktl", "qtl"], w=["b6"])
                S.op("dve", lambda e: e.tensor_tensor(out=sT[:], in0=psc, in1=maskT[:], op=ALU.mult), r=["b6", "maskT"], w=["sT"])
            for c in range(2):
                if own:
                    S.op("act", lambda e, c=c: e.activation(out=sbf[c][:], in_=state[:], func=AF.Copy), r=["state"], w=["sbf%d" % c])
                for p_ in range(2):
                    cs64 = slice(c * 64, (c + 1) * 64)
                    S.op("pe", lambda e, p_=p_, cs64=cs64: e.matmul(pkvs[:, p_, :], lhsT=kh[cs64, p_, :], rhs=vb[cs64, p_ * 256:(p_ + 1) * 256],
                                                                    start=True, stop=True), r=["kh", vbk], w=["b4"])
                for hd in range(4):
                    p_, o_ = hd // 2, (hd % 2) * 64
                    dcol = E4[o_:o_ + 64, p_, c * 64 + 63:c * 64 + 64]
                    S.op("dve", lambda e, hd=hd, p_=p_, o_=o_, dcol=dcol: e.scalar_tensor_tensor(
                        out=state[o_:o_ + 64, hd, :], in0=state[o_:o_ + 64, hd, :], scalar=dcol,
                        in1=pkvs[o_:o_ + 64, p_, (hd % 2) * 128:(hd % 2) * 128 + 128], op0=ALU.mult, op1=ALU.add),
                        r=["state", "E4", "b4"], w=["state"])
            if own:
                for hd in range(4):
                    p_, o_ = hd // 2, (hd % 2) * 64
                    S.op("pe", lambda e, hd=hd: e.matmul(po[:, hd, :], lhsT=sT[:, hd, :], rhs=vb[:, hd * 128:(hd + 1) * 128],
                                                         start=True, stop=False), r=["sT", vbk], w=["b7"])
                    for c in range(2):
                        S.op("pe", lambda e, hd=hd, p_=p_, o_=o_, c=c: e.matmul(
                            po[:, hd, :], lhsT=qhz[o_:o_ + 64, p_, c, :], rhs=sbf[c][o_:o_ + 64, hd, :], start=False, stop=(c == 1)),
                            r=["qhz", "sbf%d" % c], w=["b7"])
                for hd in range(4):
                    S.op("act", lambda e, hd=hd: e.activation(out=sqj[:], in_=po[:, hd, :], func=AF.Square, accum_out=ssq[:, hd:hd + 1]),
                         r=["b7"], w=["sqj", "ssq"])
                S.op("act", lambda e: e.activation(out=ssq[:], in_=ssq[:], func=AF.Ln, scale=1.0 / 128, bias=P["eps"][:]), r=["ssq"], w=["ssq"])
                S.op("act", lambda e: e.activation(out=ssq[:], in_=ssq[:], func=AF.Exp, scale=-0.5), r=["ssq"], w=["ssq"])
                for hd in range(4):
                    S.op("dve", lambda e, hd=hd: e.scalar_tensor_tensor(out=on[:, hd * 128:(hd + 1) * 128], in0=po[:, hd, :], scalar=ssq[:, hd:hd + 1],
                                                                        in1=gng[:], op0=ALU.mult, op1=ALU.mult), r=["b7", "ssq", "gng"], w=["on"])
                S.op("dve", lambda e: e.tensor_tensor(out=oa[:], in0=on[:], in1=rs_[:], op=ALU.mult), r=["on", rsk], w=["oa"])
                if dbg is not None:
                    S.op("dve", lambda e: e.tensor_tensor(out=oaf[:], in0=on[:], in1=rs_[:], op=ALU.mult), r=["on", rsk], w=["oaf"])
                    S.dma("sp", dbg["o_a"][(t - OWN0) * 128:(t - OWN0 + 1) * 128, :], oaf[:], r=["oaf"], w=["dbg_oa"])
                for hd in range(4):
                    S.op("pe", lambda e, hd=hd: e.transpose(out=pmx[:, hd, :], in_=oa[:, hd * 128:(hd + 1) * 128], identity=identb[:]),
                         r=["oa", "identb"], w=["b6"])
                S.op("act", lambda e: e.activation(out=M["mixT"][:, 0:4, (t - OWN0) * 128:(t - OWN0 + 1) * 128], in_=pmx, func=AF.Copy),
                     r=["b6"], w=["mixTa%d" % (t - OWN0)])
            kv4 = kv4s[:].rearrange("p (s g d) -> p s g d", s=4, g=2)
            branches = [(0, 1)] + ([(1, 2)] if t >= 44 else [])
            for br, gi in branches:
                rms_gain_rope(C, S, kv4[:, 2 * br, :, :], kvk, kn[:, br, :, :], "kn%d" % br, 2, M["gain"][:, gi, :], "gain",
                              M["csT"][:, t, :], "Tcs", RT, "rk")
                S.op("pe", lambda e, br=br: e.transpose(out=pkT[:, br, :], in_=kn[:, br, :, :].rearrange("p g d -> p (g d)"), identity=P["ident"][:]),
                     r=["kn%d" % br, "ident"], w=["b5"])
            S.op("act", lambda e: e.activation(out=M["KselT"][:, t * 128:(t + 1) * 128], in_=pkT[:, 0, :], func=AF.Copy),
                 r=["b5"], w=["KselT%d" % t])
            S.op("pool", lambda e: e.tensor_copy(out=M["Vsel"][:, t, :, 0:64], in_=kv4[:, 1, :, :]), r=[kvk], w=["Vsel%d" % t])
            if t >= 44:
                S.op("act", lambda e: e.activation(out=M["KwinT"][:, (t - 44) * 128:(t - 43) * 128], in_=pkT[:, 1, :], func=AF.Copy),
                     r=["b5"], w=["KwinT%d" % (t - 44)])
                S.op("pool", lambda e: e.tensor_copy(out=M["Vwin"][:, t - 44, :, 0:64], in_=kv4[:, 3, :, :]), r=[kvk], w=["Vwin%d" % (t - 44)])

        if tl:
            stage_a(tl[0], S)
        for n_, t in enumerate(tl):
            ra, rb = Rec(), Rec()
            if n_ + 1 < len(tl):
                stage_a(tl[n_ + 1], ra)
            stage_b(t, rb)
            interleave(S, ra, rb)
        if dbg is not None:
            S.wait_all("sp", ["dbg_oa"])
        S.wait_all("act", ["cmp_scr0", "cmp_scr1"])
        S.flush()


def alloc_cmp(C, st):
    M2 = {}
    M2["KcT"] = C.sb(st, "KcT", [128, 512], F32)
    M2["Vc"] = C.sb(st, "Vc", [128, 4, 2, 65], F32)
    M2["cbias"] = C.sb(st, "cbias", [128, 4], F32)
    return M2


def phase_cmp(C, P, M, M2, dr):
    nc, S = C.nc, C.S
    with contextlib.ExitStack() as st:
        KC2 = C.sb(st, "KC2", [128, 2, 2, 8192], BF16)
        w1 = C.sb(st, "w1", [128, 2, 16, 256], BF16)
        w2 = C.sb(st, "w2", [128, 2, 2, 64], BF16)
        pecol = C.sb(st, "pecol", [128, 2, 16], F32)
        pecb = C.sb(st, "pecb", [128, 2, 16], BF16)
        pbias = C.sb(st, "pbias", [128, 2, 2], F32)
        hT = [C.sb(st, "chT%d" % i, [128, 2, 512], BF16) for i in range(2)]
        posb = C.sb(st, "posb", [128, 4], I32)
        csB = C.sb(st, "csB", [128, 4, 16], F32)
        cval = C.sb(st, "cval", [128, 4], F32)
        kcn = C.sb(st, "kcn", [128, 2, 64], F32)
        RT = dict(sq=C.sb(st, "c_sq", [128, 2, 64], F32), ss=C.sb(st, "c_ss", [128, 2], F32),
                  t1=C.sb(st, "c_t1", [128, 2, 16], F32), t2=C.sb(st, "c_t2", [128, 2, 16], F32), eps=P["eps"])
        ph = [bank(C, st, "cph%d" % i) for i in range(2)]
        pb = bank(C, st, "cpb")
        po = [bank(C, st, "cpo%d" % i) for i in range(2)]
        pt = bank(C, st, "cpt")

        for kv in range(2):
            for g in range(2):
                S.dma("sp", KC2[0:64, kv, g, :], dr["cmp_scr"][kv, g * 64:(g + 1) * 64, :], r=["cmp_scr0", "cmp_scr1"], w=["KC2a%d%d" % (kv, g)])
                S.dma("sp", KC2[64:128, kv, g, 0:8191], dr["cmp_scr"][kv, g * 64:(g + 1) * 64, 1:8192], r=["cmp_scr0", "cmp_scr1"],
                      w=["KC2b%d%d" % (kv, g)])
            S.dma("pool", w1[:, kv, :, :], dr["nsa_cmp_w1"][kv].rearrange("(lp p) n -> p lp n", p=128), w=["w1_%d" % kv])
            S.dma("pool", w2[:, kv, :, :], dr["nsa_cmp_w2"][kv].rearrange("(hc p) n -> p hc n", p=128), w=["w2_%d" % kv])
        S.dma("sp", pecol[:], dr["cmp_pe_col"], w=["pecol"])
        S.dma("sp", posb[:], dr["posb_col"], w=["Bpos"])
        S.dma("sp", cval[:], dr["cvalid_col"], w=["cval"])
        S.op("dve", lambda e: e.tensor_copy(out=pecb[:], in_=pecol[:]), r=["pecol"], w=["pecb"])
        S.op("dve", lambda e: e.tensor_scalar(out=M2["cbias"][:], in0=cval[:], scalar1=-1.0, scalar2=1e4, op0=ALU.add, op1=ALU.mult),
             r=["cval"], w=["cbias"])
        S.op("pool", lambda e: e.memset(M2["Vc"][:, :, :, 64:65], 1.0), w=["Vc1"])
        for i in range(2):
            S.op("pool", lambda e, i=i: e.memset(hT[i][:, :, 511:512], 0.0), w=["chT%d" % i])
        rope_tables(C, S, st, posb, 4, M["invf"], csB, "B")
        for kv in range(2):
            for hc in range(2):
                for lp in range(16):
                    S.op("pe", lambda e, kv=kv, hc=hc, lp=lp: e.matmul(
                        pb[:, kv * 2 + hc:kv * 2 + hc + 1], lhsT=w1[:, kv, lp, hc * 128:(hc + 1) * 128], rhs=pecb[:, kv, lp:lp + 1],
                        start=(lp == 0), stop=(lp == 15)), r=["w1_%d" % kv, "pecb"], w=["cpb"])
        S.op("dve", lambda e: e.tensor_copy(out=pbias[:].rearrange("p a b -> p (a b)"), in_=pb[:, 0:4]), r=["cpb"], w=["pbias"])
        n = 0
        for kv in range(2):
            for g in range(2):
                h = hT[n % 2]
                hk = "chT%d" % (n % 2)
                n += 1
                for hc in range(2):
                    p = ph[hc]
                    for lp in range(16):
                        S.op("pe", lambda e, kv=kv, g=g, hc=hc, lp=lp, p=p: e.matmul(
                            p[:, 0:511], lhsT=w1[:, kv, lp, hc * 128:(hc + 1) * 128],
                            rhs=KC2[:, kv, g, 2 * lp:2 * lp + 16 * 510 + 1:16], start=(lp == 0), stop=(lp == 15)),
                            r=["w1_%d" % kv, "KC2a%d%d" % (kv, g), "KC2b%d%d" % (kv, g)], w=["cph%d" % hc])
                    S.op("act", lambda e, hc=hc, h=h, p=p, kv=kv: e.activation(out=h[:, hc, 0:511], in_=p[:, 0:511], func=GELU,
                                                                               bias=pbias[:, kv, hc:hc + 1]), r=["cph%d" % hc, "pbias"], w=[hk])
                for bc in range(4):
                    o = po[kv]
                    ok = "cpo%d" % kv
                    for hc in range(2):
                        S.op("pe", lambda e, kv=kv, g=g, hc=hc, bc=bc, h=h, o=o: e.matmul(
                            o[:, (bc * 2 + g) * 64:(bc * 2 + g + 1) * 64], lhsT=h[:, hc, bc * 128:(bc + 1) * 128], rhs=w2[:, kv, hc, :],
                            start=(hc == 0), stop=(hc == 1)), r=[hk, "w2_%d" % kv], w=[ok])
        pk4 = po[0][:].rearrange("p (b g d) -> p b g d", b=4, g=2)
        pv4 = po[1][:].rearrange("p (b g d) -> p b g d", b=4, g=2)
        ptv = pt[:].rearrange("p (a b) -> p a b", a=4)
        for bc in range(4):
            rms_gain_rope(C, S, pk4[:, bc, :, :], "cpo0", kcn[:], "kcn", 2, M["gain"][:, 0, :], "gain", csB[:, bc, :], "Bcs", RT, "ck")
            S.op("pe", lambda e, bc=bc: e.transpose(out=ptv[:, bc, :], in_=kcn[:].rearrange("p g d -> p (g d)"), identity=P["ident"][:]),
                 r=["kcn", "ident"], w=["cpt"])
        S.op("act", lambda e: e.activation(out=M2["KcT"][:], in_=pt[:], func=AF.Copy), r=["cpt"], w=["KcT"])
        S.op("dve", lambda e: e.tensor_copy(out=M2["Vc"][:, :, :, 0:64], in_=pv4), r=["cpo1"], w=["Vc"])
        S.flush()


NEGB = -30000.0


def phase_nsa(C, P, M, M2, dr, dbg=None, qtiles=None):
    nc, S = C.nc, C.S
    L = 0
    with contextlib.ExitStack() as st:
        wB = C.sb(st, "wB", [128, 8, 536], BF16)
        Esel = C.sb(st, "Esel", [128, 64, 128], BF16)
        cover = C.sb(st, "cover", [128, 4, 129], F32)
        corebias = C.sb(st, "corebias", [128, 128], F32)
        causb = C.sb(st, "causb", [128, 4, 128], BF16)
        winb = C.sb(st, "winb", [128, 4, 128], BF16)
        identb = C.sb(st, "identb", [128, 128], BF16)
        kbC = C.sb(st, "kbC", [128, NWT], F32)
        cbC = C.sb(st, "cbC", [128, 4], F32)
        gm = C.sb(st, "gm", [128, 4], F32)
        Cc = C.sb(st, "Cc", [128, 1], F32)
        xt = [C.sb(st, "xt%d" % i, [128, D], F32) for i in range(2)]
        tmpn = dict(junk=C.sb(st, "junk", [128, D], BF16), ss=C.sb(st, "ss", [128, 1], F32),
                    rs=C.sb(st, "rs", [128, 1], F32), xn=C.sb(st, "xn", [128, D], F32), eps=P["eps"], lnexp=True)
        hT = C.sb(st, "hT", [128, 8, 128], BF16)
        qn = C.sb(st, "qn", [128, 8, 64], F32)
        RT = dict(sq=C.sb(st, "q_sq", [128, 8, 64], F32), ss=C.sb(st, "q_ss", [128, 8], F32),
                  t1=C.sb(st, "q_t1", [128, 8, 16], F32), t2=C.sb(st, "q_t2", [128, 8, 16], F32), eps=P["eps"])
        QT32 = C.sb(st, "QT32", [128, 2, 4, 128], F32)
        QTb = C.sb(st, "QTb", [128, 2, 4, 128], BF16)
        gts2 = [C.sb(st, "gts%d" % i, [128, 24], F32) for i in range(2)]
        cm = [C.sb(st, "cm%d" % i, [128, 4, 128], F32) for i in range(2)]
        sbias = [C.sb(st, "sbias%d" % i, [128, 128], F32) for i in range(2)]
        PcT = [C.sb(st, "PcT%d" % i, [128, 4, 128], F32) for i in range(4)]
        PTs = [[C.sb(st, "PT%d_%d" % (g_, i), [128, 512], BF16) for i in range(3)] for g_ in range(2)]
        oTs = [[C.sb(st, "oTs%d%d" % (g_, i), [65, 512], F32) for i in range(2)] for g_ in range(2)]
        rden = C.sb(st, "rden", [128, 4], F32)
        acc = C.sb(st, "acc", [128, 128], F32)
        m8a = C.sb(st, "m8a", [128, 8], F32)
        m8b = C.sb(st, "m8b", [128, 8], F32)
        sc2 = C.sb(st, "sc2", [128, 128], F32)
        selm = C.sb(st, "selm", [128, 128], F32)
        selv = C.sb(st, "selv", [128, 128], F32)
        NBt2 = [C.sb(st, "NBt%d" % i, [128, 4, 128], BF16) for i in range(2)]
        oT = C.sb(st, "oT", [65, 512], F32)
        ob2 = [C.sb(st, "ob%d" % i, [128, 512], F32) for i in range(2)]
        obb = C.sb(st, "obb", [128, 512], BF16)
        coef = C.sb(st, "coef", [128, 4], F32)
        bks = [bank(C, st, "nb%d" % i) for i in range(8)]
        v3 = lambda a, n: a.rearrange("p (a b) -> p a b", a=n)
        pT0, pT1 = v3(bks[0][:], 4), v3(bks[1][:], 4)
        pq = bks[2]
        pqT = v3(bks[3][:], 4)
        pS = [bks[4], bks[5]]
        pO = bks[6]
        pX = bks[7]
        pW = bks[1]
        pS3 = [bks[4], bks[5], bks[3]]
        pS3k = ["nb4", "nb5", "nb3"]

        S.dma("pool", wB[:, :, 0:512], dr["w_nq_perm"].rearrange("(k p) n -> p k n", p=128), w=["wB0"])
        S.dma("pool", wB[:, :, 512:536], dr["w_in_ab"][:, 2832:2856].rearrange("(k p) n -> p k n", p=128), w=["wB1"])
        S.dma("sp", Esel[:], dr["Esel"], w=["Esel"])
        S.dma("sp", cover[:], dr["cover"], w=["cover"])
        S.dma("sp", corebias[:], dr["corebias"], w=["corebias"])
        S.dma("sp", causb[:], dr["causb"], w=["causb"])
        S.dma("sp", winb[:], dr["winb"], w=["winb"])
        S.op("dve", lambda e: e.tensor_copy(out=identb[:], in_=P["ident"][:]), r=["ident"], w=["identb"])
        S.op("pool", lambda e: e.memset(QT32[:], 0.0), w=["QT32"])
        S.op("pool", lambda e: e.memset(QTb[:], 0.0), w=["QTb"])
        S.op("dve", lambda e: e.tensor_reduce(out=gm[:], in_=M["gain"][:], axis=AX.X, op=ALU.max, apply_absolute_value=True),
             r=["gain", "gainq"], w=["gm"])
        S.op("dve", lambda e: e.tensor_reduce(out=Cc[:], in_=gm[:, 0:3], axis=AX.X, op=ALU.max), r=["gm"], w=["Cc"])
        S.op("dve", lambda e: e.tensor_scalar(out=Cc[:], in0=Cc[:], scalar1=gm[:, 3:4], scalar2=-8.0, op0=ALU.mult, op1=ALU.mult),
             r=["Cc", "gm"], w=["Cc"])
        S.op("dve", lambda e: e.tensor_scalar(out=kbC[:], in0=M["kbias"][:], scalar1=Cc[:, 0:1], scalar2=None, op0=ALU.add),
             r=["kbias", "Cc"], w=["kbC"])
        S.op("dve", lambda e: e.tensor_scalar(out=cbC[:], in0=M2["cbias"][:], scalar1=Cc[:, 0:1], scalar2=None, op0=ALU.add),
             r=["cbias", "Cc"], w=["cbC"])

        A, B = P["modA"], P["modB"]
        nS = 0
        nP = 0
        def pro(qt, S):
            T = OWN0 + qt
            i = qt % 2
            gts = gts2[i]
            ob = ob2[i]
            gk = "gts%d" % i
            obk = "ob%d" % i
            S.dma("sp", xt[i][:], dr["xw"][T * 128:(T + 1) * 128, :], w=["xt%d" % i])
            S.dma("sp", cm[i][:], dr["cmaskT"][qt], w=["cm%d" % i])
            S.dma("sp", sbias[i][:], dr["selbias"][qt], w=["sbias%d" % i])
            norm_transpose(C, S, xt[i][:], "xt%d" % i, tmpn, P["ident"], [(pT0, "nb0"), (pT1, "nb1")], "n")
            for k in range(8):
                p, pk = ((pT0, "nb0"), (pT1, "nb1"))[k // 4]
                if k % 2 == 0:
                    S.op("dve", lambda e, k=k, p=p: e.tensor_scalar(
                        out=hT[:, k, :], in0=p[:, k % 4, :], scalar1=A[:, L, 0, k:k + 1], scalar2=B[:, L, 0, k:k + 1],
                        op0=ALU.mult, op1=ALU.add), r=[pk, "modA", "modB"], w=["hT"])
                else:
                    S.op("act", lambda e, k=k, p=p: e.activation(
                        out=hT[:, k, :], in_=p[:, k % 4, :], func=AF.Identity, scale=A[:, L, 0, k:k + 1],
                        bias=B[:, L, 0, k:k + 1]), r=[pk, "modA", "modB"], w=["hT"])
            for k in range(8):
                S.op("pe", lambda e, k=k: e.matmul(pq[:], lhsT=hT[:, k, :], rhs=wB[:, k, 0:512], start=(k == 0), stop=(k == 7)),
                     r=["hT", "wB0"], w=["nb2"])
            for k in range(8):
                S.op("pe", lambda e, k=k: e.matmul(bks[0][:, 0:24], lhsT=hT[:, k, :], rhs=wB[:, k, 512:536], start=(k == 0), stop=(k == 7)),
                     r=["hT", "wB1"], w=["nb0"])
            S.op("act", lambda e: e.activation(out=gts[:], in_=bks[0][:, 0:24], func=AF.Exp, scale=-1.0), r=["nb0"], w=[gk])
            S.op("dve", lambda e: e.tensor_scalar(out=gts[:], in0=gts[:], scalar1=1.0, scalar2=None, op0=ALU.add), r=[gk], w=[gk])
            S.op("dve", lambda e: e.reciprocal(out=gts[:], in_=gts[:]), r=[gk], w=[gk])
            rms_gain_rope(C, S, pq[:].rearrange("p (h d) -> p h d", h=8), "nb2", qn[:], "qn", 8, M["gain"][:, 3, :], "gainq",
                          M["csT"][:, T, :], "Tcs", RT, "rq")
            for hh in range(4):
                S.op("pe", lambda e, hh=hh: e.transpose(out=pqT[:, hh, :], in_=qn[:, 2 * hh:2 * hh + 2, :].rearrange("p g d -> p (g d)"),
                                                        identity=P["ident"][:]), r=["qn", "ident"], w=["nb3"])
            for g in range(2):
                rg = slice(g * 64, (g + 1) * 64)
                S.op("dve", lambda e, g=g, rg=rg: e.tensor_copy(out=QT32[rg, g, :, :], in_=pqT[rg, :, :]), r=["nb3"], w=["QT32"])
                S.op("dve", lambda e, g=g, rg=rg: e.tensor_copy(out=QTb[rg, g, :, :], in_=pqT[rg, :, :]), r=["nb3"], w=["QTb"])
            S.op("dve", lambda e: e.memset(ob[:], 0.0), w=[obk])


        qlist = list(qtiles if qtiles is not None else range(NT))
        pro(qlist[0], S)
        for qi, qt in enumerate(qlist):
            T = OWN0 + qt
            i = qt % 2
            gts = gts2[i]
            ob = ob2[i]
            gk = "gts%d" % i
            obk = "ob%d" % i
            def finish_branch(g, br):
                S.op("act", lambda e: e.activation(out=oT[:], in_=pO[0:65, :], func=AF.Copy), r=["nb6"], w=["oT"])
                pXv = pX[:, 0:260].rearrange("p (h d) -> p h d", h=4)
                for hh in range(4):
                    S.op("pe", lambda e, hh=hh: e.transpose(out=pXv[:, hh, :], in_=oT[:, hh * 128:(hh + 1) * 128], identity=P["ident"][0:65, 0:65]),
                         r=["oT", "ident"], w=["nb7"])
                S.op("dve", lambda e: e.tensor_scalar(out=coef[:], in0=pXv[:, :, 64], scalar1=1e-30, scalar2=None, op0=ALU.max), r=["nb7"], w=["coef"])
                S.op("dve", lambda e: e.reciprocal(out=coef[:], in_=coef[:]), r=["coef"], w=["coef"])
                gv = gts[:].rearrange("p (g h b) -> p g h b", g=2, h=4)
                S.op("dve", lambda e: e.tensor_tensor(out=coef[:], in0=coef[:], in1=gv[:, g, :, br], op=ALU.mult), r=["coef", gk], w=["coef"])
                for hh in range(4):
                    c0 = (g * 4 + hh) * 64
                    S.op("dve", lambda e, hh=hh, c0=c0: e.scalar_tensor_tensor(
                        out=ob[:, c0:c0 + 64], in0=pXv[:, hh, 0:64], scalar=coef[:, hh:hh + 1], in1=ob[:, c0:c0 + 64],
                        op0=ALU.mult, op1=ALU.add), r=["nb7", "coef", obk], w=[obk])

            def finish_branch2(g, br, pacc, pacck):
                S.op("act", lambda e: e.activation(out=oT[:], in_=pacc[0:65, :], func=AF.Copy), r=[pacck], w=["oT"])
                pXv = pX[:, 0:260].rearrange("p (h d) -> p h d", h=4)
                for hh in range(4):
                    S.op("pe", lambda e, hh=hh: e.transpose(out=pXv[:, hh, :], in_=oT[:, hh * 128:(hh + 1) * 128], identity=P["ident"][0:65, 0:65]),
                         r=["oT", "ident"], w=["nb7"])
                S.op("dve", lambda e: e.tensor_scalar(out=coef[:], in0=pXv[:, :, 64], scalar1=1e-30, scalar2=None, op0=ALU.max), r=["nb7"], w=["coef"])
                S.op("dve", lambda e: e.reciprocal(out=coef[:], in_=coef[:]), r=["coef"], w=["coef"])
                gv = gts[:].rearrange("p (g h b) -> p g h b", g=2, h=4)
                S.op("dve", lambda e: e.tensor_tensor(out=coef[:], in0=coef[:], in1=gv[:, g, :, br], op=ALU.mult), r=["coef", gk], w=["coef"])
                for hh in range(4):
                    c0 = (g * 4 + hh) * 64
                    S.op("dve", lambda e, hh=hh, c0=c0: e.scalar_tensor_tensor(
                        out=ob[:, c0:c0 + 64], in0=pXv[:, hh, 0:64], scalar=coef[:, hh:hh + 1], in1=ob[:, c0:c0 + 64],
                        op0=ALU.mult, op1=ALU.add), r=["nb7", "coef", obk], w=[obk])

            pI2 = [[(bks[0], "nb0"), (bks[1], "nb1")], [(bks[2], "nb2"), (bks[3], "nb3")]]
            pI = [bks[0], bks[1]]
            for g in range(2):
                for bc in range(4):
                    j = nS % 2
                    nS += 1
                    pc = PcT[bc]
                    pck = "PcT%d" % bc
                    S.op("pe", lambda e, g=g, bc=bc, j=j: e.matmul(pS[j][:], lhsT=M2["KcT"][:, bc * 128:(bc + 1) * 128],
                                                                   rhs=QT32[:, g, :, :], start=True, stop=True),
                         r=["KcT", "QT32"], w=["nb%d" % (4 + j)])
                    S.op("act", lambda e, bc=bc, j=j, pc=pc: e.activation(out=pc[:].rearrange("p a b -> p (a b)"), in_=pS[j][:], func=AF.Exp,
                                                                          scale=0.125, bias=cbC[:, bc:bc + 1]),
                         r=["nb%d" % (4 + j), "cbC"], w=[pck])
                    S.op("dve", lambda e, bc=bc, pc=pc, i=i: e.tensor_tensor(out=pc[:], in0=pc[:],
                                                                             in1=cm[i][:, bc, :].unsqueeze(1).to_broadcast([128, 4, 128]), op=ALU.mult),
                         r=[pck, "cm%d" % i], w=[pck])
                for bc in range(4):
                    pc = PcT[bc]
                    pck = "PcT%d" % bc
                    S.op("pe", lambda e, g=g, bc=bc, pc=pc: e.matmul(pO[0:65, :], lhsT=M2["Vc"][:, bc, g, :], rhs=pc[:].rearrange("p a b -> p (a b)"),
                                                                     start=(bc == 0), stop=(bc == 3)), r=[pck, "Vc", "Vc1"], w=["nb6"])
                for hh in range(4):
                    pi, pik = pI2[g][hh // 2]
                    for bc in range(4):
                        pc = PcT[bc]
                        pck = "PcT%d" % bc
                        S.op("pe", lambda e, bc=bc, pc=pc, hh=hh, pi=pi: e.matmul(
                            pi[:, (hh % 2) * 129:(hh % 2) * 129 + 129], lhsT=pc[:, hh, :], rhs=cover[:, bc, :],
                            start=(bc == 0), stop=(bc == 3)), r=[pck, "cover"], w=[pik])
                finish_branch2(g, 0, pO, "nb6")
            for g in range(2):
                for hh in range(4):
                    pi, pik = pI2[g][hh // 2]
                    S.op("dve", lambda e, hh=hh, pi=pi: e.tensor_scalar(out=rden[:, hh:hh + 1], in0=pi[:, (hh % 2) * 129 + 128:(hh % 2) * 129 + 129],
                                                                        scalar1=1e-30, scalar2=None, op0=ALU.max), r=[pik], w=["rden"])
                S.op("dve", lambda e: e.reciprocal(out=rden[:], in_=rden[:]), r=["rden"], w=["rden"])
                for hh in range(4):
                    pi, pik = pI2[g][hh // 2]
                    src = pi[:, (hh % 2) * 129:(hh % 2) * 129 + 128]
                    if hh == 0:
                        S.op("dve", lambda e, src=src: e.scalar_tensor_tensor(out=acc[:], in0=src, scalar=rden[:, 0:1], in1=sbias[i][:],
                                                                              op0=ALU.mult, op1=ALU.add), r=[pik, "rden", "sbias%d" % i], w=["acc"])
                    else:
                        S.op("dve", lambda e, src=src, hh=hh: e.scalar_tensor_tensor(out=acc[:], in0=src, scalar=rden[:, hh:hh + 1], in1=acc[:],
                                                                                     op0=ALU.mult, op1=ALU.add), r=[pik, "rden", "acc"], w=["acc"])
                S.op("dve", lambda e: e.tensor_tensor(out=acc[:], in0=acc[:], in1=corebias[:], op=ALU.add), r=["acc", "corebias"], w=["acc"])
                S.op("dve", lambda e: e.max(out=m8a[:], in_=acc[:]), r=["acc"], w=["m8a"])
                S.op("dve", lambda e: e.match_replace(out=sc2[:], in_to_replace=m8a[:], in_values=acc[:], imm_value=-3e38),
                     r=["acc", "m8a"], w=["sc2"])
                S.op("dve", lambda e: e.max(out=m8b[:], in_=sc2[:]), r=["sc2"], w=["m8b"])
                S.op("dve", lambda e: e.tensor_scalar(out=selm[:], in0=acc[:], scalar1=m8b[:, 7:8], scalar2=None, op0=ALU.is_ge),
                     r=["acc", "m8b"], w=["selm"])
                S.op("dve", lambda e: e.tensor_scalar(out=selv[:], in0=acc[:], scalar1=-1e29, scalar2=None, op0=ALU.is_gt), r=["acc"], w=["selv"])
                S.op("dve", lambda e: e.tensor_tensor(out=selm[:], in0=selm[:], in1=selv[:], op=ALU.mult), r=["selm", "selv"], w=["selm"])
                if dbg is not None and "selm" in dbg:
                    S.dma("sp", dbg["selm"][qt, g], selm[:], r=["selm"], w=["dbg_selm"])
                S.op("pe", lambda e: e.transpose(out=pX[:, 384:512], in_=selm[:], identity=P["ident"][:]), r=["selm", "ident"], w=["nb7"])
                S.op("dve", lambda e, g=g: e.tensor_scalar(out=NBt2[g][:], in0=pX[:, 384:512].unsqueeze(1).to_broadcast([128, 4, 128]), scalar1=-1.0,
                                                           scalar2=-NEGB, op0=ALU.add, op1=ALU.mult), r=["nb7"], w=["NBt%d" % g])
            def finish_tail(g, br, src):
                pXv = pX[:, 0:260].rearrange("p (h d) -> p h d", h=4)
                srck = "oTs%d%d" % (g, br)
                for hh in range(4):
                    S.op("pe", lambda e, hh=hh: e.transpose(out=pXv[:, hh, :], in_=src[:, hh * 128:(hh + 1) * 128], identity=P["ident"][0:65, 0:65]),
                         r=[srck, "ident"], w=["nb7"])
                S.op("dve", lambda e: e.tensor_scalar(out=coef[:], in0=pXv[:, :, 64], scalar1=1e-30, scalar2=None, op0=ALU.max), r=["nb7"], w=["coef"])
                S.op("dve", lambda e: e.reciprocal(out=coef[:], in_=coef[:]), r=["coef"], w=["coef"])
                gv = gts[:].rearrange("p (g h b) -> p g h b", g=2, h=4)
                S.op("dve", lambda e: e.tensor_tensor(out=coef[:], in0=coef[:], in1=gv[:, g, :, br], op=ALU.mult), r=["coef", gk], w=["coef"])
                for hh in range(4):
                    c0 = (g * 4 + hh) * 64
                    S.op("dve", lambda e, hh=hh, c0=c0: e.scalar_tensor_tensor(
                        out=ob[:, c0:c0 + 64], in0=pXv[:, hh, 0:64], scalar=coef[:, hh:hh + 1], in1=ob[:, c0:c0 + 64],
                        op0=ALU.mult, op1=ALU.add), r=["nb7", "coef", obk], w=[obk])

            def stream(g, R):
                psb = [(bks[4], "nb4"), (bks[5], "nb5")] if g == 0 else [(bks[2], "nb2"), (bks[3], "nb3")]
                pOW, pOWk = (bks[6], "nb6") if g == 0 else (bks[1], "nb1")
                chunks = [("sel", kc) for kc in range(T + 1)] + [("win", wi) for wi in range(5)]
                nck = len(chunks)

                def emit_S(ci):
                    kind, a_ = chunks[ci]
                    ps, psk = psb[ci % 2]
                    if kind == "sel":
                        kc = a_
                        last = (kc == T)
                        R.op("pe", lambda e: e.matmul(ps[:], lhsT=M["KselT"][:, kc * 128:(kc + 1) * 128], rhs=QTb[:, g, :, :], start=True, stop=False),
                             r=["KselT%d" % kc, "QTb"], w=[psk])
                        R.op("pe", lambda e: e.matmul(ps[:], lhsT=Esel[:, kc, :], rhs=NBt2[g][:], start=False, stop=(not last)),
                             r=["Esel", "NBt%d" % g], w=[psk])
                        if last:
                            R.op("pe", lambda e: e.matmul(ps[:], lhsT=identb[:], rhs=causb[:], start=False, stop=True),
                                 r=["identb", "causb"], w=[psk])
                    else:
                        wi = a_
                        kc = T - 4 + wi
                        edge = wi in (0, 4)
                        R.op("pe", lambda e: e.matmul(ps[:], lhsT=M["KwinT"][:, (kc - 44) * 128:(kc - 43) * 128], rhs=QTb[:, g, :, :],
                                                      start=True, stop=(not edge)), r=["KwinT%d" % (kc - 44), "QTb"], w=[psk])
                        if edge:
                            mb = winb if wi == 0 else causb
                            R.op("pe", lambda e: e.matmul(ps[:], lhsT=identb[:], rhs=mb[:], start=False, stop=True),
                                 r=["identb", "causb", "winb"], w=[psk])

                def emit_PV(ci):
                    kind, a_ = chunks[ci]
                    ps, psk = psb[ci % 2]
                    pt_ = PTs[g][ci % 3]
                    ptk = "PT%d_%d" % (g, ci % 3)
                    kc = a_ if kind == "sel" else T - 4 + a_
                    R.op("act", lambda e: e.activation(out=pt_[:], in_=ps[:], func=AF.Exp, scale=0.125, bias=kbC[:, kc:kc + 1]),
                         r=[psk, "kbC"], w=[ptk])
                    if kind == "sel":
                        R.op("pe", lambda e: e.matmul(pOW[0:65, :], lhsT=M["Vsel"][:, kc, g, :], rhs=pt_[:], start=(kc == 0), stop=(kc == T)),
                             r=[ptk, "Vsel%d" % kc, "Vsel1"], w=[pOWk])
                        if kc == T:
                            R.op("act", lambda e: e.activation(out=oTs[g][0][:], in_=pOW[0:65, :], func=AF.Copy), r=[pOWk], w=["oTs%d0" % g])
                    else:
                        R.op("pe", lambda e: e.matmul(pOW[0:65, :], lhsT=M["Vwin"][:, kc - 44, g, :], rhs=pt_[:], start=(a_ == 0), stop=(a_ == 4)),
                             r=[ptk, "Vwin%d" % (kc - 44), "Vwin1"], w=[pOWk])
                        if a_ == 4:
                            R.op("act", lambda e: e.activation(out=oTs[g][1][:], in_=pOW[0:65, :], func=AF.Copy), r=[pOWk], w=["oTs%d1" % g])

                emit_S(0)
                for ci in range(nck):
                    if ci + 1 < nck:
                        emit_S(ci + 1)
                    emit_PV(ci)

            r0, r1 = Rec(), Rec()
            stream(0, r0)
            stream(1, r1)
            interleave(S, r0, r1)
            S_main = S
            S = Rec()
            for g in range(2):
                finish_tail(g, 1, oTs[g][0])
                finish_tail(g, 2, oTs[g][1])
            if dbg is not None and "o_b" in dbg:
                S.dma("sp", dbg["o_b"][qt * 128:(qt + 1) * 128, :], ob[:], r=[obk], w=["dbg_ob"])
            S.op("act", lambda e: e.activation(out=obb[:], in_=ob[:], func=AF.Copy), r=[obk], w=["obb"])
            pmx = bks[7][:, 0:256].bitcast(BF16).rearrange("p (a b) -> p a b", a=4)
            for hh in range(4):
                S.op("pe", lambda e, hh=hh: e.transpose(out=pmx[:, hh, :], in_=obb[:, hh * 128:(hh + 1) * 128], identity=identb[:]),
                     r=["obb", "identb"], w=["nb7"])
            S.op("act", lambda e, qt=qt: e.activation(out=M["mixT"][:, 4:8, qt * 128:(qt + 1) * 128], in_=pmx, func=AF.Copy),
                 r=["nb7"], w=["mixTb%d" % qt])
            tail = S
            S = S_main
            nxt = Rec()
            if qi + 1 < len(qlist):
                pro(qlist[qi + 1], nxt)
            interleave(S, tail, nxt)
        if dbg is not None:
            S.wait_all("sp", ["dbg_ob", "dbg_selm"])
        S.flush()


def phase_outproj(C, P, M, dr, x_out):
    nc, S = C.nc, C.S
    L = 0
    with contextlib.ExitStack() as st:
        wo = C.sb(st, "wo", [128, 8, D], BF16)
        xt = [C.sb(st, "xt%d" % i, [128, D], F32) for i in range(2)]
        xo = [C.sb(st, "xo%d" % i, [128, D], F32) for i in range(2)]
        ev = C.sb(st, "ev", [128, 512], F32)
        py = [bank(C, st, "opy%d" % i) for i in range(2)]
        S.dma("pool", wo[:], dr["w_out_ab"].rearrange("(k p) n -> p k n", p=128), w=["wo"])
        gate = P["gates"]
        for t in range(NT):
            i = t % 2
            S.dma("sp", xt[i][:], dr["xw"][(OWN0 + t) * 128:(OWN0 + t + 1) * 128, :], w=["xt%d" % i])
            for half in range(2):
                for k in range(8):
                    S.op("pe", lambda e, k=k, half=half, t=t: e.matmul(py[half][:], lhsT=M["mixT"][:, k, t * 128:(t + 1) * 128],
                                                                       rhs=wo[:, k, half * 512:(half + 1) * 512], start=(k == 0), stop=(k == 7)),
                         r=["wo", "mixTa%d" % t, "mixTb%d" % t], w=["opy%d" % half])
                S.op("dve", lambda e, half=half: e.tensor_tensor(out=ev[:], in0=py[half][:], in1=gate[:, L, 0, half * 512:(half + 1) * 512], op=ALU.mult),
                     r=["opy%d" % half, "gates"], w=["ev"])
                S.op("dve", lambda e, half=half, i=i: e.tensor_tensor(out=xo[i][:, half * 512:(half + 1) * 512], in0=ev[:],
                                                                      in1=xt[i][:, half * 512:(half + 1) * 512], op=ALU.add),
                     r=["ev", "xt%d" % i], w=["xo%d_%d" % (i, half)])
            S.dma("sp", x_out[t * 128:(t + 1) * 128, :], xo[i][:], r=["xo%d_0" % i, "xo%d_1" % i], w=["xo_%s_%d" % (x_out.name, i)])
        S.wait_all("sp", ["xo_%s_%d" % (x_out.name, j) for j in range(2)])
        S.flush()


IN_SPECS = [("ident", [128, 128], F32), ("c_col", [128, 8], F32), ("b_ada_col", [128, 2, 48], F32), ("norm_g_col", [128, 2, 2, 8], F32),
            ("w_ada", [2, 1024, 6144], F32), ("b_ada", [2, 6144], F32),
            ("xw", [8192, 1024], F32), ("w_in_ab", [1024, 2856], F32), ("gla_w_gate2", [16, 256], F32), ("gla_b_gate_col", [128, 2], F32),
            ("gla_norm_g", [128], F32), ("gla_maskT", [128, 4, 128], F32), ("valid_col", [128, 64], F32), ("invf", [8], F32),
            ("nsa_k_gain", [3, 64], F32), ("nsa_q_gain", [64], F32), ("pos_col", [128, 64], I32),
            ("nsa_cmp_w1", [2, 2048, 256], F32), ("nsa_cmp_w2", [2, 256, 64], F32), ("cmp_pe_col", [128, 2, 16], F32),
            ("posb_col", [128, 4], I32), ("cvalid_col", [128, 4], F32), ("w_nq_perm", [1024, 512], F32), ("Esel", [128, 64, 128], BF16),
            ("cover", [128, 4, 129], F32), ("corebias", [128, 128], F32), ("causb", [128, 4, 128], BF16), ("winb", [128, 4, 128], BF16),
            ("cmaskT", [16, 128, 4, 128], F32), ("selbias", [16, 128, 128], F32), ("w_out_ab", [1024, 1024], F32),
            ("w_in_c", [1024, 4096], F32), ("w_out_c", [2048, 1024], F32), ("sgu_w_s", [8, 128, 128], F32), ("sgu_b_s", [8, 128], F32),
            ("sgu_norm_g", [2048], F32), ("tril", [128, 128], F32),
            ("w_router", [1024, 16], F32), ("router_bias", [16], F32),
            ("w_exp", [2, 16, 128, 12288], F32)]


def build_full(upto=4):
    C = Ctx()
    dr = {n: C.dram_in(n, s, dt) for n, s, dt in IN_SPECS}
    dr["cmp_scr"] = C.dram_tmp("cmp_scr", [2, 128, 8192], BF16)
    xs = [C.dram_tmp("xs%d" % i, [TOK, D], F32) for i in range(3)]
    y = C.dram_out("y", [TOK, D])
    dst = lambda i: y if upto == i + 1 else xs[i]
    P = alloc_persistent(C)
    phase_init(C, P, dr)
    phase_ada(C, P, dr)
    mst = contextlib.ExitStack()
    M = alloc_mixer(C, mst)
    phase_mix_a(C, P, M, dr)
    M2 = alloc_cmp(C, mst)
    phase_cmp(C, P, M, M2, dr)
    phase_nsa(C, P, M, M2, dr)
    phase_outproj(C, P, M, dr, dst(0))
    mst.close()
    if upto >= 2:
        phase_moe(C, P, dr, 0, xs[0], dst(1))
    if upto >= 3:
        phase_gmlp(C, P, dr, xs[1], dst(2))
    if upto >= 4:
        phase_moe(C, P, dr, 1, xs[2], y)
    C.outer.close()
    return C.nc


def _col(v):
    return np.ascontiguousarray(np.asarray(v).reshape(-1, 128).T)


def _shared_inputs(d):
    f32 = np.float32
    sh = {}
    sh["ident"] = np.eye(128, dtype=f32)
    sh["b_ada_col"] = np.ascontiguousarray(d["b_ada"].reshape(2, 48, 128).transpose(2, 0, 1))
    sh["norm_g_col"] = np.ascontiguousarray(d["norm_g"].reshape(2, 2, 8, 128).transpose(3, 0, 1, 2))
    sh["w_ada"] = d["w_ada"]
    sh["b_ada"] = d["b_ada"]
    w_in = d["w_in_ab"][0]
    sh["w_in_ab"] = w_in
    sh["gla_w_gate2"] = d["gla_w_gate2"][0]
    sh["gla_b_gate_col"] = _col(d["gla_b_gate"][0])
    sh["gla_norm_g"] = d["gla_norm_g"][0]
    j = np.arange(128)[:, None]
    i = np.arange(128)[None, :]
    m = ((j // 64 == i // 64) & (j <= i)).astype(f32)
    sh["gla_maskT"] = np.ascontiguousarray(np.repeat(m[:, None, :], 4, axis=1))
    sh["invf"] = (f32(500000.0) ** (-np.arange(8, dtype=f32) / f32(8))).astype(f32)
    sh["nsa_k_gain"] = d["nsa_k_gain"][0]
    sh["nsa_q_gain"] = d["nsa_q_gain"][0]
    sh["nsa_cmp_w1"] = d["nsa_cmp_w1"][0]
    sh["nsa_cmp_w2"] = d["nsa_cmp_w2"][0]
    sh["cmp_pe_col"] = np.ascontiguousarray(d["nsa_cmp_pe"][0].reshape(2, 16, 128).transpose(2, 0, 1))
    nq = w_in[:, 1552:2064].reshape(1024, 2, 4, 64)
    sh["w_nq_perm"] = np.ascontiguousarray(nq.transpose(0, 2, 1, 3).reshape(1024, 512))
    E = np.zeros((128, 64, 128), f32)
    for c in range(64):
        E[2 * c, c, 0:64] = 1.0
        E[2 * c + 1, c, 64:128] = 1.0
    sh["Esel"] = E.astype(ml_dtypes.bfloat16)
    blk = np.arange(512)
    jj = np.arange(128)
    cov = np.zeros((512, 129), f32)
    cov[:, :128] = ((16 * blk[:, None] < 64 * jj[None, :] + 64) & (16 * blk[:, None] + 32 > 64 * jj[None, :])).astype(f32)
    cov[:, 128] = 1.0
    cov[511] = 0.0
    sh["cover"] = np.ascontiguousarray(cov.reshape(4, 128, 129).transpose(1, 0, 2))
    cm = np.zeros((16, 128, 4, 128), f32)
    sb = np.zeros((16, 128, 128), f32)
    ii = np.arange(128)
    for qt in range(16):
        wq = (OWN0 + qt) * 128 + ii
        for bc in range(4):
            b_ = bc * 128 + np.arange(128)
            cm[qt, :, bc, :] = ((16 * b_ + 31)[:, None] <= wq[None, :]).astype(f32)
        cur = wq // 64
        forced = (jj[None, :] == cur[:, None]) | (jj[None, :] == cur[:, None] - 1)
        inval = jj[None, :] > cur[:, None]
        sb[qt] = np.where(inval, -1e30, np.where(forced, 1e4, 0.0))
    sh["cmaskT"] = cm
    sh["selbias"] = sb
    caus = np.where(j <= i, 0.0, NEGB).astype(f32)
    win = np.where(j > i, 0.0, NEGB).astype(f32)
    rep4 = lambda a: np.ascontiguousarray(np.repeat(a[:, None, :], 4, axis=1))
    sh["causb"] = rep4(caus).astype(ml_dtypes.bfloat16)
    sh["winb"] = rep4(win).astype(ml_dtypes.bfloat16)
    sh["w_out_ab"] = d["w_out_ab"][0]
    sh["w_in_c"] = d["w_in_c"][0]
    sh["w_out_c"] = d["w_out_c"][0]
    sh["sgu_w_s"] = d["sgu_w_s"][0]
    sh["sgu_b_s"] = d["sgu_b_s"][0]
    sh["sgu_norm_g"] = d["sgu_norm_g"][0]
    sh["tril"] = np.tril(np.ones((128, 128), f32))
    for k in ("w_router", "router_bias"):
        sh[k] = d[k]
    wg_ = d["w_gate"].reshape(2, 16, 8, 128, 512).transpose(0, 1, 3, 2, 4).reshape(2, 16, 128, 4096)
    wu_ = d["w_up"].reshape(2, 16, 8, 128, 512).transpose(0, 1, 3, 2, 4).reshape(2, 16, 128, 4096)
    wd_ = d["w_down"].reshape(2, 16, 4, 128, 1024).transpose(0, 1, 3, 2, 4).reshape(2, 16, 128, 4096)
    sh["w_exp"] = np.ascontiguousarray(np.concatenate([wg_, wu_, wd_], axis=-1))
    return sh


def _core_inputs(d, core):
    f32 = np.float32
    b, qtr = core // 4, core % 4
    own = qtr * TOK
    a = np.arange(8192) - 6144 + own
    ok = a >= 0
    xw = np.zeros((8192, D), f32)
    xw[ok] = d["x"][b, a[ok]]
    pos = np.zeros(8192, np.int32)
    pos[ok] = d["positions"][b, a[ok]]
    co = {"xw": xw, "pos_col": _col(pos), "valid_col": _col(ok.astype(f32)), "c_col": _col(d["c"][b])}
    first_blk = 96 - 32 * qtr
    jj = np.arange(128)
    row = np.where(jj < first_blk, -1e30, np.where(jj == first_blk, 1e4, 0.0)).astype(f32)
    co["corebias"] = np.ascontiguousarray(np.broadcast_to(row, (128, 128)))
    blk = np.arange(512)
    a_start = 16 * blk - 6144 + own
    cvalid = ((a_start >= 0) & (blk <= 510))
    a_end = np.clip(16 * blk + 31 - 6144 + own, 0, 8191)
    posb = np.where(cvalid, d["positions"][b, a_end], 0).astype(np.int32)
    co["cvalid_col"] = _col(cvalid.astype(f32))
    co["posb_col"] = _col(posb)
    return co


_NC_CACHE = {}


def kernel(**inputs):
    d = {k: np.asarray(v) for k, v in inputs.items()}
    if "nc" not in _NC_CACHE:
        _NC_CACHE["nc"] = build_full()
    nc = _NC_CACHE["nc"]
    sh = _shared_inputs(d)
    in_maps = []
    for core in range(NCORES):
        m = dict(sh)
        m.update(_core_inputs(d, core))
        in_maps.append(m)
    res = run_bass_kernel_spmd(nc, in_maps, core_ids=list(range(NCORES)))
    out = np.zeros((2, 8192, D), np.float32)
    for core in range(NCORES):
        b, qtr = core // 4, core % 4
        out[b, qtr * TOK:(qtr + 1) * TOK] = res.results[core]["y"]
    return out
```

```python
import contextlib
import types
import numpy as np
import ml_dtypes
import concourse.bass as bass
import concourse.mybir as mybir
from concourse.bass_utils import run_bass_kernel_spmd

F32 = mybir.dt.float32
BF16 = mybir.dt.bfloat16
I32 = mybir.dt.int32
AF = mybir.ActivationFunctionType
ALU = mybir.AluOpType
AX = mybir.AxisListType

ENGS = ("pe", "dve", "act", "pool", "sp")
NCORES = 8
D = 1024
NT = 16
TOK = 2048
EPS = 1e-6


PSUM_KEYS = set(["b%d" % i for i in range(8)] + ["nb%d" % i for i in range(8)] +
                ["cph0", "cph1", "cpb", "cpo0", "cpo1", "cpt", "pT0", "pT1", "pg0", "pg1", "pu0", "pu1", "py0", "py1",
                 "pv0", "pv1", "pm0", "pm1", "pcol", "prow0", "prow1", "opy0", "opy1"] + ["gb%d" % i for i in range(8)])


class Sched:
    def __init__(self, nc, stack):
        self.nc = nc
        self.stack = stack
        self.q = {e: [] for e in ENGS}
        self.last_w = {}
        self.readers = {}
        self.dma_sems = {}
        self.esem = {e: stack.enter_context(nc.semaphore("s_" + e)) for e in ENGS}
        self.ecount = {e: 0 for e in ENGS}
        self.seen = {e: {} for e in ENGS}
        self.phase_end = {}
        self.phase = 0
        self.alias = {}
        self.slot_map = {}
        self.sem_pool = []

    def _deps(self, eng, r, w):
        deps = []
        r = [self.alias.get(k, k) for k in r]
        w = [self.alias.get(k, k) for k in w]
        for k in r:
            t = self.last_w.get(k)
            if t is not None:
                deps.append(t)
            if k in PSUM_KEYS:
                deps.extend(x for x in self.readers.get(k, ()) if not (x[0] == "eng" and x[1] == eng))
        for k in w:
            t = self.last_w.get(k)
            if t is not None:
                deps.append(t)
            deps.extend(self.readers.get(k, ()))
        best = {}
        for t in deps:
            key = (t[0], t[1], t[3] if t[0] == "eng" else 0)
            if key not in best or best[key] < t[2]:
                best[key] = t[2]
        out = []
        for (kind, name, ph), v in best.items():
            if kind == "eng" and name == eng and (eng == "pe" or not Sched.same_engine_sync):
                continue
            out.append((kind, name, v, ph))
        return out

    def _commit(self, tok, r, w):
        r = [self.alias.get(k, k) for k in r]
        w = [self.alias.get(k, k) for k in w]
        for k in w:
            self.last_w[k] = tok
            self.readers[k] = []
        for k in r:
            self.readers.setdefault(k, []).append(tok)

    limit = None
    count = 0
    recycle = True
    sim_mode = False
    uniq = 0
    same_engine_sync = True

    @staticmethod
    def _freeze(fn):
        if fn.__closure__ is None:
            return fn
        cells = tuple(types.CellType(c.cell_contents) for c in fn.__closure__)
        g = types.FunctionType(fn.__code__, fn.__globals__, fn.__name__, fn.__defaults__, cells)
        g.__kwdefaults__ = fn.__kwdefaults__
        return g

    def op(self, eng, fn, r=(), w=()):
        fn = self._freeze(fn)
        Sched.count += 1
        if Sched.limit is not None and Sched.count > Sched.limit:
            return
        deps = self._deps(eng, r, w)
        idx = len(self.q[eng])
        self.q[eng].append(dict(kind="op", fn=fn, deps=deps))
        self._commit(("eng", eng, idx, self.phase), r, w)

    def dma(self, eng, out, in_, r=(), w=(), slot=None, **kw):
        Sched.count += 1
        if Sched.limit is not None and Sched.count > Sched.limit:
            return
        deps = self._deps(eng, r, w)
        if slot is None:
            slot = w[0]
        if eng == "pool":
            Sched.uniq += 1
            sid = "sw%d" % Sched.uniq
            self.dma_sems[sid] = [self.stack.enter_context(self.nc.semaphore(sid)), 0]
            slot = sid
        else:
            if slot not in self.slot_map:
                n = len(self.slot_map)
                if n >= len(self.sem_pool):
                    self.sem_pool.append(n)
                    self.dma_sems[n] = [self.stack.enter_context(self.nc.semaphore("d%d" % n)), 0]
                self.slot_map[slot] = n
            slot = self.slot_map[slot]
        self.dma_sems[slot][1] += 16
        val = self.dma_sems[slot][1]
        self.q[eng].append(dict(kind="dma", out=out, in_=in_, deps=deps, slot=slot, kw=kw))
        self._commit(("dma", slot, val, 0), r, w)

    def wait_all(self, eng, keys):
        deps = self._deps(eng, keys, ())
        self.q[eng].append(dict(kind="wait", deps=deps))

    def flush(self, barrier=True):
        nc = self.nc
        ph = self.phase
        miles = {e: set() for e in ENGS}
        for e in ENGS:
            for o in self.q[e]:
                for (kind, name, v, p) in o["deps"]:
                    if kind == "eng" and p == ph:
                        miles[name].add(v)
        for e in ENGS:
            n = len(self.q[e])
            if n:
                last = max(i for i, o in enumerate(self.q[e]) if o["kind"] != "wait") if any(
                    o["kind"] != "wait" for o in self.q[e]) else None
                if last is not None and self.q[e][last]["kind"] == "op":
                    miles[e].add(last)
        rank = {}
        for e in ENGS:
            for i, v in enumerate(sorted(miles[e])):
                rank[(e, v)] = self.ecount[e] + i + 1
        prev_end = dict(self.phase_end)
        prev_dma = dict(getattr(self, 'dma_totals', {}))
        with nc.Block() as block:
            engobj = {"pe": block.tensor, "dve": block.vector, "act": block.scalar,
                      "pool": block.gpsimd, "sp": block.sync}

            def make(ename):
                def body(eng):
                    seen = self.seen[ename]

                    def wait(sem, key, v):
                        if seen.get(key, 0) >= v:
                            return
                        eng.wait_ge(sem, v)
                        seen[key] = v
                    if barrier:
                        for oe, v in prev_end.items():
                            if oe != ename and v > 0:
                                wait(self.esem[oe], oe, v)
                        for slot, tot in prev_dma.items():
                            wait(self.dma_sems[slot][0], "d:%s" % (slot,), tot)
                    for i, o in enumerate(self.q[ename]):
                        for (kind, name, v, p) in o["deps"]:
                            if kind == "eng":
                                if p == ph:
                                    if name == ename:
                                        wait(self.esem[name], name, rank[(name, v)])
                                    else:
                                        wait(self.esem[name], name, rank[(name, v)])
                                else:
                                    if name != ename and prev_end.get(name, 0) > 0:
                                        wait(self.esem[name], name, prev_end[name])
                            else:
                                wait(self.dma_sems[name][0], "d:%s" % (name,), v)
                        if o["kind"] == "op":
                            ins = o["fn"](eng)
                            if i in miles[ename]:
                                ins.then_inc(self.esem[ename], 1)
                        elif o["kind"] == "dma":
                            ins = eng.dma_start(out=o["out"], in_=o["in_"], **o["kw"])
                            ins.then_inc(self.dma_sems[o["slot"]][0], 16)
                return body
            for e in ENGS:
                if self.q[e] or barrier:
                    engobj[e](make(e))
        for e in ENGS:
            self.ecount[e] += len(miles[e])
            self.phase_end[e] = self.ecount[e]
            self.q[e] = []
        self.phase += 1
        self.dma_totals = {slot: v[1] for slot, v in self.dma_sems.items()}
        if Sched.recycle:
            self.slot_map = {}


class Rec:
    def __init__(self):
        self.items = []

    def op(self, eng, fn, r=(), w=()):
        self.items.append(("op", eng, Sched._freeze(fn), tuple(r), tuple(w), None))

    def dma(self, eng, out, in_, r=(), w=(), slot=None, **kw):
        self.items.append(("dma", eng, (out, in_, slot, kw), tuple(r), tuple(w), None))


def interleave(S, a, b):
    ia = ib = 0
    na, nb = len(a.items), len(b.items)
    while ia < na or ib < nb:
        take_a = ib >= nb or (ia < na and ia * nb <= ib * na)
        it = a.items[ia] if take_a else b.items[ib]
        if take_a:
            ia += 1
        else:
            ib += 1
        if it[0] == "op":
            S.op(it[1], it[2], r=it[3], w=it[4])
        else:
            out, in_, slot, kw = it[2]
            S.dma(it[1], out, in_, r=it[3], w=it[4], slot=slot, **kw)


class Ctx:
    def __init__(self):
        self.nc = bass.Bass("TRN2", target_bir_lowering=False)
        self.outer = contextlib.ExitStack()
        self.S = Sched(self.nc, self.outer)
        self.uid = 0

    def dram_in(self, name, shape, dt=F32):
        return self.nc.dram_tensor(name, list(shape), dt, kind="ExternalInput").ap()

    def dram_out(self, name, shape, dt=F32):
        return self.nc.dram_tensor(name, list(shape), dt, kind="ExternalOutput").ap()

    def dram_tmp(self, name, shape, dt=F32):
        return self.nc.dram_tensor(name, list(shape), dt, kind="Internal").ap()

    def sb(self, stack, name, shape, dt):
        self.uid += 1
        return stack.enter_context(self.nc.sbuf_tensor("%s_%d" % (name, self.uid), list(shape), dt))

    def ps(self, stack, name, shape, dt=F32):
        self.uid += 1
        return stack.enter_context(self.nc.psum_tensor("%s_%d" % (name, self.uid), list(shape), dt))


def norm_transpose(C, S, xt, xkey, tmp, ident, pTs, tag):
    junk, ss, rs, xn = tmp["junk"], tmp["ss"], tmp["rs"], tmp["xn"]
    kj, kss, krs, kxn = [tag + s for s in ("junk", "ss", "rs", "xn")]
    S.op("act", lambda e: e.activation(out=junk[:], in_=xt, func=AF.Square, accum_out=ss[:]),
         r=[xkey], w=[kj, kss])
    if tmp.get("lnexp"):
        S.op("act", lambda e: e.activation(out=rs[:], in_=ss[:], func=AF.Ln, scale=1.0 / D, bias=tmp["eps"][:]),
             r=[kss], w=[krs])
        S.op("act", lambda e: e.activation(out=rs[:], in_=rs[:], func=AF.Exp, scale=-0.5), r=[krs], w=[krs])
    else:
        S.op("act", lambda e: e.activation(out=rs[:], in_=ss[:], func=AF.Sqrt, scale=1.0 / D, bias=tmp["eps"][:]),
             r=[kss], w=[krs])
        S.op("dve", lambda e: e.reciprocal(out=rs[:], in_=rs[:]), r=[krs], w=[krs])
    S.op("dve", lambda e: e.tensor_scalar(out=xn[:], in0=xt, scalar1=rs[:, 0:1], scalar2=None, op0=ALU.mult),
         r=[xkey, krs], w=[kxn])
    for k in range(8):
        p, pk = pTs[k // 4]
        S.op("pe", lambda e, k=k, p=p: e.transpose(out=p[:, k % 4, :], in_=xn[:, k * 128:(k + 1) * 128],
                                                   identity=ident[:]),
             r=[kxn, "ident"], w=[pk])


def phase_ada(C, P, dr):
    nc, S = C.nc, C.S
    with contextlib.ExitStack() as st:
        ccol = C.sb(st, "ccol", [128, 8], F32)
        cond = C.sb(st, "cond", [128, 8], F32)
        condbc = C.sb(st, "condbc", [128, 8, 128], F32)
        ones = C.sb(st, "ones", [128, 128], F32)
        bcol = C.sb(st, "bcol", [128, 2, 48], F32)
        gcol = C.sb(st, "gcol", [128, 2, 2, 8], F32)
        mcol = C.sb(st, "mcol", [128, 2, 48], F32)
        wblk = [C.sb(st, "wblk%d" % i, [128, 8, 512], F32) for i in range(2)]
        brow = [C.sb(st, "brow%d" % i, [128, 512], F32) for i in range(2)]
        pcol = C.ps(st, "pcol", [128, 512], F32)
        prow = [C.ps(st, "prow%d" % i, [128, 512], F32) for i in range(2)]
        S.dma("sp", ccol[:], dr["c_col"], w=["ccol"])
        S.dma("sp", bcol[:], dr["b_ada_col"], w=["bcol"])
        S.dma("sp", gcol[:], dr["norm_g_col"], w=["gcol"])
        S.op("act", lambda e: e.activation(out=cond[:], in_=ccol[:], func=AF.Silu), r=["ccol"], w=["cond"])
        S.op("dve", lambda e: e.memset(ones[:], 1.0), w=["ones"])
        for k in range(8):
            S.op("dve", lambda e, k=k: e.tensor_scalar(out=condbc[:, k, :], in0=ones[:], scalar1=cond[:, k:k + 1],
                                                       scalar2=None, op0=ALU.mult),
                 r=["ones", "cond"], w=["condbc"])
        nrow = 0
        modrow = [C.sb(st, "modrow%d" % i, [128, 512], F32) for i in range(2)]
        pcT = C.ps(st, "pcT", [128, 4, 128], F32)
        for l in range(2):
            for nb in range(12):
                i = (l * 12 + nb) % 2
                for hk_ in range(2):
                    S.dma("sp" if hk_ == 0 else "act", wblk[i][:, hk_ * 4:(hk_ + 1) * 4, :],
                          dr["w_ada"][l, hk_ * 512:(hk_ + 1) * 512, nb * 512:(nb + 1) * 512].rearrange("(k p) n -> p k n", p=128),
                          w=["wblk%d_%d" % (i, hk_)])
                sub6 = nb // 2
                j = nrow % 2
                nrow += 1
                S.dma("sp", brow[j][:], dr["b_ada"][l, nb * 512:(nb + 1) * 512].partition_broadcast(128), w=["brow%d" % j])
                for k in range(8):
                    S.op("pe", lambda e, k=k, i=i, j=j: e.matmul(prow[j][:], lhsT=condbc[:, k, :], rhs=wblk[i][:, k, :],
                                                                 start=(k == 0), stop=(k == 7)),
                         r=["condbc", "wblk%d_0" % i, "wblk%d_1" % i], w=["prow%d" % j])
                if sub6 in (2, 5):
                    gs = 0 if sub6 == 2 else 1
                    half = nb % 2
                    S.op("dve", lambda e, j=j, l=l, gs=gs, half=half: e.tensor_tensor(
                        out=P["gates"][:, l, gs, half * 512:(half + 1) * 512], in0=prow[j][:], in1=brow[j][:], op=ALU.add),
                        r=["prow%d" % j, "brow%d" % j], w=["gates"])
                else:
                    S.op("dve", lambda e, j=j: e.tensor_tensor(out=modrow[j][:], in0=prow[j][:], in1=brow[j][:], op=ALU.add),
                         r=["prow%d" % j, "brow%d" % j], w=["modrow%d" % j])
                    for m in range(4):
                        S.op("pe", lambda e, j=j, m=m: e.transpose(out=pcT[:, m, :], in_=modrow[j][:, m * 128:(m + 1) * 128], identity=P["ident"][:]),
                             r=["modrow%d" % j, "ident"], w=["pcol"])
                    S.op("dve", lambda e, l=l, nb=nb: e.tensor_copy(out=mcol[:, l, nb * 4:nb * 4 + 4], in_=pcT[:, :, 0]),
                         r=["pcol"], w=["mcol"])
            for s in range(2):
                S.op("dve", lambda e, l=l, s=s: e.scalar_tensor_tensor(
                    out=P["modA"][:, l, s, :], in0=mcol[:, l, s * 24 + 8:s * 24 + 16], scalar=1.0, in1=gcol[:, l, s, :],
                    op0=ALU.add, op1=ALU.mult), r=["mcol", "gcol"], w=["modA"])
                S.op("dve", lambda e, l=l, s=s: e.tensor_copy(out=P["modB"][:, l, s, :], in_=mcol[:, l, s * 24:s * 24 + 8]),
                     r=["mcol"], w=["modB"])
        S.flush()


MOE_INTERLEAVE = True
MOE_LNEXP = False


def phase_moe(C, P, dr, layer, x_in, x_out):
    nc, S = C.nc, C.S
    L = layer
    with contextlib.ExitStack() as st:
        X = C.sb(st, "X", [128, NT, D], F32)
        hT = C.sb(st, "hT", [128, 8, TOK], BF16)
        comb = C.sb(st, "comb", [128, NT, 16], F32)
        tmpn2 = [dict(junk=C.sb(st, "junk%d" % i, [128, D], BF16), ss=C.sb(st, "ss%d" % i, [128, 1], F32),
                      rs=C.sb(st, "rs%d" % i, [128, 1], F32), xn=C.sb(st, "xn%d" % i, [128, D], F32), eps=P["eps"], lnexp=MOE_LNEXP) for i in range(2)]
        hTf = [C.sb(st, "hTf%d" % i, [128, 8, 128], F32) for i in range(2)]
        wr = C.sb(st, "wr", [128, 8, 16], F32)
        rb = C.sb(st, "rb", [128, 16], F32)
        rt2 = [{n: C.sb(st, "rt%d_" % i + n, [128, 16], F32) for n in
                ("sc", "sel", "eq1", "sel2", "eq2", "selm", "wts")} for i in range(2)]
        r42 = [{n: C.sb(st, "r4%d_" % i + n, [128, 4], F32) for n in ("m1", "m2", "gs", "ing")} for i in range(2)]
        r12 = [{n: C.sb(st, "r1%d_" % i + n, [128, 1], F32) for n in ("gmax", "den")} for i in range(2)]
        wpk = [C.sb(st, "wpk%d" % i, [128, 12288], BF16) for i in range(2)]
        wg = [wpk[i][:, 0:4096].rearrange("p (k n) -> p k n", k=8) for i in range(2)]
        wu = [wpk[i][:, 4096:8192].rearrange("p (k n) -> p k n", k=8) for i in range(2)]
        wd = [wpk[i][:, 8192:12288].rearrange("p (k n) -> p k n", k=4) for i in range(2)]
        sg = [C.sb(st, "sg%d" % i, [128, 512], F32) for i in range(2)]
        hid = [C.sb(st, "hid%d" % i, [128, 4, 512], BF16) for i in range(2)]
        ev = [C.sb(st, "ev%d" % i, [128, 512], F32) for i in range(2)]
        pT = [C.ps(st, "pT%d" % i, [128, 4, 128], F32) for i in range(2)]
        pg = [C.ps(st, "pg%d" % i, [128, 512], F32) for i in range(2)]
        pu = [C.ps(st, "pu%d" % i, [128, 512], F32) for i in range(2)]
        py = [C.ps(st, "py%d" % i, [128, 512], F32) for i in range(2)]
        plog2 = [pg[0], pg[1]]

        S.dma("sp", wr[:], dr["w_router"].rearrange("(k p) n -> p k n", p=128), w=["wr"])
        S.dma("sp", rb[:], dr["router_bias"].partition_broadcast(128), w=["rb"])

        def load_w(e):
            i = e % 2
            S.dma("pool", wpk[i][:], dr["w_exp"][L, e], w=["wg%d" % i, "wu%d" % i, "wd%d" % i], slot="wpk%d" % i, max_dma_last_dim=8192)

        for t in range(NT):
            S.dma("sp", X[:, t, :], x_in[t * 128:(t + 1) * 128, :], w=["X%d" % t])
        load_w(0)
        load_w(1)
        A = P["modA"]
        B = P["modB"]
        def moe_pro(t, S):
            q_ = t % 2
            tmpn = tmpn2[q_]
            rt, r4, r1 = rt2[q_], r42[q_], r12[q_]
            plog = plog2[q_]
            T_ = "m%d" % q_
            pTq = [(pT[0], "pT0"), (pT[1], "pT1")] if q_ == 0 else [(pu[0][:].rearrange("p (a b) -> p a b", a=4), "pu0"),
                                                                      (pu[1][:].rearrange("p (a b) -> p a b", a=4), "pu1")]
            norm_transpose(C, S, X[:, t, :], "X%d" % t, tmpn, P["ident"], pTq, T_)
            hf = hTf[t % 2]
            hk = "hTf%d" % (t % 2)
            for k in range(8):
                p, pk = pTq[k // 4]
                if k % 2 == 0:
                    S.op("dve", lambda e, k=k, p=p, hf=hf: e.tensor_scalar(
                        out=hf[:, k, :], in0=p[:, k % 4, :], scalar1=A[:, L, 1, k:k + 1], scalar2=B[:, L, 1, k:k + 1],
                        op0=ALU.mult, op1=ALU.add), r=[pk, "modA", "modB"], w=[hk])
                else:
                    S.op("act", lambda e, k=k, p=p, hf=hf: e.activation(
                        out=hf[:, k, :], in_=p[:, k % 4, :], func=AF.Identity, scale=A[:, L, 1, k:k + 1],
                        bias=B[:, L, 1, k:k + 1]), r=[pk, "modA", "modB"], w=[hk])
            S.op("pool", lambda e, t=t, hf=hf: e.tensor_copy(out=hT[:, :, t * 128:(t + 1) * 128], in_=hf[:]),
                 r=[hk], w=["hT%d" % t])
            for k in range(8):
                S.op("pe", lambda e, k=k, hf=hf: e.matmul(plog[:, 0:16], lhsT=hf[:, k, :], rhs=wr[:, k, :],
                                                          start=(k == 0), stop=(k == 7)),
                     r=[hk, "wr"], w=["pg%d" % q_])
            sc, sel, eq1, sel2, eq2, selm, wts = [rt[n] for n in ("sc", "sel", "eq1", "sel2", "eq2", "selm", "wts")]
            m1, m2, gs, ing = [r4[n] for n in ("m1", "m2", "gs", "ing")]
            gmax, den = r1["gmax"], r1["den"]
            v4 = lambda a: a[:].rearrange("p (g e) -> p g e", e=4)
            b4 = lambda a: a[:].unsqueeze(2).to_broadcast([128, 4, 4])
            S.op("act", lambda e: e.activation(out=sc[:], in_=plog[:, 0:16], func=AF.Sigmoid), r=["pg%d" % q_], w=[T_ + "r_sc"])
            S.op("dve", lambda e: e.tensor_tensor(out=sel[:], in0=sc[:], in1=rb[:], op=ALU.add), r=[T_ + "r_sc", "rb"], w=[T_ + "r_sel"])
            S.op("dve", lambda e: e.tensor_reduce(out=m1[:], in_=v4(sel), axis=AX.X, op=ALU.max), r=[T_ + "r_sel"], w=[T_ + "r_m1"])
            S.op("dve", lambda e: e.tensor_tensor(out=v4(eq1), in0=v4(sel), in1=b4(m1), op=ALU.is_equal),
                 r=[T_ + "r_sel", T_ + "r_m1"], w=[T_ + "r_eq1"])
            S.op("dve", lambda e: e.scalar_tensor_tensor(out=sel2[:], in0=eq1[:], scalar=-1e9, in1=sel[:],
                                                         op0=ALU.mult, op1=ALU.add), r=[T_ + "r_eq1", T_ + "r_sel"], w=[T_ + "r_sel2"])
            S.op("dve", lambda e: e.tensor_reduce(out=m2[:], in_=v4(sel2), axis=AX.X, op=ALU.max), r=[T_ + "r_sel2"], w=[T_ + "r_m2"])
            S.op("dve", lambda e: e.tensor_tensor(out=gs[:], in0=m1[:], in1=m2[:], op=ALU.add), r=[T_ + "r_m1", T_ + "r_m2"], w=[T_ + "r_gs"])
            S.op("dve", lambda e: e.tensor_reduce(out=gmax[:], in_=gs[:], axis=AX.X, op=ALU.max), r=[T_ + "r_gs"], w=[T_ + "r_gmax"])
            S.op("dve", lambda e: e.tensor_scalar(out=ing[:], in0=gs[:], scalar1=gmax[:, 0:1], scalar2=None, op0=ALU.is_equal),
                 r=[T_ + "r_gs", T_ + "r_gmax"], w=[T_ + "r_ing"])
            S.op("dve", lambda e: e.tensor_tensor(out=v4(eq2), in0=v4(sel2), in1=b4(m2), op=ALU.is_equal),
                 r=[T_ + "r_sel2", T_ + "r_m2"], w=[T_ + "r_eq2"])
            S.op("dve", lambda e: e.tensor_tensor(out=selm[:], in0=eq1[:], in1=eq2[:], op=ALU.add), r=[T_ + "r_eq1", T_ + "r_eq2"], w=[T_ + "r_selm"])
            S.op("dve", lambda e: e.tensor_tensor(out=v4(selm), in0=v4(selm), in1=b4(ing), op=ALU.mult),
                 r=[T_ + "r_selm", T_ + "r_ing"], w=[T_ + "r_selm"])
            S.op("dve", lambda e: e.tensor_tensor(out=wts[:], in0=sc[:], in1=selm[:], op=ALU.mult), r=[T_ + "r_sc", T_ + "r_selm"], w=[T_ + "r_wts"])
            S.op("dve", lambda e: e.tensor_reduce(out=den[:], in_=wts[:], axis=AX.X, op=ALU.add), r=[T_ + "r_wts"], w=[T_ + "r_den"])
            S.op("dve", lambda e: e.reciprocal(out=den[:], in_=den[:]), r=[T_ + "r_den"], w=[T_ + "r_den"])
            S.op("dve", lambda e, t=t: e.tensor_scalar(out=comb[:, t, :], in0=wts[:], scalar1=den[:, 0:1], scalar2=None, op0=ALU.mult),
                 r=[T_ + "r_wts", T_ + "r_den"], w=["comb%d" % t])

        for t in range(0, NT, 2):
            if MOE_INTERLEAVE:
                ra, rb_ = Rec(), Rec()
                moe_pro(t, ra)
                moe_pro(t + 1, rb_)
                interleave(S, ra, rb_)
            else:
                moe_pro(t, S)
                moe_pro(t + 1, S)
        gate = P["gates"]
        n_g = 0
        n_y = 0
        for ex in range(16):
            i = ex % 2
            for tg in range(4):
                hb = hid[(ex * 4 + tg) % 2]
                hbk = "hid%d" % ((ex * 4 + tg) % 2)
                for hc in range(4):
                    j = n_g % 2
                    n_g += 1
                    hkeys = ["hT%d" % t for t in range(tg * 4, tg * 4 + 4)]
                    for k in range(8):
                        S.op("pe", lambda e, k=k, i=i, j=j, hc=hc, tg=tg: e.matmul(
                            pg[j][:], lhsT=wg[i][:, k, hc * 128:(hc + 1) * 128], rhs=hT[:, k, tg * 512:(tg + 1) * 512],
                            start=(k == 0), stop=(k == 7)), r=hkeys + ["wg%d" % i], w=["pg%d" % j])
                    for k in range(8):
                        S.op("pe", lambda e, k=k, i=i, j=j, hc=hc, tg=tg: e.matmul(
                            pu[j][:], lhsT=wu[i][:, k, hc * 128:(hc + 1) * 128], rhs=hT[:, k, tg * 512:(tg + 1) * 512],
                            start=(k == 0), stop=(k == 7)), r=hkeys + ["wu%d" % i], w=["pu%d" % j])
                    S.op("act", lambda e, j=j: e.activation(out=sg[j][:], in_=pg[j][:], func=AF.Silu),
                         r=["pg%d" % j], w=["sg%d" % j])
                    S.op("dve", lambda e, j=j, hb=hb, hc=hc: e.tensor_tensor(out=hb[:, hc, :], in0=sg[j][:], in1=pu[j][:], op=ALU.mult),
                         r=["sg%d" % j, "pu%d" % j], w=[hbk + "_%d" % hc])
                for tt in range(4):
                    t = tg * 4 + tt
                    for half in range(2):
                        j = n_y % 2
                        n_y += 1
                        for hc in range(4):
                            S.op("pe", lambda e, j=j, hb=hb, hc=hc, tt=tt, half=half, i=i: e.matmul(
                                py[j][:], lhsT=hb[:, hc, tt * 128:(tt + 1) * 128], rhs=wd[i][:, hc, half * 512:(half + 1) * 512],
                                start=(hc == 0), stop=(hc == 3)), r=[hbk + "_%d" % hc, "wd%d" % i], w=["py%d" % j])
                        S.op("dve", lambda e, j=j, t=t, ex=ex, half=half: e.scalar_tensor_tensor(
                            out=ev[j][:], in0=py[j][:], scalar=comb[:, t, ex:ex + 1], in1=gate[:, L, 1, half * 512:(half + 1) * 512],
                            op0=ALU.mult, op1=ALU.mult), r=["py%d" % j, "comb%d" % t, "gates"], w=["ev%d" % j])
                        S.op("pool", lambda e, j=j, t=t, half=half: e.tensor_tensor(
                            out=X[:, t, half * 512:(half + 1) * 512], in0=X[:, t, half * 512:(half + 1) * 512], in1=ev[j][:], op=ALU.add),
                            r=["ev%d" % j, "X%d" % t], w=["X%d" % t])
            if ex + 2 < 16:
                load_w(ex + 2)
        for t in range(NT):
            S.dma("sp", x_out[t * 128:(t + 1) * 128, :], X[:, t, :], r=["X%d" % t], w=["xo_%s_%d" % (x_out.name, t % 4)])
        S.wait_all("sp", ["xo_%s_%d" % (x_out.name, j) for j in range(4)])
        S.flush()


def alloc_persistent(C):
    st = C.outer
    P = {}
    P["ident"] = C.sb(st, "ident", [128, 128], F32)
    P["eps"] = C.sb(st, "eps", [128, 1], F32)
    P["modA"] = C.sb(st, "modA", [128, 2, 2, 8], F32)
    P["modB"] = C.sb(st, "modB", [128, 2, 2, 8], F32)
    P["gates"] = C.sb(st, "gates", [128, 2, 2, D], F32)
    return P


def phase_init(C, P, dr):
    S = C.S
    S.dma("sp", P["ident"][:], dr["ident"], w=["ident"])
    S.op("dve", lambda e: e.memset(P["eps"][:], EPS), w=["eps"])


GELU = AF.Gelu_apprx_tanh


def phase_gmlp(C, P, dr, x_in, x_out):
    nc, S0 = C.nc, C.S
    L = 1
    with contextlib.ExitStack() as st:
        win = C.sb(st, "win", [128, 8, 4096], BF16)
        wout = C.sb(st, "wout", [128, 16, 1024], BF16)
        WT = C.sb(st, "WT", [128, 8, 128], BF16)
        wsf = C.sb(st, "wsf", [128, 8, 128], F32)
        tril = C.sb(st, "tril", [128, 128], F32)
        grow = C.sb(st, "grow", [128, 2048], F32)
        bsr = C.sb(st, "bsr", [1, 8, 128], BF16)
        bsf = C.sb(st, "bsf", [1, 8, 128], F32)
        ones1 = C.sb(st, "ones1", [1, 128], BF16)
        xt = [C.sb(st, "xt%d" % i, [128, D], F32) for i in range(2)]
        xo = [C.sb(st, "xo%d" % i, [128, D], F32) for i in range(2)]
        junk_sh = C.sb(st, "junk_sh", [128, D], BF16)
        tmpn2 = [dict(junk=junk_sh, ss=C.sb(st, "ss%d" % i, [128, 1], F32),
                      rs=C.sb(st, "rs%d" % i, [128, 1], F32), xn=C.sb(st, "xn%d" % i, [128, D], F32), eps=P["eps"], lnexp=True) for i in range(2)]
        hT = [C.sb(st, "hT%d" % i, [128, 8, 128], BF16) for i in range(2)]
        vz2 = [C.sb(st, "vz%d" % i, [128, 2048], F32) for i in range(2)]
        vss2 = [C.sb(st, "vss%d" % i, [128, 4], F32) for i in range(2)]
        vs12 = [C.sb(st, "vs1%d" % i, [128, 1], F32) for i in range(2)]
        vn2 = [C.sb(st, "vn%d" % i, [128, 2048], BF16) for i in range(2)]
        uT2 = [C.sb(st, "uT%d" % i, [128, 16, 128], BF16) for i in range(2)]
        pTt2 = [C.sb(st, "pTt%d" % i, [128, 16, 128], BF16) for i in range(2)]
        ev2 = [C.sb(st, "ev%d" % i, [128, 512], F32) for i in range(2)]
        bks = [bank(C, st, "gb%d" % i) for i in range(8)]
        v3 = lambda a_, n: a_.rearrange("p (a b) -> p a b", a=n)
        S = S0
        wsrc = dr["w_in_c"].rearrange("(k p) n -> p k n", p=128)
        S.dma("pool", win[:, :, 2048:4096], wsrc[:, :, 2048:4096], w=["win%d" % k for k in range(4)], slot="winv", max_dma_last_dim=8192)
        S.dma("pool", win[:, :, 0:2048], wsrc[:, :, 0:2048], w=["win%d" % k for k in range(4, 8)], slot="winu", max_dma_last_dim=8192)
        S.dma("pool", wout[:], dr["w_out_c"].rearrange("(k p) n -> p k n", p=128), w=["wout"])
        S.dma("sp", wsf[:], dr["sgu_w_s"].rearrange("g t s -> t g s"), w=["wsf"])
        S.dma("sp", tril[:], dr["tril"], w=["tril"])
        S.dma("sp", grow[:], dr["sgu_norm_g"].partition_broadcast(128), w=["grow"])
        S.dma("sp", bsf[:], dr["sgu_b_s"].rearrange("(o g) t -> o g t", o=1), w=["bsf"])
        S.op("dve", lambda e: e.tensor_copy(out=bsr[:], in_=bsf[:]), r=["bsf"], w=["bsr"])
        S.op("dve", lambda e: e.memset(ones1[:], 1.0), w=["ones1"])
        for g in range(8):
            S.op("dve", lambda e, g=g: e.tensor_tensor(out=wsf[:, g, :], in0=wsf[:, g, :], in1=tril[:], op=ALU.mult),
                 r=["wsf", "tril"], w=["wsf"])
        for g in range(8):
            p = v3(bks[g // 4][:], 4)
            S.op("pe", lambda e, g=g, p=p: e.transpose(out=p[:, g % 4, :], in_=wsf[:, g, :], identity=P["ident"][:]),
                 r=["wsf", "ident"], w=["gb%d" % (g // 4)])
        for i in range(2):
            S.op("dve", lambda e, i=i: e.tensor_copy(out=WT[:, i * 4:(i + 1) * 4, :], in_=v3(bks[i][:], 4)), r=["gb%d" % i], w=["WT"])

        A = P["modA"]
        B = P["modB"]
        gate = P["gates"]
        winkeys = ["win%d" % k for k in range(8)]

        def tile(t, S):
            i = t % 2
            q0, q1, q2, q3 = [bks[i * 4 + n] for n in range(4)]
            k0, k1, k2, k3 = ["gb%d" % (i * 4 + n) for n in range(4)]
            pT = [(v3(q0[:], 4), k0), (v3(q1[:], 4), k1)]
            pvb = [(q0, k0), (q1, k1)]
            pu, puk = v3(q2[:], 4), k2
            pm, pmk = v3(q3[:], 4), k3
            tmpn, vz, vss, vs1, vn, uT, pTt, ev = tmpn2[i], vz2[i], vss2[i], vs12[i], vn2[i], uT2[i], pTt2[i], ev2[i]
            T_ = "g%d" % i
            S.dma("sp", xt[i][:], x_in[t * 128:(t + 1) * 128, :], w=["xt%d" % i])
            norm_transpose(C, S, xt[i][:], "xt%d" % i, tmpn, P["ident"], pT, T_)
            h = hT[i]
            hk = "hT%d" % i
            for k in range(8):
                p, pk = pT[k // 4]
                if k % 2 == 0:
                    S.op("dve", lambda e, k=k, p=p, h=h: e.tensor_scalar(
                        out=h[:, k, :], in0=p[:, k % 4, :], scalar1=A[:, L, 0, k:k + 1], scalar2=B[:, L, 0, k:k + 1],
                        op0=ALU.mult, op1=ALU.add), r=[pk, "modA", "modB"], w=[hk])
                else:
                    S.op("act", lambda e, k=k, p=p, h=h: e.activation(
                        out=h[:, k, :], in_=p[:, k % 4, :], func=AF.Identity, scale=A[:, L, 0, k:k + 1],
                        bias=B[:, L, 0, k:k + 1]), r=[pk, "modA", "modB"], w=[hk])
            for n in range(4):
                pv_, pvk = pvb[n % 2]
                for k in range(8):
                    S.op("pe", lambda e, k=k, n=n, pv_=pv_, h=h: e.matmul(
                        pv_[:], lhsT=h[:, k, :], rhs=win[:, k, 2048 + n * 512: 2048 + (n + 1) * 512],
                        start=(k == 0), stop=(k == 7)), r=[hk] + winkeys, w=[pvk])
                S.op("act", lambda e, n=n, pv_=pv_: e.activation(out=vz[:, n * 512:(n + 1) * 512], in_=pv_[:], func=GELU),
                     r=[pvk], w=[T_ + "vz%d" % n])
                S.op("act", lambda e, n=n: e.activation(out=junk_sh[:, 0:512], in_=vz[:, n * 512:(n + 1) * 512], func=AF.Square,
                                                        accum_out=vss[:, n:n + 1]), r=[T_ + "vz%d" % n], w=[T_ + "vss%d" % n])
            S.op("dve", lambda e: e.tensor_reduce(out=vs1[:], in_=vss[:], axis=AX.X, op=ALU.add),
                 r=[T_ + "vss%d" % n for n in range(4)], w=[T_ + "vs1"])
            S.op("act", lambda e: e.activation(out=vs1[:], in_=vs1[:], func=AF.Ln, scale=1.0 / 2048, bias=P["eps"][:]),
                 r=[T_ + "vs1"], w=[T_ + "vs1"])
            S.op("act", lambda e: e.activation(out=vs1[:], in_=vs1[:], func=AF.Exp, scale=-0.5), r=[T_ + "vs1"], w=[T_ + "vs1"])
            for n in range(4):
                S.op("dve", lambda e, n=n: e.scalar_tensor_tensor(
                    out=vn[:, n * 512:(n + 1) * 512], in0=vz[:, n * 512:(n + 1) * 512], scalar=vs1[:, 0:1],
                    in1=grow[:, n * 512:(n + 1) * 512], op0=ALU.mult, op1=ALU.mult),
                    r=[T_ + "vz%d" % n, T_ + "vs1", "grow"], w=[T_ + "vn%d" % n])
            for q in range(4):
                for m in range(4):
                    c = q * 4 + m
                    for k in range(8):
                        S.op("pe", lambda e, k=k, c=c, m=m, h=h: e.matmul(
                            pu[:, m, :], lhsT=win[:, k, c * 128:(c + 1) * 128], rhs=h[:, k, :],
                            start=(k == 0), stop=(k == 7)), r=[hk] + winkeys, w=[puk])
                S.op("act", lambda e, q=q: e.activation(out=uT[:, q * 4:(q + 1) * 4, :], in_=pu, func=GELU),
                     r=[puk], w=[T_ + "uT%d" % q])
                for m in range(4):
                    c = q * 4 + m
                    g = c // 2
                    S.op("pe", lambda e, c=c, m=m, g=g: e.matmul(
                        pm[:, m, :], lhsT=vn[:, c * 128:(c + 1) * 128], rhs=WT[:, g, :], start=True, stop=False),
                        r=[T_ + "vn%d" % (c // 4), "WT"], w=[pmk])
                    S.op("pe", lambda e, c=c, m=m, g=g: e.matmul(
                        pm[:, m, :], lhsT=ones1[0:1, :], rhs=bsr[0:1, g, :], start=False, stop=True),
                        r=["ones1", "bsr"], w=[pmk])
                S.op("dve", lambda e, q=q: e.tensor_tensor(out=pTt[:, q * 4:(q + 1) * 4, :], in0=pm, in1=uT[:, q * 4:(q + 1) * 4, :],
                                                           op=ALU.mult), r=[pmk, T_ + "uT%d" % q], w=[T_ + "pTt%d" % q])
            o = xo[i]
            for half in range(2):
                pv_, pvk = pvb[half]
                for c in range(16):
                    S.op("pe", lambda e, c=c, half=half, pv_=pv_: e.matmul(
                        pv_[:], lhsT=pTt[:, c, :], rhs=wout[:, c, half * 512:(half + 1) * 512],
                        start=(c == 0), stop=(c == 15)), r=[T_ + "pTt%d" % (c // 4), "wout"], w=[pvk])
                S.op("dve", lambda e, half=half, pv_=pv_: e.tensor_tensor(
                    out=ev[:], in0=pv_[:], in1=gate[:, L, 0, half * 512:(half + 1) * 512], op=ALU.mult),
                    r=[pvk, "gates"], w=[T_ + "ev"])
                S.op("dve", lambda e, half=half, o=o: e.tensor_tensor(
                    out=o[:, half * 512:(half + 1) * 512], in0=ev[:], in1=xt[i][:, half * 512:(half + 1) * 512], op=ALU.add),
                    r=[T_ + "ev", "xt%d" % i], w=["xo%d_%d" % (i, half)])
            S.dma("sp", x_out[t * 128:(t + 1) * 128, :], o[:], r=["xo%d_0" % i, "xo%d_1" % i], w=["xo_%s_%d" % (x_out.name, i)])

        for t in range(0, NT, 2):
            ra, rb_ = Rec(), Rec()
            tile(t, ra)
            tile(t + 1, rb_)
            interleave(S0, ra, rb_)
        S0.wait_all("sp", ["xo_%s_%d" % (x_out.name, j) for j in range(2)])
        S0.flush()


NWT = 64
OWN0 = 48
DBG_SKIP = set()
TWO_PI = 6.283185307179586
CW1 = 6.28125
CW2 = TWO_PI - CW1
CA_Q, CA_K, CA_V, CA_GLR, CA_R = 0, 256, 512, 1024, 1040
CA_KC, CA_VC, CA_KV4 = 1552, 1680, 1808
NA = 2320


def bank(C, st, name):
    return C.ps(st, name, [128, 512], F32)


def rope_tables(C, S, st, pos_i, ncol, invf, out_cs, tag):
    pf = C.sb(st, tag + "pf", [128, ncol], F32)
    ang = C.sb(st, tag + "ang", [128, ncol, 8], F32)
    ki = C.sb(st, tag + "ki", [128, ncol, 8], I32)
    kf = C.sb(st, tag + "kf", [128, ncol, 8], F32)
    r = C.sb(st, tag + "r", [128, ncol, 8], F32)
    y = C.sb(st, tag + "y", [128, ncol, 8], F32)
    m = C.sb(st, tag + "m", [128, ncol, 8], F32)
    k = lambda s: tag + s
    S.op("dve", lambda e: e.tensor_copy(out=pf[:], in_=pos_i[:]), r=[k("pos")], w=[k("pf")])
    S.op("dve", lambda e: e.tensor_tensor(out=ang[:], in0=pf[:].unsqueeze(2).to_broadcast([128, ncol, 8]),
                                          in1=invf[:].unsqueeze(1).to_broadcast([128, ncol, 8]), op=ALU.mult),
         r=[k("pf"), "invf"], w=[k("ang")])
    S.op("dve", lambda e: e.tensor_scalar(out=ki[:], in0=ang[:], scalar1=1.0 / TWO_PI, scalar2=None, op0=ALU.mult),
         r=[k("ang")], w=[k("ki")])
    S.op("dve", lambda e: e.tensor_copy(out=kf[:], in_=ki[:]), r=[k("ki")], w=[k("kf")])
    S.op("dve", lambda e: e.scalar_tensor_tensor(out=r[:], in0=kf[:], scalar=-CW1, in1=ang[:], op0=ALU.mult, op1=ALU.add),
         r=[k("kf"), k("ang")], w=[k("r")])
    S.op("dve", lambda e: e.scalar_tensor_tensor(out=r[:], in0=kf[:], scalar=-CW2, in1=r[:], op0=ALU.mult, op1=ALU.add),
         r=[k("kf"), k("r")], w=[k("r")])
    for which, shift in ((1, 0.0), (0, np.pi / 2)):
        S.op("dve", lambda e, shift=shift: e.tensor_scalar(out=y[:], in0=r[:], scalar1=float(shift), scalar2=None, op0=ALU.add),
             r=[k("r")], w=[k("y")])
        S.op("dve", lambda e: e.tensor_scalar(out=m[:], in0=y[:], scalar1=float(np.pi), scalar2=None, op0=ALU.is_gt),
             r=[k("y")], w=[k("m")])
        S.op("dve", lambda e: e.scalar_tensor_tensor(out=y[:], in0=m[:], scalar=-TWO_PI, in1=y[:], op0=ALU.mult, op1=ALU.add),
             r=[k("m"), k("y")], w=[k("y")])
        S.op("dve", lambda e: e.tensor_scalar(out=y[:], in0=y[:], scalar1=float(np.pi), scalar2=-float(np.pi), op0=ALU.min, op1=ALU.max),
             r=[k("y")], w=[k("y")])
        S.op("act", lambda e, which=which: e.activation(out=out_cs[:, :, which * 8:(which + 1) * 8], in_=y[:], func=AF.Sin),
             r=[k("y")], w=[k("cs")])


def rms_gain_rope(C, S, src, srckey, dst, dstkey, ngrp, gain_row, gainkey, cs, cskey, T, tag):
    sq, ss, t1, t2 = T["sq"], T["ss"], T["t1"], T["t2"]
    k = lambda s: tag + s
    S.op("act", lambda e: e.activation(out=sq[:, 0:ngrp, :], in_=src, func=AF.Square), r=[srckey], w=[k("sq")])
    S.op("dve", lambda e: e.tensor_reduce(out=ss[:, 0:ngrp], in_=sq[:, 0:ngrp, :], axis=AX.X, op=ALU.add), r=[k("sq")], w=[k("ss")])
    S.op("act", lambda e: e.activation(out=ss[:, 0:ngrp], in_=ss[:, 0:ngrp], func=AF.Ln, scale=1.0 / 64, bias=T["eps"][:]),
         r=[k("ss")], w=[k("ss")])
    S.op("act", lambda e: e.activation(out=ss[:, 0:ngrp], in_=ss[:, 0:ngrp], func=AF.Exp, scale=-0.5), r=[k("ss")], w=[k("ss")])
    S.op("dve", lambda e: e.tensor_tensor(out=dst, in0=src, in1=ss[:, 0:ngrp].unsqueeze(2).to_broadcast([128, ngrp, 64]), op=ALU.mult),
         r=[srckey, k("ss")], w=[dstkey])
    S.op("dve", lambda e: e.tensor_tensor(out=dst, in0=dst, in1=gain_row.unsqueeze(1).to_broadcast([128, ngrp, 64]), op=ALU.mult),
         r=[dstkey, gainkey], w=[dstkey])
    cosb = cs[:, 0:8].unsqueeze(1).to_broadcast([128, ngrp, 8])
    sinb = cs[:, 8:16].unsqueeze(1).to_broadcast([128, ngrp, 8])
    x1 = dst[:, :, 0:8]
    x2 = dst[:, :, 8:16]
    S.op("dve", lambda e: e.tensor_tensor(out=t1[:, 0:ngrp, 0:8], in0=x1, in1=cosb, op=ALU.mult), r=[dstkey, cskey], w=[k("t1a")])
    S.op("dve", lambda e: e.tensor_tensor(out=t1[:, 0:ngrp, 8:16], in0=x2, in1=cosb, op=ALU.mult), r=[dstkey, cskey], w=[k("t1b")])
    S.op("dve", lambda e: e.tensor_tensor(out=t2[:, 0:ngrp, 0:8], in0=x2, in1=sinb, op=ALU.mult), r=[dstkey, cskey], w=[k("t2a")])
    S.op("dve", lambda e: e.tensor_tensor(out=t2[:, 0:ngrp, 8:16], in0=x1, in1=sinb, op=ALU.mult), r=[dstkey, cskey], w=[k("t2b")])
    S.op("dve", lambda e: e.tensor_tensor(out=x1, in0=t1[:, 0:ngrp, 0:8], in1=t2[:, 0:ngrp, 0:8], op=ALU.subtract),
         r=[k("t1a"), k("t2a"), k("t1b"), k("t2b")], w=[dstkey])
    S.op("dve", lambda e: e.tensor_tensor(out=x2, in0=t1[:, 0:ngrp, 8:16], in1=t2[:, 0:ngrp, 8:16], op=ALU.add),
         r=[k("t1b"), k("t2b")], w=[dstkey])


def alloc_mixer(C, st):
    M = {}
    M["KselT"] = C.sb(st, "KselT", [128, NWT * 128], BF16)
    M["Vsel"] = C.sb(st, "Vsel", [128, NWT, 2, 65], BF16)
    M["KwinT"] = C.sb(st, "KwinT", [128, 20 * 128], BF16)
    M["Vwin"] = C.sb(st, "Vwin", [128, 20, 2, 65], BF16)
    M["mixT"] = C.sb(st, "mixT", [128, 8, TOK], BF16)
    M["csT"] = C.sb(st, "csT", [128, NWT, 16], F32)
    M["kbias"] = C.sb(st, "kbias", [128, NWT], F32)
    M["gain"] = C.sb(st, "gain", [128, 4, 64], F32)
    M["invf"] = C.sb(st, "invf", [128, 8], F32)
    return M


def phase_mix_a(C, P, M, dr, dbg=None, tiles=None):
    nc, S = C.nc, C.S
    L = 0
    with contextlib.ExitStack() as st:
        wA = C.sb(st, "wA", [128, 8, NA], BF16)
        w2g = C.sb(st, "w2g", [16, 256], F32)
        negb = C.sb(st, "negb", [128, 2], F32)
        gng = C.sb(st, "gng", [128, 128], F32)
        maskT = C.sb(st, "maskT", [128, 4, 128], F32)
        posi = C.sb(st, "posi", [128, NWT], I32)
        valid = C.sb(st, "valid", [128, NWT], F32)
        onesc = C.sb(st, "onesc", [128, 64], F32)
        state = C.sb(st, "state", [128, 4, 128], F32)
        sbf = [C.sb(st, "sbf%d" % i, [128, 4, 128], BF16) for i in range(2)]
        xt = [C.sb(st, "xt%d" % i, [128, D], F32) for i in range(2)]
        tmpn = dict(junk=C.sb(st, "junk", [128, D], BF16), ss=C.sb(st, "ss", [128, 1], F32),
                    rs=C.sb(st, "rs", [128, 1], F32), xn=C.sb(st, "xn", [128, D], F32), eps=P["eps"], lnexp=True)
        hT = C.sb(st, "hT", [128, 8, 128], BF16)
        glrT = C.sb(st, "glrT", [16, 128], F32)
        vb2 = [C.sb(st, "vb%d" % i, [128, 512], BF16) for i in range(2)]
        qk2 = [C.sb(st, "qk%d" % i, [128, 4, 128], F32) for i in range(2)]
        sp2 = [C.sb(st, "sp%d" % i, [128, 2, 128], F32) for i in range(2)]
        kv42 = [C.sb(st, "kv4s%d" % i, [128, 512], F32) for i in range(2)]
        rs2 = [C.sb(st, "rsl%d" % i, [128, 512], F32) for i in range(2)]
        cmpT = [C.sb(st, "cmpT%d" % i, [128, 2, 128], BF16) for i in range(2)]
        cs_ = C.sb(st, "cs_", [128, 2, 128], F32)
        m16 = C.sb(st, "m16", [128, 2, 2, 2], F32)
        nm16 = C.sb(st, "nm16", [128, 2, 2, 2], F32)
        oaf = C.sb(st, "oaf", [128, 512], F32)
        E1 = C.sb(st, "E1", [128, 2, 128], F32)
        E2 = C.sb(st, "E2", [128, 2, 128], F32)
        E3 = C.sb(st, "E3", [128, 2, 128], F32)
        E4 = C.sb(st, "E4", [128, 2, 128], F32)
        qtl = C.sb(st, "qtl", [128, 2, 2, 128], BF16)
        ktl = C.sb(st, "ktl", [128, 2, 128], BF16)
        khT = C.sb(st, "khT", [128, 2, 128], F32)
        kh = C.sb(st, "kh", [128, 2, 128], BF16)
        qhz = C.sb(st, "qhz", [128, 2, 2, 128], BF16)
        sT = C.sb(st, "sT", [128, 4, 128], BF16)
        ssq = C.sb(st, "ssq", [128, 4], F32)
        sqj = C.sb(st, "sqj", [128, 128], F32)
        on = C.sb(st, "on", [128, 512], F32)
        oa = C.sb(st, "oa", [128, 512], BF16)
        identb = C.sb(st, "identb", [128, 128], BF16)
        kn = C.sb(st, "kn", [128, 2, 2, 64], F32)
        RT = dict(sq=C.sb(st, "r_sq", [128, 2, 64], F32), ss=C.sb(st, "r_ss", [128, 2], F32),
                  t1=C.sb(st, "r_t1", [128, 2, 16], F32), t2=C.sb(st, "r_t2", [128, 2, 16], F32), eps=P["eps"])
        b = [bank(C, st, "bk%d" % i) for i in range(8)]
        v3 = lambda a, n: a.rearrange("p (a b) -> p a b", a=n)
        pT0, pT1 = v3(b[0][:], 4), v3(b[1][:], 4)
        pv, pr = b[0], b[0]
        pkv4 = b[1]
        pqk = v3(b[2][:], 4)
        pcmp = v3(b[3][:, 0:256], 2)
        pz = v3(b[3][:, 256:512], 2)
        pglr = b[3][0:16, 256:384]
        pkvs = v3(b[4][:], 2)
        pkh = v3(b[5][:, 0:256], 2)
        pkT = v3(b[5][:, 256:512], 2)
        psc = v3(b[6][:], 4)
        pmx = b[6][:, 0:256].bitcast(BF16).rearrange("p (a b) -> p a b", a=4)
        po = v3(b[7][:], 4)

        S.dma("pool", wA[:, :, 0:1552], dr["w_in_ab"][:, 0:1552].rearrange("(k p) n -> p k n", p=128), w=["wA0"])
        S.dma("pool", wA[:, :, 1552:NA], dr["w_in_ab"][:, 2064:2832].rearrange("(k p) n -> p k n", p=128), w=["wA1"])
        S.dma("sp", w2g[:], dr["gla_w_gate2"], w=["w2g"])
        S.dma("sp", negb[:], dr["gla_b_gate_col"], w=["negb"])
        S.dma("sp", gng[:], dr["gla_norm_g"].partition_broadcast(128), w=["gng"])
        S.dma("sp", maskT[:], dr["gla_maskT"], w=["maskT"])
        S.dma("sp", posi[:], dr["pos_col"], w=["Tpos"])
        S.dma("sp", valid[:], dr["valid_col"], w=["valid"])
        S.dma("sp", M["invf"][:], dr["invf"].partition_broadcast(128), w=["invf"])
        S.dma("sp", M["gain"][:, 0:3, :], dr["nsa_k_gain"].partition_broadcast(128), w=["gain"])
        S.dma("sp", M["gain"][:, 3, :], dr["nsa_q_gain"].partition_broadcast(128), w=["gainq"])
        S.op("dve", lambda e: e.tensor_scalar(out=negb[:], in0=negb[:], scalar1=-1.0, scalar2=None, op0=ALU.mult), r=["negb"], w=["negb"])
        S.op("dve", lambda e: e.memset(onesc[:], 1.0), w=["onesc"])
        S.op("dve", lambda e: e.memset(state[:], 0.0), w=["state"])
        S.op("pool", lambda e: e.memset(qhz[:], 0.0), w=["qhz"])
        S.op("pool", lambda e: e.memset(qtl[:], 0.0), w=["qtl"])
        S.op("pool", lambda e: e.memset(M["Vsel"][:, :, :, 64:65], 1.0), w=["Vsel1"])
        S.op("pool", lambda e: e.memset(M["Vwin"][:, :, :, 64:65], 1.0), w=["Vwin1"])
        S.op("dve", lambda e: e.tensor_copy(out=identb[:], in_=P["ident"][:]), r=["ident"], w=["identb"])
        S.op("dve", lambda e: e.tensor_scalar(out=M["kbias"][:], in0=valid[:], scalar1=-1.0, scalar2=1e4, op0=ALU.add, op1=ALU.mult),
             r=["valid"], w=["kbias"])
        rope_tables(C, S, st, posi, NWT, M["invf"], M["csT"], "T")

        A, B = P["modA"], P["modB"]
        wk = ["wA0", "wA1"]
        tl = list(tiles if tiles is not None else range(NWT))

        def stage_a(t, S):
            own = t >= OWN0
            i = t % 2
            S.dma("sp", xt[i][:], dr["xw"][t * 128:(t + 1) * 128, :], w=["xt%d" % i])
            norm_transpose(C, S, xt[i][:], "xt%d" % i, tmpn, P["ident"], [(pT0, "b0"), (pT1, "b1")], "a")
            for k in range(8):
                p, pk = ((pT0, "b0"), (pT1, "b1"))[k // 4]
                if k % 2 == 0:
                    S.op("dve", lambda e, k=k, p=p: e.tensor_scalar(
                        out=hT[:, k, :], in0=p[:, k % 4, :], scalar1=A[:, L, 0, k:k + 1], scalar2=B[:, L, 0, k:k + 1],
                        op0=ALU.mult, op1=ALU.add), r=[pk, "modA", "modB"], w=["hT"])
                else:
                    S.op("act", lambda e, k=k, p=p: e.activation(
                        out=hT[:, k, :], in_=p[:, k % 4, :], func=AF.Identity, scale=A[:, L, 0, k:k + 1],
                        bias=B[:, L, 0, k:k + 1]), r=[pk, "modA", "modB"], w=["hT"])
            for m in range(4):
                if m < 2 and not own:
                    continue
                for k in range(8):
                    S.op("pe", lambda e, k=k, m=m: e.matmul(pqk[:, m, :], lhsT=wA[:, k, m * 128:(m + 1) * 128], rhs=hT[:, k, :],
                                                            start=(k == 0), stop=(k == 7)), r=["hT"] + wk, w=["b2"])
            for k in range(8):
                S.op("pe", lambda e, k=k: e.matmul(pglr, lhsT=wA[:, k, CA_GLR:CA_GLR + 16], rhs=hT[:, k, :],
                                                   start=(k == 0), stop=(k == 7)), r=["hT"] + wk, w=["b3"])
            for m in range(2):
                for k in range(8):
                    S.op("pe", lambda e, k=k, m=m: e.matmul(pcmp[:, m, :], lhsT=wA[:, k, CA_KC + m * 128:CA_KC + (m + 1) * 128],
                                                            rhs=hT[:, k, :], start=(k == 0), stop=(k == 7)), r=["hT"] + wk, w=["b3"])
            for k in range(8):
                S.op("pe", lambda e, k=k: e.matmul(pv[:], lhsT=hT[:, k, :], rhs=wA[:, k, CA_V:CA_V + 512],
                                                   start=(k == 0), stop=(k == 7)), r=["hT"] + wk, w=["b0"])
            for k in range(8):
                S.op("pe", lambda e, k=k: e.matmul(pkv4[:], lhsT=hT[:, k, :], rhs=wA[:, k, CA_KV4:CA_KV4 + 512],
                                                   start=(k == 0), stop=(k == 7)), r=["hT"] + wk, w=["b1"])
            S.op("dve", lambda e: e.tensor_copy(out=glrT[:], in_=pglr), r=["b3"], w=["glrT"])
            ct = cmpT[i]
            S.op("act", lambda e: e.activation(out=ct[:], in_=pcmp, func=AF.Copy), r=["b3"], w=["cmpT%d" % i])
            for m in range(2):
                S.dma("act", dr["cmp_scr"][m, :, t * 128:(t + 1) * 128], ct[:, m, :], r=["cmpT%d" % i], w=["cmp_scr%d" % i])
            m0 = 0 if own else 2
            S.op("dve", lambda e: e.tensor_copy(out=qk2[i][:, m0:4, :], in_=pqk[:, m0:4, :]), r=["b2"], w=["qk%d" % i])
            S.op("act", lambda e: e.activation(out=vb2[i][:], in_=pv[:], func=AF.Copy), r=["b0"], w=["vb%d" % i])
            S.op("dve", lambda e: e.tensor_copy(out=kv42[i][:], in_=pkv4[:]), r=["b1"], w=["kv4s%d" % i])
            for p_ in range(2):
                S.op("pe", lambda e, p_=p_: e.matmul(pz[:, p_, :], lhsT=w2g[:, p_ * 128:(p_ + 1) * 128], rhs=glrT[:],
                                                     start=True, stop=True), r=["w2g", "glrT"], w=["b3"])
            if own:
                for k in range(8):
                    S.op("pe", lambda e, k=k: e.matmul(pr[:], lhsT=hT[:, k, :], rhs=wA[:, k, CA_R:CA_R + 512],
                                                       start=(k == 0), stop=(k == 7)), r=["hT"] + wk, w=["b0"])
            for p_ in range(2):
                S.op("act", lambda e, p_=p_: e.activation(out=sp2[i][:, p_, :], in_=pz[:, p_, :], func=AF.Exp, scale=-1.0,
                                                          bias=negb[:, p_:p_ + 1]), r=["b3", "negb"], w=["sp%d" % i])
            if own:
                S.op("act", lambda e: e.activation(out=rs2[i][:], in_=pr[:], func=AF.Exp, scale=-1.0), r=["b0"], w=["rsl%d" % i])
                S.op("dve", lambda e: e.tensor_scalar(out=rs2[i][:], in0=rs2[i][:], scalar1=1.0, scalar2=None, op0=ALU.add), r=["rsl%d" % i], w=["rsl%d" % i])
                S.op("dve", lambda e: e.reciprocal(out=rs2[i][:], in_=rs2[i][:]), r=["rsl%d" % i], w=["rsl%d" % i])
                S.op("dve", lambda e: e.tensor_tensor(out=rs2[i][:], in0=rs2[i][:], in1=pr[:], op=ALU.mult), r=["rsl%d" % i, "b0"], w=["rsl%d" % i])

        def stage_b(t, S):
            own = t >= OWN0
            i = t % 2
            sp, qk, vb, kv4s, rs_ = sp2[i], qk2[i], vb2[i], kv42[i], rs2[i]
            spk, qkk, vbk, kvk, rsk = "sp%d" % i, "qk%d" % i, "vb%d" % i, "kv4s%d" % i, "rsl%d" % i
            S.op("act", lambda e: e.activation(out=sp[:], in_=sp[:], func=AF.Ln, bias=1.0, scale=1.0), r=[spk], w=[spk])
            for p_ in range(2):
                for c in range(2):
                    S.op("dve", lambda e, p_=p_, c=c: e.tensor_tensor_scan(
                        out=cs_[:, p_, c * 64:(c + 1) * 64], data0=onesc[:], data1=sp[:, p_, c * 64:(c + 1) * 64], initial=0.0,
                        op0=ALU.mult, op1=ALU.add), r=[spk, "onesc"], w=["cs_"])
            csv = cs_[:].rearrange("p a (c t) -> p a c t", c=2)
            S.op("dve", lambda e: e.tensor_scalar(out=m16[:, :, :, 0:1], in0=csv[:, :, :, 32:33], scalar1=1.0 / 16, scalar2=None, op0=ALU.mult),
                 r=["cs_"], w=["m16"])
            S.op("dve", lambda e: e.tensor_scalar(out=m16[:, :, :, 1:2], in0=csv[:, :, :, 63:64], scalar1=1.0 / 16, scalar2=None, op0=ALU.mult),
                 r=["cs_"], w=["m16"])
            S.op("dve", lambda e: e.tensor_scalar(out=nm16[:], in0=m16[:], scalar1=-1.0, scalar2=None, op0=ALU.mult), r=["m16"], w=["nm16"])
            for p_ in range(2):
                for c in range(2):
                    sl = slice(c * 64, (c + 1) * 64)
                    S.op("act", lambda e, p_=p_, c=c, sl=sl: e.activation(out=E3[:, p_, sl], in_=cs_[:, p_, sl], func=AF.Exp,
                                                                          scale=1.0 / 16, bias=nm16[:, p_, c, 1:2]), r=["cs_", "nm16"], w=["E3"])
            S.op("act", lambda e: e.activation(out=E4[:], in_=cs_[:], func=AF.Exp, scale=-1.0 / 16), r=["cs_"], w=["E4"])
            if own:
                for p_ in range(2):
                    for c in range(2):
                        sl = slice(c * 64, (c + 1) * 64)
                        S.op("act", lambda e, p_=p_, c=c, sl=sl: e.activation(out=E1[:, p_, sl], in_=cs_[:, p_, sl], func=AF.Exp,
                                                                              scale=-1.0 / 16, bias=m16[:, p_, c, 0:1]), r=["cs_", "m16"], w=["E1"])
                        S.op("act", lambda e, p_=p_, c=c, sl=sl: e.activation(out=E2[:, p_, sl], in_=cs_[:, p_, sl], func=AF.Exp,
                                                                              scale=1.0 / 16, bias=nm16[:, p_, c, 0:1]), r=["cs_", "nm16"], w=["E2"])
            S.op("dve", lambda e: e.tensor_tensor(out=khT[:], in0=qk[:, 2:4, :], in1=E3[:], op=ALU.mult), r=[qkk, "E3"], w=["khT"])
            for p_ in range(2):
                S.op("pe", lambda e, p_=p_: e.transpose(out=pkh[:, p_, :], in_=khT[:, p_, :], identity=P["ident"][:]),
                     r=["khT", "ident"], w=["b5"])
            S.op("dve", lambda e: e.tensor_scalar(out=kh[:], in0=pkh, scalar1=valid[:, t:t + 1], scalar2=None, op0=ALU.mult),
                 r=["b5", "valid"], w=["kh"])
            if own:
                S.op("dve", lambda e: e.tensor_tensor(out=ktl[:], in0=qk[:, 2:4, :], in1=E2[:], op=ALU.mult), r=[qkk, "E2"], w=["ktl"])
                for p_ in range(2):
                    for hh in range(2):
                        rs64 = slice(hh * 64, (hh + 1) * 64)
                        S.op("dve", lambda e, p_=p_, hh=hh, rs64=rs64: e.scalar_tensor_tensor(
                            out=qtl[rs64, p_, hh, :], in0=qk[rs64, p_, :], scalar=0.125, in1=E1[rs64, p_, :],
                            op0=ALU.mult, op1=ALU.mult), r=[qkk, "E1"], w=["qtl"])
                    for c in range(2):
                        sl = slice(c * 64, (c + 1) * 64)
                        S.op("dve", lambda e, p_=p_, c=c, sl=sl: e.scalar_tensor_tensor(
                            out=qhz[:, p_, c, sl], in0=qk[:, p_, sl], scalar=0.125, in1=E4[:, p_, sl], op0=ALU.mult, op1=ALU.mult),
                            r=[qkk, "E4"], w=["qhz"])
                for hd in range(4):
                    p_ = hd // 2
                    S.op("pe", lambda e, hd=hd, p_=p_: e.matmul(psc[:, hd, :], lhsT=ktl[:, p_, :], rhs=qtl[:, p_, hd % 2, :],
                                                                start=True, stop=True), r=["ktl", "qtl"], w=["b6"])
                S.op("dve", lambda e: e.tensor_tensor(out=sT[:], in0=psc, in1=maskT[:], op=ALU.mult), r=["b6", "maskT"], w=["sT"])
            for c in range(2):
                if own:
                    S.op("act", lambda e, c=c: e.activation(out=sbf[c][:], in_=state[:], func=AF.Copy), r=["state"], w=["sbf%d" % c])
                for p_ in range(2):
                    cs64 = slice(c * 64, (c + 1) * 64)
                    S.op("pe", lambda e, p_=p_, cs64=cs64: e.matmul(pkvs[:, p_, :], lhsT=kh[cs64, p_, :], rhs=vb[cs64, p_ * 256:(p_ + 1) * 256],
                                                                    start=True, stop=True), r=["kh", vbk], w=["b4"])
                for hd in range(4):
                    p_, o_ = hd // 2, (hd % 2) * 64
                    dcol = E4[o_:o_ + 64, p_, c * 64 + 63:c * 64 + 64]
                    S.op("dve", lambda e, hd=hd, p_=p_, o_=o_, dcol=dcol: e.scalar_tensor_tensor(
                        out=state[o_:o_ + 64, hd, :], in0=state[o_:o_ + 64, hd, :], scalar=dcol,
                        in1=pkvs[o_:o_ + 64, p_, (hd % 2) * 128:(hd % 2) * 128 + 128], op0=ALU.mult, op1=ALU.add),
                        r=["state", "E4", "b4"], w=["state"])
            if own:
                for hd in range(4):
                    p_, o_ = hd // 2, (hd % 2) * 64
                    S.op("pe", lambda e, hd=hd: e.matmul(po[:, hd, :], lhsT=sT[:, hd, :], rhs=vb[:, hd * 128:(hd + 1) * 128],
                                                         start=True, stop=False), r=["sT", vbk], w=["b7"])
                    for c in range(2):
                        S.op("pe", lambda e, hd=hd, p_=p_, o_=o_, c=c: e.matmul(
                            po[:, hd, :], lhsT=qhz[o_:o_ + 64, p_, c, :], rhs=sbf[c][o_:o_ + 64, hd, :], start=False, stop=(c == 1)),
                            r=["qhz", "sbf%d" % c], w=["b7"])
                for hd in range(4):
                    S.op("act", lambda e, hd=hd: e.activation(out=sqj[:], in_=po[:, hd, :], func=AF.Square, accum_out=ssq[:, hd:hd + 1]),
                         r=["b7"], w=["sqj", "ssq"])
                S.op("act", lambda e: e.activation(out=ssq[:], in_=ssq[:], func=AF.Ln, scale=1.0 / 128, bias=P["eps"][:]), r=["ssq"], w=["ssq"])
                S.op("act", lambda e: e.activation(out=ssq[:], in_=ssq[:], func=AF.Exp, scale=-0.5), r=["ssq"], w=["ssq"])
                for hd in range(4):
                    S.op("dve", lambda e, hd=hd: e.scalar_tensor_tensor(out=on[:, hd * 128:(hd + 1) * 128], in0=po[:, hd, :], scalar=ssq[:, hd:hd + 1],
                                                                        in1=gng[:], op0=ALU.mult, op1=ALU.mult), r=["b7", "ssq", "gng"], w=["on"])
                S.op("dve", lambda e: e.tensor_tensor(out=oa[:], in0=on[:], in1=rs_[:], op=ALU.mult), r=["on", rsk], w=["oa"])
                if dbg is not None:
                    S.op("dve", lambda e: e.tensor_tensor(out=oaf[:], in0=on[:], in1=rs_[:], op=ALU.mult), r=["on", rsk], w=["oaf"])
                    S.dma("sp", dbg["o_a"][(t - OWN0) * 128:(t - OWN0 + 1) * 128, :], oaf[:], r=["oaf"], w=["dbg_oa"])
                for hd in range(4):
                    S.op("pe", lambda e, hd=hd: e.transpose(out=pmx[:, hd, :], in_=oa[:, hd * 128:(hd + 1) * 128], identity=identb[:]),
                         r=["oa", "identb"], w=["b6"])
                S.op("act", lambda e: e.activation(out=M["mixT"][:, 0:4, (t - OWN0) * 128:(t - OWN0 + 1) * 128], in_=pmx, func=AF.Copy),
                     r=["b6"], w=["mixTa%d" % (t - OWN0)])
            kv4 = kv4s[:].rearrange("p (s g d) -> p s g d", s=4, g=2)
            branches = [(0, 1)] + ([(1, 2)] if t >= 44 else [])
            for br, gi in branches:
                rms_gain_rope(C, S, kv4[:, 2 * br, :, :], kvk, kn[:, br, :, :], "kn%d" % br, 2, M["gain"][:, gi, :], "gain",
                              M["csT"][:, t, :], "Tcs", RT, "rk")
                S.op("pe", lambda e, br=br: e.transpose(out=pkT[:, br, :], in_=kn[:, br, :, :].rearrange("p g d -> p (g d)"), identity=P["ident"][:]),
                     r=["kn%d" % br, "ident"], w=["b5"])
            S.op("act", lambda e: e.activation(out=M["KselT"][:, t * 128:(t + 1) * 128], in_=pkT[:, 0, :], func=AF.Copy),
                 r=["b5"], w=["KselT%d" % t])
            S.op("pool", lambda e: e.tensor_copy(out=M["Vsel"][:, t, :, 0:64], in_=kv4[:, 1, :, :]), r=[kvk], w=["Vsel%d" % t])
            if t >= 44:
                S.op("act", lambda e: e.activation(out=M["KwinT"][:, (t - 44) * 128:(t - 43) * 128], in_=pkT[:, 1, :], func=AF.Copy),
                     r=["b5"], w=["KwinT%d" % (t - 44)])
                S.op("pool", lambda e: e.tensor_copy(out=M["Vwin"][:, t - 44, :, 0:64], in_=kv4[:, 3, :, :]), r=[kvk], w=["Vwin%d" % (t - 44)])

        if tl:
            stage_a(tl[0], S)
        for n_, t in enumerate(tl):
            ra, rb = Rec(), Rec()
            if n_ + 1 < len(tl):
                stage_a(tl[n_ + 1], ra)
            stage_b(t, rb)
            interleave(S, ra, rb)
        if dbg is not None:
            S.wait_all("sp", ["dbg_oa"])
        S.wait_all("act", ["cmp_scr0", "cmp_scr1"])
        S.flush()


def alloc_cmp(C, st):
    M2 = {}
    M2["KcT"] = C.sb(st, "KcT", [128, 512], F32)
    M2["Vc"] = C.sb(st, "Vc", [128, 4, 2, 65], F32)
    M2["cbias"] = C.sb(st, "cbias", [128, 4], F32)
    return M2


def phase_cmp(C, P, M, M2, dr):
    nc, S = C.nc, C.S
    with contextlib.ExitStack() as st:
        KC2 = C.sb(st, "KC2", [128, 2, 2, 8192], BF16)
        w1 = C.sb(st, "w1", [128, 2, 16, 256], BF16)
        w2 = C.sb(st, "w2", [128, 2, 2, 64], BF16)
        pecol = C.sb(st, "pecol", [128, 2, 16], F32)
        pecb = C.sb(st, "pecb", [128, 2, 16], BF16)
        pbias = C.sb(st, "pbias", [128, 2, 2], F32)
        hT = [C.sb(st, "chT%d" % i, [128, 2, 512], BF16) for i in range(2)]
        posb = C.sb(st, "posb", [128, 4], I32)
        csB = C.sb(st, "csB", [128, 4, 16], F32)
        cval = C.sb(st, "cval", [128, 4], F32)
        kcn = C.sb(st, "kcn", [128, 2, 64], F32)
        RT = dict(sq=C.sb(st, "c_sq", [128, 2, 64], F32), ss=C.sb(st, "c_ss", [128, 2], F32),
                  t1=C.sb(st, "c_t1", [128, 2, 16], F32), t2=C.sb(st, "c_t2", [128, 2, 16], F32), eps=P["eps"])
        ph = [bank(C, st, "cph%d" % i) for i in range(2)]
        pb = bank(C, st, "cpb")
        po = [bank(C, st, "cpo%d" % i) for i in range(2)]
        pt = bank(C, st, "cpt")

        for kv in range(2):
            for g in range(2):
                S.dma("sp", KC2[0:64, kv, g, :], dr["cmp_scr"][kv, g * 64:(g + 1) * 64, :], r=["cmp_scr0", "cmp_scr1"], w=["KC2a%d%d" % (kv, g)])
                S.dma("sp", KC2[64:128, kv, g, 0:8191], dr["cmp_scr"][kv, g * 64:(g + 1) * 64, 1:8192], r=["cmp_scr0", "cmp_scr1"],
                      w=["KC2b%d%d" % (kv, g)])
            S.dma("pool", w1[:, kv, :, :], dr["nsa_cmp_w1"][kv].rearrange("(lp p) n -> p lp n", p=128), w=["w1_%d" % kv])
            S.dma("pool", w2[:, kv, :, :], dr["nsa_cmp_w2"][kv].rearrange("(hc p) n -> p hc n", p=128), w=["w2_%d" % kv])
        S.dma("sp", pecol[:], dr["cmp_pe_col"], w=["pecol"])
        S.dma("sp", posb[:], dr["posb_col"], w=["Bpos"])
        S.dma("sp", cval[:], dr["cvalid_col"], w=["cval"])
        S.op("dve", lambda e: e.tensor_copy(out=pecb[:], in_=pecol[:]), r=["pecol"], w=["pecb"])
        S.op("dve", lambda e: e.tensor_scalar(out=M2["cbias"][:], in0=cval[:], scalar1=-1.0, scalar2=1e4, op0=ALU.add, op1=ALU.mult),
             r=["cval"], w=["cbias"])
        S.op("pool", lambda e: e.memset(M2["Vc"][:, :, :, 64:65], 1.0), w=["Vc1"])
        for i in range(2):
            S.op("pool", lambda e, i=i: e.memset(hT[i][:, :, 511:512], 0.0), w=["chT%d" % i])
        rope_tables(C, S, st, posb, 4, M["invf"], csB, "B")
        for kv in range(2):
            for hc in range(2):
                for lp in range(16):
                    S.op("pe", lambda e, kv=kv, hc=hc, lp=lp: e.matmul(
                        pb[:, kv * 2 + hc:kv * 2 + hc + 1], lhsT=w1[:, kv, lp, hc * 128:(hc + 1) * 128], rhs=pecb[:, kv, lp:lp + 1],
                        start=(lp == 0), stop=(lp == 15)), r=["w1_%d" % kv, "pecb"], w=["cpb"])
        S.op("dve", lambda e: e.tensor_copy(out=pbias[:].rearrange("p a b -> p (a b)"), in_=pb[:, 0:4]), r=["cpb"], w=["pbias"])
        n = 0
        for kv in range(2):
            for g in range(2):
                h = hT[n % 2]
                hk = "chT%d" % (n % 2)
                n += 1
                for hc in range(2):
                    p = ph[hc]
                    for lp in range(16):
                        S.op("pe", lambda e, kv=kv, g=g, hc=hc, lp=lp, p=p: e.matmul(
                            p[:, 0:511], lhsT=w1[:, kv, lp, hc * 128:(hc + 1) * 128],
                            rhs=KC2[:, kv, g, 2 * lp:2 * lp + 16 * 510 + 1:16], start=(lp == 0), stop=(lp == 15)),
                            r=["w1_%d" % kv, "KC2a%d%d" % (kv, g), "KC2b%d%d" % (kv, g)], w=["cph%d" % hc])
                    S.op("act", lambda e, hc=hc, h=h, p=p, kv=kv: e.activation(out=h[:, hc, 0:511], in_=p[:, 0:511], func=GELU,
                                                                               bias=pbias[:, kv, hc:hc + 1]), r=["cph%d" % hc, "pbias"], w=[hk])
                for bc in range(4):
                    o = po[kv]
                    ok = "cpo%d" % kv
                    for hc in range(2):
                        S.op("pe", lambda e, kv=kv, g=g, hc=hc, bc=bc, h=h, o=o: e.matmul(
                            o[:, (bc * 2 + g) * 64:(bc * 2 + g + 1) * 64], lhsT=h[:, hc, bc * 128:(bc + 1) * 128], rhs=w2[:, kv, hc, :],
                            start=(hc == 0), stop=(hc == 1)), r=[hk, "w2_%d" % kv], w=[ok])
        pk4 = po[0][:].rearrange("p (b g d) -> p b g d", b=4, g=2)
        pv4 = po[1][:].rearrange("p (b g d) -> p b g d", b=4, g=2)
        ptv = pt[:].rearrange("p (a b) -> p a b", a=4)
        for bc in range(4):
            rms_gain_rope(C, S, pk4[:, bc, :, :], "cpo0", kcn[:], "kcn", 2, M["gain"][:, 0, :], "gain", csB[:, bc, :], "Bcs", RT, "ck")
            S.op("pe", lambda e, bc=bc: e.transpose(out=ptv[:, bc, :], in_=kcn[:].rearrange("p g d -> p (g d)"), identity=P["ident"][:]),
                 r=["kcn", "ident"], w=["cpt"])
        S.op("act", lambda e: e.activation(out=M2["KcT"][:], in_=pt[:], func=AF.Copy), r=["cpt"], w=["KcT"])
        S.op("dve", lambda e: e.tensor_copy(out=M2["Vc"][:, :, :, 0:64], in_=pv4), r=["cpo1"], w=["Vc"])
        S.flush()


NEGB = -30000.0


def phase_nsa(C, P, M, M2, dr, dbg=None, qtiles=None):
    nc, S = C.nc, C.S
    L = 0
    with contextlib.ExitStack() as st:
        wB = C.sb(st, "wB", [128, 8, 536], BF16)
        Esel = C.sb(st, "Esel", [128, 64, 128], BF16)
        cover = C.sb(st, "cover", [128, 4, 129], F32)
        corebias = C.sb(st, "corebias", [128, 128], F32)
        causb = C.sb(st, "causb", [128, 4, 128], BF16)
        winb = C.sb(st, "winb", [128, 4, 128], BF16)
        identb = C.sb(st, "identb", [128, 128], BF16)
        kbC = C.sb(st, "kbC", [128, NWT], F32)
        cbC = C.sb(st, "cbC", [128, 4], F32)
        gm = C.sb(st, "gm", [128, 4], F32)
        Cc = C.sb(st, "Cc", [128, 1], F32)
        xt = [C.sb(st, "xt%d" % i, [128, D], F32) for i in range(2)]
        tmpn = dict(junk=C.sb(st, "junk", [128, D], BF16), ss=C.sb(st, "ss", [128, 1], F32),
                    rs=C.sb(st, "rs", [128, 1], F32), xn=C.sb(st, "xn", [128, D], F32), eps=P["eps"], lnexp=True)
        hT = C.sb(st, "hT", [128, 8, 128], BF16)
        qn = C.sb(st, "qn", [128, 8, 64], F32)
        RT = dict(sq=C.sb(st, "q_sq", [128, 8, 64], F32), ss=C.sb(st, "q_ss", [128, 8], F32),
                  t1=C.sb(st, "q_t1", [128, 8, 16], F32), t2=C.sb(st, "q_t2", [128, 8, 16], F32), eps=P["eps"])
        QT32 = C.sb(st, "QT32", [128, 2, 4, 128], F32)
        QTb = C.sb(st, "QTb", [128, 2, 4, 128], BF16)
        gts2 = [C.sb(st, "gts%d" % i, [128, 24], F32) for i in range(2)]
        cm = [C.sb(st, "cm%d" % i, [128, 4, 128], F32) for i in range(2)]
        sbias = [C.sb(st, "sbias%d" % i, [128, 128], F32) for i in range(2)]
        PcT = [C.sb(st, "PcT%d" % i, [128, 4, 128], F32) for i in range(4)]
        PTs = [[C.sb(st, "PT%d_%d" % (g_, i), [128, 512], BF16) for i in range(3)] for g_ in range(2)]
        oTs = [[C.sb(st, "oTs%d%d" % (g_, i), [65, 512], F32) for i in range(2)] for g_ in range(2)]
        rden = C.sb(st, "rden", [128, 4], F32)
        acc = C.sb(st, "acc", [128, 128], F32)
        m8a = C.sb(st, "m8a", [128, 8], F32)
        m8b = C.sb(st, "m8b", [128, 8], F32)
        sc2 = C.sb(st, "sc2", [128, 128], F32)
        selm = C.sb(st, "selm", [128, 128], F32)
        selv = C.sb(st, "selv", [128, 128], F32)
        NBt2 = [C.sb(st, "NBt%d" % i, [128, 4, 128], BF16) for i in range(2)]
        oT = C.sb(st, "oT", [65, 512], F32)
        ob2 = [C.sb(st, "ob%d" % i, [128, 512], F32) for i in range(2)]
        obb = C.sb(st, "obb", [128, 512], BF16)
        coef = C.sb(st, "coef", [128, 4], F32)
        bks = [bank(C, st, "nb%d" % i) for i in range(8)]
        v3 = lambda a, n: a.rearrange("p (a b) -> p a b", a=n)
        pT0, pT1 = v3(bks[0][:], 4), v3(bks[1][:], 4)
        pq = bks[2]
        pqT = v3(bks[3][:], 4)
        pS = [bks[4], bks[5]]
        pO = bks[6]
        pX = bks[7]
        pW = bks[1]
        pS3 = [bks[4], bks[5], bks[3]]
        pS3k = ["nb4", "nb5", "nb3"]

        S.dma("pool", wB[:, :, 0:512], dr["w_nq_perm"].rearrange("(k p) n -> p k n", p=128), w=["wB0"])
        S.dma("pool", wB[:, :, 512:536], dr["w_in_ab"][:, 2832:2856].rearrange("(k p) n -> p k n", p=128), w=["wB1"])
        S.dma("sp", Esel[:], dr["Esel"], w=["Esel"])
        S.dma("sp", cover[:], dr["cover"], w=["cover"])
        S.dma("sp", corebias[:], dr["corebias"], w=["corebias"])
        S.dma("sp", causb[:], dr["causb"], w=["causb"])
        S.dma("sp", winb[:], dr["winb"], w=["winb"])
        S.op("dve", lambda e: e.tensor_copy(out=identb[:], in_=P["ident"][:]), r=["ident"], w=["identb"])
        S.op("pool", lambda e: e.memset(QT32[:], 0.0), w=["QT32"])
        S.op("pool", lambda e: e.memset(QTb[:], 0.0), w=["QTb"])
        S.op("dve", lambda e: e.tensor_reduce(out=gm[:], in_=M["gain"][:], axis=AX.X, op=ALU.max, apply_absolute_value=True),
             r=["gain", "gainq"], w=["gm"])
        S.op("dve", lambda e: e.tensor_reduce(out=Cc[:], in_=gm[:, 0:3], axis=AX.X, op=ALU.max), r=["gm"], w=["Cc"])
        S.op("dve", lambda e: e.tensor_scalar(out=Cc[:], in0=Cc[:], scalar1=gm[:, 3:4], scalar2=-8.0, op0=ALU.mult, op1=ALU.mult),
             r=["Cc", "gm"], w=["Cc"])
        S.op("dve", lambda e: e.tensor_scalar(out=kbC[:], in0=M["kbias"][:], scalar1=Cc[:, 0:1], scalar2=None, op0=ALU.add),
             r=["kbias", "Cc"], w=["kbC"])
        S.op("dve", lambda e: e.tensor_scalar(out=cbC[:], in0=M2["cbias"][:], scalar1=Cc[:, 0:1], scalar2=None, op0=ALU.add),
             r=["cbias", "Cc"], w=["cbC"])

        A, B = P["modA"], P["modB"]
        nS = 0
        nP = 0
        def pro(qt, S):
            T = OWN0 + qt
            i = qt % 2
            gts = gts2[i]
            ob = ob2[i]
            gk = "gts%d" % i
            obk = "ob%d" % i
            S.dma("sp", xt[i][:], dr["xw"][T * 128:(T + 1) * 128, :], w=["xt%d" % i])
            S.dma("sp", cm[i][:], dr["cmaskT"][qt], w=["cm%d" % i])
            S.dma("sp", sbias[i][:], dr["selbias"][qt], w=["sbias%d" % i])
            norm_transpose(C, S, xt[i][:], "xt%d" % i, tmpn, P["ident"], [(pT0, "nb0"), (pT1, "nb1")], "n")
            for k in range(8):
                p, pk = ((pT0, "nb0"), (pT1, "nb1"))[k // 4]
                if k % 2 == 0:
                    S.op("dve", lambda e, k=k, p=p: e.tensor_scalar(
                        out=hT[:, k, :], in0=p[:, k % 4, :], scalar1=A[:, L, 0, k:k + 1], scalar2=B[:, L, 0, k:k + 1],
                        op0=ALU.mult, op1=ALU.add), r=[pk, "modA", "modB"], w=["hT"])
                else:
                    S.op("act", lambda e, k=k, p=p: e.activation(
                        out=hT[:, k, :], in_=p[:, k % 4, :], func=AF.Identity, scale=A[:, L, 0, k:k + 1],
                        bias=B[:, L, 0, k:k + 1]), r=[pk, "modA", "modB"], w=["hT"])
            for k in range(8):
                S.op("pe", lambda e, k=k: e.matmul(pq[:], lhsT=hT[:, k, :], rhs=wB[:, k, 0:512], start=(k == 0), stop=(k == 7)),
                     r=["hT", "wB0"], w=["nb2"])
            for k in range(8):
                S.op("pe", lambda e, k=k: e.matmul(bks[0][:, 0:24], lhsT=hT[:, k, :], rhs=wB[:, k, 512:536], start=(k == 0), stop=(k == 7)),
                     r=["hT", "wB1"], w=["nb0"])
            S.op("act", lambda e: e.activation(out=gts[:], in_=bks[0][:, 0:24], func=AF.Exp, scale=-1.0), r=["nb0"], w=[gk])
            S.op("dve", lambda e: e.tensor_scalar(out=gts[:], in0=gts[:], scalar1=1.0, scalar2=None, op0=ALU.add), r=[gk], w=[gk])
            S.op("dve", lambda e: e.reciprocal(out=gts[:], in_=gts[:]), r=[gk], w=[gk])
            rms_gain_rope(C, S, pq[:].rearrange("p (h d) -> p h d", h=8), "nb2", qn[:], "qn", 8, M["gain"][:, 3, :], "gainq",
                          M["csT"][:, T, :], "Tcs", RT, "rq")
            for hh in range(4):
                S.op("pe", lambda e, hh=hh: e.transpose(out=pqT[:, hh, :], in_=qn[:, 2 * hh:2 * hh + 2, :].rearrange("p g d -> p (g d)"),
                                                        identity=P["ident"][:]), r=["qn", "ident"], w=["nb3"])
            for g in range(2):
                rg = slice(g * 64, (g + 1) * 64)
                S.op("dve", lambda e, g=g, rg=rg: e.tensor_copy(out=QT32[rg, g, :, :], in_=pqT[rg, :, :]), r=["nb3"], w=["QT32"])
                S.op("dve", lambda e, g=g, rg=rg: e.tensor_copy(out=QTb[rg, g, :, :], in_=pqT[rg, :, :]), r=["nb3"], w=["QTb"])
            S.op("dve", lambda e: e.memset(ob[:], 0.0), w=[obk])


        qlist = list(qtiles if qtiles is not None else range(NT))
        pro(qlist[0], S)
        for qi, qt in enumerate(qlist):
            T = OWN0 + qt
            i = qt % 2
            gts = gts2[i]
            ob = ob2[i]
            gk = "gts%d" % i
            obk = "ob%d" % i
            def finish_branch(g, br):
                S.op("act", lambda e: e.activation(out=oT[:], in_=pO[0:65, :], func=AF.Copy), r=["nb6"], w=["oT"])
                pXv = pX[:, 0:260].rearrange("p (h d) -> p h d", h=4)
                for hh in range(4):
                    S.op("pe", lambda e, hh=hh: e.transpose(out=pXv[:, hh, :], in_=oT[:, hh * 128:(hh + 1) * 128], identity=P["ident"][0:65, 0:65]),
                         r=["oT", "ident"], w=["nb7"])
                S.op("dve", lambda e: e.tensor_scalar(out=coef[:], in0=pXv[:, :, 64], scalar1=1e-30, scalar2=None, op0=ALU.max), r=["nb7"], w=["coef"])
                S.op("dve", lambda e: e.reciprocal(out=coef[:], in_=coef[:]), r=["coef"], w=["coef"])
                gv = gts[:].rearrange("p (g h b) -> p g h b", g=2, h=4)
                S.op("dve", lambda e: e.tensor_tensor(out=coef[:], in0=coef[:], in1=gv[:, g, :, br], op=ALU.mult), r=["coef", gk], w=["coef"])
                for hh in range(4):
                    c0 = (g * 4 + hh) * 64
                    S.op("dve", lambda e, hh=hh, c0=c0: e.scalar_tensor_tensor(
                        out=ob[:, c0:c0 + 64], in0=pXv[:, hh, 0:64], scalar=coef[:, hh:hh + 1], in1=ob[:, c0:c0 + 64],
                        op0=ALU.mult, op1=ALU.add), r=["nb7", "coef", obk], w=[obk])

            def finish_branch2(g, br, pacc, pacck):
                S.op("act", lambda e: e.activation(out=oT[:], in_=pacc[0:65, :], func=AF.Copy), r=[pacck], w=["oT"])
                pXv = pX[:, 0:260].rearrange("p (h d) -> p h d", h=4)
                for hh in range(4):
                    S.op("pe", lambda e, hh=hh: e.transpose(out=pXv[:, hh, :], in_=oT[:, hh * 128:(hh + 1) * 128], identity=P["ident"][0:65, 0:65]),
                         r=["oT", "ident"], w=["nb7"])
                S.op("dve", lambda e: e.tensor_scalar(out=coef[:], in0=pXv[:, :, 64], scalar1=1e-30, scalar2=None, op0=ALU.max), r=["nb7"], w=["coef"])
                S.op("dve", lambda e: e.reciprocal(out=coef[:], in_=coef[:]), r=["coef"], w=["coef"])
                gv = gts[:].rearrange("p (g h b) -> p g h b", g=2, h=4)
                S.op("dve", lambda e: e.tensor_tensor(out=coef[:], in0=coef[:], in1=gv[:, g, :, br], op=ALU.mult), r=["coef", gk], w=["coef"])
                for hh in range(4):
                    c0 = (g * 4 + hh) * 64
                    S.op("dve", lambda e, hh=hh, c0=c0: e.scalar_tensor_tensor(
                        out=ob[:, c0:c0 + 64], in0=pXv[:, hh, 0:64], scalar=coef[:, hh:hh + 1], in1=ob[:, c0:c0 + 64],
                        op0=ALU.mult, op1=ALU.add), r=["nb7", "coef", obk], w=[obk])

            pI2 = [[(bks[0], "nb0"), (bks[1], "nb1")], [(bks[2], "nb2"), (bks[3], "nb3")]]
            pI = [bks[0], bks[1]]
            for g in range(2):
                for bc in range(4):
                    j = nS % 2
                    nS += 1
                    pc = PcT[bc]
                    pck = "PcT%d" % bc
                    S.op("pe", lambda e, g=g, bc=bc, j=j: e.matmul(pS[j][:], lhsT=M2["KcT"][:, bc * 128:(bc + 1) * 128],
                                                                   rhs=QT32[:, g, :, :], start=True, stop=True),
                         r=["KcT", "QT32"], w=["nb%d" % (4 + j)])
                    S.op("act", lambda e, bc=bc, j=j, pc=pc: e.activation(out=pc[:].rearrange("p a b -> p (a b)"), in_=pS[j][:], func=AF.Exp,
                                                                          scale=0.125, bias=cbC[:, bc:bc + 1]),
                         r=["nb%d" % (4 + j), "cbC"], w=[pck])
                    S.op("dve", lambda e, bc=bc, pc=pc, i=i: e.tensor_tensor(out=pc[:], in0=pc[:],
                                                                             in1=cm[i][:, bc, :].unsqueeze(1).to_broadcast([128, 4, 128]), op=ALU.mult),
                         r=[pck, "cm%d" % i], w=[pck])
                for bc in range(4):
                    pc = PcT[bc]
                    pck = "PcT%d" % bc
                    S.op("pe", lambda e, g=g, bc=bc, pc=pc: e.matmul(pO[0:65, :], lhsT=M2["Vc"][:, bc, g, :], rhs=pc[:].rearrange("p a b -> p (a b)"),
                                                                     start=(bc == 0), stop=(bc == 3)), r=[pck, "Vc", "Vc1"], w=["nb6"])
                for hh in range(4):
                    pi, pik = pI2[g][hh // 2]
                    for bc in range(4):
                        pc = PcT[bc]
                        pck = "PcT%d" % bc
                        S.op("pe", lambda e, bc=bc, pc=pc, hh=hh, pi=pi: e.matmul(
                            pi[:, (hh % 2) * 129:(hh % 2) * 129 + 129], lhsT=pc[:, hh, :], rhs=cover[:, bc, :],
                            start=(bc == 0), stop=(bc == 3)), r=[pck, "cover"], w=[pik])
                finish_branch2(g, 0, pO, "nb6")
            for g in range(2):
                for hh in range(4):
                    pi, pik = pI2[g][hh // 2]
                    S.op("dve", lambda e, hh=hh, pi=pi: e.tensor_scalar(out=rden[:, hh:hh + 1], in0=pi[:, (hh % 2) * 129 + 128:(hh % 2) * 129 + 129],
                                                                        scalar1=1e-30, scalar2=None, op0=ALU.max), r=[pik], w=["rden"])
                S.op("dve", lambda e: e.reciprocal(out=rden[:], in_=rden[:]), r=["rden"], w=["rden"])
                for hh in range(4):
                    pi, pik = pI2[g][hh // 2]
                    src = pi[:, (hh % 2) * 129:(hh % 2) * 129 + 128]
                    if hh == 0:
                        S.op("dve", lambda e, src=src: e.scalar_tensor_tensor(out=acc[:], in0=src, scalar=rden[:, 0:1], in1=sbias[i][:],
                                                                              op0=ALU.mult, op1=ALU.add), r=[pik, "rden", "sbias%d" % i], w=["acc"])
                    else:
                        S.op("dve", lambda e, src=src, hh=hh: e.scalar_tensor_tensor(out=acc[:], in0=src, scalar=rden[:, hh:hh + 1], in1=acc[:],
                                                                                     op0=ALU.mult, op1=ALU.add), r=[pik, "rden", "acc"], w=["acc"])
                S.op("dve", lambda e: e.tensor_tensor(out=acc[:], in0=acc[:], in1=corebias[:], op=ALU.add), r=["acc", "corebias"], w=["acc"])
                S.op("dve", lambda e: e.max(out=m8a[:], in_=acc[:]), r=["acc"], w=["m8a"])
                S.op("dve", lambda e: e.match_replace(out=sc2[:], in_to_replace=m8a[:], in_values=acc[:], imm_value=-3e38),
                     r=["acc", "m8a"], w=["sc2"])
                S.op("dve", lambda e: e.max(out=m8b[:], in_=sc2[:]), r=["sc2"], w=["m8b"])
                S.op("dve", lambda e: e.tensor_scalar(out=selm[:], in0=acc[:], scalar1=m8b[:, 7:8], scalar2=None, op0=ALU.is_ge),
                     r=["acc", "m8b"], w=["selm"])
                S.op("dve", lambda e: e.tensor_scalar(out=selv[:], in0=acc[:], scalar1=-1e29, scalar2=None, op0=ALU.is_gt), r=["acc"], w=["selv"])
                S.op("dve", lambda e: e.tensor_tensor(out=selm[:], in0=selm[:], in1=selv[:], op=ALU.mult), r=["selm", "selv"], w=["selm"])
                if dbg is not None and "selm" in dbg:
                    S.dma("sp", dbg["selm"][qt, g], selm[:], r=["selm"], w=["dbg_selm"])
                S.op("pe", lambda e: e.transpose(out=pX[:, 384:512], in_=selm[:], identity=P["ident"][:]), r=["selm", "ident"], w=["nb7"])
                S.op("dve", lambda e, g=g: e.tensor_scalar(out=NBt2[g][:], in0=pX[:, 384:512].unsqueeze(1).to_broadcast([128, 4, 128]), scalar1=-1.0,
                                                           scalar2=-NEGB, op0=ALU.add, op1=ALU.mult), r=["nb7"], w=["NBt%d" % g])
            def finish_tail(g, br, src):
                pXv = pX[:, 0:260].rearrange("p (h d) -> p h d", h=4)
                srck = "oTs%d%d" % (g, br)
                for hh in range(4):
                    S.op("pe", lambda e, hh=hh: e.transpose(out=pXv[:, hh, :], in_=src[:, hh * 128:(hh + 1) * 128], identity=P["ident"][0:65, 0:65]),
                         r=[srck, "ident"], w=["nb7"])
                S.op("dve", lambda e: e.tensor_scalar(out=coef[:], in0=pXv[:, :, 64], scalar1=1e-30, scalar2=None, op0=ALU.max), r=["nb7"], w=["coef"])
                S.op("dve", lambda e: e.reciprocal(out=coef[:], in_=coef[:]), r=["coef"], w=["coef"])
                gv = gts[:].rearrange("p (g h b) -> p g h b", g=2, h=4)
                S.op("dve", lambda e: e.tensor_tensor(out=coef[:], in0=coef[:], in1=gv[:, g, :, br], op=ALU.mult), r=["coef", gk], w=["coef"])
                for hh in range(4):
                    c0 = (g * 4 + hh) * 64
                    S.op("dve", lambda e, hh=hh, c0=c0: e.scalar_tensor_tensor(
                        out=ob[:, c0:c0 + 64], in0=pXv[:, hh, 0:64], scalar=coef[:, hh:hh + 1], in1=ob[:, c0:c0 + 64],
                        op0=ALU.mult, op1=ALU.add), r=["nb7", "coef", obk], w=[obk])

            def stream(g, R):
                psb = [(bks[4], "nb4"), (bks[5], "nb5")] if g == 0 else [(bks[2], "nb2"), (bks[3], "nb3")]
                pOW, pOWk = (bks[6], "nb6") if g == 0 else (bks[1], "nb1")
                chunks = [("sel", kc) for kc in range(T + 1)] + [("win", wi) for wi in range(5)]
                nck = len(chunks)

                def emit_S(ci):
                    kind, a_ = chunks[ci]
                    ps, psk = psb[ci % 2]
                    if kind == "sel":
                        kc = a_
                        last = (kc == T)
                        R.op("pe", lambda e: e.matmul(ps[:], lhsT=M["KselT"][:, kc * 128:(kc + 1) * 128], rhs=QTb[:, g, :, :], start=True, stop=False),
                             r=["KselT%d" % kc, "QTb"], w=[psk])
                        R.op("pe", lambda e: e.matmul(ps[:], lhsT=Esel[:, kc, :], rhs=NBt2[g][:], start=False, stop=(not last)),
                             r=["Esel", "NBt%d" % g], w=[psk])
                        if last:
                            R.op("pe", lambda e: e.matmul(ps[:], lhsT=identb[:], rhs=causb[:], start=False, stop=True),
                                 r=["identb", "causb"], w=[psk])
                    else:
                        wi = a_
                        kc = T - 4 + wi
                        edge = wi in (0, 4)
                        R.op("pe", lambda e: e.matmul(ps[:], lhsT=M["KwinT"][:, (kc - 44) * 128:(kc - 43) * 128], rhs=QTb[:, g, :, :],
                                                      start=True, stop=(not edge)), r=["KwinT%d" % (kc - 44), "QTb"], w=[psk])
                        if edge:
                            mb = winb if wi == 0 else causb
                            R.op("pe", lambda e: e.matmul(ps[:], lhsT=identb[:], rhs=mb[:], start=False, stop=True),
                                 r=["identb", "causb", "winb"], w=[psk])

                def emit_PV(ci):
                    kind, a_ = chunks[ci]
                    ps, psk = psb[ci % 2]
                    pt_ = PTs[g][ci % 3]
                    ptk = "PT%d_%d" % (g, ci % 3)
                    kc = a_ if kind == "sel" else T - 4 + a_
                    R.op("act", lambda e: e.activation(out=pt_[:], in_=ps[:], func=AF.Exp, scale=0.125, bias=kbC[:, kc:kc + 1]),
                         r=[psk, "kbC"], w=[ptk])
                    if kind == "sel":
                        R.op("pe", lambda e: e.matmul(pOW[0:65, :], lhsT=M["Vsel"][:, kc, g, :], rhs=pt_[:], start=(kc == 0), stop=(kc == T)),
                             r=[ptk, "Vsel%d" % kc, "Vsel1"], w=[pOWk])
                        if kc == T:
                            R.op("act", lambda e: e.activation(out=oTs[g][0][:], in_=pOW[0:65, :], func=AF.Copy), r=[pOWk], w=["oTs%d0" % g])
                    else:
                        R.op("pe", lambda e: e.matmul(pOW[0:65, :], lhsT=M["Vwin"][:, kc - 44, g, :], rhs=pt_[:], start=(a_ == 0), stop=(a_ == 4)),
                             r=[ptk, "Vwin%d" % (kc - 44), "Vwin1"], w=[pOWk])
                        if a_ == 4:
                            R.op("act", lambda e: e.activation(out=oTs[g][1][:], in_=pOW[0:65, :], func=AF.Copy), r=[pOWk], w=["oTs%d1" % g])

                emit_S(0)
                for ci in range(nck):
                    if ci + 1 < nck:
                        emit_S(ci + 1)
                    emit_PV(ci)

            r0, r1 = Rec(), Rec()
            stream(0, r0)
            stream(1, r1)
            interleave(S, r0, r1)
            S_main = S
            S = Rec()
            for g in range(2):
                finish_tail(g, 1, oTs[g][0])
                finish_tail(g, 2, oTs[g][1])
            if dbg is not None and "o_b" in dbg:
                S.dma("sp", dbg["o_b"][qt * 128:(qt + 1) * 128, :], ob[:], r=[obk], w=["dbg_ob"])
            S.op("act", lambda e: e.activation(out=obb[:], in_=ob[:], func=AF.Copy), r=[obk], w=["obb"])
            pmx = bks[7][:, 0:256].bitcast(BF16).rearrange("p (a b) -> p a b", a=4)
            for hh in range(4):
                S.op("pe", lambda e, hh=hh: e.transpose(out=pmx[:, hh, :], in_=obb[:, hh * 128:(hh + 1) * 128], identity=identb[:]),
                     r=["obb", "identb"], w=["nb7"])
            S.op("act", lambda e, qt=qt: e.activation(out=M["mixT"][:, 4:8, qt * 128:(qt + 1) * 128], in_=pmx, func=AF.Copy),
                 r=["nb7"], w=["mixTb%d" % qt])
            tail = S
            S = S_main
            nxt = Rec()
            if qi + 1 < len(qlist):
                pro(qlist[qi + 1], nxt)
            interleave(S, tail, nxt)
        if dbg is not None:
            S.wait_all("sp", ["dbg_ob", "dbg_selm"])
        S.flush()


def phase_outproj(C, P, M, dr, x_out):
    nc, S = C.nc, C.S
    L = 0
    with contextlib.ExitStack() as st:
        wo = C.sb(st, "wo", [128, 8, D], BF16)
        xt = [C.sb(st, "xt%d" % i, [128, D], F32) for i in range(2)]
        xo = [C.sb(st, "xo%d" % i, [128, D], F32) for i in range(2)]
        ev = C.sb(st, "ev", [128, 512], F32)
        py = [bank(C, st, "opy%d" % i) for i in range(2)]
        S.dma("pool", wo[:], dr["w_out_ab"].rearrange("(k p) n -> p k n", p=128), w=["wo"])
        gate = P["gates"]
        for t in range(NT):
            i = t % 2
            S.dma("sp", xt[i][:], dr["xw"][(OWN0 + t) * 128:(OWN0 + t + 1) * 128, :], w=["xt%d" % i])
            for half in range(2):
                for k in range(8):
                    S.op("pe", lambda e, k=k, half=half, t=t: e.matmul(py[half][:], lhsT=M["mixT"][:, k, t * 128:(t + 1) * 128],
                                                                       rhs=wo[:, k, half * 512:(half + 1) * 512], start=(k == 0), stop=(k == 7)),
                         r=["wo", "mixTa%d" % t, "mixTb%d" % t], w=["opy%d" % half])
                S.op("dve", lambda e, half=half: e.tensor_tensor(out=ev[:], in0=py[half][:], in1=gate[:, L, 0, half * 512:(half + 1) * 512], op=ALU.mult),
                     r=["opy%d" % half, "gates"], w=["ev"])
                S.op("dve", lambda e, half=half, i=i: e.tensor_tensor(out=xo[i][:, half * 512:(half + 1) * 512], in0=ev[:],
                                                                      in1=xt[i][:, half * 512:(half + 1) * 512], op=ALU.add),
                     r=["ev", "xt%d" % i], w=["xo%d_%d" % (i, half)])
            S.dma("sp", x_out[t * 128:(t + 1) * 128, :], xo[i][:], r=["xo%d_0" % i, "xo%d_1" % i], w=["xo_%s_%d" % (x_out.name, i)])
        S.wait_all("sp", ["xo_%s_%d" % (x_out.name, j) for j in range(2)])
        S.flush()


IN_SPECS = [("ident", [128, 128], F32), ("c_col", [128, 8], F32), ("b_ada_col", [128, 2, 48], F32), ("norm_g_col", [128, 2, 2, 8], F32),
            ("w_ada", [2, 1024, 6144], F32), ("b_ada", [2, 6144], F32),
            ("xw", [8192, 1024], F32), ("w_in_ab", [1024, 2856], F32), ("gla_w_gate2", [16, 256], F32), ("gla_b_gate_col", [128, 2], F32),
            ("gla_norm_g", [128], F32), ("gla_maskT", [128, 4, 128], F32), ("valid_col", [128, 64], F32), ("invf", [8], F32),
            ("nsa_k_gain", [3, 64], F32), ("nsa_q_gain", [64], F32), ("pos_col", [128, 64], I32),
            ("nsa_cmp_w1", [2, 2048, 256], F32), ("nsa_cmp_w2", [2, 256, 64], F32), ("cmp_pe_col", [128, 2, 16], F32),
            ("posb_col", [128, 4], I32), ("cvalid_col", [128, 4], F32), ("w_nq_perm", [1024, 512], F32), ("Esel", [128, 64, 128], BF16),
            ("cover", [128, 4, 129], F32), ("corebias", [128, 128], F32), ("causb", [128, 4, 128], BF16), ("winb", [128, 4, 128], BF16),
            ("cmaskT", [16, 128, 4, 128], F32), ("selbias", [16, 128, 128], F32), ("w_out_ab", [1024, 1024], F32),
            ("w_in_c", [1024, 4096], F32), ("w_out_c", [2048, 1024], F32), ("sgu_w_s", [8, 128, 128], F32), ("sgu_b_s", [8, 128], F32),
            ("sgu_norm_g", [2048], F32), ("tril", [128, 128], F32),
            ("w_router", [1024, 16], F32), ("router_bias", [16], F32),
            ("w_exp", [2, 16, 128, 12288], F32)]


def build_full(upto=4):
    C = Ctx()
    dr = {n: C.dram_in(n, s, dt) for n, s, dt in IN_SPECS}
    dr["cmp_scr"] = C.dram_tmp("cmp_scr", [2, 128, 8192], BF16)
    xs = [C.dram_tmp("xs%d" % i, [TOK, D], F32) for i in range(3)]
    y = C.dram_out("y", [TOK, D])
    dst = lambda i: y if upto == i + 1 else xs[i]
    P = alloc_persistent(C)
    phase_init(C, P, dr)
    phase_ada(C, P, dr)
    mst = contextlib.ExitStack()
    M = alloc_mixer(C, mst)
    phase_mix_a(C, P, M, dr)
    M2 = alloc_cmp(C, mst)
    phase_cmp(C, P, M, M2, dr)
    phase_nsa(C, P, M, M2, dr)
    phase_outproj(C, P, M, dr, dst(0))
    mst.close()
    if upto >= 2:
        phase_moe(C, P, dr, 0, xs[0], dst(1))
    if upto >= 3:
        phase_gmlp(C, P, dr, xs[1], dst(2))
    if upto >= 4:
        phase_moe(C, P, dr, 1, xs[2], y)
    C.outer.close()
    return C.nc


def _col(v):
    return np.ascontiguousarray(np.asarray(v).reshape(-1, 128).T)


def _shared_inputs(d):
    f32 = np.float32
    sh = {}
    sh["ident"] = np.eye(128, dtype=f32)
    sh["b_ada_col"] = np.ascontiguousarray(d["b_ada"].reshape(2, 48, 128).transpose(2, 0, 1))
    sh["norm_g_col"] = np.ascontiguousarray(d["norm_g"].reshape(2, 2, 8, 128).transpose(3, 0, 1, 2))
    sh["w_ada"] = d["w_ada"]
    sh["b_ada"] = d["b_ada"]
    w_in = d["w_in_ab"][0]
    sh["w_in_ab"] = w_in
    sh["gla_w_gate2"] = d["gla_w_gate2"][0]
    sh["gla_b_gate_col"] = _col(d["gla_b_gate"][0])
    sh["gla_norm_g"] = d["gla_norm_g"][0]
    j = np.arange(128)[:, None]
    i = np.arange(128)[None, :]
    m = ((j // 64 == i // 64) & (j <= i)).astype(f32)
    sh["gla_maskT"] = np.ascontiguousarray(np.repeat(m[:, None, :], 4, axis=1))
    sh["invf"] = (f32(500000.0) ** (-np.arange(8, dtype=f32) / f32(8))).astype(f32)
    sh["nsa_k_gain"] = d["nsa_k_gain"][0]
    sh["nsa_q_gain"] = d["nsa_q_gain"][0]
    sh["nsa_cmp_w1"] = d["nsa_cmp_w1"][0]
    sh["nsa_cmp_w2"] = d["nsa_cmp_w2"][0]
    sh["cmp_pe_col"] = np.ascontiguousarray(d["nsa_cmp_pe"][0].reshape(2, 16, 128).transpose(2, 0, 1))
    nq = w_in[:, 1552:2064].reshape(1024, 2, 4, 64)
    sh["w_nq_perm"] = np.ascontiguousarray(nq.transpose(0, 2, 1, 3).reshape(1024, 512))
    E = np.zeros((128, 64, 128), f32)
    for c in range(64):
        E[2 * c, c, 0:64] = 1.0
        E[2 * c + 1, c, 64:128] = 1.0
    sh["Esel"] = E.astype(ml_dtypes.bfloat16)
    blk = np.arange(512)
    jj = np.arange(128)
    cov = np.zeros((512, 129), f32)
    cov[:, :128] = ((16 * blk[:, None] < 64 * jj[None, :] + 64) & (16 * blk[:, None] + 32 > 64 * jj[None, :])).astype(f32)
    cov[:, 128] = 1.0
    cov[511] = 0.0
    sh["cover"] = np.ascontiguousarray(cov.reshape(4, 128, 129).transpose(1, 0, 2))
    cm = np.zeros((16, 128, 4, 128), f32)
    sb = np.zeros((16, 128, 128), f32)
    ii = np.arange(128)
    for qt in range(16):
        wq = (OWN0 + qt) * 128 + ii
        for bc in range(4):
            b_ = bc * 128 + np.arange(128)
            cm[qt, :, bc, :] = ((16 * b_ + 31)[:, None] <= wq[None, :]).astype(f32)
        cur = wq // 64
        forced = (jj[None, :] == cur[:, None]) | (jj[None, :] == cur[:, None] - 1)
        inval = jj[None, :] > cur[:, None]
        sb[qt] = np.where(inval, -1e30, np.where(forced, 1e4, 0.0))
    sh["cmaskT"] = cm
    sh["selbias"] = sb
    caus = np.where(j <= i, 0.0, NEGB).astype(f32)
    win = np.where(j > i, 0.0, NEGB).astype(f32)
    rep4 = lambda a: np.ascontiguousarray(np.repeat(a[:, None, :], 4, axis=1))
    sh["causb"] = rep4(caus).astype(ml_dtypes.bfloat16)
    sh["winb"] = rep4(win).astype(ml_dtypes.bfloat16)
    sh["w_out_ab"] = d["w_out_ab"][0]
    sh["w_in_c"] = d["w_in_c"][0]
    sh["w_out_c"] = d["w_out_c"][0]
    sh["sgu_w_s"] = d["sgu_w_s"][0]
    sh["sgu_b_s"] = d["sgu_b_s"][0]
    sh["sgu_norm_g"] = d["sgu_norm_g"][0]
    sh["tril"] = np.tril(np.ones((128, 128), f32))
    for k in ("w_router", "router_bias"):
        sh[k] = d[k]
    wg_ = d["w_gate"].reshape(2, 16, 8, 128, 512).transpose(0, 1, 3, 2, 4).reshape(2, 16, 128, 4096)
    wu_ = d["w_up"].reshape(2, 16, 8, 128, 512).transpose(0, 1, 3, 2, 4).reshape(2, 16, 128, 4096)
    wd_ = d["w_down"].reshape(2, 16, 4, 128, 1024).transpose(0, 1, 3, 2, 4).reshape(2, 16, 128, 4096)
    sh["w_exp"] = np.ascontiguousarray(np.concatenate([wg_, wu_, wd_], axis=-1))
    return sh


def _core_inputs(d, core):
    f32 = np.float32
    b, qtr = core // 4, core % 4
    own = qtr * TOK
    a = np.arange(8192) - 6144 + own
    ok = a >= 0
    xw = np.zeros((8192, D), f32)
    xw[ok] = d["x"][b, a[ok]]
    pos = np.zeros(8192, np.int32)
    pos[ok] = d["positions"][b, a[ok]]
    co = {"xw": xw, "pos_col": _col(pos), "valid_col": _col(ok.astype(f32)), "c_col": _col(d["c"][b])}
    first_blk = 96 - 32 * qtr
    jj = np.arange(128)
    row = np.where(jj < first_blk, -1e30, np.where(jj == first_blk, 1e4, 0.0)).astype(f32)
    co["corebias"] = np.ascontiguousarray(np.broadcast_to(row, (128, 128)))
    blk = np.arange(512)
    a_start = 16 * blk - 6144 + own
    cvalid = ((a_start >= 0) & (blk <= 510))
    a_end = np.clip(16 * blk + 31 - 6144 + own, 0, 8191)
    posb = np.where(cvalid, d["positions"][b, a_end], 0).astype(np.int32)
    co["cvalid_col"] = _col(cvalid.astype(f32))
    co["posb_col"] = _col(posb)
    return co


_NC_CACHE = {}


def kernel(**inputs):
    d = {k: np.asarray(v) for k, v in inputs.items()}
    if "nc" not in _NC_CACHE:
        _NC_CACHE["nc"] = build_full()
    nc = _NC_CACHE["nc"]
    sh = _shared_inputs(d)
    in_maps = []
    for core in range(NCORES):
        m = dict(sh)
        m.update(_core_inputs(d, core))
        in_maps.append(m)
    res = run_bass_kernel_spmd(nc, in_maps, core_ids=list(range(NCORES)))
    out = np.zeros((2, 8192, D), np.float32)
    for core in range(NCORES):
        b, qtr = core // 4, core % 4
        out[b, qtr * TOK:(qtr + 1) * TOK] = res.results[core]["y"]
    return out
```

```python
import contextlib
import types
import numpy as np
import ml_dtypes
import concourse.bass as bass
import concourse.mybir as mybir
from concourse.bass_utils import run_bass_kernel_spmd

F32 = mybir.dt.float32
BF16 = mybir.dt.bfloat16
I32 = mybir.dt.int32
AF = mybir.ActivationFunctionType
ALU = mybir.AluOpType
AX = mybir.AxisListType

ENGS = ("pe", "dve", "act", "pool", "sp")
NCORES = 8
D = 1024
NT = 16
TOK = 2048
EPS = 1e-6


PSUM_KEYS = set(["b%d" % i for i in range(8)] + ["nb%d" % i for i in range(8)] +
                ["cph0", "cph1", "cpb", "cpo0", "cpo1", "cpt", "pT0", "pT1", "pg0", "pg1", "pu0", "pu1", "py0", "py1",
                 "pv0", "pv1", "pm0", "pm1", "pcol", "prow0", "prow1", "opy0", "opy1"] + ["gb%d" % i for i in range(8)])


class Sched:
    def __init__(self, nc, stack):
        self.nc = nc
        self.stack = stack
        self.q = {e: [] for e in ENGS}
        self.last_w = {}
        self.readers = {}
        self.dma_sems = {}
        self.esem = {e: stack.enter_context(nc.semaphore("s_" + e)) for e in ENGS}
        self.ecount = {e: 0 for e in ENGS}
        self.seen = {e: {} for e in ENGS}
        self.phase_end = {}
        self.phase = 0
        self.alias = {}
        self.slot_map = {}
        self.sem_pool = []

    def _deps(self, eng, r, w):
        deps = []
        r = [self.alias.get(k, k) for k in r]
        w = [self.alias.get(k, k) for k in w]
        for k in r:
            t = self.last_w.get(k)
            if t is not None:
                deps.append(t)
            if k in PSUM_KEYS:
                deps.extend(x for x in self.readers.get(k, ()) if not (x[0] == "eng" and x[1] == eng))
        for k in w:
            t = self.last_w.get(k)
            if t is not None:
                deps.append(t)
            deps.extend(self.readers.get(k, ()))
        best = {}
        for t in deps:
            key = (t[0], t[1], t[3] if t[0] == "eng" else 0)
            if key not in best or best[key] < t[2]:
                best[key] = t[2]
        out = []
        for (kind, name, ph), v in best.items():
            if kind == "eng" and name == eng and (eng == "pe" or not Sched.same_engine_sync):
                continue
            out.append((kind, name, v, ph))
        return out

    def _commit(self, tok, r, w):
        r = [self.alias.get(k, k) for k in r]
        w = [self.alias.get(k, k) for k in w]
        for k in w:
            self.last_w[k] = tok
            self.readers[k] = []
        for k in r:
            self.readers.setdefault(k, []).append(tok)

    limit = None
    count = 0
    recycle = True
    sim_mode = False
    uniq = 0
    same_engine_sync = True

    @staticmethod
    def _freeze(fn):
        if fn.__closure__ is None:
            return fn
        cells = tuple(types.CellType(c.cell_contents) for c in fn.__closure__)
        g = types.FunctionType(fn.__code__, fn.__globals__, fn.__name__, fn.__defaults__, cells)
        g.__kwdefaults__ = fn.__kwdefaults__
        return g

    def op(self, eng, fn, r=(), w=()):
        fn = self._freeze(fn)
        Sched.count += 1
        if Sched.limit is not None and Sched.count > Sched.limit:
            return
        deps = self._deps(eng, r, w)
        idx = len(self.q[eng])
        self.q[eng].append(dict(kind="op", fn=fn, deps=deps))
        self._commit(("eng", eng, idx, self.phase), r, w)

    def dma(self, eng, out, in_, r=(), w=(), slot=None, **kw):
        Sched.count += 1
        if Sched.limit is not None and Sched.count > Sched.limit:
            return
        deps = self._deps(eng, r, w)
        if slot is None:
            slot = w[0]
        if eng == "pool":
            Sched.uniq += 1
            sid = "sw%d" % Sched.uniq
            self.dma_sems[sid] = [self.stack.enter_context(self.nc.semaphore(sid)), 0]
            slot = sid
        else:
            if slot not in self.slot_map:
                n = len(self.slot_map)
                if n >= len(self.sem_pool):
                    self.sem_pool.append(n)
                    self.dma_sems[n] = [self.stack.enter_context(self.nc.semaphore("d%d" % n)), 0]
                self.slot_map[slot] = n
            slot = self.slot_map[slot]
        self.dma_sems[slot][1] += 16
        val = self.dma_sems[slot][1]
        self.q[eng].append(dict(kind="dma", out=out, in_=in_, deps=deps, slot=slot, kw=kw))
        self._commit(("dma", slot, val, 0), r, w)

    def wait_all(self, eng, keys):
        deps = self._deps(eng, keys, ())
        self.q[eng].append(dict(kind="wait", deps=deps))

    def flush(self, barrier=True):
        nc = self.nc
        ph = self.phase
        miles = {e: set() for e in ENGS}
        for e in ENGS:
            for o in self.q[e]:
                for (kind, name, v, p) in o["deps"]:
                    if kind == "eng" and p == ph:
                        miles[name].add(v)
        for e in ENGS:
            n = len(self.q[e])
            if n:
                last = max(i for i, o in enumerate(self.q[e]) if o["kind"] != "wait") if any(
                    o["kind"] != "wait" for o in self.q[e]) else None
                if last is not None and self.q[e][last]["kind"] == "op":
                    miles[e].add(last)
        rank = {}
        for e in ENGS:
            for i, v in enumerate(sorted(miles[e])):
                rank[(e, v)] = self.ecount[e] + i + 1
        prev_end = dict(self.phase_end)
        prev_dma = dict(getattr(self, 'dma_totals', {}))
        with nc.Block() as block:
            engobj = {"pe": block.tensor, "dve": block.vector, "act": block.scalar,
                      "pool": block.gpsimd, "sp": block.sync}

            def make(ename):
                def body(eng):
                    seen = self.seen[ename]

                    def wait(sem, key, v):
                        if seen.get(key, 0) >= v:
                            return
                        eng.wait_ge(sem, v)
                        seen[key] = v
                    if barrier:
                        for oe, v in prev_end.items():
                            if oe != ename and v > 0:
                                wait(self.esem[oe], oe, v)
                        for slot, tot in prev_dma.items():
                            wait(self.dma_sems[slot][0], "d:%s" % (slot,), tot)
                    for i, o in enumerate(self.q[ename]):
                        for (kind, name, v, p) in o["deps"]:
                            if kind == "eng":
                                if p == ph:
                                    if name == ename:
                                        wait(self.esem[name], name, rank[(name, v)])
                                    else:
                                        wait(self.esem[name], name, rank[(name, v)])
                                else:
                                    if name != ename and prev_end.get(name, 0) > 0:
                                        wait(self.esem[name], name, prev_end[name])
                            else:
                                wait(self.dma_sems[name][0], "d:%s" % (name,), v)
                        if o["kind"] == "op":
                            ins = o["fn"](eng)
                            if i in miles[ename]:
                                ins.then_inc(self.esem[ename], 1)
                        elif o["kind"] == "dma":
                            ins = eng.dma_start(out=o["out"], in_=o["in_"], **o["kw"])
                            ins.then_inc(self.dma_sems[o["slot"]][0], 16)
                return body
            for e in ENGS:
                if self.q[e] or barrier:
                    engobj[e](make(e))
        for e in ENGS:
            self.ecount[e] += len(miles[e])
            self.phase_end[e] = self.ecount[e]
            self.q[e] = []
        self.phase += 1
        self.dma_totals = {slot: v[1] for slot, v in self.dma_sems.items()}
        if Sched.recycle:
            self.slot_map = {}


class Rec:
    def __init__(self):
        self.items = []

    def op(self, eng, fn, r=(), w=()):
        self.items.append(("op", eng, Sched._freeze(fn), tuple(r), tuple(w), None))

    def dma(self, eng, out, in_, r=(), w=(), slot=None, **kw):
        self.items.append(("dma", eng, (out, in_, slot, kw), tuple(r), tuple(w), None))


def interleave(S, a, b):
    ia = ib = 0
    na, nb = len(a.items), len(b.items)
    while ia < na or ib < nb:
        take_a = ib >= nb or (ia < na and ia * nb <= ib * na)
        it = a.items[ia] if take_a else b.items[ib]
        if take_a:
            ia += 1
        else:
            ib += 1
        if it[0] == "op":
            S.op(it[1], it[2], r=it[3], w=it[4])
        else:
            out, in_, slot, kw = it[2]
            S.dma(it[1], out, in_, r=it[3], w=it[4], slot=slot, **kw)


class Ctx:
    def __init__(self):
        self.nc = bass.Bass("TRN2", target_bir_lowering=False)
        self.outer = contextlib.ExitStack()
        self.S = Sched(self.nc, self.outer)
        self.uid = 0

    def dram_in(self, name, shape, dt=F32):
        return self.nc.dram_tensor(name, list(shape), dt, kind="ExternalInput").ap()

    def dram_out(self, name, shape, dt=F32):
        return self.nc.dram_tensor(name, list(shape), dt, kind="ExternalOutput").ap()

    def dram_tmp(self, name, shape, dt=F32):
        return self.nc.dram_tensor(name, list(shape), dt, kind="Internal").ap()

    def sb(self, stack, name, shape, dt):
        self.uid += 1
        return stack.enter_context(self.nc.sbuf_tensor("%s_%d" % (name, self.uid), list(shape), dt))

    def ps(self, stack, name, shape, dt=F32):
        self.uid += 1
        return stack.enter_context(self.nc.psum_tensor("%s_%d" % (name, self.uid), list(shape), dt))


def norm_transpose(C, S, xt, xkey, tmp, ident, pTs, tag):
    junk, ss, rs, xn = tmp["junk"], tmp["ss"], tmp["rs"], tmp["xn"]
    kj, kss, krs, kxn = [tag + s for s in ("junk", "ss", "rs", "xn")]
    S.op("act", lambda e: e.activation(out=junk[:], in_=xt, func=AF.Square, accum_out=ss[:]),
         r=[xkey], w=[kj, kss])
    if tmp.get("lnexp"):
        S.op("act", lambda e: e.activation(out=rs[:], in_=ss[:], func=AF.Ln, scale=1.0 / D, bias=tmp["eps"][:]),
             r=[kss], w=[krs])
        S.op("act", lambda e: e.activation(out=rs[:], in_=rs[:], func=AF.Exp, scale=-0.5), r=[krs], w=[krs])
    else:
        S.op("act", lambda e: e.activation(out=rs[:], in_=ss[:], func=AF.Sqrt, scale=1.0 / D, bias=tmp["eps"][:]),
             r=[kss], w=[krs])
        S.op("dve", lambda e: e.reciprocal(out=rs[:], in_=rs[:]), r=[krs], w=[krs])
    S.op("dve", lambda e: e.tensor_scalar(out=xn[:], in0=xt, scalar1=rs[:, 0:1], scalar2=None, op0=ALU.mult),
         r=[xkey, krs], w=[kxn])
    for k in range(8):
        p, pk = pTs[k // 4]
        S.op("pe", lambda e, k=k, p=p: e.transpose(out=p[:, k % 4, :], in_=xn[:, k * 128:(k + 1) * 128],
                                                   identity=ident[:]),
             r=[kxn, "ident"], w=[pk])


def phase_ada(C, P, dr):
    nc, S = C.nc, C.S
    with contextlib.ExitStack() as st:
        ccol = C.sb(st, "ccol", [128, 8], F32)
        cond = C.sb(st, "cond", [128, 8], F32)
        condbc = C.sb(st, "condbc", [128, 8, 128], F32)
        ones = C.sb(st, "ones", [128, 128], F32)
        bcol = C.sb(st, "bcol", [128, 2, 48], F32)
        gcol = C.sb(st, "gcol", [128, 2, 2, 8], F32)
        mcol = C.sb(st, "mcol", [128, 2, 48], F32)
        wblk = [C.sb(st, "wblk%d" % i, [128, 8, 512], F32) for i in range(2)]
        brow = [C.sb(st, "brow%d" % i, [128, 512], F32) for i in range(2)]
        pcol = C.ps(st, "pcol", [128, 512], F32)
        prow = [C.ps(st, "prow%d" % i, [128, 512], F32) for i in range(2)]
        S.dma("sp", ccol[:], dr["c_col"], w=["ccol"])
        S.dma("sp", bcol[:], dr["b_ada_col"], w=["bcol"])
        S.dma("sp", gcol[:], dr["norm_g_col"], w=["gcol"])
        S.op("act", lambda e: e.activation(out=cond[:], in_=ccol[:], func=AF.Silu), r=["ccol"], w=["cond"])
        S.op("dve", lambda e: e.memset(ones[:], 1.0), w=["ones"])
        for k in range(8):
            S.op("dve", lambda e, k=k: e.tensor_scalar(out=condbc[:, k, :], in0=ones[:], scalar1=cond[:, k:k + 1],
                                                       scalar2=None, op0=ALU.mult),
                 r=["ones", "cond"], w=["condbc"])
        nrow = 0
        modrow = [C.sb(st, "modrow%d" % i, [128, 512], F32) for i in range(2)]
        pcT = C.ps(st, "pcT", [128, 4, 128], F32)
        for l in range(2):
            for nb in range(12):
                i = (l * 12 + nb) % 2
                for hk_ in range(2):
                    S.dma("sp" if hk_ == 0 else "act", wblk[i][:, hk_ * 4:(hk_ + 1) * 4, :],
                          dr["w_ada"][l, hk_ * 512:(hk_ + 1) * 512, nb * 512:(nb + 1) * 512].rearrange("(k p) n -> p k n", p=128),
                          w=["wblk%d_%d" % (i, hk_)])
                sub6 = nb // 2
                j = nrow % 2
                nrow += 1
                S.dma("sp", brow[j][:], dr["b_ada"][l, nb * 512:(nb + 1) * 512].partition_broadcast(128), w=["brow%d" % j])
                for k in range(8):
                    S.op("pe", lambda e, k=k, i=i, j=j: e.matmul(prow[j][:], lhsT=condbc[:, k, :], rhs=wblk[i][:, k, :],
                                                                 start=(k == 0), stop=(k == 7)),
                         r=["condbc", "wblk%d_0" % i, "wblk%d_1" % i], w=["prow%d" % j])
                if sub6 in (2, 5):
                    gs = 0 if sub6 == 2 else 1
                    half = nb % 2
                    S.op("dve", lambda e, j=j, l=l, gs=gs, half=half: e.tensor_tensor(
                        out=P["gates"][:, l, gs, half * 512:(half + 1) * 512], in0=prow[j][:], in1=brow[j][:], op=ALU.add),
                        r=["prow%d" % j, "brow%d" % j], w=["gates"])
                else:
                    S.op("dve", lambda e, j=j: e.tensor_tensor(out=modrow[j][:], in0=prow[j][:], in1=brow[j][:], op=ALU.add),
                         r=["prow%d" % j, "brow%d" % j], w=["modrow%d" % j])
                    for m in range(4):
                        S.op("pe", lambda e, j=j, m=m: e.transpose(out=pcT[:, m, :], in_=modrow[j][:, m * 128:(m + 1) * 128], identity=P["ident"][:]),
                             r=["modrow%d" % j, "ident"], w=["pcol"])
                    S.op("dve", lambda e, l=l, nb=nb: e.tensor_copy(out=mcol[:, l, nb * 4:nb * 4 + 4], in_=pcT[:, :, 0]),
                         r=["pcol"], w=["mcol"])
            for s in range(2):
                S.op("dve", lambda e, l=l, s=s: e.scalar_tensor_tensor(
                    out=P["modA"][:, l, s, :], in0=mcol[:, l, s * 24 + 8:s * 24 + 16], scalar=1.0, in1=gcol[:, l, s, :],
                    op0=ALU.add, op1=ALU.mult), r=["mcol", "gcol"], w=["modA"])
                S.op("dve", lambda e, l=l, s=s: e.tensor_copy(out=P["modB"][:, l, s, :], in_=mcol[:, l, s * 24:s * 24 + 8]),
                     r=["mcol"], w=["modB"])
        S.flush()


MOE_INTERLEAVE = True
MOE_LNEXP = False


def phase_moe(C, P, dr, layer, x_in, x_out):
    nc, S = C.nc, C.S
    L = layer
    with contextlib.ExitStack() as st:
        X = C.sb(st, "X", [128, NT, D], F32)
        hT = C.sb(st, "hT", [128, 8, TOK], BF16)
        comb = C.sb(st, "comb", [128, NT, 16], F32)
        tmpn2 = [dict(junk=C.sb(st, "junk%d" % i, [128, D], BF16), ss=C.sb(st, "ss%d" % i, [128, 1], F32),
                      rs=C.sb(st, "rs%d" % i, [128, 1], F32), xn=C.sb(st, "xn%d" % i, [128, D], F32), eps=P["eps"], lnexp=MOE_LNEXP) for i in range(2)]
        hTf = [C.sb(st, "hTf%d" % i, [128, 8, 128], F32) for i in range(2)]
        wr = C.sb(st, "wr", [128, 8, 16], F32)
        rb = C.sb(st, "rb", [128, 16], F32)
        rt2 = [{n: C.sb(st, "rt%d_" % i + n, [128, 16], F32) for n in
                ("sc", "sel", "eq1", "sel2", "eq2", "selm", "wts")} for i in range(2)]
        r42 = [{n: C.sb(st, "r4%d_" % i + n, [128, 4], F32) for n in ("m1", "m2", "gs", "ing")} for i in range(2)]
        r12 = [{n: C.sb(st, "r1%d_" % i + n, [128, 1], F32) for n in ("gmax", "den")} for i in range(2)]
        wpk = [C.sb(st, "wpk%d" % i, [128, 12288], BF16) for i in range(2)]
        wg = [wpk[i][:, 0:4096].rearrange("p (k n) -> p k n", k=8) for i in range(2)]
        wu = [wpk[i][:, 4096:8192].rearrange("p (k n) -> p k n", k=8) for i in range(2)]
        wd = [wpk[i][:, 8192:12288].rearrange("p (k n) -> p k n", k=4) for i in range(2)]
        sg = [C.sb(st, "sg%d" % i, [128, 512], F32) for i in range(2)]
        hid = [C.sb(st, "hid%d" % i, [128, 4, 512], BF16) for i in range(2)]
        ev = [C.sb(st, "ev%d" % i, [128, 512], F32) for i in range(2)]
        pT = [C.ps(st, "pT%d" % i, [128, 4, 128], F32) for i in range(2)]
        pg = [C.ps(st, "pg%d" % i, [128, 512], F32) for i in range(2)]
        pu = [C.ps(st, "pu%d" % i, [128, 512], F32) for i in range(2)]
        py = [C.ps(st, "py%d" % i, [128, 512], F32) for i in range(2)]
        plog2 = [pg[0], pg[1]]

        S.dma("sp", wr[:], dr["w_router"].rearrange("(k p) n -> p k n", p=128), w=["wr"])
        S.dma("sp", rb[:], dr["router_bias"].partition_broadcast(128), w=["rb"])

        def load_w(e):
            i = e % 2
            S.dma("pool", wpk[i][:], dr["w_exp"][L, e], w=["wg%d" % i, "wu%d" % i, "wd%d" % i], slot="wpk%d" % i, max_dma_last_dim=8192)

        for t in range(NT):
            S.dma("sp", X[:, t, :], x_in[t * 128:(t + 1) * 128, :], w=["X%d" % t])
        load_w(0)
        load_w(1)
        A = P["modA"]
        B = P["modB"]
        def moe_pro(t, S):
            q_ = t % 2
            tmpn = tmpn2[q_]
            rt, r4, r1 = rt2[q_], r42[q_], r12[q_]
            plog = plog2[q_]
            T_ = "m%d" % q_
            pTq = [(pT[0], "pT0"), (pT[1], "pT1")] if q_ == 0 else [(pu[0][:].rearrange("p (a b) -> p a b", a=4), "pu0"),
                                                                      (pu[1][:].rearrange("p (a b) -> p a b", a=4), "pu1")]
            norm_transpose(C, S, X[:, t, :], "X%d" % t, tmpn, P["ident"], pTq, T_)
            hf = hTf[t % 2]
            hk = "hTf%d" % (t % 2)
            for k in range(8):
                p, pk = pTq[k // 4]
                if k % 2 == 0:
                    S.op("dve", lambda e, k=k, p=p, hf=hf: e.tensor_scalar(
                        out=hf[:, k, :], in0=p[:, k % 4, :], scalar1=A[:, L, 1, k:k + 1], scalar2=B[:, L, 1, k:k + 1],
                        op0=ALU.mult, op1=ALU.add), r=[pk, "modA", "modB"], w=[hk])
                else:
                    S.op("act", lambda e, k=k, p=p, hf=hf: e.activation(
                        out=hf[:, k, :], in_=p[:, k % 4, :], func=AF.Identity, scale=A[:, L, 1, k:k + 1],
                        bias=B[:, L, 1, k:k + 1]), r=[pk, "modA", "modB"], w=[hk])
            S.op("pool", lambda e, t=t, hf=hf: e.tensor_copy(out=hT[:, :, t * 128:(t + 1) * 128], in_=hf[:]),
                 r=[hk], w=["hT%d" % t])
            for k in range(8):
                S.op("pe", lambda e, k=k, hf=hf: e.matmul(plog[:, 0:16], lhsT=hf[:, k, :], rhs=wr[:, k, :],
                                                          start=(k == 0), stop=(k == 7)),
                     r=[hk, "wr"], w=["pg%d" % q_])
            sc, sel, eq1, sel2, eq2, selm, wts = [rt[n] for n in ("sc", "sel", "eq1", "sel2", "eq2", "selm", "wts")]
            m1, m2, gs, ing = [r4[n] for n in ("m1", "m2", "gs", "ing")]
            gmax, den = r1["gmax"], r1["den"]
            v4 = lambda a: a[:].rearrange("p (g e) -> p g e", e=4)
            b4 = lambda a: a[:].unsqueeze(2).to_broadcast([128, 4, 4])
            S.op("act", lambda e: e.activation(out=sc[:], in_=plog[:, 0:16], func=AF.Sigmoid), r=["pg%d" % q_], w=[T_ + "r_sc"])
            S.op("dve", lambda e: e.tensor_tensor(out=sel[:], in0=sc[:], in1=rb[:], op=ALU.add), r=[T_ + "r_sc", "rb"], w=[T_ + "r_sel"])
            S.op("dve", lambda e: e.tensor_reduce(out=m1[:], in_=v4(sel), axis=AX.X, op=ALU.max), r=[T_ + "r_sel"], w=[T_ + "r_m1"])
            S.op("dve", lambda e: e.tensor_tensor(out=v4(eq1), in0=v4(sel), in1=b4(m1), op=ALU.is_equal),
                 r=[T_ + "r_sel", T_ + "r_m1"], w=[T_ + "r_eq1"])
            S.op("dve", lambda e: e.scalar_tensor_tensor(out=sel2[:], in0=eq1[:], scalar=-1e9, in1=sel[:],
                                                         op0=ALU.mult, op1=ALU.add), r=[T_ + "r_eq1", T_ + "r_sel"], w=[T_ + "r_sel2"])
            S.op("dve", lambda e: e.tensor_reduce(out=m2[:], in_=v4(sel2), axis=AX.X, op=ALU.max), r=[T_ + "r_sel2"], w=[T_ + "r_m2"])
            S.op("dve", lambda e: e.tensor_tensor(out=gs[:], in0=m1[:], in1=m2[:], op=ALU.add), r=[T_ + "r_m1", T_ + "r_m2"], w=[T_ + "r_gs"])
            S.op("dve", lambda e: e.tensor_reduce(out=gmax[:], in_=gs[:], axis=AX.X, op=ALU.max), r=[T_ + "r_gs"], w=[T_ + "r_gmax"])
            S.op("dve", lambda e: e.tensor_scalar(out=ing[:], in0=gs[:], scalar1=gmax[:, 0:1], scalar2=None, op0=ALU.is_equal),
                 r=[T_ + "r_gs", T_ + "r_gmax"], w=[T_ + "r_ing"])
            S.op("dve", lambda e: e.tensor_tensor(out=v4(eq2), in0=v4(sel2), in1=b4(m2), op=ALU.is_equal),
                 r=[T_ + "r_sel2", T_ + "r_m2"], w=[T_ + "r_eq2"])
            S.op("dve", lambda e: e.tensor_tensor(out=selm[:], in0=eq1[:], in1=eq2[:], op=ALU.add), r=[T_ + "r_eq1", T_ + "r_eq2"], w=[T_ + "r_selm"])
            S.op("dve", lambda e: e.tensor_tensor(out=v4(selm), in0=v4(selm), in1=b4(ing), op=ALU.mult),
                 r=[T_ + "r_selm", T_ + "r_ing"], w=[T_ + "r_selm"])
            S.op("dve", lambda e: e.tensor_tensor(out=wts[:], in0=sc[:], in1=selm[:], op=ALU.mult), r=[T_ + "r_sc", T_ + "r_selm"], w=[T_ + "r_wts"])
            S.op("dve", lambda e: e.tensor_reduce(out=den[:], in_=wts[:], axis=AX.X, op=ALU.add), r=[T_ + "r_wts"], w=[T_ + "r_den"])
            S.op("dve", lambda e: e.reciprocal(out=den[:], in_=den[:]), r=[T_ + "r_den"], w=[T_ + "r_den"])
            S.op("dve", lambda e, t=t: e.tensor_scalar(out=comb[:, t, :], in0=wts[:], scalar1=den[:, 0:1], scalar2=None, op0=ALU.mult),
                 r=[T_ + "r_wts", T_ + "r_den"], w=["comb%d" % t])

        for t in range(0, NT, 2):
            if MOE_INTERLEAVE:
                ra, rb_ = Rec(), Rec()
                moe_pro(t, ra)
                moe_pro(t + 1, rb_)
                interleave(S, ra, rb_)
            else:
                moe_pro(t, S)
                moe_pro(t + 1, S)
        gate = P["gates"]
        n_g = 0
        n_y = 0
        for ex in range(16):
            i = ex % 2
            for tg in range(4):
                hb = hid[(ex * 4 + tg) % 2]
                hbk = "hid%d" % ((ex * 4 + tg) % 2)
                for hc in range(4):
                    j = n_g % 2
                    n_g += 1
                    hkeys = ["hT%d" % t for t in range(tg * 4, tg * 4 + 4)]
                    for k in range(8):
                        S.op("pe", lambda e, k=k, i=i, j=j, hc=hc, tg=tg: e.matmul(
                            pg[j][:], lhsT=wg[i][:, k, hc * 128:(hc + 1) * 128], rhs=hT[:, k, tg * 512:(tg + 1) * 512],
                            start=(k == 0), stop=(k == 7)), r=hkeys + ["wg%d" % i], w=["pg%d" % j])
                    for k in range(8):
                        S.op("pe", lambda e, k=k, i=i, j=j, hc=hc, tg=tg: e.matmul(
                            pu[j][:], lhsT=wu[i][:, k, hc * 128:(hc + 1) * 128], rhs=hT[:, k, tg * 512:(tg + 1) * 512],
                            start=(k == 0), stop=(k == 7)), r=hkeys + ["wu%d" % i], w=["pu%d" % j])
                    S.op("act", lambda e, j=j: e.activation(out=sg[j][:], in_=pg[j][:], func=AF.Silu),
                         r=["pg%d" % j], w=["sg%d" % j])
                    S.op("dve", lambda e, j=j, hb=hb, hc=hc: e.tensor_tensor(out=hb[:, hc, :], in0=sg[j][:], in1=pu[j][:], op=ALU.mult),
                         r=["sg%d" % j, "pu%d" % j], w=[hbk + "_%d" % hc])
                for tt in range(4):
                    t = tg * 4 + tt
                    for half in range(2):
                        j = n_y % 2
                        n_y += 1
                        for hc in range(4):
                            S.op("pe", lambda e, j=j, hb=hb, hc=hc, tt=tt, half=half, i=i: e.matmul(
                                py[j][:], lhsT=hb[:, hc, tt * 128:(tt + 1) * 128], rhs=wd[i][:, hc, half * 512:(half + 1) * 512],
                                start=(hc == 0), stop=(hc == 3)), r=[hbk + "_%d" % hc, "wd%d" % i], w=["py%d" % j])
                        S.op("dve", lambda e, j=j, t=t, ex=ex, half=half: e.scalar_tensor_tensor(
                            out=ev[j][:], in0=py[j][:], scalar=comb[:, t, ex:ex + 1], in1=gate[:, L, 1, half * 512:(half + 1) * 512],
                            op0=ALU.mult, op1=ALU.mult), r=["py%d" % j, "comb%d" % t, "gates"], w=["ev%d" % j])
                        S.op("pool", lambda e, j=j, t=t, half=half: e.tensor_tensor(
                            out=X[:, t, half * 512:(half + 1) * 512], in0=X[:, t, half * 512:(half + 1) * 512], in1=ev[j][:], op=ALU.add),
                            r=["ev%d" % j, "X%d" % t], w=["X%d" % t])
            if ex + 2 < 16:
                load_w(ex + 2)
        for t in range(NT):
            S.dma("sp", x_out[t * 128:(t + 1) * 128, :], X[:, t, :], r=["X%d" % t], w=["xo_%s_%d" % (x_out.name, t % 4)])
        S.wait_all("sp", ["xo_%s_%d" % (x_out.name, j) for j in range(4)])
        S.flush()


def alloc_persistent(C):
    st = C.outer
    P = {}
    P["ident"] = C.sb(st, "ident", [128, 128], F32)
    P["eps"] = C.sb(st, "eps", [128, 1], F32)
    P["modA"] = C.sb(st, "modA", [128, 2, 2, 8], F32)
    P["modB"] = C.sb(st, "modB", [128, 2, 2, 8], F32)
    P["gates"] = C.sb(st, "gates", [128, 2, 2, D], F32)
    return P


def phase_init(C, P, dr):
    S = C.S
    S.dma("sp", P["ident"][:], dr["ident"], w=["ident"])
    S.op("dve", lambda e: e.memset(P["eps"][:], EPS), w=["eps"])


GELU = AF.Gelu_apprx_tanh


def phase_gmlp(C, P, dr, x_in, x_out):
    nc, S0 = C.nc, C.S
    L = 1
    with contextlib.ExitStack() as st:
        win = C.sb(st, "win", [128, 8, 4096], BF16)
        wout = C.sb(st, "wout", [128, 16, 1024], BF16)
        WT = C.sb(st, "WT", [128, 8, 128], BF16)
        wsf = C.sb(st, "wsf", [128, 8, 128], F32)
        tril = C.sb(st, "tril", [128, 128], F32)
        grow = C.sb(st, "grow", [128, 2048], F32)
        bsr = C.sb(st, "bsr", [1, 8, 128], BF16)
        bsf = C.sb(st, "bsf", [1, 8, 128], F32)
        ones1 = C.sb(st, "ones1", [1, 128], BF16)
        xt = [C.sb(st, "xt%d" % i, [128, D], F32) for i in range(2)]
        xo = [C.sb(st, "xo%d" % i, [128, D], F32) for i in range(2)]
        junk_sh = C.sb(st, "junk_sh", [128, D], BF16)
        tmpn2 = [dict(junk=junk_sh, ss=C.sb(st, "ss%d" % i, [128, 1], F32),
                      rs=C.sb(st, "rs%d" % i, [128, 1], F32), xn=C.sb(st, "xn%d" % i, [128, D], F32), eps=P["eps"], lnexp=True) for i in range(2)]
        hT = [C.sb(st, "hT%d" % i, [128, 8, 128], BF16) for i in range(2)]
        vz2 = [C.sb(st, "vz%d" % i, [128, 2048], F32) for i in range(2)]
        vss2 = [C.sb(st, "vss%d" % i, [128, 4], F32) for i in range(2)]
        vs12 = [C.sb(st, "vs1%d" % i, [128, 1], F32) for i in range(2)]
        vn2 = [C.sb(st, "vn%d" % i, [128, 2048], BF16) for i in range(2)]
        uT2 = [C.sb(st, "uT%d" % i, [128, 16, 128], BF16) for i in range(2)]
        pTt2 = [C.sb(st, "pTt%d" % i, [128, 16, 128], BF16) for i in range(2)]
        ev2 = [C.sb(st, "ev%d" % i, [128, 512], F32) for i in range(2)]
        bks = [bank(C, st, "gb%d" % i) for i in range(8)]
        v3 = lambda a_, n: a_.rearrange("p (a b) -> p a b", a=n)
        S = S0
        wsrc = dr["w_in_c"].rearrange("(k p) n -> p k n", p=128)
        S.dma("pool", win[:, :, 2048:4096], wsrc[:, :, 2048:4096], w=["win%d" % k for k in range(4)], slot="winv", max_dma_last_dim=8192)
        S.dma("pool", win[:, :, 0:2048], wsrc[:, :, 0:2048], w=["win%d" % k for k in range(4, 8)], slot="winu", max_dma_last_dim=8192)
        S.dma("pool", wout[:], dr["w_out_c"].rearrange("(k p) n -> p k n", p=128), w=["wout"])
        S.dma("sp", wsf[:], dr["sgu_w_s"].rearrange("g t s -> t g s"), w=["wsf"])
        S.dma("sp", tril[:], dr["tril"], w=["tril"])
        S.dma("sp", grow[:], dr["sgu_norm_g"].partition_broadcast(128), w=["grow"])
        S.dma("sp", bsf[:], dr["sgu_b_s"].rearrange("(o g) t -> o g t", o=1), w=["bsf"])
        S.op("dve", lambda e: e.tensor_copy(out=bsr[:], in_=bsf[:]), r=["bsf"], w=["bsr"])
        S.op("dve", lambda e: e.memset(ones1[:], 1.0), w=["ones1"])
        for g in range(8):
            S.op("dve", lambda e, g=g: e.tensor_tensor(out=wsf[:, g, :], in0=wsf[:, g, :], in1=tril[:], op=ALU.mult),
                 r=["wsf", "tril"], w=["wsf"])
        for g in range(8):
            p = v3(bks[g // 4][:], 4)
            S.op("pe", lambda e, g=g, p=p: e.transpose(out=p[:, g % 4, :], in_=wsf[:, g, :], identity=P["ident"][:]),
                 r=["wsf", "ident"], w=["gb%d" % (g // 4)])
        for i in range(2):
            S.op("dve", lambda e, i=i: e.tensor_copy(out=WT[:, i * 4:(i + 1) * 4, :], in_=v3(bks[i][:], 4)), r=["gb%d" % i], w=["WT"])

        A = P["modA"]
        B = P["modB"]
        gate = P["gates"]
        winkeys = ["win%d" % k for k in range(8)]

        def tile(t, S):
            i = t % 2
            q0, q1, q2, q3 = [bks[i * 4 + n] for n in range(4)]
            k0, k1, k2, k3 = ["gb%d" % (i * 4 + n) for n in range(4)]
            pT = [(v3(q0[:], 4), k0), (v3(q1[:], 4), k1)]
            pvb = [(q0, k0), (q1, k1)]
            pu, puk = v3(q2[:], 4), k2
            pm, pmk = v3(q3[:], 4), k3
            tmpn, vz, vss, vs1, vn, uT, pTt, ev = tmpn2[i], vz2[i], vss2[i], vs12[i], vn2[i], uT2[i], pTt2[i], ev2[i]
            T_ = "g%d" % i
            S.dma("sp", xt[i][:], x_in[t * 128:(t + 1) * 128, :], w=["xt%d" % i])
            norm_transpose(C, S, xt[i][:], "xt%d" % i, tmpn, P["ident"], pT, T_)
            h = hT[i]
            hk = "hT%d" % i
            for k in range(8):
                p, pk = pT[k // 4]
                if k % 2 == 0:
                    S.op("dve", lambda e, k=k, p=p, h=h: e.tensor_scalar(
                        out=h[:, k, :], in0=p[:, k % 4, :], scalar1=A[:, L, 0, k:k + 1], scalar2=B[:, L, 0, k:k + 1],
                        op0=ALU.mult, op1=ALU.add), r=[pk, "modA", "modB"], w=[hk])
                else:
                    S.op("act", lambda e, k=k, p=p, h=h: e.activation(
                        out=h[:, k, :], in_=p[:, k % 4, :], func=AF.Identity, scale=A[:, L, 0, k:k + 1],
                        bias=B[:, L, 0, k:k + 1]), r=[pk, "modA", "modB"], w=[hk])
            for n in range(4):
                pv_, pvk = pvb[n % 2]
                for k in range(8):
                    S.op("pe", lambda e, k=k, n=n, pv_=pv_, h=h: e.matmul(
                        pv_[:], lhsT=h[:, k, :], rhs=win[:, k, 2048 + n * 512: 2048 + (n + 1) * 512],
                        start=(k == 0), stop=(k == 7)), r=[hk] + winkeys, w=[pvk])
                S.op("act", lambda e, n=n, pv_=pv_: e.activation(out=vz[:, n * 512:(n + 1) * 512], in_=pv_[:], func=GELU),
                     r=[pvk], w=[T_ + "vz%d" % n])
                S.op("act", lambda e, n=n: e.activation(out=junk_sh[:, 0:512], in_=vz[:, n * 512:(n + 1) * 512], func=AF.Square,
                                                        accum_out=vss[:, n:n + 1]), r=[T_ + "vz%d" % n], w=[T_ + "vss%d" % n, "junk_sh"])
            S.op("dve", lambda e: e.tensor_reduce(out=vs1[:], in_=vss[:], axis=AX.X, op=ALU.add),
                 r=[T_ + "vss%d" % n for n in range(4)], w=[T_ + "vs1"])
            S.op("act", lambda e: e.activation(out=vs1[:], in_=vs1[:], func=AF.Ln, scale=1.0 / 2048, bias=P["eps"][:]),
                 r=[T_ + "vs1"], w=[T_ + "vs1"])
            S.op("act", lambda e: e.activation(out=vs1[:], in_=vs1[:], func=AF.Exp, scale=-0.5), r=[T_ + "vs1"], w=[T_ + "vs1"])
            for n in range(4):
                S.op("dve", lambda e, n=n: e.scalar_tensor_tensor(
                    out=vn[:, n * 512:(n + 1) * 512], in0=vz[:, n * 512:(n + 1) * 512], scalar=vs1[:, 0:1],
                    in1=grow[:, n * 512:(n + 1) * 512], op0=ALU.mult, op1=ALU.mult),
                    r=[T_ + "vz%d" % n, T_ + "vs1", "grow"], w=[T_ + "vn%d" % n])
            for q in range(4):
                for m in range(4):
                    c = q * 4 + m
                    for k in range(8):
                        S.op("pe", lambda e, k=k, c=c, m=m, h=h: e.matmul(
                            pu[:, m, :], lhsT=win[:, k, c * 128:(c + 1) * 128], rhs=h[:, k, :],
                            start=(k == 0), stop=(k == 7)), r=[hk] + winkeys, w=[puk])
                S.op("act", lambda e, q=q: e.activation(out=uT[:, q * 4:(q + 1) * 4, :], in_=pu, func=GELU),
                     r=[puk], w=[T_ + "uT%d" % q])
                for m in range(4):
                    c = q * 4 + m
                    g = c // 2
                    S.op("pe", lambda e, c=c, m=m, g=g: e.matmul(
                        pm[:, m, :], lhsT=vn[:, c * 128:(c + 1) * 128], rhs=WT[:, g, :], start=True, stop=False),
                        r=[T_ + "vn%d" % (c // 4), "WT"], w=[pmk])
                    S.op("pe", lambda e, c=c, m=m, g=g: e.matmul(
                        pm[:, m, :], lhsT=ones1[0:1, :], rhs=bsr[0:1, g, :], start=False, stop=True),
                        r=["ones1", "bsr"], w=[pmk])
                S.op("dve", lambda e, q=q: e.tensor_tensor(out=pTt[:, q * 4:(q + 1) * 4, :], in0=pm, in1=uT[:, q * 4:(q + 1) * 4, :],
                                                           op=ALU.mult), r=[pmk, T_ + "uT%d" % q], w=[T_ + "pTt%d" % q])
            o = xo[i]
            for half in range(2):
                pv_, pvk = pvb[half]
                for c in range(16):
                    S.op("pe", lambda e, c=c, half=half, pv_=pv_: e.matmul(
                        pv_[:], lhsT=pTt[:, c, :], rhs=wout[:, c, half * 512:(half + 1) * 512],
                        start=(c == 0), stop=(c == 15)), r=[T_ + "pTt%d" % (c // 4), "wout"], w=[pvk])
                S.op("dve", lambda e, half=half, pv_=pv_: e.tensor_tensor(
                    out=ev[:], in0=pv_[:], in1=gate[:, L, 0, half * 512:(half + 1) * 512], op=ALU.mult),
                    r=[pvk, "gates"], w=[T_ + "ev"])
                S.op("dve", lambda e, half=half, o=o: e.tensor_tensor(
                    out=o[:, half * 512:(half + 1) * 512], in0=ev[:], in1=xt[i][:, half * 512:(half + 1) * 512], op=ALU.add),
                    r=[T_ + "ev", "xt%d" % i], w=["xo%d_%d" % (i, half)])
            S.dma("sp", x_out[t * 128:(t + 1) * 128, :], o[:], r=["xo%d_0" % i, "xo%d_1" % i], w=["xo_%s_%d" % (x_out.name, i)])

        S0.alias = {"g0junk": "junk_sh", "g1junk": "junk_sh"}
        for t in range(0, NT, 2):
            ra, rb_ = Rec(), Rec()
            tile(t, ra)
            tile(t + 1, rb_)
            interleave(S0, ra, rb_)
        S0.wait_all("sp", ["xo_%s_%d" % (x_out.name, j) for j in range(2)])
        S0.flush()
        S0.alias = {}


NWT = 64
OWN0 = 48
DBG_SKIP = set()
TWO_PI = 6.283185307179586
CW1 = 6.28125
CW2 = TWO_PI - CW1
CA_Q, CA_K, CA_V, CA_GLR, CA_R = 0, 256, 512, 1024, 1040
CA_KC, CA_VC, CA_KV4 = 1552, 1680, 1808
NA = 2320


def bank(C, st, name):
    return C.ps(st, name, [128, 512], F32)


def rope_tables(C, S, st, pos_i, ncol, invf, out_cs, tag):
    pf = C.sb(st, tag + "pf", [128, ncol], F32)
    ang = C.sb(st, tag + "ang", [128, ncol, 8], F32)
    ki = C.sb(st, tag + "ki", [128, ncol, 8], I32)
    kf = C.sb(st, tag + "kf", [128, ncol, 8], F32)
    r = C.sb(st, tag + "r", [128, ncol, 8], F32)
    y = C.sb(st, tag + "y", [128, ncol, 8], F32)
    m = C.sb(st, tag + "m", [128, ncol, 8], F32)
    k = lambda s: tag + s
    S.op("dve", lambda e: e.tensor_copy(out=pf[:], in_=pos_i[:]), r=[k("pos")], w=[k("pf")])
    S.op("dve", lambda e: e.tensor_tensor(out=ang[:], in0=pf[:].unsqueeze(2).to_broadcast([128, ncol, 8]),
                                          in1=invf[:].unsqueeze(1).to_broadcast([128, ncol, 8]), op=ALU.mult),
         r=[k("pf"), "invf"], w=[k("ang")])
    S.op("dve", lambda e: e.tensor_scalar(out=ki[:], in0=ang[:], scalar1=1.0 / TWO_PI, scalar2=None, op0=ALU.mult),
         r=[k("ang")], w=[k("ki")])
    S.op("dve", lambda e: e.tensor_copy(out=kf[:], in_=ki[:]), r=[k("ki")], w=[k("kf")])
    S.op("dve", lambda e: e.scalar_tensor_tensor(out=r[:], in0=kf[:], scalar=-CW1, in1=ang[:], op0=ALU.mult, op1=ALU.add),
         r=[k("kf"), k("ang")], w=[k("r")])
    S.op("dve", lambda e: e.scalar_tensor_tensor(out=r[:], in0=kf[:], scalar=-CW2, in1=r[:], op0=ALU.mult, op1=ALU.add),
         r=[k("kf"), k("r")], w=[k("r")])
    for which, shift in ((1, 0.0), (0, np.pi / 2)):
        S.op("dve", lambda e, shift=shift: e.tensor_scalar(out=y[:], in0=r[:], scalar1=float(shift), scalar2=None, op0=ALU.add),
             r=[k("r")], w=[k("y")])
        S.op("dve", lambda e: e.tensor_scalar(out=m[:], in0=y[:], scalar1=float(np.pi), scalar2=None, op0=ALU.is_gt),
             r=[k("y")], w=[k("m")])
        S.op("dve", lambda e: e.scalar_tensor_tensor(out=y[:], in0=m[:], scalar=-TWO_PI, in1=y[:], op0=ALU.mult, op1=ALU.add),
             r=[k("m"), k("y")], w=[k("y")])
        S.op("dve", lambda e: e.tensor_scalar(out=y[:], in0=y[:], scalar1=float(np.pi), scalar2=-float(np.pi), op0=ALU.min, op1=ALU.max),
             r=[k("y")], w=[k("y")])
        S.op("act", lambda e, which=which: e.activation(out=out_cs[:, :, which * 8:(which + 1) * 8], in_=y[:], func=AF.Sin),
             r=[k("y")], w=[k("cs")])


def rms_gain_rope(C, S, src, srckey, dst, dstkey, ngrp, gain_row, gainkey, cs, cskey, T, tag):
    sq, ss, t1, t2 = T["sq"], T["ss"], T["t1"], T["t2"]
    k = lambda s: tag + s
    S.op("act", lambda e: e.activation(out=sq[:, 0:ngrp, :], in_=src, func=AF.Square), r=[srckey], w=[k("sq")])
    S.op("dve", lambda e: e.tensor_reduce(out=ss[:, 0:ngrp], in_=sq[:, 0:ngrp, :], axis=AX.X, op=ALU.add), r=[k("sq")], w=[k("ss")])
    S.op("act", lambda e: e.activation(out=ss[:, 0:ngrp], in_=ss[:, 0:ngrp], func=AF.Ln, scale=1.0 / 64, bias=T["eps"][:]),
         r=[k("ss")], w=[k("ss")])
    S.op("act", lambda e: e.activation(out=ss[:, 0:ngrp], in_=ss[:, 0:ngrp], func=AF.Exp, scale=-0.5), r=[k("ss")], w=[k("ss")])
    S.op("dve", lambda e: e.tensor_tensor(out=dst, in0=src, in1=ss[:, 0:ngrp].unsqueeze(2).to_broadcast([128, ngrp, 64]), op=ALU.mult),
         r=[srckey, k("ss")], w=[dstkey])
    S.op("dve", lambda e: e.tensor_tensor(out=dst, in0=dst, in1=gain_row.unsqueeze(1).to_broadcast([128, ngrp, 64]), op=ALU.mult),
         r=[dstkey, gainkey], w=[dstkey])
    cosb = cs[:, 0:8].unsqueeze(1).to_broadcast([128, ngrp, 8])
    sinb = cs[:, 8:16].unsqueeze(1).to_broadcast([128, ngrp, 8])
    x1 = dst[:, :, 0:8]
    x2 = dst[:, :, 8:16]
    S.op("dve", lambda e: e.tensor_tensor(out=t1[:, 0:ngrp, 0:8], in0=x1, in1=cosb, op=ALU.mult), r=[dstkey, cskey], w=[k("t1a")])
    S.op("dve", lambda e: e.tensor_tensor(out=t1[:, 0:ngrp, 8:16], in0=x2, in1=cosb, op=ALU.mult), r=[dstkey, cskey], w=[k("t1b")])
    S.op("dve", lambda e: e.tensor_tensor(out=t2[:, 0:ngrp, 0:8], in0=x2, in1=sinb, op=ALU.mult), r=[dstkey, cskey], w=[k("t2a")])
    S.op("dve", lambda e: e.tensor_tensor(out=t2[:, 0:ngrp, 8:16], in0=x1, in1=sinb, op=ALU.mult), r=[dstkey, cskey], w=[k("t2b")])
    S.op("dve", lambda e: e.tensor_tensor(out=x1, in0=t1[:, 0:ngrp, 0:8], in1=t2[:, 0:ngrp, 0:8], op=ALU.subtract),
         r=[k("t1a"), k("t2a"), k("t1b"), k("t2b")], w=[dstkey])
    S.op("dve", lambda e: e.tensor_tensor(out=x2, in0=t1[:, 0:ngrp, 8:16], in1=t2[:, 0:ngrp, 8:16], op=ALU.add),
         r=[k("t1b"), k("t2b")], w=[dstkey])


def alloc_mixer(C, st):
    M = {}
    M["KselT"] = C.sb(st, "KselT", [128, NWT * 128], BF16)
    M["Vsel"] = C.sb(st, "Vsel", [128, NWT, 2, 65], BF16)
    M["KwinT"] = C.sb(st, "KwinT", [128, 20 * 128], BF16)
    M["Vwin"] = C.sb(st, "Vwin", [128, 20, 2, 65], BF16)
    M["mixT"] = C.sb(st, "mixT", [128, 8, TOK], BF16)
    M["csT"] = C.sb(st, "csT", [128, NWT, 16], F32)
    M["kbias"] = C.sb(st, "kbias", [128, NWT], F32)
    M["gain"] = C.sb(st, "gain", [128, 4, 64], F32)
    M["invf"] = C.sb(st, "invf", [128, 8], F32)
    return M


def phase_mix_a(C, P, M, dr, dbg=None, tiles=None):
    nc, S = C.nc, C.S
    L = 0
    with contextlib.ExitStack() as st:
        wA = C.sb(st, "wA", [128, 8, NA], BF16)
        w2g = C.sb(st, "w2g", [16, 256], F32)
        negb = C.sb(st, "negb", [128, 2], F32)
        gng = C.sb(st, "gng", [128, 128], F32)
        maskT = C.sb(st, "maskT", [128, 4, 128], F32)
        posi = C.sb(st, "posi", [128, NWT], I32)
        valid = C.sb(st, "valid", [128, NWT], F32)
        onesc = C.sb(st, "onesc", [128, 64], F32)
        state = C.sb(st, "state", [128, 4, 128], F32)
        sbf = [C.sb(st, "sbf%d" % i, [128, 4, 128], BF16) for i in range(2)]
        xt = [C.sb(st, "xt%d" % i, [128, D], F32) for i in range(2)]
        tmpn = dict(junk=C.sb(st, "junk", [128, D], BF16), ss=C.sb(st, "ss", [128, 1], F32),
                    rs=C.sb(st, "rs", [128, 1], F32), xn=C.sb(st, "xn", [128, D], F32), eps=P["eps"], lnexp=True)
        hT = C.sb(st, "hT", [128, 8, 128], BF16)
        glrT = C.sb(st, "glrT", [16, 128], F32)
        vb2 = [C.sb(st, "vb%d" % i, [128, 512], BF16) for i in range(2)]
        qk2 = [C.sb(st, "qk%d" % i, [128, 4, 128], F32) for i in range(2)]
        sp2 = [C.sb(st, "sp%d" % i, [128, 2, 128], F32) for i in range(2)]
        kv42 = [C.sb(st, "kv4s%d" % i, [128, 512], F32) for i in range(2)]
        rs2 = [C.sb(st, "rsl%d" % i, [128, 512], F32) for i in range(2)]
        cmpT = [C.sb(st, "cmpT%d" % i, [128, 2, 128], BF16) for i in range(2)]
        cs_ = C.sb(st, "cs_", [128, 2, 128], F32)
        m16 = C.sb(st, "m16", [128, 2, 2, 2], F32)
        nm16 = C.sb(st, "nm16", [128, 2, 2, 2], F32)
        oaf = C.sb(st, "oaf", [128, 512], F32)
        E1 = C.sb(st, "E1", [128, 2, 128], F32)
        E2 = C.sb(st, "E2", [128, 2, 128], F32)
        E3 = C.sb(st, "E3", [128, 2, 128], F32)
        E4 = C.sb(st, "E4", [128, 2, 128], F32)
        qtl = C.sb(st, "qtl", [128, 2, 2, 128], BF16)
        ktl = C.sb(st, "ktl", [128, 2, 128], BF16)
        khT = C.sb(st, "khT", [128, 2, 128], F32)
        kh = C.sb(st, "kh", [128, 2, 128], BF16)
        qhz = C.sb(st, "qhz", [128, 2, 2, 128], BF16)
        sT = C.sb(st, "sT", [128, 4, 128], BF16)
        ssq = C.sb(st, "ssq", [128, 4], F32)
        sqj = C.sb(st, "sqj", [128, 128], F32)
        on = C.sb(st, "on", [128, 512], F32)
        oa = C.sb(st, "oa", [128, 512], BF16)
        identb = C.sb(st, "identb", [128, 128], BF16)
        kn = C.sb(st, "kn", [128, 2, 2, 64], F32)
        RT = dict(sq=C.sb(st, "r_sq", [128, 2, 64], F32), ss=C.sb(st, "r_ss", [128, 2], F32),
                  t1=C.sb(st, "r_t1", [128, 2, 16], F32), t2=C.sb(st, "r_t2", [128, 2, 16], F32), eps=P["eps"])
        b = [bank(C, st, "bk%d" % i) for i in range(8)]
        v3 = lambda a, n: a.rearrange("p (a b) -> p a b", a=n)
        pT0, pT1 = v3(b[0][:], 4), v3(b[1][:], 4)
        pv, pr = b[0], b[0]
        pkv4 = b[1]
        pqk = v3(b[2][:], 4)
        pcmp = v3(b[3][:, 0:256], 2)
        pz = v3(b[3][:, 256:512], 2)
        pglr = b[3][0:16, 256:384]
        pkvs = v3(b[4][:], 2)
        pkh = v3(b[5][:, 0:256], 2)
        pkT = v3(b[5][:, 256:512], 2)
        psc = v3(b[6][:], 4)
        pmx = b[6][:, 0:256].bitcast(BF16).rearrange("p (a b) -> p a b", a=4)
        po = v3(b[7][:], 4)

        S.dma("pool", wA[:, :, 0:1552], dr["w_in_ab"][:, 0:1552].rearrange("(k p) n -> p k n", p=128), w=["wA0"])
        S.dma("pool", wA[:, :, 1552:NA], dr["w_in_ab"][:, 2064:2832].rearrange("(k p) n -> p k n", p=128), w=["wA1"])
        S.dma("sp", w2g[:], dr["gla_w_gate2"], w=["w2g"])
        S.dma("sp", negb[:], dr["gla_b_gate_col"], w=["negb"])
        S.dma("sp", gng[:], dr["gla_norm_g"].partition_broadcast(128), w=["gng"])
        S.dma("sp", maskT[:], dr["gla_maskT"], w=["maskT"])
        S.dma("sp", posi[:], dr["pos_col"], w=["Tpos"])
        S.dma("sp", valid[:], dr["valid_col"], w=["valid"])
        S.dma("sp", M["invf"][:], dr["invf"].partition_broadcast(128), w=["invf"])
        S.dma("sp", M["gain"][:, 0:3, :], dr["nsa_k_gain"].partition_broadcast(128), w=["gain"])
        S.dma("sp", M["gain"][:, 3, :], dr["nsa_q_gain"].partition_broadcast(128), w=["gainq"])
        S.op("dve", lambda e: e.tensor_scalar(out=negb[:], in0=negb[:], scalar1=-1.0, scalar2=None, op0=ALU.mult), r=["negb"], w=["negb"])
        S.op("dve", lambda e: e.memset(onesc[:], 1.0), w=["onesc"])
        S.op("dve", lambda e: e.memset(state[:], 0.0), w=["state"])
        S.op("pool", lambda e: e.memset(qhz[:], 0.0), w=["qhz"])
        S.op("pool", lambda e: e.memset(qtl[:], 0.0), w=["qtl"])
        S.op("pool", lambda e: e.memset(M["Vsel"][:, :, :, 64:65], 1.0), w=["Vsel1"])
        S.op("pool", lambda e: e.memset(M["Vwin"][:, :, :, 64:65], 1.0), w=["Vwin1"])
        S.op("dve", lambda e: e.tensor_copy(out=identb[:], in_=P["ident"][:]), r=["ident"], w=["identb"])
        S.op("dve", lambda e: e.tensor_scalar(out=M["kbias"][:], in0=valid[:], scalar1=-1.0, scalar2=1e4, op0=ALU.add, op1=ALU.mult),
             r=["valid"], w=["kbias"])
        rope_tables(C, S, st, posi, NWT, M["invf"], M["csT"], "T")

        A, B = P["modA"], P["modB"]
        wk = ["wA0", "wA1"]
        tl = list(tiles if tiles is not None else range(NWT))

        def stage_a(t, S):
            own = t >= OWN0
            i = t % 2
            S.dma("sp", xt[i][:], dr["xw"][t * 128:(t + 1) * 128, :], w=["xt%d" % i])
            norm_transpose(C, S, xt[i][:], "xt%d" % i, tmpn, P["ident"], [(pT0, "b0"), (pT1, "b1")], "a")
            for k in range(8):
                p, pk = ((pT0, "b0"), (pT1, "b1"))[k // 4]
                if k % 2 == 0:
                    S.op("dve", lambda e, k=k, p=p: e.tensor_scalar(
                        out=hT[:, k, :], in0=p[:, k % 4, :], scalar1=A[:, L, 0, k:k + 1], scalar2=B[:, L, 0, k:k + 1],
                        op0=ALU.mult, op1=ALU.add), r=[pk, "modA", "modB"], w=["hT"])
                else:
                    S.op("act", lambda e, k=k, p=p: e.activation(
                        out=hT[:, k, :], in_=p[:, k % 4, :], func=AF.Identity, scale=A[:, L, 0, k:k + 1],
                        bias=B[:, L, 0, k:k + 1]), r=[pk, "modA", "modB"], w=["hT"])
            for m in range(4):
                if m < 2 and not own:
                    continue
                for k in range(8):
                    S.op("pe", lambda e, k=k, m=m: e.matmul(pqk[:, m, :], lhsT=wA[:, k, m * 128:(m + 1) * 128], rhs=hT[:, k, :],
                                                            start=(k == 0), stop=(k == 7)), r=["hT"] + wk, w=["b2"])
            for k in range(8):
                S.op("pe", lambda e, k=k: e.matmul(pglr, lhsT=wA[:, k, CA_GLR:CA_GLR + 16], rhs=hT[:, k, :],
                                                   start=(k == 0), stop=(k == 7)), r=["hT"] + wk, w=["b3"])
            for m in range(2):
                for k in range(8):
                    S.op("pe", lambda e, k=k, m=m: e.matmul(pcmp[:, m, :], lhsT=wA[:, k, CA_KC + m * 128:CA_KC + (m + 1) * 128],
                                                            rhs=hT[:, k, :], start=(k == 0), stop=(k == 7)), r=["hT"] + wk, w=["b3"])
            for k in range(8):
                S.op("pe", lambda e, k=k: e.matmul(pv[:], lhsT=hT[:, k, :], rhs=wA[:, k, CA_V:CA_V + 512],
                                                   start=(k == 0), stop=(k == 7)), r=["hT"] + wk, w=["b0"])
            for k in range(8):
                S.op("pe", lambda e, k=k: e.matmul(pkv4[:], lhsT=hT[:, k, :], rhs=wA[:, k, CA_KV4:CA_KV4 + 512],
                                                   start=(k == 0), stop=(k == 7)), r=["hT"] + wk, w=["b1"])
            S.op("dve", lambda e: e.tensor_copy(out=glrT[:], in_=pglr), r=["b3"], w=["glrT"])
            ct = cmpT[i]
            S.op("act", lambda e: e.activation(out=ct[:], in_=pcmp, func=AF.Copy), r=["b3"], w=["cmpT%d" % i])
            for m in range(2):
                S.dma("act", dr["cmp_scr"][m, :, t * 128:(t + 1) * 128], ct[:, m, :], r=["cmpT%d" % i], w=["cmp_scr%d" % i])
            m0 = 0 if own else 2
            S.op("dve", lambda e: e.tensor_copy(out=qk2[i][:, m0:4, :], in_=pqk[:, m0:4, :]), r=["b2"], w=["qk%d" % i])
            S.op("act", lambda e: e.activation(out=vb2[i][:], in_=pv[:], func=AF.Copy), r=["b0"], w=["vb%d" % i])
            S.op("dve", lambda e: e.tensor_copy(out=kv42[i][:], in_=pkv4[:]), r=["b1"], w=["kv4s%d" % i])
            for p_ in range(2):
                S.op("pe", lambda e, p_=p_: e.matmul(pz[:, p_, :], lhsT=w2g[:, p_ * 128:(p_ + 1) * 128], rhs=glrT[:],
                                                     start=True, stop=True), r=["w2g", "glrT"], w=["b3"])
            if own:
                for k in range(8):
                    S.op("pe", lambda e, k=k: e.matmul(pr[:], lhsT=hT[:, k, :], rhs=wA[:, k, CA_R:CA_R + 512],
                                                       start=(k == 0), stop=(k == 7)), r=["hT"] + wk, w=["b0"])
            for p_ in range(2):
                S.op("act", lambda e, p_=p_: e.activation(out=sp2[i][:, p_, :], in_=pz[:, p_, :], func=AF.Exp, scale=-1.0,
                                                          bias=negb[:, p_:p_ + 1]), r=["b3", "negb"], w=["sp%d" % i])
            if own:
                S.op("act", lambda e: e.activation(out=rs2[i][:], in_=pr[:], func=AF.Exp, scale=-1.0), r=["b0"], w=["rsl%d" % i])
                S.op("dve", lambda e: e.tensor_scalar(out=rs2[i][:], in0=rs2[i][:], scalar1=1.0, scalar2=None, op0=ALU.add), r=["rsl%d" % i], w=["rsl%d" % i])
                S.op("dve", lambda e: e.reciprocal(out=rs2[i][:], in_=rs2[i][:]), r=["rsl%d" % i], w=["rsl%d" % i])
                S.op("dve", lambda e: e.tensor_tensor(out=rs2[i][:], in0=rs2[i][:], in1=pr[:], op=ALU.mult), r=["rsl%d" % i, "b0"], w=["rsl%d" % i])

        def stage_b(t, S):
            own = t >= OWN0
            i = t % 2
            sp, qk, vb, kv4s, rs_ = sp2[i], qk2[i], vb2[i], kv42[i], rs2[i]
            spk, qkk, vbk, kvk, rsk = "sp%d" % i, "qk%d" % i, "vb%d" % i, "kv4s%d" % i, "rsl%d" % i
            S.op("act", lambda e: e.activation(out=sp[:], in_=sp[:], func=AF.Ln, bias=1.0, scale=1.0), r=[spk], w=[spk])
            for p_ in range(2):
                for c in range(2):
                    S.op("dve", lambda e, p_=p_, c=c: e.tensor_tensor_scan(
                        out=cs_[:, p_, c * 64:(c + 1) * 64], data0=onesc[:], data1=sp[:, p_, c * 64:(c + 1) * 64], initial=0.0,
                        op0=ALU.mult, op1=ALU.add), r=[spk, "onesc"], w=["cs_"])
            csv = cs_[:].rearrange("p a (c t) -> p a c t", c=2)
            S.op("dve", lambda e: e.tensor_scalar(out=m16[:, :, :, 0:1], in0=csv[:, :, :, 32:33], scalar1=1.0 / 16, scalar2=None, op0=ALU.mult),
                 r=["cs_"], w=["m16"])
            S.op("dve", lambda e: e.tensor_scalar(out=m16[:, :, :, 1:2], in0=csv[:, :, :, 63:64], scalar1=1.0 / 16, scalar2=None, op0=ALU.mult),
                 r=["cs_"], w=["m16"])
            S.op("dve", lambda e: e.tensor_scalar(out=nm16[:], in0=m16[:], scalar1=-1.0, scalar2=None, op0=ALU.mult), r=["m16"], w=["nm16"])
            for p_ in range(2):
                for c in range(2):
                    sl = slice(c * 64, (c + 1) * 64)
                    S.op("act", lambda e, p_=p_, c=c, sl=sl: e.activation(out=E3[:, p_, sl], in_=cs_[:, p_, sl], func=AF.Exp,
                                                                          scale=1.0 / 16, bias=nm16[:, p_, c, 1:2]), r=["cs_", "nm16"], w=["E3"])
            S.op("act", lambda e: e.activation(out=E4[:], in_=cs_[:], func=AF.Exp, scale=-1.0 / 16), r=["cs_"], w=["E4"])
            if own:
                for p_ in range(2):
                    for c in range(2):
                        sl = slice(c * 64, (c + 1) * 64)
                        S.op("act", lambda e, p_=p_, c=c, sl=sl: e.activation(out=E1[:, p_, sl], in_=cs_[:, p_, sl], func=AF.Exp,
                                                                              scale=-1.0 / 16, bias=m16[:, p_, c, 0:1]), r=["cs_", "m16"], w=["E1"])
                        S.op("act", lambda e, p_=p_, c=c, sl=sl: e.activation(out=E2[:, p_, sl], in_=cs_[:, p_, sl], func=AF.Exp,
                                                                              scale=1.0 / 16, bias=nm16[:, p_, c, 0:1]), r=["cs_", "nm16"], w=["E2"])
            S.op("dve", lambda e: e.tensor_tensor(out=khT[:], in0=qk[:, 2:4, :], in1=E3[:], op=ALU.mult), r=[qkk, "E3"], w=["khT"])
            for p_ in range(2):
                S.op("pe", lambda e, p_=p_: e.transpose(out=pkh[:, p_, :], in_=khT[:, p_, :], identity=P["ident"][:]),
                     r=["khT", "ident"], w=["b5"])
            S.op("dve", lambda e: e.tensor_scalar(out=kh[:], in0=pkh, scalar1=valid[:, t:t + 1], scalar2=None, op0=ALU.mult),
                 r=["b5", "valid"], w=["kh"])
            if own:
                S.op("dve", lambda e: e.tensor_tensor(out=ktl[:], in0=qk[:, 2:4, :], in1=E2[:], op=ALU.mult), r=[qkk, "E2"], w=["ktl"])
                for p_ in range(2):
                    for hh in range(2):
                        rs64 = slice(hh * 64, (hh + 1) * 64)
                        S.op("dve", lambda e, p_=p_, hh=hh, rs64=rs64: e.scalar_tensor_tensor(
                            out=qtl[rs64, p_, hh, :], in0=qk[rs64, p_, :], scalar=0.125, in1=E1[rs64, p_, :],
                            op0=ALU.mult, op1=ALU.mult), r=[qkk, "E1"], w=["qtl"])
                    for c in range(2):
                        sl = slice(c * 64, (c + 1) * 64)
                        S.op("dve", lambda e, p_=p_, c=c, sl=sl: e.scalar_tensor_tensor(
                            out=qhz[:, p_, c, sl], in0=qk[:, p_, sl], scalar=0.125, in1=E4[:, p_, sl], op0=ALU.mult, op1=ALU.mult),
                            r=[qkk, "E4"], w=["qhz"])
                for hd in range(4):
                    p_ = hd // 2
                    S.op("pe", lambda e, hd=hd, p_=p_: e.matmul(psc[:, hd, :], lhsT=ktl[:, p_, :], rhs=qtl[:, p_, hd % 2, :],
                                                                start=True, stop=True), r=["ktl", "qtl"], w=["b6"])
                S.op("dve", lambda e: e.tensor_tensor(out=sT[:], in0=psc, in1=maskT[:], op=ALU.mult), r=["b6", "maskT"], w=["sT"])
            for c in range(2):
                if own:
                    S.op("act", lambda e, c=c: e.activation(out=sbf[c][:], in_=state[:], func=AF.Copy), r=["state"], w=["sbf%d" % c])
                for p_ in range(2):
                    cs64 = slice(c * 64, (c + 1) * 64)
                    S.op("pe", lambda e, p_=p_, cs64=cs64: e.matmul(pkvs[:, p_, :], lhsT=kh[cs64, p_, :], rhs=vb[cs64, p_ * 256:(p_ + 1) * 256],
                                                                    start=True, stop=True), r=["kh", vbk], w=["b4"])
                for hd in range(4):
                    p_, o_ = hd // 2, (hd % 2) * 64
                    dcol = E4[o_:o_ + 64, p_, c * 64 + 63:c * 64 + 64]
                    S.op("dve", lambda e, hd=hd, p_=p_, o_=o_, dcol=dcol: e.scalar_tensor_tensor(
                        out=state[o_:o_ + 64, hd, :], in0=state[o_:o_ + 64, hd, :], scalar=dcol,
                        in1=pkvs[o_:o_ + 64, p_, (hd % 2) * 128:(hd % 2) * 128 + 128], op0=ALU.mult, op1=ALU.add),
                        r=["state", "E4", "b4"], w=["state"])
            if own:
                for hd in range(4):
                    p_, o_ = hd // 2, (hd % 2) * 64
                    S.op("pe", lambda e, hd=hd: e.matmul(po[:, hd, :], lhsT=sT[:, hd, :], rhs=vb[:, hd * 128:(hd + 1) * 128],
                                                         start=True, stop=False), r=["sT", vbk], w=["b7"])
                    for c in range(2):
                        S.op("pe", lambda e, hd=hd, p_=p_, o_=o_, c=c: e.matmul(
                            po[:, hd, :], lhsT=qhz[o_:o_ + 64, p_, c, :], rhs=sbf[c][o_:o_ + 64, hd, :], start=False, stop=(c == 1)),
                            r=["qhz", "sbf%d" % c], w=["b7"])
                for hd in range(4):
                    S.op("act", lambda e, hd=hd: e.activation(out=sqj[:], in_=po[:, hd, :], func=AF.Square, accum_out=ssq[:, hd:hd + 1]),
                         r=["b7"], w=["sqj", "ssq"])
                S.op("act", lambda e: e.activation(out=ssq[:], in_=ssq[:], func=AF.Ln, scale=1.0 / 128, bias=P["eps"][:]), r=["ssq"], w=["ssq"])
                S.op("act", lambda e: e.activation(out=ssq[:], in_=ssq[:], func=AF.Exp, scale=-0.5), r=["ssq"], w=["ssq"])
                for hd in range(4):
                    S.op("dve", lambda e, hd=hd: e.scalar_tensor_tensor(out=on[:, hd * 128:(hd + 1) * 128], in0=po[:, hd, :], scalar=ssq[:, hd:hd + 1],
                                                                        in1=gng[:], op0=ALU.mult, op1=ALU.mult), r=["b7", "ssq", "gng"], w=["on"])
                S.op("dve", lambda e: e.tensor_tensor(out=oa[:], in0=on[:], in1=rs_[:], op=ALU.mult), r=["on", rsk], w=["oa"])
                if dbg is not None:
                    S.op("dve", lambda e: e.tensor_tensor(out=oaf[:], in0=on[:], in1=rs_[:], op=ALU.mult), r=["on", rsk], w=["oaf"])
                    S.dma("sp", dbg["o_a"][(t - OWN0) * 128:(t - OWN0 + 1) * 128, :], oaf[:], r=["oaf"], w=["dbg_oa"])
                for hd in range(4):
                    S.op("pe", lambda e, hd=hd: e.transpose(out=pmx[:, hd, :], in_=oa[:, hd * 128:(hd + 1) * 128], identity=identb[:]),
                         r=["oa", "identb"], w=["b6"])
                S.op("act", lambda e: e.activation(out=M["mixT"][:, 0:4, (t - OWN0) * 128:(t - OWN0 + 1) * 128], in_=pmx, func=AF.Copy),
                     r=["b6"], w=["mixTa%d" % (t - OWN0)])
            kv4 = kv4s[:].rearrange("p (s g d) -> p s g d", s=4, g=2)
            branches = [(0, 1)] + ([(1, 2)] if t >= 44 else [])
            for br, gi in branches:
                rms_gain_rope(C, S, kv4[:, 2 * br, :, :], kvk, kn[:, br, :, :], "kn%d" % br, 2, M["gain"][:, gi, :], "gain",
                              M["csT"][:, t, :], "Tcs", RT, "rk")
                S.op("pe", lambda e, br=br: e.transpose(out=pkT[:, br, :], in_=kn[:, br, :, :].rearrange("p g d -> p (g d)"), identity=P["ident"][:]),
                     r=["kn%d" % br, "ident"], w=["b5"])
            S.op("act", lambda e: e.activation(out=M["KselT"][:, t * 128:(t + 1) * 128], in_=pkT[:, 0, :], func=AF.Copy),
                 r=["b5"], w=["KselT%d" % t])
            S.op("pool", lambda e: e.tensor_copy(out=M["Vsel"][:, t, :, 0:64], in_=kv4[:, 1, :, :]), r=[kvk], w=["Vsel%d" % t])
            if t >= 44:
                S.op("act", lambda e: e.activation(out=M["KwinT"][:, (t - 44) * 128:(t - 43) * 128], in_=pkT[:, 1, :], func=AF.Copy),
                     r=["b5"], w=["KwinT%d" % (t - 44)])
                S.op("pool", lambda e: e.tensor_copy(out=M["Vwin"][:, t - 44, :, 0:64], in_=kv4[:, 3, :, :]), r=[kvk], w=["Vwin%d" % (t - 44)])

        if tl:
            stage_a(tl[0], S)
        for n_, t in enumerate(tl):
            ra, rb = Rec(), Rec()
            if n_ + 1 < len(tl):
                stage_a(tl[n_ + 1], ra)
            stage_b(t, rb)
            interleave(S, ra, rb)
        if dbg is not None:
            S.wait_all("sp", ["dbg_oa"])
        S.wait_all("act", ["cmp_scr0", "cmp_scr1"])
        S.flush()


def alloc_cmp(C, st):
    M2 = {}
    M2["KcT"] = C.sb(st, "KcT", [128, 512], F32)
    M2["Vc"] = C.sb(st, "Vc", [128, 4, 2, 65], F32)
    M2["cbias"] = C.sb(st, "cbias", [128, 4], F32)
    return M2


def phase_cmp(C, P, M, M2, dr):
    nc, S = C.nc, C.S
    with contextlib.ExitStack() as st:
        KC2 = C.sb(st, "KC2", [128, 2, 2, 8192], BF16)
        w1 = C.sb(st, "w1", [128, 2, 16, 256], BF16)
        w2 = C.sb(st, "w2", [128, 2, 2, 64], BF16)
        pecol = C.sb(st, "pecol", [128, 2, 16], F32)
        pecb = C.sb(st, "pecb", [128, 2, 16], BF16)
        pbias = C.sb(st, "pbias", [128, 2, 2], F32)
        hT = [C.sb(st, "chT%d" % i, [128, 2, 512], BF16) for i in range(2)]
        posb = C.sb(st, "posb", [128, 4], I32)
        csB = C.sb(st, "csB", [128, 4, 16], F32)
        cval = C.sb(st, "cval", [128, 4], F32)
        kcn = C.sb(st, "kcn", [128, 2, 64], F32)
        RT = dict(sq=C.sb(st, "c_sq", [128, 2, 64], F32), ss=C.sb(st, "c_ss", [128, 2], F32),
                  t1=C.sb(st, "c_t1", [128, 2, 16], F32), t2=C.sb(st, "c_t2", [128, 2, 16], F32), eps=P["eps"])
        ph = [bank(C, st, "cph%d" % i) for i in range(2)]
        pb = bank(C, st, "cpb")
        po = [bank(C, st, "cpo%d" % i) for i in range(2)]
        pt = bank(C, st, "cpt")

        for kv in range(2):
            for g in range(2):
                S.dma("sp", KC2[0:64, kv, g, :], dr["cmp_scr"][kv, g * 64:(g + 1) * 64, :], r=["cmp_scr0", "cmp_scr1"], w=["KC2a%d%d" % (kv, g)])
                S.dma("sp", KC2[64:128, kv, g, 0:8191], dr["cmp_scr"][kv, g * 64:(g + 1) * 64, 1:8192], r=["cmp_scr0", "cmp_scr1"],
                      w=["KC2b%d%d" % (kv, g)])
            S.dma("pool", w1[:, kv, :, :], dr["nsa_cmp_w1"][kv].rearrange("(lp p) n -> p lp n", p=128), w=["w1_%d" % kv])
            S.dma("pool", w2[:, kv, :, :], dr["nsa_cmp_w2"][kv].rearrange("(hc p) n -> p hc n", p=128), w=["w2_%d" % kv])
        S.dma("sp", pecol[:], dr["cmp_pe_col"], w=["pecol"])
        S.dma("sp", posb[:], dr["posb_col"], w=["Bpos"])
        S.dma("sp", cval[:], dr["cvalid_col"], w=["cval"])
        S.op("dve", lambda e: e.tensor_copy(out=pecb[:], in_=pecol[:]), r=["pecol"], w=["pecb"])
        S.op("dve", lambda e: e.tensor_scalar(out=M2["cbias"][:], in0=cval[:], scalar1=-1.0, scalar2=1e4, op0=ALU.add, op1=ALU.mult),
             r=["cval"], w=["cbias"])
        S.op("pool", lambda e: e.memset(M2["Vc"][:, :, :, 64:65], 1.0), w=["Vc1"])
        for i in range(2):
            S.op("pool", lambda e, i=i: e.memset(hT[i][:, :, 511:512], 0.0), w=["chT%d" % i])
        rope_tables(C, S, st, posb, 4, M["invf"], csB, "B")
        for kv in range(2):
            for hc in range(2):
                for lp in range(16):
                    S.op("pe", lambda e, kv=kv, hc=hc, lp=lp: e.matmul(
                        pb[:, kv * 2 + hc:kv * 2 + hc + 1], lhsT=w1[:, kv, lp, hc * 128:(hc + 1) * 128], rhs=pecb[:, kv, lp:lp + 1],
                        start=(lp == 0), stop=(lp == 15)), r=["w1_%d" % kv, "pecb"], w=["cpb"])
        S.op("dve", lambda e: e.tensor_copy(out=pbias[:].rearrange("p a b -> p (a b)"), in_=pb[:, 0:4]), r=["cpb"], w=["pbias"])
        n = 0
        for kv in range(2):
            for g in range(2):
                h = hT[n % 2]
                hk = "chT%d" % (n % 2)
                n += 1
                for hc in range(2):
                    p = ph[hc]
                    for lp in range(16):
                        S.op("pe", lambda e, kv=kv, g=g, hc=hc, lp=lp, p=p: e.matmul(
                            p[:, 0:511], lhsT=w1[:, kv, lp, hc * 128:(hc + 1) * 128],
                            rhs=KC2[:, kv, g, 2 * lp:2 * lp + 16 * 510 + 1:16], start=(lp == 0), stop=(lp == 15)),
                            r=["w1_%d" % kv, "KC2a%d%d" % (kv, g), "KC2b%d%d" % (kv, g)], w=["cph%d" % hc])
                    S.op("act", lambda e, hc=hc, h=h, p=p, kv=kv: e.activation(out=h[:, hc, 0:511], in_=p[:, 0:511], func=GELU,
                                                                               bias=pbias[:, kv, hc:hc + 1]), r=["cph%d" % hc, "pbias"], w=[hk])
                for bc in range(4):
                    o = po[kv]
                    ok = "cpo%d" % kv
                    for hc in range(2):
                        S.op("pe", lambda e, kv=kv, g=g, hc=hc, bc=bc, h=h, o=o: e.matmul(
                            o[:, (bc * 2 + g) * 64:(bc * 2 + g + 1) * 64], lhsT=h[:, hc, bc * 128:(bc + 1) * 128], rhs=w2[:, kv, hc, :],
                            start=(hc == 0), stop=(hc == 1)), r=[hk, "w2_%d" % kv], w=[ok])
        pk4 = po[0][:].rearrange("p (b g d) -> p b g d", b=4, g=2)
        pv4 = po[1][:].rearrange("p (b g d) -> p b g d", b=4, g=2)
        ptv = pt[:].rearrange("p (a b) -> p a b", a=4)
        for bc in range(4):
            rms_gain_rope(C, S, pk4[:, bc, :, :], "cpo0", kcn[:], "kcn", 2, M["gain"][:, 0, :], "gain", csB[:, bc, :], "Bcs", RT, "ck")
            S.op("pe", lambda e, bc=bc: e.transpose(out=ptv[:, bc, :], in_=kcn[:].rearrange("p g d -> p (g d)"), identity=P["ident"][:]),
                 r=["kcn", "ident"], w=["cpt"])
        S.op("act", lambda e: e.activation(out=M2["KcT"][:], in_=pt[:], func=AF.Copy), r=["cpt"], w=["KcT"])
        S.op("dve", lambda e: e.tensor_copy(out=M2["Vc"][:, :, :, 0:64], in_=pv4), r=["cpo1"], w=["Vc"])
        S.flush()


NEGB = -30000.0


def phase_nsa(C, P, M, M2, dr, dbg=None, qtiles=None):
    nc, S = C.nc, C.S
    L = 0
    with contextlib.ExitStack() as st:
        wB = C.sb(st, "wB", [128, 8, 536], BF16)
        Esel = C.sb(st, "Esel", [128, 64, 128], BF16)
        cover = C.sb(st, "cover", [128, 4, 129], F32)
        corebias = C.sb(st, "corebias", [128, 128], F32)
        causb = C.sb(st, "causb", [128, 4, 128], BF16)
        winb = C.sb(st, "winb", [128, 4, 128], BF16)
        identb = C.sb(st, "identb", [128, 128], BF16)
        kbC = C.sb(st, "kbC", [128, NWT], F32)
        cbC = C.sb(st, "cbC", [128, 4], F32)
        gm = C.sb(st, "gm", [128, 4], F32)
        Cc = C.sb(st, "Cc", [128, 1], F32)
        xt = [C.sb(st, "xt%d" % i, [128, D], F32) for i in range(2)]
        tmpn = dict(junk=C.sb(st, "junk", [128, D], BF16), ss=C.sb(st, "ss", [128, 1], F32),
                    rs=C.sb(st, "rs", [128, 1], F32), xn=C.sb(st, "xn", [128, D], F32), eps=P["eps"], lnexp=True)
        hT = C.sb(st, "hT", [128, 8, 128], BF16)
        qn = C.sb(st, "qn", [128, 8, 64], F32)
        RT = dict(sq=C.sb(st, "q_sq", [128, 8, 64], F32), ss=C.sb(st, "q_ss", [128, 8], F32),
                  t1=C.sb(st, "q_t1", [128, 8, 16], F32), t2=C.sb(st, "q_t2", [128, 8, 16], F32), eps=P["eps"])
        QT32 = C.sb(st, "QT32", [128, 2, 4, 128], F32)
        QTb = C.sb(st, "QTb", [128, 2, 4, 128], BF16)
        gts2 = [C.sb(st, "gts%d" % i, [128, 24], F32) for i in range(2)]
        cm = [C.sb(st, "cm%d" % i, [128, 4, 128], F32) for i in range(2)]
        sbias = [C.sb(st, "sbias%d" % i, [128, 128], F32) for i in range(2)]
        PcT = [C.sb(st, "PcT%d" % i, [128, 4, 128], F32) for i in range(4)]
        PTs = [[C.sb(st, "PT%d_%d" % (g_, i), [128, 512], BF16) for i in range(3)] for g_ in range(2)]
        oTs = [[C.sb(st, "oTs%d%d" % (g_, i), [65, 512], F32) for i in range(2)] for g_ in range(2)]
        rden = C.sb(st, "rden", [128, 4], F32)
        acc = C.sb(st, "acc", [128, 128], F32)
        m8a = C.sb(st, "m8a", [128, 8], F32)
        m8b = C.sb(st, "m8b", [128, 8], F32)
        sc2 = C.sb(st, "sc2", [128, 128], F32)
        selm = C.sb(st, "selm", [128, 128], F32)
        selv = C.sb(st, "selv", [128, 128], F32)
        NBt2 = [C.sb(st, "NBt%d" % i, [128, 4, 128], BF16) for i in range(2)]
        oT = C.sb(st, "oT", [65, 512], F32)
        ob2 = [C.sb(st, "ob%d" % i, [128, 512], F32) for i in range(2)]
        obb = C.sb(st, "obb", [128, 512], BF16)
        coef = C.sb(st, "coef", [128, 4], F32)
        bks = [bank(C, st, "nb%d" % i) for i in range(8)]
        v3 = lambda a, n: a.rearrange("p (a b) -> p a b", a=n)
        pT0, pT1 = v3(bks[0][:], 4), v3(bks[1][:], 4)
        pq = bks[2]
        pqT = v3(bks[3][:], 4)
        pS = [bks[4], bks[5]]
        pO = bks[6]
        pX = bks[7]
        pW = bks[1]
        pS3 = [bks[4], bks[5], bks[3]]
        pS3k = ["nb4", "nb5", "nb3"]

        S.dma("pool", wB[:, :, 0:512], dr["w_nq_perm"].rearrange("(k p) n -> p k n", p=128), w=["wB0"])
        S.dma("pool", wB[:, :, 512:536], dr["w_in_ab"][:, 2832:2856].rearrange("(k p) n -> p k n", p=128), w=["wB1"])
        S.dma("sp", Esel[:], dr["Esel"], w=["Esel"])
        S.dma("sp", cover[:], dr["cover"], w=["cover"])
        S.dma("sp", corebias[:], dr["corebias"], w=["corebias"])
        S.dma("sp", causb[:], dr["causb"], w=["causb"])
        S.dma("sp", winb[:], dr["winb"], w=["winb"])
        S.op("dve", lambda e: e.tensor_copy(out=identb[:], in_=P["ident"][:]), r=["ident"], w=["identb"])
        S.op("pool", lambda e: e.memset(QT32[:], 0.0), w=["QT32"])
        S.op("pool", lambda e: e.memset(QTb[:], 0.0), w=["QTb"])
        S.op("dve", lambda e: e.tensor_reduce(out=gm[:], in_=M["gain"][:], axis=AX.X, op=ALU.max, apply_absolute_value=True),
             r=["gain", "gainq"], w=["gm"])
        S.op("dve", lambda e: e.tensor_reduce(out=Cc[:], in_=gm[:, 0:3], axis=AX.X, op=ALU.max), r=["gm"], w=["Cc"])
        S.op("dve", lambda e: e.tensor_scalar(out=Cc[:], in0=Cc[:], scalar1=gm[:, 3:4], scalar2=-8.0, op0=ALU.mult, op1=ALU.mult),
             r=["Cc", "gm"], w=["Cc"])
        S.op("dve", lambda e: e.tensor_scalar(out=kbC[:], in0=M["kbias"][:], scalar1=Cc[:, 0:1], scalar2=None, op0=ALU.add),
             r=["kbias", "Cc"], w=["kbC"])
        S.op("dve", lambda e: e.tensor_scalar(out=cbC[:], in0=M2["cbias"][:], scalar1=Cc[:, 0:1], scalar2=None, op0=ALU.add),
             r=["cbias", "Cc"], w=["cbC"])

        A, B = P["modA"], P["modB"]
        nS = 0
        nP = 0
        def pro(qt, S):
            T = OWN0 + qt
            i = qt % 2
            gts = gts2[i]
            ob = ob2[i]
            gk = "gts%d" % i
            obk = "ob%d" % i
            S.dma("sp", xt[i][:], dr["xw"][T * 128:(T + 1) * 128, :], w=["xt%d" % i])
            S.dma("sp", cm[i][:], dr["cmaskT"][qt], w=["cm%d" % i])
            S.dma("sp", sbias[i][:], dr["selbias"][qt], w=["sbias%d" % i])
            norm_transpose(C, S, xt[i][:], "xt%d" % i, tmpn, P["ident"], [(pT0, "nb0"), (pT1, "nb1")], "n")
            for k in range(8):
                p, pk = ((pT0, "nb0"), (pT1, "nb1"))[k // 4]
                if k % 2 == 0:
                    S.op("dve", lambda e, k=k, p=p: e.tensor_scalar(
                        out=hT[:, k, :], in0=p[:, k % 4, :], scalar1=A[:, L, 0, k:k + 1], scalar2=B[:, L, 0, k:k + 1],
                        op0=ALU.mult, op1=ALU.add), r=[pk, "modA", "modB"], w=["hT"])
                else:
                    S.op("act", lambda e, k=k, p=p: e.activation(
                        out=hT[:, k, :], in_=p[:, k % 4, :], func=AF.Identity, scale=A[:, L, 0, k:k + 1],
                        bias=B[:, L, 0, k:k + 1]), r=[pk, "modA", "modB"], w=["hT"])
            for k in range(8):
                S.op("pe", lambda e, k=k: e.matmul(pq[:], lhsT=hT[:, k, :], rhs=wB[:, k, 0:512], start=(k == 0), stop=(k == 7)),
                     r=["hT", "wB0"], w=["nb2"])
            for k in range(8):
                S.op("pe", lambda e, k=k: e.matmul(bks[0][:, 0:24], lhsT=hT[:, k, :], rhs=wB[:, k, 512:536], start=(k == 0), stop=(k == 7)),
                     r=["hT", "wB1"], w=["nb0"])
            S.op("act", lambda e: e.activation(out=gts[:], in_=bks[0][:, 0:24], func=AF.Exp, scale=-1.0), r=["nb0"], w=[gk])
            S.op("dve", lambda e: e.tensor_scalar(out=gts[:], in0=gts[:], scalar1=1.0, scalar2=None, op0=ALU.add), r=[gk], w=[gk])
            S.op("dve", lambda e: e.reciprocal(out=gts[:], in_=gts[:]), r=[gk], w=[gk])
            rms_gain_rope(C, S, pq[:].rearrange("p (h d) -> p h d", h=8), "nb2", qn[:], "qn", 8, M["gain"][:, 3, :], "gainq",
                          M["csT"][:, T, :], "Tcs", RT, "rq")
            for hh in range(4):
                S.op("pe", lambda e, hh=hh: e.transpose(out=pqT[:, hh, :], in_=qn[:, 2 * hh:2 * hh + 2, :].rearrange("p g d -> p (g d)"),
                                                        identity=P["ident"][:]), r=["qn", "ident"], w=["nb3"])
            for g in range(2):
                rg = slice(g * 64, (g + 1) * 64)
                S.op("dve", lambda e, g=g, rg=rg: e.tensor_copy(out=QT32[rg, g, :, :], in_=pqT[rg, :, :]), r=["nb3"], w=["QT32"])
                S.op("dve", lambda e, g=g, rg=rg: e.tensor_copy(out=QTb[rg, g, :, :], in_=pqT[rg, :, :]), r=["nb3"], w=["QTb"])
            S.op("dve", lambda e: e.memset(ob[:], 0.0), w=[obk])


        qlist = list(qtiles if qtiles is not None else range(NT))
        pro(qlist[0], S)
        for qi, qt in enumerate(qlist):
            T = OWN0 + qt
            i = qt % 2
            gts = gts2[i]
            ob = ob2[i]
            gk = "gts%d" % i
            obk = "ob%d" % i
            def finish_branch(g, br):
                S.op("act", lambda e: e.activation(out=oT[:], in_=pO[0:65, :], func=AF.Copy), r=["nb6"], w=["oT"])
                pXv = pX[:, 0:260].rearrange("p (h d) -> p h d", h=4)
                for hh in range(4):
                    S.op("pe", lambda e, hh=hh: e.transpose(out=pXv[:, hh, :], in_=oT[:, hh * 128:(hh + 1) * 128], identity=P["ident"][0:65, 0:65]),
                         r=["oT", "ident"], w=["nb7"])
                S.op("dve", lambda e: e.tensor_scalar(out=coef[:], in0=pXv[:, :, 64], scalar1=1e-30, scalar2=None, op0=ALU.max), r=["nb7"], w=["coef"])
                S.op("dve", lambda e: e.reciprocal(out=coef[:], in_=coef[:]), r=["coef"], w=["coef"])
                gv = gts[:].rearrange("p (g h b) -> p g h b", g=2, h=4)
                S.op("dve", lambda e: e.tensor_tensor(out=coef[:], in0=coef[:], in1=gv[:, g, :, br], op=ALU.mult), r=["coef", gk], w=["coef"])
                for hh in range(4):
                    c0 = (g * 4 + hh) * 64
                    S.op("dve", lambda e, hh=hh, c0=c0: e.scalar_tensor_tensor(
                        out=ob[:, c0:c0 + 64], in0=pXv[:, hh, 0:64], scalar=coef[:, hh:hh + 1], in1=ob[:, c0:c0 + 64],
                        op0=ALU.mult, op1=ALU.add), r=["nb7", "coef", obk], w=[obk])

            def finish_branch2(g, br, pacc, pacck):
                S.op("act", lambda e: e.activation(out=oT[:], in_=pacc[0:65, :], func=AF.Copy), r=[pacck], w=["oT"])
                pXv = pX[:, 0:260].rearrange("p (h d) -> p h d", h=4)
                for hh in range(4):
                    S.op("pe", lambda e, hh=hh: e.transpose(out=pXv[:, hh, :], in_=oT[:, hh * 128:(hh + 1) * 128], identity=P["ident"][0:65, 0:65]),
                         r=["oT", "ident"], w=["nb7"])
                S.op("dve", lambda e: e.tensor_scalar(out=coef[:], in0=pXv[:, :, 64], scalar1=1e-30, scalar2=None, op0=ALU.max), r=["nb7"], w=["coef"])
                S.op("dve", lambda e: e.reciprocal(out=coef[:], in_=coef[:]), r=["coef"], w=["coef"])
                gv = gts[:].rearrange("p (g h b) -> p g h b", g=2, h=4)
                S.op("dve", lambda e: e.tensor_tensor(out=coef[:], in0=coef[:], in1=gv[:, g, :, br], op=ALU.mult), r=["coef", gk], w=["coef"])
                for hh in range(4):
                    c0 = (g * 4 + hh) * 64
                    S.op("dve", lambda e, hh=hh, c0=c0: e.scalar_tensor_tensor(
                        out=ob[:, c0:c0 + 64], in0=pXv[:, hh, 0:64], scalar=coef[:, hh:hh + 1], in1=ob[:, c0:c0 + 64],
                        op0=ALU.mult, op1=ALU.add), r=["nb7", "coef", obk], w=[obk])

            pI2 = [[(bks[0], "nb0"), (bks[1], "nb1")], [(bks[2], "nb2"), (bks[3], "nb3")]]
            pI = [bks[0], bks[1]]
            for g in range(2):
                for bc in range(4):
                    j = nS % 2
                    nS += 1
                    pc = PcT[bc]
                    pck = "PcT%d" % bc
                    S.op("pe", lambda e, g=g, bc=bc, j=j: e.matmul(pS[j][:], lhsT=M2["KcT"][:, bc * 128:(bc + 1) * 128],
                                                                   rhs=QT32[:, g, :, :], start=True, stop=True),
                         r=["KcT", "QT32"], w=["nb%d" % (4 + j)])
                    S.op("act", lambda e, bc=bc, j=j, pc=pc: e.activation(out=pc[:].rearrange("p a b -> p (a b)"), in_=pS[j][:], func=AF.Exp,
                                                                          scale=0.125, bias=cbC[:, bc:bc + 1]),
                         r=["nb%d" % (4 + j), "cbC"], w=[pck])
                    S.op("dve", lambda e, bc=bc, pc=pc, i=i: e.tensor_tensor(out=pc[:], in0=pc[:],
                                                                             in1=cm[i][:, bc, :].unsqueeze(1).to_broadcast([128, 4, 128]), op=ALU.mult),
                         r=[pck, "cm%d" % i], w=[pck])
                for bc in range(4):
                    pc = PcT[bc]
                    pck = "PcT%d" % bc
                    S.op("pe", lambda e, g=g, bc=bc, pc=pc: e.matmul(pO[0:65, :], lhsT=M2["Vc"][:, bc, g, :], rhs=pc[:].rearrange("p a b -> p (a b)"),
                                                                     start=(bc == 0), stop=(bc == 3)), r=[pck, "Vc", "Vc1"], w=["nb6"])
                for hh in range(4):
                    pi, pik = pI2[g][hh // 2]
                    for bc in range(4):
                        pc = PcT[bc]
                        pck = "PcT%d" % bc
                        S.op("pe", lambda e, bc=bc, pc=pc, hh=hh, pi=pi: e.matmul(
                            pi[:, (hh % 2) * 129:(hh % 2) * 129 + 129], lhsT=pc[:, hh, :], rhs=cover[:, bc, :],
                            start=(bc == 0), stop=(bc == 3)), r=[pck, "cover"], w=[pik])
                finish_branch2(g, 0, pO, "nb6")
            for g in range(2):
                for hh in range(4):
                    pi, pik = pI2[g][hh // 2]
                    S.op("dve", lambda e, hh=hh, pi=pi: e.tensor_scalar(out=rden[:, hh:hh + 1], in0=pi[:, (hh % 2) * 129 + 128:(hh % 2) * 129 + 129],
                                                                        scalar1=1e-30, scalar2=None, op0=ALU.max), r=[pik], w=["rden"])
                S.op("dve", lambda e: e.reciprocal(out=rden[:], in_=rden[:]), r=["rden"], w=["rden"])
                for hh in range(4):
                    pi, pik = pI2[g][hh // 2]
                    src = pi[:, (hh % 2) * 129:(hh % 2) * 129 + 128]
                    if hh == 0:
                        S.op("dve", lambda e, src=src: e.scalar_tensor_tensor(out=acc[:], in0=src, scalar=rden[:, 0:1], in1=sbias[i][:],
                                                                              op0=ALU.mult, op1=ALU.add), r=[pik, "rden", "sbias%d" % i], w=["acc"])
                    else:
                        S.op("dve", lambda e, src=src, hh=hh: e.scalar_tensor_tensor(out=acc[:], in0=src, scalar=rden[:, hh:hh + 1], in1=acc[:],
                                                                                     op0=ALU.mult, op1=ALU.add), r=[pik, "rden", "acc"], w=["acc"])
                S.op("dve", lambda e: e.tensor_tensor(out=acc[:], in0=acc[:], in1=corebias[:], op=ALU.add), r=["acc", "corebias"], w=["acc"])
                S.op("dve", lambda e: e.max(out=m8a[:], in_=acc[:]), r=["acc"], w=["m8a"])
                S.op("dve", lambda e: e.match_replace(out=sc2[:], in_to_replace=m8a[:], in_values=acc[:], imm_value=-3e38),
                     r=["acc", "m8a"], w=["sc2"])
                S.op("dve", lambda e: e.max(out=m8b[:], in_=sc2[:]), r=["sc2"], w=["m8b"])
                S.op("dve", lambda e: e.tensor_scalar(out=selm[:], in0=acc[:], scalar1=m8b[:, 7:8], scalar2=None, op0=ALU.is_ge),
                     r=["acc", "m8b"], w=["selm"])
                S.op("dve", lambda e: e.tensor_scalar(out=selv[:], in0=acc[:], scalar1=-1e29, scalar2=None, op0=ALU.is_gt), r=["acc"], w=["selv"])
                S.op("dve", lambda e: e.tensor_tensor(out=selm[:], in0=selm[:], in1=selv[:], op=ALU.mult), r=["selm", "selv"], w=["selm"])
                if dbg is not None and "selm" in dbg:
                    S.dma("sp", dbg["selm"][qt, g], selm[:], r=["selm"], w=["dbg_selm"])
                S.op("pe", lambda e: e.transpose(out=pX[:, 384:512], in_=selm[:], identity=P["ident"][:]), r=["selm", "ident"], w=["nb7"])
                S.op("dve", lambda e, g=g: e.tensor_scalar(out=NBt2[g][:], in0=pX[:, 384:512].unsqueeze(1).to_broadcast([128, 4, 128]), scalar1=-1.0,
                                                           scalar2=-NEGB, op0=ALU.add, op1=ALU.mult), r=["nb7"], w=["NBt%d" % g])
            def finish_tail(g, br, src):
                pXv = pX[:, 0:260].rearrange("p (h d) -> p h d", h=4)
                srck = "oTs%d%d" % (g, br)
                for hh in range(4):
                    S.op("pe", lambda e, hh=hh: e.transpose(out=pXv[:, hh, :], in_=src[:, hh * 128:(hh + 1) * 128], identity=P["ident"][0:65, 0:65]),
                         r=[srck, "ident"], w=["nb7"])
                S.op("dve", lambda e: e.tensor_scalar(out=coef[:], in0=pXv[:, :, 64], scalar1=1e-30, scalar2=None, op0=ALU.max), r=["nb7"], w=["coef"])
                S.op("dve", lambda e: e.reciprocal(out=coef[:], in_=coef[:]), r=["coef"], w=["coef"])
                gv = gts[:].rearrange("p (g h b) -> p g h b", g=2, h=4)
                S.op("dve", lambda e: e.tensor_tensor(out=coef[:], in0=coef[:], in1=gv[:, g, :, br], op=ALU.mult), r=["coef", gk], w=["coef"])
                for hh in range(4):
                    c0 = (g * 4 + hh) * 64
                    S.op("dve", lambda e, hh=hh, c0=c0: e.scalar_tensor_tensor(
                        out=ob[:, c0:c0 + 64], in0=pXv[:, hh, 0:64], scalar=coef[:, hh:hh + 1], in1=ob[:, c0:c0 + 64],
                        op0=ALU.mult, op1=ALU.add), r=["nb7", "coef", obk], w=[obk])

            def stream(g, R):
                psb = [(bks[4], "nb4"), (bks[5], "nb5")] if g == 0 else [(bks[2], "nb2"), (bks[3], "nb3")]
                pOW, pOWk = (bks[6], "nb6") if g == 0 else (bks[1], "nb1")
                chunks = [("sel", kc) for kc in range(T + 1)] + [("win", wi) for wi in range(5)]
                nck = len(chunks)

                def emit_S(ci):
                    kind, a_ = chunks[ci]
                    ps, psk = psb[ci % 2]
                    if kind == "sel":
                        kc = a_
                        last = (kc == T)
                        R.op("pe", lambda e: e.matmul(ps[:], lhsT=M["KselT"][:, kc * 128:(kc + 1) * 128], rhs=QTb[:, g, :, :], start=True, stop=False),
                             r=["KselT%d" % kc, "QTb"], w=[psk])
                        R.op("pe", lambda e: e.matmul(ps[:], lhsT=Esel[:, kc, :], rhs=NBt2[g][:], start=False, stop=(not last)),
                             r=["Esel", "NBt%d" % g], w=[psk])
                        if last:
                            R.op("pe", lambda e: e.matmul(ps[:], lhsT=identb[:], rhs=causb[:], start=False, stop=True),
                                 r=["identb", "causb"], w=[psk])
                    else:
                        wi = a_
                        kc = T - 4 + wi
                        edge = wi in (0, 4)
                        R.op("pe", lambda e: e.matmul(ps[:], lhsT=M["KwinT"][:, (kc - 44) * 128:(kc - 43) * 128], rhs=QTb[:, g, :, :],
                                                      start=True, stop=(not edge)), r=["KwinT%d" % (kc - 44), "QTb"], w=[psk])
                        if edge:
                            mb = winb if wi == 0 else causb
                            R.op("pe", lambda e: e.matmul(ps[:], lhsT=identb[:], rhs=mb[:], start=False, stop=True),
                                 r=["identb", "causb", "winb"], w=[psk])

                def emit_PV(ci):
                    kind, a_ = chunks[ci]
                    ps, psk = psb[ci % 2]
                    pt_ = PTs[g][ci % 3]
                    ptk = "PT%d_%d" % (g, ci % 3)
                    kc = a_ if kind == "sel" else T - 4 + a_
                    R.op("act", lambda e: e.activation(out=pt_[:], in_=ps[:], func=AF.Exp, scale=0.125, bias=kbC[:, kc:kc + 1]),
                         r=[psk, "kbC"], w=[ptk])
                    if kind == "sel":
                        R.op("pe", lambda e: e.matmul(pOW[0:65, :], lhsT=M["Vsel"][:, kc, g, :], rhs=pt_[:], start=(kc == 0), stop=(kc == T)),
                             r=[ptk, "Vsel%d" % kc, "Vsel1"], w=[pOWk])
                        if kc == T:
                            R.op("act", lambda e: e.activation(out=oTs[g][0][:], in_=pOW[0:65, :], func=AF.Copy), r=[pOWk], w=["oTs%d0" % g])
                    else:
                        R.op("pe", lambda e: e.matmul(pOW[0:65, :], lhsT=M["Vwin"][:, kc - 44, g, :], rhs=pt_[:], start=(a_ == 0), stop=(a_ == 4)),
                             r=[ptk, "Vwin%d" % (kc - 44), "Vwin1"], w=[pOWk])
                        if a_ == 4:
                            R.op("act", lambda e: e.activation(out=oTs[g][1][:], in_=pOW[0:65, :], func=AF.Copy), r=[pOWk], w=["oTs%d1" % g])

                emit_S(0)
                for ci in range(nck):
                    if ci + 1 < nck:
                        emit_S(ci + 1)
                    emit_PV(ci)

            r0, r1 = Rec(), Rec()
            stream(0, r0)
            stream(1, r1)
            interleave(S, r0, r1)
            S_main = S
            S = Rec()
            for g in range(2):
                finish_tail(g, 1, oTs[g][0])
                finish_tail(g, 2, oTs[g][1])
            if dbg is not None and "o_b" in dbg:
                S.dma("sp", dbg["o_b"][qt * 128:(qt + 1) * 128, :], ob[:], r=[obk], w=["dbg_ob"])
            S.op("act", lambda e: e.activation(out=obb[:], in_=ob[:], func=AF.Copy), r=[obk], w=["obb"])
            pmx = bks[7][:, 0:256].bitcast(BF16).rearrange("p (a b) -> p a b", a=4)
            for hh in range(4):
                S.op("pe", lambda e, hh=hh: e.transpose(out=pmx[:, hh, :], in_=obb[:, hh * 128:(hh + 1) * 128], identity=identb[:]),
                     r=["obb", "identb"], w=["nb7"])
            S.op("act", lambda e, qt=qt: e.activation(out=M["mixT"][:, 4:8, qt * 128:(qt + 1) * 128], in_=pmx, func=AF.Copy),
                 r=["nb7"], w=["mixTb%d" % qt])
            tail = S
            S = S_main
            nxt = Rec()
            if qi + 1 < len(qlist):
                pro(qlist[qi + 1], nxt)
            interleave(S, tail, nxt)
        if dbg is not None:
            S.wait_all("sp", ["dbg_ob", "dbg_selm"])
        S.flush()


def phase_outproj(C, P, M, dr, x_out):
    nc, S = C.nc, C.S
    L = 0
    with contextlib.ExitStack() as st:
        wo = C.sb(st, "wo", [128, 8, D], BF16)
        xt = [C.sb(st, "xt%d" % i, [128, D], F32) for i in range(2)]
        xo = [C.sb(st, "xo%d" % i, [128, D], F32) for i in range(2)]
        ev = C.sb(st, "ev", [128, 512], F32)
        py = [bank(C, st, "opy%d" % i) for i in range(2)]
        S.dma("pool", wo[:], dr["w_out_ab"].rearrange("(k p) n -> p k n", p=128), w=["wo"])
        gate = P["gates"]
        for t in range(NT):
            i = t % 2
            S.dma("sp", xt[i][:], dr["xw"][(OWN0 + t) * 128:(OWN0 + t + 1) * 128, :], w=["xt%d" % i])
            for half in range(2):
                for k in range(8):
                    S.op("pe", lambda e, k=k, half=half, t=t: e.matmul(py[half][:], lhsT=M["mixT"][:, k, t * 128:(t + 1) * 128],
                                                                       rhs=wo[:, k, half * 512:(half + 1) * 512], start=(k == 0), stop=(k == 7)),
                         r=["wo", "mixTa%d" % t, "mixTb%d" % t], w=["opy%d" % half])
                S.op("dve", lambda e, half=half: e.tensor_tensor(out=ev[:], in0=py[half][:], in1=gate[:, L, 0, half * 512:(half + 1) * 512], op=ALU.mult),
                     r=["opy%d" % half, "gates"], w=["ev"])
                S.op("dve", lambda e, half=half, i=i: e.tensor_tensor(out=xo[i][:, half * 512:(half + 1) * 512], in0=ev[:],
                                                                      in1=xt[i][:, half * 512:(half + 1) * 512], op=ALU.add),
                     r=["ev", "xt%d" % i], w=["xo%d_%d" % (i, half)])
            S.dma("sp", x_out[t * 128:(t + 1) * 128, :], xo[i][:], r=["xo%d_0" % i, "xo%d_1" % i], w=["xo_%s_%d" % (x_out.name, i)])
        S.wait_all("sp", ["xo_%s_%d" % (x_out.name, j) for j in range(2)])
        S.flush()


IN_SPECS = [("ident", [128, 128], F32), ("c_col", [128, 8], F32), ("b_ada_col", [128, 2, 48], F32), ("norm_g_col", [128, 2, 2, 8], F32),
            ("w_ada", [2, 1024, 6144], F32), ("b_ada", [2, 6144], F32),
            ("xw", [8192, 1024], F32), ("w_in_ab", [1024, 2856], F32), ("gla_w_gate2", [16, 256], F32), ("gla_b_gate_col", [128, 2], F32),
            ("gla_norm_g", [128], F32), ("gla_maskT", [128, 4, 128], F32), ("valid_col", [128, 64], F32), ("invf", [8], F32),
            ("nsa_k_gain", [3, 64], F32), ("nsa_q_gain", [64], F32), ("pos_col", [128, 64], I32),
            ("nsa_cmp_w1", [2, 2048, 256], F32), ("nsa_cmp_w2", [2, 256, 64], F32), ("cmp_pe_col", [128, 2, 16], F32),
            ("posb_col", [128, 4], I32), ("cvalid_col", [128, 4], F32), ("w_nq_perm", [1024, 512], F32), ("Esel", [128, 64, 128], BF16),
            ("cover", [128, 4, 129], F32), ("corebias", [128, 128], F32), ("causb", [128, 4, 128], BF16), ("winb", [128, 4, 128], BF16),
            ("cmaskT", [16, 128, 4, 128], F32), ("selbias", [16, 128, 128], F32), ("w_out_ab", [1024, 1024], F32),
            ("w_in_c", [1024, 4096], F32), ("w_out_c", [2048, 1024], F32), ("sgu_w_s", [8, 128, 128], F32), ("sgu_b_s", [8, 128], F32),
            ("sgu_norm_g", [2048], F32), ("tril", [128, 128], F32),
            ("w_router", [1024, 16], F32), ("router_bias", [16], F32),
            ("w_exp", [2, 16, 128, 12288], F32)]


def build_full(upto=4):
    C = Ctx()
    dr = {n: C.dram_in(n, s, dt) for n, s, dt in IN_SPECS}
    dr["cmp_scr"] = C.dram_tmp("cmp_scr", [2, 128, 8192], BF16)
    xs = [C.dram_tmp("xs%d" % i, [TOK, D], F32) for i in range(3)]
    y = C.dram_out("y", [TOK, D])
    dst = lambda i: y if upto == i + 1 else xs[i]
    P = alloc_persistent(C)
    phase_init(C, P, dr)
    phase_ada(C, P, dr)
    mst = contextlib.ExitStack()
    M = alloc_mixer(C, mst)
    phase_mix_a(C, P, M, dr)
    M2 = alloc_cmp(C, mst)
    phase_cmp(C, P, M, M2, dr)
    phase_nsa(C, P, M, M2, dr)
    phase_outproj(C, P, M, dr, dst(0))
    mst.close()
    if upto >= 2:
        phase_moe(C, P, dr, 0, xs[0], dst(1))
    if upto >= 3:
        phase_gmlp(C, P, dr, xs[1], dst(2))
    if upto >= 4:
        phase_moe(C, P, dr, 1, xs[2], y)
    C.outer.close()
    return C.nc


def _col(v):
    return np.ascontiguousarray(np.asarray(v).reshape(-1, 128).T)


def _shared_inputs(d):
    f32 = np.float32
    sh = {}
    sh["ident"] = np.eye(128, dtype=f32)
    sh["b_ada_col"] = np.ascontiguousarray(d["b_ada"].reshape(2, 48, 128).transpose(2, 0, 1))
    sh["norm_g_col"] = np.ascontiguousarray(d["norm_g"].reshape(2, 2, 8, 128).transpose(3, 0, 1, 2))
    sh["w_ada"] = d["w_ada"]
    sh["b_ada"] = d["b_ada"]
    w_in = d["w_in_ab"][0]
    sh["w_in_ab"] = w_in
    sh["gla_w_gate2"] = d["gla_w_gate2"][0]
    sh["gla_b_gate_col"] = _col(d["gla_b_gate"][0])
    sh["gla_norm_g"] = d["gla_norm_g"][0]
    j = np.arange(128)[:, None]
    i = np.arange(128)[None, :]
    m = ((j // 64 == i // 64) & (j <= i)).astype(f32)
    sh["gla_maskT"] = np.ascontiguousarray(np.repeat(m[:, None, :], 4, axis=1))
    sh["invf"] = (f32(500000.0) ** (-np.arange(8, dtype=f32) / f32(8))).astype(f32)
    sh["nsa_k_gain"] = d["nsa_k_gain"][0]
    sh["nsa_q_gain"] = d["nsa_q_gain"][0]
    sh["nsa_cmp_w1"] = d["nsa_cmp_w1"][0]
    sh["nsa_cmp_w2"] = d["nsa_cmp_w2"][0]
    sh["cmp_pe_col"] = np.ascontiguousarray(d["nsa_cmp_pe"][0].reshape(2, 16, 128).transpose(2, 0, 1))
    nq = w_in[:, 1552:2064].reshape(1024, 2, 4, 64)
    sh["w_nq_perm"] = np.ascontiguousarray(nq.transpose(0, 2, 1, 3).reshape(1024, 512))
    E = np.zeros((128, 64, 128), f32)
    for c in range(64):
        E[2 * c, c, 0:64] = 1.0
        E[2 * c + 1, c, 64:128] = 1.0
    sh["Esel"] = E.astype(ml_dtypes.bfloat16)
    blk = np.arange(512)
    jj = np.arange(128)
    cov = np.zeros((512, 129), f32)
    cov[:, :128] = ((16 * blk[:, None] < 64 * jj[None, :] + 64) & (16 * blk[:, None] + 32 > 64 * jj[None, :])).astype(f32)
    cov[:, 128] = 1.0
    cov[511] = 0.0
    sh["cover"] = np.ascontiguousarray(cov.reshape(4, 128, 129).transpose(1, 0, 2))
    cm = np.zeros((16, 128, 4, 128), f32)
    sb = np.zeros((16, 128, 128), f32)
    ii = np.arange(128)
    for qt in range(16):
        wq = (OWN0 + qt) * 128 + ii
        for bc in range(4):
            b_ = bc * 128 + np.arange(128)
            cm[qt, :, bc, :] = ((16 * b_ + 31)[:, None] <= wq[None, :]).astype(f32)
        cur = wq // 64
        forced = (jj[None, :] == cur[:, None]) | (jj[None, :] == cur[:, None] - 1)
        inval = jj[None, :] > cur[:, None]
        sb[qt] = np.where(inval, -1e30, np.where(forced, 1e4, 0.0))
    sh["cmaskT"] = cm
    sh["selbias"] = sb
    caus = np.where(j <= i, 0.0, NEGB).astype(f32)
    win = np.where(j > i, 0.0, NEGB).astype(f32)
    rep4 = lambda a: np.ascontiguousarray(np.repeat(a[:, None, :], 4, axis=1))
    sh["causb"] = rep4(caus).astype(ml_dtypes.bfloat16)
    sh["winb"] = rep4(win).astype(ml_dtypes.bfloat16)
    sh["w_out_ab"] = d["w_out_ab"][0]
    sh["w_in_c"] = d["w_in_c"][0]
    sh["w_out_c"] = d["w_out_c"][0]
    sh["sgu_w_s"] = d["sgu_w_s"][0]
    sh["sgu_b_s"] = d["sgu_b_s"][0]
    sh["sgu_norm_g"] = d["sgu_norm_g"][0]
    sh["tril"] = np.tril(np.ones((128, 128), f32))
    for k in ("w_router", "router_bias"):
        sh[k] = d[k]
    wg_ = d["w_gate"].reshape(2, 16, 8, 128, 512).transpose(0, 1, 3, 2, 4).reshape(2, 16, 128, 4096)
    wu_ = d["w_up"].reshape(2, 16, 8, 128, 512).transpose(0, 1, 3, 2, 4).reshape(2, 16, 128, 4096)
    wd_ = d["w_down"].reshape(2, 16, 4, 128, 1024).transpose(0, 1, 3, 2, 4).reshape(2, 16, 128, 4096)
    sh["w_exp"] = np.ascontiguousarray(np.concatenate([wg_, wu_, wd_], axis=-1))
    return sh


def _core_inputs(d, core):
    f32 = np.float32
    b, qtr = core // 4, core % 4
    own = qtr * TOK
    a = np.arange(8192) - 6144 + own
    ok = a >= 0
    xw = np.zeros((8192, D), f32)
    xw[ok] = d["x"][b, a[ok]]
    pos = np.zeros(8192, np.int32)
    pos[ok] = d["positions"][b, a[ok]]
    co = {"xw": xw, "pos_col": _col(pos), "valid_col": _col(ok.astype(f32)), "c_col": _col(d["c"][b])}
    first_blk = 96 - 32 * qtr
    jj = np.arange(128)
    row = np.where(jj < first_blk, -1e30, np.where(jj == first_blk, 1e4, 0.0)).astype(f32)
    co["corebias"] = np.ascontiguousarray(np.broadcast_to(row, (128, 128)))
    blk = np.arange(512)
    a_start = 16 * blk - 6144 + own
    cvalid = ((a_start >= 0) & (blk <= 510))
    a_end = np.clip(16 * blk + 31 - 6144 + own, 0, 8191)
    posb = np.where(cvalid, d["positions"][b, a_end], 0).astype(np.int32)
    co["cvalid_col"] = _col(cvalid.astype(f32))
    co["posb_col"] = _col(posb)
    return co


_NC_CACHE = {}


def kernel(**inputs):
    d = {k: np.asarray(v) for k, v in inputs.items()}
    if "nc" not in _NC_CACHE:
        _NC_CACHE["nc"] = build_full()
    nc = _NC_CACHE["nc"]
    sh = _shared_inputs(d)
    in_maps = []
    for core in range(NCORES):
        m = dict(sh)
        m.update(_core_inputs(d, core))
        in_maps.append(m)
    res = run_bass_kernel_spmd(nc, in_maps, core_ids=list(range(NCORES)))
    out = np.zeros((2, 8192, D), np.float32)
    for core in range(NCORES):
        b, qtr = core // 4, core % 4
        out[b, qtr * TOK:(qtr + 1) * TOK] = res.results[core]["y"]
    return out
```
